# Optimizing a Trainium2 kernel written in Bass

```python
import jax, jax.numpy as jnp
from jax import lax
import numpy as np

D_MODEL = 1024
BATCH = 8
SEQ = 2048
DEPTH = 2

HEAD_DIM = 64
ROPE_THETA = 10000.0
Q_BLOCK = 128
A_HEADS = 8
A_KV_HEADS = 2
A_WINDOW = 128
B_HEADS = 4
IDX_HEADS = 4
IDX_DIM = 64
IDX_TOPK_MAX = 256
IDX_W_SCALE = (IDX_HEADS * IDX_DIM) ** -0.5
C_HEADS = 4
C_NOPE = 64
C_ROPE = 32
C_V = 64
C_Q_RANK = 256
C_KV_RANK = 128
A_Q = A_HEADS * HEAD_DIM
A_KV = A_KV_HEADS * HEAD_DIM
B_Q = B_HEADS * HEAD_DIM
B_KV = HEAD_DIM
IDX_Q = IDX_HEADS * IDX_DIM
IN_SPLITS = (A_Q, A_KV, A_KV, B_Q, B_KV, B_KV, IDX_Q, IDX_DIM, IDX_HEADS, C_Q_RANK, C_KV_RANK, C_ROPE)
IN_COLS = A_Q + 2 * A_KV + B_Q + 2 * B_KV + IDX_Q + IDX_DIM + IDX_HEADS + C_Q_RANK + C_KV_RANK + C_ROPE
MIX_WIDTH = A_HEADS * HEAD_DIM + B_HEADS * HEAD_DIM + C_HEADS * C_V
N_EXPERTS = 16
N_GROUPS = 4
EXPERTS_PER_GROUP = N_EXPERTS // N_GROUPS
TOP_K = 2
D_EXPERT = 256
DEEPNORM_ALPHA = (2 * DEPTH) ** 0.25
DEEPNORM_BETA = (8 * DEPTH) ** -0.25
LN_EPS = 1e-5
RMS_EPS = 1e-6

kernel_name = 'hybrid_swa_dsa_mla_grouped_moe_deepnorm'


def rope_tables(pos, dim):
    inv = 1.0 / (ROPE_THETA ** (jnp.arange(0, dim, 2, dtype=jnp.float32) / dim))
    ang = pos.astype(jnp.float32)[:, None] * inv[None, :]
    return jnp.cos(ang), jnp.sin(ang)


def apply_rope(x, cos, sin):
    d2 = x.shape[-1] // 2
    shape = (1, cos.shape[0]) + (1,) * (x.ndim - 3) + (d2,)
    c = cos.reshape(shape)
    s = sin.reshape(shape)
    xf = x.astype(jnp.float32)
    x1, x2 = xf[..., :d2], xf[..., d2:]
    return jnp.concatenate([x1 * c - x2 * s, x2 * c + x1 * s], axis=-1).astype(x.dtype)


def layer_norm(x, g, b):
    xf = x.astype(jnp.float32)
    mu = jnp.mean(xf, axis=-1, keepdims=True)
    var = jnp.mean(jnp.square(xf - mu), axis=-1, keepdims=True)
    return ((xf - mu) * lax.rsqrt(var + LN_EPS) * g.astype(jnp.float32) + b.astype(jnp.float32)).astype(x.dtype)


def rms_norm(x, g):
    xf = x.astype(jnp.float32)
    return (xf * lax.rsqrt(jnp.mean(jnp.square(xf), axis=-1, keepdims=True) + RMS_EPS) * g.astype(jnp.float32)).astype(x.dtype)


def split_cols(h):
    offs = np.cumsum(IN_SPLITS)[:-1].tolist()
    return jnp.split(h, offs, axis=-1)


def to_blocks(t):
    b, s = t.shape[0], t.shape[1]
    return jnp.moveaxis(t.reshape((b, s // Q_BLOCK, Q_BLOCK) + t.shape[2:]), 1, 0)


def sliding_window_sink_attn(q, k, v, sinks):
    B, S, Hq, D = q.shape
    Hkv = k.shape[2]
    G = Hq // Hkv
    W = A_WINDOW
    nb = S // W
    qb = q.reshape(B, nb, W, Hkv, G, D)

    def band(t):
        tb = t.reshape(B, nb, W, Hkv, D)
        prev = jnp.pad(tb[:, :-1], ((0, 0), (1, 0), (0, 0), (0, 0), (0, 0)))
        return jnp.concatenate([prev, tb], axis=2)

    kb, vb = band(k), band(v)
    s = jnp.einsum('bnqhgd,bnkhd->bnhgqk', qb, kb).astype(jnp.float32) * (D ** -0.5)
    qi = jnp.arange(W)[:, None]
    kj = jnp.arange(2 * W)[None, :]
    diff = qi + W - kj
    in_window = (diff >= 0) & (diff < W)
    not_pad = (jnp.arange(nb)[:, None, None] > 0) | (kj[None] >= W)
    mask = (in_window[None] & not_pad)[None, :, None, None]
    s = jnp.where(mask, s, -jnp.inf)
    sink = jnp.broadcast_to(sinks.astype(jnp.float32).reshape(1, 1, Hkv, G, 1, 1), s.shape[:-1] + (1,))
    p = jax.nn.softmax(jnp.concatenate([s, sink], axis=-1), axis=-1)[..., :-1]
    o = jnp.einsum('bnhgqk,bnkhd->bnqhgd', p.astype(v.dtype), vb)
    return o.reshape(B, S, Hq * D)


def indexer_sparse_attn(q, k, v, q_idx, k_idx, w_idx):
    B, S, H, D = q.shape
    topk = min(IDX_TOPK_MAX, S // 4)
    nb = S // Q_BLOCK
    key_pos = jnp.arange(S)
    bidx = jnp.arange(B)[:, None]

    def block_fn(args):
        qb, qib, wb, t0 = args
        tpos = t0 + jnp.arange(Q_BLOCK)
        logits = jnp.einsum('bthd,bsd->bths', qib, k_idx).astype(jnp.float32)
        score = jnp.einsum('bth,bths->bts', wb.astype(jnp.float32), jax.nn.relu(logits))
        causal = key_pos[None, :] <= tpos[:, None]
        score = jnp.where(causal[None], score, -jnp.inf)
        _, idx = lax.top_k(score, topk)
        flat = idx.reshape(B, Q_BLOCK * topk)
        kg = k[bidx, flat].reshape(B, Q_BLOCK, topk, D)
        vg = v[bidx, flat].reshape(B, Q_BLOCK, topk, D)
        valid = idx <= tpos[None, :, None]
        s = jnp.einsum('bthd,btkd->bthk', qb, kg).astype(jnp.float32) * (D ** -0.5)
        s = jnp.where(valid[:, :, None, :], s, -jnp.inf)
        p = jax.nn.softmax(s, axis=-1)
        return jnp.einsum('bthk,btkd->bthd', p.astype(vg.dtype), vg)

    t0s = jnp.arange(nb, dtype=jnp.int32) * Q_BLOCK
    out = lax.map(block_fn, (to_blocks(q), to_blocks(q_idx), to_blocks(w_idx), t0s))
    return jnp.moveaxis(out, 0, 1).reshape(B, S, H * D)


def mla_attn(q_nope, q_rope, k_nope, k_rope, v):
    B, S, H, Dv = v.shape
    scale = (q_nope.shape[-1] + q_rope.shape[-1]) ** -0.5
    nb = S // Q_BLOCK
    key_pos = jnp.arange(S)

    def block_fn(args):
        qn, qr, t0 = args
        tpos = t0 + jnp.arange(Q_BLOCK)
        s = (jnp.einsum('bthd,bshd->bhts', qn, k_nope) + jnp.einsum('bthd,bsd->bhts', qr, k_rope)).astype(jnp.float32) * scale
        causal = key_pos[None, :] <= tpos[:, None]
        s = jnp.where(causal[None, None], s, -jnp.inf)
        p = jax.nn.softmax(s, axis=-1)
        return jnp.einsum('bhts,bshd->bthd', p.astype(v.dtype), v)

    t0s = jnp.arange(nb, dtype=jnp.int32) * Q_BLOCK
    out = lax.map(block_fn, (to_blocks(q_nope), to_blocks(q_rope), t0s))
    return jnp.moveaxis(out, 0, 1).reshape(B, S, H * Dv)


def moe_ffn(x, w_router, router_bias, w_gate, w_up, w_down):
    B, S, D = x.shape
    N = B * S
    xt = x.reshape(N, D)
    scores = jax.nn.sigmoid(jnp.dot(xt, w_router).astype(jnp.float32))
    biased = (scores + router_bias.astype(jnp.float32)).reshape(N, N_GROUPS, EXPERTS_PER_GROUP)
    group_score = jnp.sum(lax.top_k(biased, TOP_K)[0], axis=-1)
    g_sel = jnp.argmax(group_score, axis=-1)
    in_group = jnp.take_along_axis(biased, g_sel[:, None, None], axis=1)[:, 0]
    _, local = lax.top_k(in_group, TOP_K)
    expert_idx = g_sel[:, None] * EXPERTS_PER_GROUP + local
    w = jnp.take_along_axis(scores, expert_idx, axis=1)
    w = w / jnp.sum(w, axis=-1, keepdims=True)
    gates = jnp.sum(jax.nn.one_hot(expert_idx, N_EXPERTS, dtype=jnp.float32) * w[..., None], axis=1)
    h = jax.nn.silu(jnp.einsum('nd,edf->enf', xt, w_gate)) * jnp.einsum('nd,edf->enf', xt, w_up)
    h = h * gates.T.astype(h.dtype)[:, :, None]
    y = jnp.einsum('enf,efd->nd', h, w_down)
    return y.reshape(B, S, D)


def setup_inputs(seed: int = 0) -> dict:
    key = jax.random.key(seed)
    ks = jax.random.split(key, 20)
    f32 = jnp.float32
    nrm = lambda k, shape, scale: jax.random.normal(k, shape, f32) * scale
    return {
        'x': nrm(ks[0], (BATCH, SEQ, D_MODEL), 1.0),
        'w_in': nrm(ks[1], (DEPTH, D_MODEL, IN_COLS), D_MODEL ** -0.5),
        'attn_sinks': nrm(ks[2], (DEPTH, A_HEADS), 0.5),
        'c_q_norm_g': 1.0 + nrm(ks[3], (DEPTH, C_Q_RANK), 0.02),
        'c_kv_norm_g': 1.0 + nrm(ks[4], (DEPTH, C_KV_RANK), 0.02),
        'w_uq': nrm(ks[5], (DEPTH, C_Q_RANK, C_HEADS * (C_NOPE + C_ROPE)), C_Q_RANK ** -0.5),
        'w_ukv': nrm(ks[6], (DEPTH, C_KV_RANK, C_HEADS * (C_NOPE + C_V)), C_KV_RANK ** -0.5),
        'w_out': nrm(ks[7], (DEPTH, MIX_WIDTH, D_MODEL), DEEPNORM_BETA * MIX_WIDTH ** -0.5),
        'ln1_g': 1.0 + nrm(ks[8], (DEPTH, D_MODEL), 0.02),
        'ln1_b': nrm(ks[9], (DEPTH, D_MODEL), 0.02),
        'w_router': nrm(ks[10], (D_MODEL, N_EXPERTS), D_MODEL ** -0.5),
        'router_bias': nrm(ks[11], (N_EXPERTS,), 0.01),
        'w_gate': nrm(ks[12], (DEPTH, N_EXPERTS, D_MODEL, D_EXPERT), D_MODEL ** -0.5),
        'w_up': nrm(ks[13], (DEPTH, N_EXPERTS, D_MODEL, D_EXPERT), D_MODEL ** -0.5),
        'w_down': nrm(ks[14], (DEPTH, N_EXPERTS, D_EXPERT, D_MODEL), DEEPNORM_BETA * D_EXPERT ** -0.5),
        'ln2_g': 1.0 + nrm(ks[15], (DEPTH, D_MODEL), 0.02),
        'ln2_b': nrm(ks[16], (DEPTH, D_MODEL), 0.02),
    }


def reference(x, w_in, attn_sinks, c_q_norm_g, c_kv_norm_g, w_uq, w_ukv, w_out, ln1_g, ln1_b, w_router, router_bias, w_gate, w_up, w_down, ln2_g, ln2_b):
    B, S, _ = x.shape
    pos = jnp.arange(S)
    cos_h, sin_h = rope_tables(pos, HEAD_DIM)
    cos_r, sin_r = rope_tables(pos, C_ROPE)
    for l in range(DEPTH):
        h = jnp.einsum('bsd,dc->bsc', x, w_in[l])
        a_q, a_k, a_v, b_q, b_k, b_v, i_q, i_k, i_w, c_q, c_kv, c_kr = split_cols(h)
        o_a = sliding_window_sink_attn(
            apply_rope(a_q.reshape(B, S, A_HEADS, HEAD_DIM), cos_h, sin_h),
            apply_rope(a_k.reshape(B, S, A_KV_HEADS, HEAD_DIM), cos_h, sin_h),
            a_v.reshape(B, S, A_KV_HEADS, HEAD_DIM), attn_sinks[l])
        o_b = indexer_sparse_attn(
            apply_rope(b_q.reshape(B, S, B_HEADS, HEAD_DIM), cos_h, sin_h),
            apply_rope(b_k, cos_h, sin_h), b_v,
            apply_rope(i_q.reshape(B, S, IDX_HEADS, IDX_DIM), cos_h, sin_h),
            apply_rope(i_k, cos_h, sin_h), i_w * IDX_W_SCALE)
        q_c = jnp.einsum('bsr,rc->bsc', rms_norm(c_q, c_q_norm_g[l]), w_uq[l]).reshape(B, S, C_HEADS, C_NOPE + C_ROPE)
        kv_c = jnp.einsum('bsr,rc->bsc', rms_norm(c_kv, c_kv_norm_g[l]), w_ukv[l]).reshape(B, S, C_HEADS, C_NOPE + C_V)
        o_c = mla_attn(q_c[..., :C_NOPE], apply_rope(q_c[..., C_NOPE:], cos_r, sin_r),
                       kv_c[..., :C_NOPE], apply_rope(c_kr, cos_r, sin_r), kv_c[..., C_NOPE:])
        mix = jnp.concatenate([o_a, o_b, o_c], axis=-1)
        x = layer_norm(DEEPNORM_ALPHA * x + jnp.einsum('bsm,md->bsd', mix, w_out[l]), ln1_g[l], ln1_b[l])
        x = layer_norm(DEEPNORM_ALPHA * x + moe_ffn(x, w_router, router_bias, w_gate[l], w_up[l], w_down[l]), ln2_g[l], ln2_b[l])
    return x
```

```python
import contextlib
import numpy as np
import concourse.bass as bass
import concourse.mybir as mybir
from concourse.bass_utils import run_bass_kernel_spmd

F32 = mybir.dt.float32
BF16 = mybir.dt.bfloat16
AF = mybir.ActivationFunctionType
ALU = mybir.AluOpType
AX = mybir.AxisListType

S = 2048
D = 1024
NT = 16
NCORES = 8
DEPTH = 2
ALPHA = float((2 * DEPTH) ** 0.25)
IDXW = float((4 * 64) ** -0.5)
SC64 = float(64 ** -0.5)
SC96 = float(96 ** -0.5)
TOPK = 256
NBIS = 14
NEG = -1.0e30
NDMA_SEMS = 8
ACC_ENG = "pool"
LNP_ENG = "pool"


class Prog:
    ENGS = ("pe", "act", "dve", "pool", "sp")

    def __init__(self):
        self.ops = {e: [] for e in self.ENGS}
        self.cnt = {e: 0 for e in self.ENGS}
        self.last_w = {}
        self.readers = {}
        self.waited = {e: {} for e in self.ENGS}
        self.dma_val = {}
        self.dma_rr = {"sp": 0, "pool": 0}
        self.final_tokens = []

    def _deps(self, eng, reads, writes):
        deps = {}

        def add(tok, raw):
            src, val = tok
            if src == eng and eng == "pe":
                return
            if deps.get(src, 0) < val:
                deps[src] = val

        for k in reads:
            if k in self.last_w:
                add(self.last_w[k], True)
            if isinstance(k, tuple) and k[0] == "ps":
                for r in self.readers.get(k, ()):
                    if r[0] != eng:
                        add(r, False)
        for k in writes:
            if k in self.last_w:
                add(self.last_w[k], False)
            for r in self.readers.get(k, ()):
                add(r, False)
        waits = []
        for src, val in deps.items():
            if self.waited[eng].get(src, 0) >= val:
                continue
            self.waited[eng][src] = val
            waits.append((src, val))
        return waits

    def _record(self, tok, reads, writes):
        for k in writes:
            self.last_w[k] = tok
            self.readers[k] = []
        for k in reads:
            self.readers.setdefault(k, []).append(tok)

    def op(self, eng, fn, reads=(), writes=(), inc=True):
        waits = self._deps(eng, reads, writes)
        if inc:
            self.cnt[eng] += 1
            idx = self.cnt[eng]
        else:
            idx = self.cnt[eng] + 1
        tok = (eng, idx)
        self._record(tok, reads, writes)
        self.ops[eng].append((waits, fn, ("eng", eng) if inc else None))
        return tok

    def dma(self, q, out_ap, in_ap, reads=(), writes=(), final=False):
        i = self.dma_rr[q]
        self.dma_rr[q] = (i + 1) % NDMA_SEMS
        src = ("dma", q, i)
        prev = self.dma_val.get(src, 0)
        waits = self._deps(q, reads, writes)
        if prev and self.waited[q].get(src, 0) < prev:
            self.waited[q][src] = prev
            waits.append((src, prev))
        val = prev + 16
        self.dma_val[src] = val
        tok = (src, val)
        self._record(tok, reads, writes)

        def fn(e, out_ap=out_ap, in_ap=in_ap):
            return e.dma_start(out=out_ap, in_=in_ap)

        self.ops[q].append((waits, fn, ("dma", src)))
        if final:
            self.final_tokens.append(tok)
        return tok

    def barrier(self):
        snap = [(e, self.cnt[e]) for e in self.ENGS if self.cnt[e] > 0]
        snap += [(src, v) for src, v in self.dma_val.items()]
        for e in self.ENGS:
            waits = []
            for src, val in snap:
                if src == e:
                    continue
                if self.waited[e].get(src, 0) >= val:
                    continue
                self.waited[e][src] = val
                waits.append((src, val))
            if waits:
                self.ops[e].append((waits, None, None))

    def emit(self, nc, sems):
        fin = list(self.final_tokens)

        def replay(eng, e):
            for waits, fn, inc in self.ops[eng]:
                for src, val in waits:
                    e.wait_ge(sems[src], val)
                if fn is None:
                    continue
                ins = fn(e)
                if inc is not None:
                    if inc[0] == "eng":
                        ins.then_inc(sems[inc[1]], 1)
                    else:
                        ins.then_inc(sems[inc[1]], 16)
            if eng == "sp":
                for src, val in fin:
                    e.wait_ge(sems[src], val)

        with nc.Block() as block:
            @block.tensor
            def _(e):
                replay("pe", e)

            @block.scalar
            def _(e):
                replay("act", e)

            @block.vector
            def _(e):
                replay("dve", e)

            @block.gpsimd
            def _(e):
                replay("pool", e)

            @block.sync
            def _(e):
                replay("sp", e)


def _consts():
    pos = np.arange(S, dtype=np.float64)
    c = {}
    for dim, nm in ((64, "64"), (32, "32")):
        inv = 1.0 / (10000.0 ** (np.arange(0, dim, 2, dtype=np.float64) / dim))
        inv = inv.astype(np.float32).astype(np.float64)
        ang = (pos.astype(np.float32)[:, None] * inv.astype(np.float32)[None, :]).astype(np.float32)
        c["cos" + nm] = np.cos(ang.astype(np.float64)).astype(np.float32)
        c["sin" + nm] = np.sin(ang.astype(np.float64)).astype(np.float32)
    c["ident"] = np.eye(128, dtype=np.float32)
    qi = np.arange(128)[:, None]
    kj = np.arange(256)[None, :]
    diff = qi + 128 - kj
    c["maskA"] = ((diff >= 0) & (diff < 128)).astype(np.float32)
    kk = np.arange(128)[None, :]
    c["causal01"] = (kk <= qi).astype(np.float32)
    c["negmask"] = np.where(kk <= qi, 0.0, NEG).astype(np.float32)
    c["causalT"] = (qi <= kk).astype(np.float32)
    c["prevT"] = (qi > kk).astype(np.float32)
    c["maskPC"] = np.concatenate([c["prevT"], c["causalT"]], axis=1)
    return c


def build_program(nlayers=DEPTH, debug_mix=False, stop_after=None):
    nc = bass.Bass("TRN2", target_bir_lowering=False)

    def din(name, shape):
        return nc.dram_tensor(name, list(shape), F32, kind="ExternalInput").ap()

    x_d = din("x", [S, D])
    w_in_d = din("w_in", [DEPTH, D, 1892])
    sinks_d = din("attn_sinks", [DEPTH, 8])
    gq_d = din("c_q_norm_g", [DEPTH, 256])
    gkv_d = din("c_kv_norm_g", [DEPTH, 128])
    w_uq_d = din("w_uq", [DEPTH, 256, 384])
    w_ukv_d = din("w_ukv", [DEPTH, 128, 512])
    w_out_d = din("w_out", [DEPTH, D, D])
    ln1g_d = din("ln1_g", [DEPTH, D])
    ln1b_d = din("ln1_b", [DEPTH, D])
    w_router_d = din("w_router", [D, 16])
    rbias_d = din("router_bias", [16])
    w_gate_d = din("w_gate", [DEPTH, 16, D, 256])
    w_up_d = din("w_up", [DEPTH, 16, D, 256])
    w_down_d = din("w_down", [DEPTH, 16, 256, D])
    ln2g_d = din("ln2_g", [DEPTH, D])
    ln2b_d = din("ln2_b", [DEPTH, D])
    cos64_d = din("cos64", [S, 32])
    sin64_d = din("sin64", [S, 32])
    cos32_d = din("cos32", [S, 16])
    sin32_d = din("sin32", [S, 16])
    ident_d = din("ident", [128, 128])
    maskA_d = din("maskA", [128, 256])
    causal_d = din("causal01", [128, 128])
    negmask_d = din("negmask", [128, 128])
    causalT_d = din("causalT", [128, 128])
    prevT_d = din("prevT", [128, 128])
    maskPC_d = din("maskPC", [128, 256])
    out_d = nc.dram_tensor("out", [S, D], F32, kind="ExternalOutput").ap()
    dbg_d = None
    if debug_mix:
        dbg_d = nc.dram_tensor("dbg", [S, D], F32, kind="ExternalOutput").ap()

    P = Prog()
    st = contextlib.ExitStack()
    with st:
        sems = {}
        for e in Prog.ENGS:
            sems[e] = st.enter_context(nc.semaphore("s_" + e))
        for q in ("sp", "pool"):
            for i in range(NDMA_SEMS):
                sems[("dma", q, i)] = st.enter_context(nc.semaphore(f"d_{q}{i}"))

        def T(name, shape, dt):
            return st.enter_context(nc.sbuf_tensor("sb_" + name, list(shape), dt))

        X = T("X", [128, NT, D], F32)
        xT = T("xT", [128, 8, S], BF16)
        cos64 = T("cos64", [128, NT, 32], F32)
        sin64 = T("sin64", [128, NT, 32], F32)
        nsin64 = T("nsin64", [128, NT, 32], F32)
        cos32 = T("cos32", [128, NT, 16], F32)
        sin32 = T("sin32", [128, NT, 16], F32)
        nsin32 = T("nsin32", [128, NT, 16], F32)
        identf = T("identf", [128, 128], F32)
        identb = T("identb", [128, 128], BF16)
        maskA = T("maskA", [128, 256], BF16)
        causalb = T("causalb", [128, 128], BF16)
        negmask = T("negmask", [128, 128], F32)
        causalT = T("causalT", [128, 128], BF16)
        prevT = T("prevT", [128, 128], BF16)
        maskPC = T("maskPC", [128, 256], BF16)
        ones1 = T("ones1", [1, 128], F32)
        lnp = T("lnp", [128, 2, D], F32)
        gates = T("gates", [128, NT, 16], F32)
        widx = T("widx", [128, NT, 4], F32)
        wr = T("wr", [128, 8, 16], F32)
        rb = T("rb", [128, 16], F32)
        sinks = T("sinks", [128, 8], F32)
        gq = T("gq", [128, 256], F32)
        gkv = T("gkv", [128, 128], F32)
        sm = T("sm", [128, 256], F32)
        ARW = 22272
        arena = T("arena", [128, ARW], F32)
        psum = st.enter_context(nc.psum_tensor("psum", [128, 4096], F32))

        def bank(i):
            return psum[:, 512 * i:512 * (i + 1)]

        def bankb(i):
            return psum[:, 512 * i:512 * (i + 1)].bitcast(BF16)

        class Arena:
            def __init__(self):
                self.off = 0

            def f32(self, n):
                o = self.off
                self.off += n
                assert self.off <= ARW, self.off
                return arena[:, o:o + n]

            def bf16(self, n):
                w = (n + 1) // 2
                o = self.off
                self.off += w
                assert self.off <= ARW, self.off
                return arena[:, o:o + w].bitcast(BF16)

        A = Arena()
        WIN = A.bf16(8 * 768)
        WOUT = A.bf16(4 * 1024)
        HSB = [A.f32(768), A.f32(768)]
        phase_qkr_off = A.off
        QKR = [A.f32(768), A.f32(768)]
        SQ = A.f32(768)
        PB = [A.bf16(512), A.bf16(512)]
        PTB = [A.bf16(512), A.bf16(512)]
        ROPET = A.f32(640)
        MIXT = A.bf16(512)
        MIXTT = A.bf16(512)
        phase_base = A.off

        rot = [0]
        rot4 = [0]
        inpipe = [False]

        def rbank():
            i = rot[0]
            rot[0] = (i + 1) % 3
            return i

        def mbank():
            if inpipe[0]:
                return 3
            i = rot4[0]
            rot4[0] = (i + 1) % 4
            return i

        def mm(out, lhsT, rhs, start, stop, reads, writes, inc):
            P.op("pe", lambda e, o=out, l=lhsT, r=rhs, s0=start, s1=stop: e.matmul(o, lhsT=l, rhs=r, start=s0, stop=s1),
                 reads=reads, writes=writes, inc=inc)

        def tr(out, in_, ident, reads, writes, inc=True):
            P.op("pe", lambda e, o=out, i=in_, d=ident: e.transpose(out=o, in_=i, identity=d),
                 reads=reads, writes=writes, inc=inc)

        def act(out, in_, func, reads, writes, bias=None, scale=None, accum=None):
            kw = {}
            if bias is not None:
                kw["bias"] = bias
            if scale is not None:
                kw["scale"] = scale
            if accum is not None:
                kw["accum_out"] = accum
            P.op("act", lambda e, o=out, i=in_, f=func, kw=kw: e.activation(out=o, in_=i, func=f, **kw),
                 reads=reads, writes=writes)

        def tt(out, in0, in1, op, reads, writes, eng="dve"):
            P.op(eng, lambda e, o=out, a=in0, b=in1, p=op: e.tensor_tensor(out=o, in0=a, in1=b, op=p),
                 reads=reads, writes=writes)

        def ts(out, in0, s1, s2, op0, op1, reads, writes, accum=None, eng="dve"):
            def fn(e, o=out, a=in0, s1=s1, s2=s2, op0=op0, op1=op1, accum=accum):
                kw = {}
                if op1 is not None:
                    kw["op1"] = op1
                if accum is not None:
                    kw["accum_out"] = accum
                return e.tensor_scalar(out=o, in0=a, scalar1=s1, scalar2=s2, op0=op0, **kw)
            P.op(eng, fn, reads=reads, writes=writes)

        def stt(out, in0, scalar, in1, op0, op1, reads, writes, eng="dve"):
            P.op(eng, lambda e, o=out, a=in0, s=scalar, b=in1, p0=op0, p1=op1:
                 e.scalar_tensor_tensor(out=o, in0=a, scalar=s, in1=b, op0=p0, op1=p1),
                 reads=reads, writes=writes)

        def cp(eng, out, in_, reads, writes):
            if eng == "act":
                act(out, in_, AF.Copy, reads, writes)
            else:
                P.op(eng, lambda e, o=out, i=in_: e.tensor_copy(out=o, in_=i), reads=reads, writes=writes)

        def red(out, in_, op, reads, writes, absv=False):
            def fn(e, o=out, i=in_, p=op, a=absv):
                if a:
                    return e.tensor_reduce(out=o, in_=i, axis=AX.X, op=p, apply_absolute_value=True)
                return e.tensor_reduce(out=o, in_=i, axis=AX.X, op=p)
            P.op("dve", fn, reads=reads, writes=writes)

        def memset(ap, val, writes, eng="dve"):
            P.op(eng, lambda e, a=ap, v=val: e.memset(a, v), writes=writes)

        def recip(out, in_, reads, writes):
            P.op("dve", lambda e, o=out, i=in_: e.reciprocal(out=o, in_=i), reads=reads, writes=writes)

        bg = []

        def bg_run(k):
            for _ in range(k):
                if not bg:
                    return
                bg.pop(0)()

        def pipeline(items, s1, s2, s3, bg_per_iter=0):
            n = len(items)
            inpipe[0] = True
            for i in range(n + 2):
                if i < n:
                    s1(items[i])
                if 0 <= i - 1 < n:
                    s2(items[i - 1])
                if 0 <= i - 2 < n:
                    s3(items[i - 2])
                if bg_per_iter:
                    bg_run(bg_per_iter)
            bg_run(len(bg))
            inpipe[0] = False

        def run_tiles(Pf, Rf):
            Pf(0)
            for t in range(NT):
                if t + 1 < NT:
                    Pf(t + 1)
                Rf(t)

        def tcols(t):
            return slice(t * 128, (t + 1) * 128)

        PBs = [PB[0], PB[1], PTB[0]]
        pbrot = [0]

        def st1(it):
            b = rbank()
            it["b"] = b
            k = it["kind"]
            if k == "att":
                it["pi"] = pbrot[0]
                pbrot[0] = (pbrot[0] + 1) % 3
                sl = it["slots"]
                for j, s in enumerate(sl):
                    ka, kk = s["K"]
                    qa, qk = s["Q"]
                    mm(bank(b)[:, j * 128:(j + 1) * 128], ka, qa, True, True, kk + qk, [("ps", b)], inc=(j == len(sl) - 1))
            elif k == "idx":
                mm(bank(b)[:, 0:it["n"]], it["lhsT"], it["rhs"], True, True, it["keys"], [("ps", b)], True)
            elif k == "mskT":
                kts = it["kts"]
                for j, kt in enumerate(kts):
                    tr(bankb(b)[:, j * 128:(j + 1) * 128], it["src"][:, kt * 128:(kt + 1) * 128], identb[:],
                       [it["srckey"], "identb"], [("ps", b)], inc=(j == len(kts) - 1))

        def st2(it):
            b = it["b"]
            k = it["kind"]
            if k == "att":
                pi = it["pi"]
                n = 128 * len(it["slots"])
                act(PBs[pi][:, 0:n], bank(b)[:, 0:n], AF.Exp, [("ps", b), "negc"], [("PB", pi)], bias=it["negc"], scale=it["scale"])
                for (c0, c1, view, m_ap, mk) in it["masks"]:
                    pv = PBs[pi][:, c0:c1]
                    if view is not None:
                        pv = pv.rearrange(view[0], **view[1])
                    tt(pv, pv, m_ap, ALU.mult, [("PB", pi)] + mk, [("PB", pi)])
            elif k == "idx":
                ri, n, h, qb, k0 = it["ri"], it["n"], it["h"], it["qb"], it["k0"]
                sco, sk = it["sco"], it["scokey"]
                act(it["rb"][:, 0:n], bank(b)[:, 0:n], AF.Relu, [("ps", b)], ["RB%d" % ri])
                if h == 0:
                    ts(sco[:, k0:k0 + n], it["rb"][:, 0:n], widx[:, qb, 0:1], None, ALU.mult, None,
                       ["RB%d" % ri, ("widx", qb)], [sk], eng=ACC_ENG)
                elif ACC_ENG == "dve":
                    stt(sco[:, k0:k0 + n], it["rb"][:, 0:n], widx[:, qb, h:h + 1], sco[:, k0:k0 + n],
                        ALU.mult, ALU.add, ["RB%d" % ri, ("widx", qb), sk], [sk])
                else:
                    ts(it["rb"][:, 0:n], it["rb"][:, 0:n], widx[:, qb, h:h + 1], None, ALU.mult, None,
                       ["RB%d" % ri, ("widx", qb)], ["RB%d" % ri], eng=ACC_ENG)
                    tt(sco[:, k0:k0 + n], sco[:, k0:k0 + n], it["rb"][:, 0:n], ALU.add, ["RB%d" % ri, sk], [sk], eng=ACC_ENG)
            elif k == "mskT":
                kts = it["kts"]
                nj = len(kts)
                cp("act", it["dst"][:, kts[0]:kts[0] + nj, :],
                   bankb(b)[:, 0:nj * 128].rearrange("p (c n) -> p c n", c=nj), [("ps", b)], [it["dstkey"]])

        def st3(it):
            if it["kind"] != "att":
                return
            pi = it["pi"]
            sl = it["slots"]
            for j, s in enumerate(sl):
                va, vk = s["V"]
                ob = s["ob"]
                mm(bank(ob)[:, s["oreg"]:s["oreg"] + 65], PBs[pi][:, j * 128:(j + 1) * 128], va, s["start"], s["stop"],
                   [("PB", pi)] + vk, [("ps", ob)], inc=(j == len(sl) - 1 or sl[j + 1]["ob"] != ob))
            if it.get("fin") is not None:
                it["fin"]()

        def run_seq(seq, bg_per_iter=0):
            n = len(seq)
            inpipe[0] = True
            for i in range(n + 2):
                if i < n and seq[i][0] is not None:
                    st1(seq[i][0])
                if 0 <= i - 1 < n and seq[i - 1][0] is not None:
                    st2(seq[i - 1][0])
                if 0 <= i - 2 < n and seq[i - 2][0] is not None:
                    st3(seq[i - 2][0])
                if i < n:
                    for c in seq[i][1]:
                        c()
                if bg_per_iter:
                    bg_run(bg_per_iter)
            bg_run(len(bg))
            inpipe[0] = False

        def fin_generic(qb, ob, nheads, nchunks_w, mix_c0, epilogue):
            def fn():
                o3 = bank(ob)[:, 0:nheads * 65].rearrange("p (h d) -> p h d", h=nheads)
                rc = sm[:, 72:72 + nheads]
                recip(rc, o3[:, :, 64], [("ps", ob)], ["rc"])
                tt(MIXT[:, 0:nheads * 64].rearrange("p (h d) -> p h d", h=nheads), o3[:, :, 0:64],
                   rc.unsqueeze(2).to_broadcast([128, nheads, 64]), ALU.mult, [("ps", ob), "rc"], ["MIXT"])
                dbg_store(qb, mix_c0, nheads * 64)
                out_proj_partial(qb, nchunks_w, False, after=epilogue)
            return fn

        def tm(ap_d):
            return ap_d.rearrange("(t p) d -> p t d", p=128)

        P.dma("sp", cos64[:], tm(cos64_d), writes=["cos64"])
        P.dma("sp", sin64[:], tm(sin64_d), writes=["sin64"])
        P.dma("sp", cos32[:], tm(cos32_d), writes=["cos32"])
        P.dma("sp", sin32[:], tm(sin32_d), writes=["sin32"])
        P.dma("sp", identf[:], ident_d, writes=["identf"])
        P.dma("pool", maskA[:], maskA_d, writes=["maskA"])
        P.dma("pool", causalb[:], causal_d, writes=["causalb"])
        P.dma("pool", identb[:], ident_d, writes=["identb"])
        P.dma("pool", causalT[:], causalT_d, writes=["causalT"])
        P.dma("pool", prevT[:], prevT_d, writes=["prevT"])
        P.dma("pool", maskPC[:], maskPC_d, writes=["maskPC"])
        P.dma("sp", negmask[:], negmask_d, writes=["negmask"])
        P.dma("sp", wr[:], w_router_d.rearrange("(c p) n -> p c n", p=128), writes=["wr"])
        P.dma("sp", rb[:], rbias_d.partition_broadcast(128), writes=["rb"])
        xv = tm(x_d)
        for t4 in range(4):
            P.dma("sp", X[:, 4 * t4:4 * t4 + 4, :], xv[:, 4 * t4:4 * t4 + 4, :],
                  writes=[("X", t) for t in range(4 * t4, 4 * t4 + 4)])
        ts(nsin64[:], sin64[:], -1.0, None, ALU.mult, None, ["sin64"], ["nsin64"])
        ts(nsin32[:], sin32[:], -1.0, None, ALU.mult, None, ["sin32"], ["nsin32"])
        memset(ones1[:], 1.0, ["ones1"])

        def build_xT(t):
            for half in range(2):
                b = mbank()
                for j in range(4):
                    c = half * 4 + j
                    tr(bank(b)[:, j * 128:(j + 1) * 128], X[:, t, c * 128:(c + 1) * 128], identf[:],
                       [("X", t), "identf"], [("ps", b)], inc=(j == 3))
                cp("act" if half == 0 else "dve",
                   xT[:, half * 4:half * 4 + 4, tcols(t)],
                   bank(b).rearrange("p (c n) -> p c n", c=4),
                   [("ps", b)], [("xT", t)])

        def in_proj(t, ncols, wview, hs):
            n0 = 0
            while n0 < ncols:
                n1 = min(ncols, n0 + 512)
                b = mbank()
                for c in range(8):
                    mm(bank(b)[:, 0:n1 - n0], xT[:, c, tcols(t)], wview[:, c, n0:n1], c == 0, c == 7,
                       [("xT", t), "WIN"], [("ps", b)], inc=(c == 7))
                cp("act", hs[:, n0:n1], bank(b)[:, 0:n1 - n0], [("ps", b)], ["HSB%d" % (t % 2)])
                n0 = n1

        def rope(src, dst, nh, hd, t, cosT, sinT, nsinT, rk, wk, tmp=None):
            h2 = hd // 2
            cb = cosT[:, t, :].unsqueeze(1).unsqueeze(1).to_broadcast([128, nh, 2, h2])
            sb = sinT[:, t, :].unsqueeze(1).to_broadcast([128, nh, h2])
            nb = nsinT[:, t, :].unsqueeze(1).to_broadcast([128, nh, h2])
            tv = ROPET[:, 0:nh * hd].rearrange("p (h t d) -> p h t d", h=nh, t=2)
            rk = rk + ["cos64", "sin64", "nsin64", "cos32", "sin32", "nsin32"]
            tt(dst, src, cb, ALU.mult, rk, wk)
            tt(tv[:, :, 0, :], src[:, :, 1, :], nb, ALU.mult, rk, ["ropetmp"])
            tt(tv[:, :, 1, :], src[:, :, 0, :], sb, ALU.mult, rk, ["ropetmp"])
            tt(dst, dst, tv, ALU.add, wk + ["ropetmp"], wk)

        def global_bound(mt, nh, qsl, ksl, scale, negc):
            b = mbank()
            tr(bank(b)[0:nh, 0:128], mt, identf[:], ["mt", "identf"], [("ps", b)])
            red(sm[0:nh, 0:1], bank(b)[0:nh, 0:128], ALU.max, [("ps", b)], ["gb1"])
            b2 = mbank()
            tr(bank(b2)[0:1, 0:nh], sm[0:nh, 0:1], identf[0:nh, 0:nh], ["gb1", "identf"], [("ps", b2)])
            red(sm[0:1, 1:2], bank(b2)[0:1, qsl], ALU.max, [("ps", b2)], ["gb2"])
            red(sm[0:1, 2:3], bank(b2)[0:1, ksl], ALU.max, [("ps", b2)], ["gb3"])
            tt(sm[0:1, 3:4], sm[0:1, 1:2], sm[0:1, 2:3], ALU.mult, ["gb2", "gb3"], ["gb4"])
            act(sm[0:1, 4:5], sm[0:1, 3:4], AF.Ln, ["gb4"], ["gb5"])
            act(sm[0:1, 5:6], sm[0:1, 4:5], AF.Exp, ["gb5"], ["gb6"], scale=0.5)
            ts(sm[0:1, 6:7], sm[0:1, 5:6], -scale, None, ALU.mult, None, ["gb6"], ["gb7"])
            b3 = mbank()
            mm(bank(b3)[:, 0:1], ones1[0:1, :], sm[0:1, 6:7], True, True, ["gb7", "ones1"], [("ps", b3)], True)
            cp("dve", negc, bank(b3)[:, 0:1], [("ps", b3)], ["negc"])

        def head_sumsq(src, ncols, nh, mt, first, rk):
            act(SQ[:, 0:ncols], src, AF.Square, rk, ["SQ"])
            hd = ncols // nh
            if first:
                red(mt, SQ[:, 0:ncols].rearrange("p (h d) -> p h d", h=nh), ALU.add, ["SQ"], ["mt"])
            else:
                red(sm[:, 16:16 + nh], SQ[:, 0:ncols].rearrange("p (h d) -> p h d", h=nh), ALU.add, ["SQ"], ["hs"])
                tt(mt, mt, sm[:, 16:16 + nh], ALU.max, ["mt", "hs"], ["mt"])

        def out_proj_T(qb, nchunks):
            b = mbank()
            for c in range(nchunks):
                tr(bankb(b)[:, c * 128:(c + 1) * 128], MIXT[:, c * 128:(c + 1) * 128], identb[:],
                   ["MIXT", "identb"], [("ps", b)], inc=(c == nchunks - 1))
            cp("act", MIXTT[:, 0:nchunks * 128], bankb(b)[:, 0:nchunks * 128], [("ps", b)], ["MIXTT"])

        def out_proj_M(qb, nchunks, first):
            wv = WOUT.rearrange("p (c n) -> p c n", n=1024)
            for half in range(2):
                yb = 6 + half
                for c in range(nchunks):
                    mm(bank(yb), MIXTT[:, c * 128:(c + 1) * 128], wv[:, c, half * 512:(half + 1) * 512],
                       c == 0, c == nchunks - 1, ["MIXTT", "WOUT"], [("ps", yb)], inc=(c == nchunks - 1))
                xs = X[:, qb, half * 512:(half + 1) * 512]
                if first:
                    stt(xs, xs, ALPHA, bank(yb), ALU.mult, ALU.add, [("X", qb), ("ps", yb)], [("X", qb)])
                else:
                    tt(xs, xs, bank(yb), ALU.add, [("X", qb), ("ps", yb)], [("X", qb)])

        def out_proj_partial(qb, nchunks, first, after=None):
            bg.append(lambda: out_proj_T(qb, nchunks))

            def part2():
                out_proj_M(qb, nchunks, first)
                if after is not None:
                    after(qb)
            bg.append(part2)

        def dbg_store(qb, c0, ncols):
            if dbg_d is None:
                return
            cp("dve", ROPET[:, 0:ncols], MIXT[:, 0:ncols], ["MIXT"], ["ropetmp"])
            P.dma("sp", tm(dbg_d)[:, qb, c0:c0 + ncols], ROPET[:, 0:ncols], reads=["ropetmp"], final=True)

        def ln_stats(t, k):
            o = 32 + 16 * k
            ks = "ln%d" % k
            P.op("dve", lambda e, o_=sm[:, o:o + 6], i=X[:, t, 0:512]: e.bn_stats(out=o_, in_=i), reads=[("X", t)], writes=[ks + "a"])
            P.op("dve", lambda e, o_=sm[:, o + 6:o + 12], i=X[:, t, 512:1024]: e.bn_stats(out=o_, in_=i), reads=[("X", t)], writes=[ks + "b"])
            P.op("dve", lambda e, o_=sm[:, o + 12:o + 14], i=sm[:, o:o + 12]: e.bn_aggr(out=o_, in_=i), reads=[ks + "a", ks + "b"], writes=[ks + "mv"])
            act(sm[:, o + 14:o + 15], sm[:, o + 13:o + 14], AF.Ln, [ks + "mv"], [ks + "lv"], bias=1e-5)
            act(sm[:, o + 15:o + 16], sm[:, o + 14:o + 15], AF.Exp, [ks + "lv"], [ks + "rs"], scale=-0.5)

        def ln_apply(t, k):
            o = 32 + 16 * k
            ks = "ln%d" % k
            xs = X[:, t, :]
            ts(xs, xs, sm[:, o + 12:o + 13], sm[:, o + 15:o + 16], ALU.subtract, ALU.mult, [("X", t), ks + "mv", ks + "rs"], [("X", t)])
            tt(xs, xs, lnp[:, 0, :], ALU.mult, [("X", t), "lnp"], [("X", t)], eng=LNP_ENG)
            tt(xs, xs, lnp[:, 1, :], ALU.add, [("X", t), "lnp"], [("X", t)], eng=LNP_ENG)

        def layer_norm(t, k=0):
            ln_stats(t, k)
            ln_apply(t, k)

        def load_ln(g_d, b_d, l):
            P.dma("sp", lnp[:, 0, :], g_d[l].partition_broadcast(128), writes=["lnp"])
            P.dma("sp", lnp[:, 1, :], b_d[l].partition_broadcast(128), writes=["lnp"])

        def phase_A(l):
            A.off = phase_base
            FM = A.bf16(6 * S).rearrange("p (c n) -> p c n", c=6)
            VA = A.bf16(NT * 2 * 65).rearrange("p (t g d) -> p t g d", t=NT, g=2)
            mt = A.f32(16)[:, 0:10]
            negc = A.f32(2)[:, 0:1]
            esink = A.f32(8)
            den = A.f32(8)
            wv = WIN[:, 0:8 * 768].rearrange("p (c n) -> p c n", c=8)
            P.dma("pool", wv, w_in_d[l].rearrange("(c p) n -> p c n", p=128)[:, :, 0:768], writes=["WIN"])
            P.dma("pool", WOUT[:, 0:4096].rearrange("p (c n) -> p c n", c=4),
                  w_out_d[l, 0:512, :].rearrange("(c p) n -> p c n", p=128), writes=["WOUT"])
            P.dma("sp", sinks[:], sinks_d[l].partition_broadcast(128), writes=["sinks"])
            memset(VA[:, :, :, 64:65], 1.0, ["VAones"])
            def Pf(t):
                in_proj(t, 768, wv, HSB[t % 2])

            def Rf(t):
                hs = HSB[t % 2]
                qk = QKR[t % 2]
                hk = "HSB%d" % (t % 2)
                qkk = "QKR%d" % (t % 2)
                head_sumsq(hs[:, 0:640], 640, 10, mt, t == 0, [hk])
                rope(hs[:, 0:512].rearrange("p (h t d) -> p h t d", h=8, t=2),
                     qk[:, 0:512].rearrange("p (h t d) -> p h t d", h=8, t=2),
                     8, 64, t, cos64, sin64, nsin64, [hk], [qkk])
                kdst = qk[:, 512:768].rearrange("p (g r d) -> p g r d", g=2, r=2)
                rope(hs[:, 512:640].rearrange("p (h t d) -> p h t d", h=2, t=2),
                     kdst[:, :, 0, :].rearrange("p g (t d) -> p g t d", t=2),
                     2, 64, t, cos64, sin64, nsin64, [hk], [qkk])
                cp("dve", kdst[:, :, 1, :], kdst[:, :, 0, :], [qkk], [qkk])
                cp("act", VA[:, t, :, 0:64], hs[:, 640:768].rearrange("p (g d) -> p g d", g=2), [hk], [("VA", t)])
                for part, (c0, nchk) in enumerate(((0, 4), (4, 2))):
                    b = mbank()
                    for j in range(nchk):
                        c = c0 + j
                        tr(bank(b)[:, j * 128:(j + 1) * 128], qk[:, c * 128:(c + 1) * 128], identf[:],
                           [qkk, "identf"], [("ps", b)], inc=(j == nchk - 1))
                    cp("act" if part == 0 else "dve", FM[:, c0:c0 + nchk, tcols(t)],
                       bank(b)[:, 0:nchk * 128].rearrange("p (c n) -> p c n", c=nchk),
                       [("ps", b)], [("FM", t)])

            run_tiles(Pf, Rf)
            if stop_after == "A1":
                return
            global_bound(mt, 10, slice(0, 8), slice(8, 10), SC64, negc)
            act(esink, sinks[:], AF.Exp, ["sinks", "negc"], ["esink"], bias=negc)
            if stop_after == "A2":
                return

            def finA(qb, g):
                def fn():
                    ob = 4 + g
                    o3 = bank(ob)[:, 0:260].rearrange("p (h d) -> p h d", h=4)
                    tt(den[:, 4 * g:4 * g + 4], o3[:, :, 64], esink[:, 4 * g:4 * g + 4], ALU.add,
                       [("ps", ob), "esink"], ["den"])
                    recip(den[:, 4 * g:4 * g + 4], den[:, 4 * g:4 * g + 4], ["den"], ["den"])
                    tt(MIXT[:, g * 256:(g + 1) * 256].rearrange("p (h d) -> p h d", h=4), o3[:, :, 0:64],
                       den[:, 4 * g:4 * g + 4].unsqueeze(2).to_broadcast([128, 4, 64]), ALU.mult,
                       [("ps", ob), "den"], ["MIXT"])
                    if g == 1:
                        dbg_store(qb, 0, 512)
                        out_proj_partial(qb, 4, True)
                return fn

            seq = []
            for qb in range(NT):
                kts = [kt for kt in (qb - 1, qb) if kt >= 0]
                nk_ = len(kts)
                for g in range(2):
                    for par in range(2):
                        po = 64 * par
                        slots = []
                        for h in (4 * g + par, 4 * g + par + 2):
                            for kt in kts:
                                slots.append({"K": (FM[po:po + 64, 4 + g, tcols(kt)], [("FM", kt)]),
                                              "Q": (FM[po:po + 64, h // 2, tcols(qb)], [("FM", qb)]),
                                              "V": (VA[:, kt, g, :], [("VA", kt), "VAones"]),
                                              "ob": 4 + g, "oreg": (h % 4) * 65,
                                              "start": kt == kts[0], "stop": kt == kts[-1]})
                        if nk_ == 2:
                            masks = [(0, 512, ("p (h n) -> p h n", {"h": 2}),
                                      maskPC[:].unsqueeze(1).to_broadcast([128, 2, 256]), ["maskPC"])]
                        else:
                            masks = [(0, 256, ("p (h n) -> p h n", {"h": 2}),
                                      maskPC[:, 128:256].unsqueeze(1).to_broadcast([128, 2, 128]), ["maskPC"])]
                        seq.append(({"kind": "att", "slots": slots, "scale": SC64, "negc": negc, "masks": masks,
                                     "fin": finA(qb, g) if par == 1 else None}, []))
            run_seq(seq, bg_per_iter=1)

        def phase_B(l):
            A.off = phase_base
            FM = A.bf16(6 * S).rearrange("p (c n) -> p c n", c=6)
            VB = A.bf16(NT * 65 + 1)[:, 0:NT * 65].rearrange("p (t d) -> p t d", t=NT)
            SCO = A.f32(S)
            MSK = A.bf16(S)
            RB = [HSB[0][:, 0:512], HSB[1][:, 0:512]]
            mt = A.f32(16)[:, 0:5]
            negc = A.f32(2)[:, 0:1]
            bis = A.f32(8)
            wv = WIN[:, 0:8 * 708].rearrange("p (c n) -> p c n", c=8)
            P.dma("pool", wv, w_in_d[l].rearrange("(c p) n -> p c n", p=128)[:, :, 768:1476], writes=["WIN"])
            P.dma("pool", WOUT[:, 0:2048].rearrange("p (c n) -> p c n", c=2),
                  w_out_d[l, 512:768, :].rearrange("(c p) n -> p c n", p=128), writes=["WOUT"])
            memset(VB[:, :, 64:65], 1.0, ["VBones"])
            def Pf(t):
                in_proj(t, 708, wv, HSB[t % 2])

            def Rf(t):
                hs = HSB[t % 2]
                qk = QKR[t % 2]
                hk = "HSB%d" % (t % 2)
                qkk = "QKR%d" % (t % 2)
                head_sumsq(hs[:, 0:320], 320, 5, mt, t == 0, [hk])
                for (s0, d0) in ((0, 0), (384, 384)):
                    rope(hs[:, s0:s0 + 320].rearrange("p (h t d) -> p h t d", h=5, t=2),
                         qk[:, d0:d0 + 320].rearrange("p (h t d) -> p h t d", h=5, t=2),
                         5, 64, t, cos64, sin64, nsin64, [hk], [qkk])
                    cp("dve", qk[:, d0 + 320:d0 + 384], qk[:, d0 + 256:d0 + 320], [qkk], [qkk])
                cp("act", VB[:, t, 0:64], hs[:, 320:384], [hk], [("VB", t)])
                ts(widx[:, t, :], hs[:, 704:708], IDXW, None, ALU.mult, None, [hk], [("widx", t)])
                for part, (c0, nchk) in enumerate(((0, 4), (4, 2))):
                    b = mbank()
                    for j in range(nchk):
                        c = c0 + j
                        tr(bank(b)[:, j * 128:(j + 1) * 128], qk[:, c * 128:(c + 1) * 128], identf[:],
                           [qkk, "identf"], [("ps", b)], inc=(j == nchk - 1))
                    cp("act" if part == 0 else "dve", FM[:, c0:c0 + nchk, tcols(t)],
                       bank(b)[:, 0:nchk * 128].rearrange("p (c n) -> p c n", c=nchk),
                       [("ps", b)], [("FM", t)])

            run_tiles(Pf, Rf)
            global_bound(mt, 5, slice(0, 4), slice(4, 5), SC64, negc)

            P.barrier()
            SCOs = [SCO, arena[:, 0:S]]
            a_qkr = phase_qkr_off
            MSKT = [arena[:, a_qkr + 1024 * i:a_qkr + 1024 * (i + 1)].bitcast(BF16).rearrange("p (t n) -> p t n", t=NT)
                    for i in range(2)]

            def idx_items(qb):
                nk = (qb + 1) * 128
                par = qb % 2
                out = []
                for k0 in range(0, nk, 512):
                    n = min(512, nk - k0)
                    for h in range(4):
                        po = 64 * (h % 2)
                        ri = (len(out)) % 2
                        out.append({"kind": "idx", "n": n, "h": h, "qb": qb, "k0": k0, "ri": ri, "rb": RB[ri],
                                    "lhsT": FM[po:po + 64, 3 + h // 2, tcols(qb)], "rhs": FM[po:po + 64, 5, k0:k0 + n],
                                    "keys": [("FM", t) for t in range(qb + 1)],
                                    "sco": SCOs[par], "scokey": "SCO%d" % par})
                return out

            def bis_closures(qb):
                nk = (qb + 1) * 128
                par = qb % 2
                sco = SCOs[par]
                sk = "SCO%d" % par
                cl = []

                def init():
                    if qb >= 2:
                        red(bis[:, 0:1], sco[:, 0:nk], ALU.max, [sk], ["bnd"], absv=True)
                    tt(sco[:, qb * 128:nk], sco[:, qb * 128:nk], negmask[:], ALU.add, [sk, "negmask"], [sk])
                    if qb >= 2:
                        ts(bis[:, 1:2], bis[:, 0:1], -1.0, -1.0, ALU.mult, ALU.add, ["bnd"], ["lo"])
                        ts(bis[:, 2:3], bis[:, 0:1], 2.0, 2.0, ALU.mult, ALU.add, ["bnd"], ["w0"])
                    else:
                        ts(MSK[:, 0:nk], sco[:, 0:nk], -1.0e29, None, ALU.is_ge, None, [sk], ["MSK"])
                cl.append(init)
                if qb >= 2:
                    def it_fn(k):
                        def fn():
                            f = 2.0 ** -(k + 1)
                            stt(bis[:, 3:4], bis[:, 2:3], f, bis[:, 1:2], ALU.mult, ALU.add, ["w0", "lo"], ["mid"])
                            ts(MSK[:, 0:nk], sco[:, 0:nk], bis[:, 3:4], None, ALU.is_ge, ALU.add, [sk, "mid"],
                               ["MSK", "cnt"], accum=bis[:, 4:5])
                            ts(bis[:, 5:6], bis[:, 4:5], float(TOPK), bis[:, 2:3], ALU.is_ge, ALU.mult,
                               ["cnt", "w0"], ["stp"])
                            stt(bis[:, 1:2], bis[:, 5:6], f, bis[:, 1:2], ALU.mult, ALU.add, ["stp", "lo"], ["lo"])
                        return fn
                    for k in range(NBIS):
                        cl.append(it_fn(k))
                    cl.append(lambda: ts(MSK[:, 0:nk], sco[:, 0:nk], bis[:, 1:2], None, ALU.is_ge, None, [sk, "lo"], ["MSK"]))
                return cl

            def mskT_items(qb):
                par = qb % 2
                out = []
                for kt0 in range(0, qb + 1, 4):
                    kts = list(range(kt0, min(kt0 + 4, qb + 1)))
                    out.append({"kind": "mskT", "kts": kts, "src": MSK, "srckey": "MSK",
                                "dst": MSKT[par], "dstkey": ("MSKT", par)})
                return out

            def att_items(qb):
                par = qb % 2
                out = []
                for h in range(4):
                    po = 64 * (h % 2)
                    for kt0 in range(0, qb + 1, 4):
                        kts = list(range(kt0, min(kt0 + 4, qb + 1)))
                        slots = [{"K": (FM[po:po + 64, 2, tcols(kt)], [("FM", kt)]),
                                  "Q": (FM[po:po + 64, h // 2, tcols(qb)], [("FM", qb)]),
                                  "V": (VB[:, kt, :], [("VB", kt), "VBones"]),
                                  "ob": 4 + par, "oreg": h * 65, "start": kt == 0, "stop": kt == qb} for kt in kts]
                        ns = len(kts)
                        masks = [(0, 128 * ns, ("p (c n) -> p c n", {"c": ns}), MSKT[par][:, kt0:kt0 + ns, :], [("MSKT", par)])]
                        last = (h == 3 and kts[-1] == qb)
                        out.append({"kind": "att", "slots": slots, "scale": SC64, "negc": negc, "masks": masks,
                                    "fin": fin_generic(qb, 4 + par, 4, 2, 512, None) if last else None})
                return out

            def merge(a, b):
                out = []
                na, nb = len(a), len(b)
                ia = ib = 0
                while ia < na or ib < nb:
                    if ib >= nb or (ia < na and ia * nb <= ib * na):
                        out.append(a[ia])
                        ia += 1
                    else:
                        out.append(b[ib])
                        ib += 1
                return out

            seq = []
            for r in range(-2, NT):
                ia = att_items(r) if r >= 0 else []
                ii = idx_items(r + 2) if r + 2 < NT else []
                cl = bis_closures(r + 1) if 0 <= r + 1 < NT else []
                im = mskT_items(r + 1) if 0 <= r + 1 < NT else []
                ents = [[it, []] for it in merge(ia, ii)]
                if not ents:
                    ents = [[None, []]]
                ne = len(ents)
                for ci, c in enumerate(cl):
                    ents[min(ne - 1, (ci * ne) // max(1, len(cl)))][1].append(c)
                seq.extend((e[0], e[1]) for e in ents)
                seq.extend((it, []) for it in im)
            run_seq(seq, bg_per_iter=1)

        def phase_C(l):
            A.off = phase_base
            FQ = A.bf16(4 * S).rearrange("p (c n) -> p c n", c=4)
            FK = A.bf16(4 * S).rearrange("p (c n) -> p c n", c=4)
            VC = A.bf16(NT * 4 * 65).rearrange("p (t h d) -> p t h d", t=NT, h=4)
            QCF = QKR[0]
            KCF = QKR[1]
            CQN = A.bf16(256)
            CKN = A.bf16(128)
            CQT = A.bf16(256)
            CKT = A.bf16(128)
            mt = A.f32(16)[:, 0:8]
            negc = A.f32(2)[:, 0:1]
            rt = A.f32(128)
            wv = WIN[:, 0:8 * 416].rearrange("p (c n) -> p c n", c=8)
            WUQ = WIN[:, 8 * 416:8 * 416 + 768].rearrange("p (c n) -> p c n", c=2)
            WUKV = WIN[:, 8 * 416 + 768:8 * 416 + 768 + 512]
            P.dma("pool", wv, w_in_d[l].rearrange("(c p) n -> p c n", p=128)[:, :, 1476:1892], writes=["WIN"])
            P.dma("pool", WUQ, w_uq_d[l].rearrange("(c p) n -> p c n", p=128), writes=["WIN"])
            P.dma("pool", WUKV, w_ukv_d[l], writes=["WIN"])
            P.dma("pool", WOUT[:, 0:2048].rearrange("p (c n) -> p c n", c=2),
                  w_out_d[l, 768:1024, :].rearrange("(c p) n -> p c n", p=128), writes=["WOUT"])
            P.dma("sp", gq[:], gq_d[l].partition_broadcast(128), writes=["gq"])
            P.dma("sp", gkv[:], gkv_d[l].partition_broadcast(128), writes=["gkv"])
            load_ln(ln1g_d, ln1b_d, l)
            memset(VC[:, :, :, 64:65], 1.0, ["VCones"])
            def Pf(t):
                in_proj(t, 416, wv, HSB[t % 2])

            def Rf(t):
                hs = HSB[t % 2]
                hk = "HSB%d" % (t % 2)
                act(SQ[:, 0:256], hs[:, 0:256], AF.Square, [hk], ["SQ"], accum=sm[:, 64:65])
                act(SQ[:, 256:384], hs[:, 256:384], AF.Square, [hk], ["SQ2"], accum=sm[:, 65:66])
                act(sm[:, 66:67], sm[:, 64:65], AF.Ln, ["SQ"], ["ln1"], scale=1.0 / 256, bias=1e-6)
                act(sm[:, 67:68], sm[:, 65:66], AF.Ln, ["SQ2"], ["ln2"], scale=1.0 / 128, bias=1e-6)
                act(sm[:, 68:69], sm[:, 66:67], AF.Exp, ["ln1"], ["rs1"], scale=-0.5)
                act(sm[:, 69:70], sm[:, 67:68], AF.Exp, ["ln2"], ["rs2"], scale=-0.5)
                stt(CQN[:, 0:256], hs[:, 0:256], sm[:, 68:69], gq[:], ALU.mult, ALU.mult, [hk, "rs1", "gq"], ["CQN"])
                stt(CKN[:, 0:128], hs[:, 256:384], sm[:, 69:70], gkv[:], ALU.mult, ALU.mult, [hk, "rs2", "gkv"], ["CKN"])
                b = mbank()
                tr(bankb(b)[:, 0:128], CQN[:, 0:128], identb[:], ["CQN", "identb"], [("ps", b)], inc=False)
                tr(bankb(b)[:, 128:256], CQN[:, 128:256], identb[:], ["CQN", "identb"], [("ps", b)], inc=False)
                tr(bankb(b)[:, 256:384], CKN[:, 0:128], identb[:], ["CKN", "identb"], [("ps", b)], inc=True)
                cp("act", CQT[:, 0:256], bankb(b)[:, 0:256], [("ps", b)], ["CQT"])
                cp("dve", CKT[:, 0:128], bankb(b)[:, 256:384], [("ps", b)], ["CKT"])
                bq = mbank()
                mm(bank(bq)[:, 0:384], CQT[:, 0:128], WUQ[:, 0, :], True, False, ["CQT", "WIN"], [("ps", bq)], False)
                mm(bank(bq)[:, 0:384], CQT[:, 128:256], WUQ[:, 1, :], False, True, ["CQT", "WIN"], [("ps", bq)], True)
                bk = mbank()
                mm(bank(bk)[:, 0:512], CKT[:, 0:128], WUKV, True, True, ["CKT", "WIN"], [("ps", bk)], True)
                cp("act", QCF[:, 0:384], bank(bq)[:, 0:384], [("ps", bq)], ["QKR0"])
                q4 = QCF[:, 0:384].rearrange("p (h d) -> p h d", h=4)
                qr = q4[:, :, 64:96].rearrange("p h (t d) -> p h t d", t=2)
                cp("dve", rt[:, 0:128].rearrange("p (h d) -> p h d", h=4), q4[:, :, 64:96], ["QKR0"], ["rt"])
                rope(rt[:, 0:128].rearrange("p (h t d) -> p h t d", h=4, t=2), qr, 4, 32, t,
                     cos32, sin32, nsin32, ["rt"], ["QKR0"])
                kv4 = bank(bk)[:, 0:512].rearrange("p (h d) -> p h d", h=4)
                k4 = KCF[:, 0:384].rearrange("p (h d) -> p h d", h=4)
                cp("act", k4[:, :, 0:64], kv4[:, :, 0:64], [("ps", bk)], ["QKR1"])
                cp("dve", VC[:, t, :, 0:64], kv4[:, :, 64:128], [("ps", bk)], [("VC", t)])
                rope(hs[:, 384:416].rearrange("p (h t d) -> p h t d", h=1, t=2),
                     rt[:, 0:32].rearrange("p (h t d) -> p h t d", h=1, t=2), 1, 32, t,
                     cos32, sin32, nsin32, [hk], ["rt"])
                cp("dve", k4[:, :, 64:96], rt[:, 0:32].unsqueeze(1).to_broadcast([128, 4, 32]), ["rt"], ["QKR1"])
                act(SQ[:, 0:384], QCF[:, 0:384], AF.Square, ["QKR0"], ["SQ"])
                act(SQ[:, 384:768], KCF[:, 0:384], AF.Square, ["QKR1"], ["SQ"])
                if t == 0:
                    red(mt, SQ[:, 0:768].rearrange("p (h d) -> p h d", h=8), ALU.add, ["SQ"], ["mt"])
                else:
                    red(sm[:, 16:24], SQ[:, 0:768].rearrange("p (h d) -> p h d", h=8), ALU.add, ["SQ"], ["hs"])
                    tt(mt, mt, sm[:, 16:24], ALU.max, ["mt", "hs"], ["mt"])
                for (src, dstF, key, eng) in ((QCF, FQ, "QKR0", "act"), (KCF, FK, "QKR1", "dve")):
                    b = mbank()
                    for h in range(4):
                        tr(bank(b)[0:96, h * 128:(h + 1) * 128], src[:, h * 96:(h + 1) * 96], identf[:],
                           [key, "identf"], [("ps", b)], inc=(h == 3))
                    cp(eng, dstF[0:96, :, tcols(t)], bank(b)[0:96, :].rearrange("p (c n) -> p c n", c=4),
                       [("ps", b)], [("F" + key, t)])

            run_tiles(Pf, Rf)
            global_bound(mt, 8, slice(0, 4), slice(4, 8), SC96, negc)

            P.barrier()
            RT = arena[:, phase_qkr_off:phase_qkr_off + 2304]

            def epilogue(qb):
                k = qb % 2
                bg.append(lambda: ln_stats(qb, k))
                bg.append(lambda: ln_apply(qb, k))

                def xpose(half):
                    b = mbank()
                    for j in range(4):
                        c = half * 4 + j
                        tr(bank(b)[:, j * 128:(j + 1) * 128], X[:, qb, c * 128:(c + 1) * 128], identf[:],
                           [("X", qb), "identf"], [("ps", b)], inc=(j == 3))
                    cp("act", xT[:, half * 4:half * 4 + 4, tcols(qb)], bank(b).rearrange("p (c n) -> p c n", c=4),
                       [("ps", b)], [("xT", qb)])
                    cp("dve", HSB[half][:, 0:512], bank(b), [("ps", b)], ["HSB%d" % half])

                bg.append(lambda: xpose(0))
                bg.append(lambda: xpose(1))
                def router_a():
                    b = mbank()
                    for c in range(8):
                        mm(bank(b)[:, 0:16], HSB[c // 4][:, (c % 4) * 128:(c % 4 + 1) * 128], wr[:, c, :], c == 0, c == 7,
                           ["HSB0", "HSB1", "wr"], [("ps", b)], inc=(c == 7))
                    act(RT[:, qb * 16:(qb + 1) * 16], bank(b)[:, 0:16], AF.Exp, [("ps", b)], [("r_sc", qb)], scale=-1.0)

                bg.append(router_a)

            seq = []
            for qb in range(NT):
                par = qb % 2
                for h in range(4):
                    for kt0 in range(0, qb + 1, 4):
                        kts = list(range(kt0, min(kt0 + 4, qb + 1)))
                        slots = [{"K": (FK[0:96, h, tcols(kt)], [("FQKR1", kt)]),
                                  "Q": (FQ[0:96, h, tcols(qb)], [("FQKR0", qb)]),
                                  "V": (VC[:, kt, h, :], [("VC", kt), "VCones"]),
                                  "ob": 4 + par, "oreg": h * 65, "start": kt == 0, "stop": kt == qb} for kt in kts]
                        ns = len(kts)
                        masks = []
                        if kts[-1] == qb:
                            masks = [(128 * (ns - 1), 128 * ns, None, causalT[:], ["causalT"])]
                        last = (h == 3 and kts[-1] == qb)
                        seq.append(({"kind": "att", "slots": slots, "scale": SC96, "negc": negc, "masks": masks,
                                     "fin": fin_generic(qb, 4 + par, 4, 2, 768, epilogue) if last else None}, []))
            run_seq(seq, bg_per_iter=2)

            sc = RT[:, 0:256]
            bi = RT[:, 256:512]
            m1 = RT[:, 512:576]
            eq = RT[:, 576:832]
            msk = RT[:, 832:1088]
            m2 = RT[:, 1088:1152]
            gs = RT[:, 1152:1216]
            gm = RT[:, 1216:1232]
            gsel = RT[:, 1232:1296]
            t2 = RT[:, 1296:1552]
            wgt = RT[:, 1552:1808]
            ws = RT[:, 1808:1824]
            rsk = [("r_sc", t) for t in range(NT)]

            def v3(ap, a):
                return ap.rearrange("p (a b) -> p a b", a=a)

            ts(sc, sc, 1.0, None, ALU.add, None, rsk, ["r_s"])
            recip(sc, sc, ["r_s"], ["r_s"])
            tt(v3(bi, 16), v3(sc, 16), rb[:].unsqueeze(1).to_broadcast([128, 16, 16]), ALU.add, ["r_s", "rb"], ["r_bi"])
            red(m1, v3(bi, 64), ALU.max, ["r_bi"], ["r_m1"])
            tt(v3(eq, 64), v3(bi, 64), m1.unsqueeze(2).to_broadcast([128, 64, 4]), ALU.is_equal, ["r_bi", "r_m1"], ["r_eq"])
            stt(msk, eq, NEG, bi, ALU.mult, ALU.add, ["r_eq", "r_bi"], ["r_msk"])
            red(m2, v3(msk, 64), ALU.max, ["r_msk"], ["r_m2"])
            tt(gs, m1, m2, ALU.add, ["r_m1", "r_m2"], ["r_gs"])
            red(gm, v3(gs, 16), ALU.max, ["r_gs"], ["r_gm"])
            tt(v3(gsel, 16), v3(gs, 16), gm.unsqueeze(2).to_broadcast([128, 16, 4]), ALU.is_equal, ["r_gs", "r_gm"], ["r_gsel"])
            tt(v3(t2, 64), v3(bi, 64), m2.unsqueeze(2).to_broadcast([128, 64, 4]), ALU.is_ge, ["r_bi", "r_m2"], ["r_t2"])
            tt(v3(t2, 64), v3(t2, 64), gsel.unsqueeze(2).to_broadcast([128, 64, 4]), ALU.mult, ["r_t2", "r_gsel"], ["r_t2"])
            tt(wgt, sc, t2, ALU.mult, ["r_s", "r_t2"], ["r_w"])
            red(ws, v3(wgt, 16), ALU.add, ["r_w"], ["r_ws"])
            recip(ws, ws, ["r_ws"], ["r_ws"])
            tt(gates[:], v3(wgt, 16), ws.unsqueeze(2).to_broadcast([128, 16, 16]), ALU.mult, ["r_w", "r_ws"],
               [("gates", t) for t in range(NT)])

        def phase_moe(l, last_layer):
            A.off = 0
            NSLOT = 7
            WGU = [A.bf16(8 * 512).rearrange("p (c n) -> p c n", c=8) for _ in range(NSLOT)]
            WD = [A.bf16(2 * 1024).rearrange("p (c n) -> p c n", c=2) for _ in range(NSLOT)]
            SB = [A.bf16(256) for _ in range(2)]
            TB = [A.bf16(256) for _ in range(2)]
            TTB = [A.bf16(256) for _ in range(2)]
            load_ln(ln2g_d, ln2b_d, l)
            def load_expert(e):
                s = e % NSLOT
                P.dma("pool", WGU[s][:, :, 0:256], w_gate_d[l, e].rearrange("(c p) f -> p c f", p=128), writes=[("WGU", s)])
                P.dma("pool", WGU[s][:, :, 256:512], w_up_d[l, e].rearrange("(c p) f -> p c f", p=128), writes=[("WGU", s)])
                P.dma("pool", WD[s], w_down_d[l, e].rearrange("(c p) d -> p c d", p=128), writes=[("WD", s)])

            for e in range(NSLOT):
                load_expert(e)
            items = [(G, t, e) for G in range(4) for t in range(NT) for e in range(4 * G, 4 * G + 4)]
            sd = {}
            cnt = [0]

            def s1(it):
                G, t, e = it
                s = e % NSLOT
                b = rbank()
                sd[it] = {"b": b, "i": cnt[0] % 2}
                cnt[0] += 1
                for c in range(8):
                    mm(bank(b), xT[:, c, tcols(t)], WGU[s][:, c, :], c == 0, c == 7,
                       [("xT", t), ("WGU", s)], [("ps", b)], inc=(c == 7))

            def s2(it):
                G, t, e = it
                d = sd[it]
                b, i = d["b"], d["i"]
                act(SB[i][:, 0:256], bank(b)[:, 0:256], AF.Silu, [("ps", b)], [("SB", i)])
                stt(TB[i][:, 0:256], bank(b)[:, 256:512], gates[:, t, e:e + 1], SB[i][:, 0:256], ALU.mult, ALU.mult,
                    [("ps", b), ("gates", t), ("SB", i)], [("TB", i)])
                b2 = rbank()
                d["b2"] = b2
                for fc in range(2):
                    tr(bankb(b2)[:, fc * 128:(fc + 1) * 128], TB[i][:, fc * 128:(fc + 1) * 128], identb[:],
                       [("TB", i), "identb"], [("ps", b2)], inc=(fc == 1))

            def s3(it):
                G, t, e = it
                d = sd[it]
                b2, i = d["b2"], d["i"]
                s = e % NSLOT
                cp("act", TTB[i][:, 0:256], bankb(b2)[:, 0:256], [("ps", b2)], [("TTB", i)])
                first = (e % 4 == 0)
                lastx = (e % 4 == 3)
                for half in range(2):
                    yb = 4 + 2 * (t % 2) + half
                    for fc in range(2):
                        mm(bank(yb), TTB[i][:, fc * 128:(fc + 1) * 128], WD[s][:, fc, half * 512:(half + 1) * 512],
                           first and fc == 0, lastx and fc == 1, [("TTB", i), ("WD", s)], [("ps", yb)],
                           inc=(fc == 1))
                if t == NT - 1 and e + NSLOT < 16:
                    load_expert(e + NSLOT)
                if lastx:
                    for half in range(2):
                        yb = 4 + 2 * (t % 2) + half
                        xs = X[:, t, half * 512:(half + 1) * 512]
                        if G == 0:
                            stt(xs, xs, ALPHA, bank(yb), ALU.mult, ALU.add, [("X", t), ("ps", yb)], [("X", t)])
                        else:
                            tt(xs, xs, bank(yb), ALU.add, [("X", t), ("ps", yb)], [("X", t)])
                    if G == 3:
                        ln_stats(t, t % 2)

                        def fin(t=t):
                            ln_apply(t, t % 2)
                            if last_layer:
                                P.dma("sp", tm(out_d)[:, t, :], X[:, t, :], reads=[("X", t)], final=True)
                        bg.append(lambda: None)
                        bg.append(fin)

            pipeline(items, s1, s2, s3, bg_per_iter=1)

        def run_layers():
            for l in range(nlayers):
                for t in range(NT):
                    build_xT(t)
                if stop_after == "xT":
                    return False
                phase_A(l)
                P.barrier()
                if stop_after in ("A", "A1", "A2"):
                    return False
                phase_B(l)
                P.barrier()
                if stop_after == "B":
                    return False
                phase_C(l)
                P.barrier()
                if stop_after == "C":
                    return False
                phase_moe(l, l == nlayers - 1)
                P.barrier()
            return True

        if not run_layers():
            for t in range(NT):
                P.dma("sp", tm(out_d)[:, t, :], X[:, t, :], reads=[("X", t)], final=True)

        P.emit(nc, sems)
    return nc


_CACHE = {}


def kernel(**inputs):
    consts = _consts()
    if "nc" not in _CACHE:
        _CACHE["nc"] = build_program()
    nc = _CACHE["nc"]
    x = np.ascontiguousarray(np.asarray(inputs["x"], dtype=np.float32))
    shared = {}
    for k, v in inputs.items():
        if k == "x":
            continue
        shared[k] = np.ascontiguousarray(np.asarray(v, dtype=np.float32))
    shared.update(consts)
    in_maps = []
    for c in range(NCORES):
        m = dict(shared)
        m["x"] = x[c]
        in_maps.append(m)
    res = run_bass_kernel_spmd(nc, in_maps, core_ids=list(range(NCORES)))
    out = np.stack([np.asarray(r["out"], dtype=np.float32) for r in res.results], axis=0)
    return out
```

```python
import contextlib
import numpy as np
import concourse.bass as bass
import concourse.mybir as mybir
from concourse.bass_utils import run_bass_kernel_spmd

F32 = mybir.dt.float32
BF16 = mybir.dt.bfloat16
AF = mybir.ActivationFunctionType
ALU = mybir.AluOpType
AX = mybir.AxisListType

S = 2048
D = 1024
NT = 16
NCORES = 8
DEPTH = 2
ALPHA = float((2 * DEPTH) ** 0.25)
IDXW = float((4 * 64) ** -0.5)
SC64 = float(64 ** -0.5)
SC96 = float(96 ** -0.5)
TOPK = 256
NBIS = 14
NEG = -1.0e30
NDMA_SEMS = 8
ACC_ENG = "dve"
LNP_ENG = "dve"


class Prog:
    ENGS = ("pe", "act", "dve", "pool", "sp")

    def __init__(self):
        self.ops = {e: [] for e in self.ENGS}
        self.cnt = {e: 0 for e in self.ENGS}
        self.last_w = {}
        self.readers = {}
        self.waited = {e: {} for e in self.ENGS}
        self.dma_val = {}
        self.dma_rr = {"sp": 0, "pool": 0}
        self.final_tokens = []

    def _deps(self, eng, reads, writes):
        deps = {}

        def add(tok, raw):
            src, val = tok
            if src == eng and eng == "pe":
                return
            if deps.get(src, 0) < val:
                deps[src] = val

        for k in reads:
            if k in self.last_w:
                add(self.last_w[k], True)
            if isinstance(k, tuple) and k[0] == "ps":
                for r in self.readers.get(k, ()):
                    if r[0] != eng:
                        add(r, False)
        for k in writes:
            if k in self.last_w:
                add(self.last_w[k], False)
            for r in self.readers.get(k, ()):
                add(r, False)
        waits = []
        for src, val in deps.items():
            if self.waited[eng].get(src, 0) >= val:
                continue
            self.waited[eng][src] = val
            waits.append((src, val))
        return waits

    def _record(self, tok, reads, writes):
        for k in writes:
            self.last_w[k] = tok
            self.readers[k] = []
        for k in reads:
            self.readers.setdefault(k, []).append(tok)

    def op(self, eng, fn, reads=(), writes=(), inc=True):
        waits = self._deps(eng, reads, writes)
        if inc:
            self.cnt[eng] += 1
            idx = self.cnt[eng]
        else:
            idx = self.cnt[eng] + 1
        tok = (eng, idx)
        self._record(tok, reads, writes)
        self.ops[eng].append((waits, fn, ("eng", eng) if inc else None))
        return tok

    def dma(self, q, out_ap, in_ap, reads=(), writes=(), final=False):
        i = self.dma_rr[q]
        self.dma_rr[q] = (i + 1) % NDMA_SEMS
        src = ("dma", q, i)
        prev = self.dma_val.get(src, 0)
        waits = self._deps(q, reads, writes)
        if prev and self.waited[q].get(src, 0) < prev:
            self.waited[q][src] = prev
            waits.append((src, prev))
        val = prev + 16
        self.dma_val[src] = val
        tok = (src, val)
        self._record(tok, reads, writes)

        def fn(e, out_ap=out_ap, in_ap=in_ap):
            return e.dma_start(out=out_ap, in_=in_ap)

        self.ops[q].append((waits, fn, ("dma", src)))
        if final:
            self.final_tokens.append(tok)
        return tok

    def barrier(self):
        snap = [(e, self.cnt[e]) for e in self.ENGS if self.cnt[e] > 0]
        snap += [(src, v) for src, v in self.dma_val.items()]
        for e in self.ENGS:
            waits = []
            for src, val in snap:
                if src == e:
                    continue
                if self.waited[e].get(src, 0) >= val:
                    continue
                self.waited[e][src] = val
                waits.append((src, val))
            if waits:
                self.ops[e].append((waits, None, None))

    def emit(self, nc, sems):
        fin = list(self.final_tokens)

        def replay(eng, e):
            for waits, fn, inc in self.ops[eng]:
                for src, val in waits:
                    e.wait_ge(sems[src], val)
                if fn is None:
                    continue
                ins = fn(e)
                if inc is not None:
                    if inc[0] == "eng":
                        ins.then_inc(sems[inc[1]], 1)
                    else:
                        ins.then_inc(sems[inc[1]], 16)
            if eng == "sp":
                for src, val in fin:
                    e.wait_ge(sems[src], val)

        with nc.Block() as block:
            @block.tensor
            def _(e):
                replay("pe", e)

            @block.scalar
            def _(e):
                replay("act", e)

            @block.vector
            def _(e):
                replay("dve", e)

            @block.gpsimd
            def _(e):
                replay("pool", e)

            @block.sync
            def _(e):
                replay("sp", e)


def _consts():
    pos = np.arange(S, dtype=np.float64)
    c = {}
    for dim, nm in ((64, "64"), (32, "32")):
        inv = 1.0 / (10000.0 ** (np.arange(0, dim, 2, dtype=np.float64) / dim))
        inv = inv.astype(np.float32).astype(np.float64)
        ang = (pos.astype(np.float32)[:, None] * inv.astype(np.float32)[None, :]).astype(np.float32)
        c["cos" + nm] = np.cos(ang.astype(np.float64)).astype(np.float32)
        c["sin" + nm] = np.sin(ang.astype(np.float64)).astype(np.float32)
    c["ident"] = np.eye(128, dtype=np.float32)
    qi = np.arange(128)[:, None]
    kj = np.arange(256)[None, :]
    diff = qi + 128 - kj
    c["maskA"] = ((diff >= 0) & (diff < 128)).astype(np.float32)
    kk = np.arange(128)[None, :]
    c["causal01"] = (kk <= qi).astype(np.float32)
    c["negmask"] = np.where(kk <= qi, 0.0, NEG).astype(np.float32)
    c["causalT"] = (qi <= kk).astype(np.float32)
    c["prevT"] = (qi > kk).astype(np.float32)
    c["maskPC"] = np.concatenate([c["prevT"], c["causalT"]], axis=1)
    return c


def build_program(nlayers=DEPTH, debug_mix=False, stop_after=None):
    nc = bass.Bass("TRN2", target_bir_lowering=False)

    def din(name, shape):
        return nc.dram_tensor(name, list(shape), F32, kind="ExternalInput").ap()

    x_d = din("x", [S, D])
    w_in_d = din("w_in", [DEPTH, D, 1892])
    sinks_d = din("attn_sinks", [DEPTH, 8])
    gq_d = din("c_q_norm_g", [DEPTH, 256])
    gkv_d = din("c_kv_norm_g", [DEPTH, 128])
    w_uq_d = din("w_uq", [DEPTH, 256, 384])
    w_ukv_d = din("w_ukv", [DEPTH, 128, 512])
    w_out_d = din("w_out", [DEPTH, D, D])
    ln1g_d = din("ln1_g", [DEPTH, D])
    ln1b_d = din("ln1_b", [DEPTH, D])
    w_router_d = din("w_router", [D, 16])
    rbias_d = din("router_bias", [16])
    w_gate_d = din("w_gate", [DEPTH, 16, D, 256])
    w_up_d = din("w_up", [DEPTH, 16, D, 256])
    w_down_d = din("w_down", [DEPTH, 16, 256, D])
    ln2g_d = din("ln2_g", [DEPTH, D])
    ln2b_d = din("ln2_b", [DEPTH, D])
    cos64_d = din("cos64", [S, 32])
    sin64_d = din("sin64", [S, 32])
    cos32_d = din("cos32", [S, 16])
    sin32_d = din("sin32", [S, 16])
    ident_d = din("ident", [128, 128])
    maskA_d = din("maskA", [128, 256])
    causal_d = din("causal01", [128, 128])
    negmask_d = din("negmask", [128, 128])
    causalT_d = din("causalT", [128, 128])
    prevT_d = din("prevT", [128, 128])
    maskPC_d = din("maskPC", [128, 256])
    out_d = nc.dram_tensor("out", [S, D], F32, kind="ExternalOutput").ap()
    dbg_d = None
    if debug_mix:
        dbg_d = nc.dram_tensor("dbg", [S, D], F32, kind="ExternalOutput").ap()

    P = Prog()
    st = contextlib.ExitStack()
    with st:
        sems = {}
        for e in Prog.ENGS:
            sems[e] = st.enter_context(nc.semaphore("s_" + e))
        for q in ("sp", "pool"):
            for i in range(NDMA_SEMS):
                sems[("dma", q, i)] = st.enter_context(nc.semaphore(f"d_{q}{i}"))

        def T(name, shape, dt):
            return st.enter_context(nc.sbuf_tensor("sb_" + name, list(shape), dt))

        X = T("X", [128, NT, D], F32)
        xT = T("xT", [128, 8, S], BF16)
        cos64 = T("cos64", [128, NT, 32], F32)
        sin64 = T("sin64", [128, NT, 32], F32)
        nsin64 = T("nsin64", [128, NT, 32], F32)
        cos32 = T("cos32", [128, NT, 16], F32)
        sin32 = T("sin32", [128, NT, 16], F32)
        nsin32 = T("nsin32", [128, NT, 16], F32)
        identf = T("identf", [128, 128], F32)
        identb = T("identb", [128, 128], BF16)
        maskA = T("maskA", [128, 256], BF16)
        causalb = T("causalb", [128, 128], BF16)
        negmask = T("negmask", [128, 128], F32)
        causalT = T("causalT", [128, 128], BF16)
        prevT = T("prevT", [128, 128], BF16)
        maskPC = T("maskPC", [128, 256], BF16)
        ones1 = T("ones1", [1, 128], F32)
        lnp = T("lnp", [128, 2, D], F32)
        gates = T("gates", [128, NT, 16], F32)
        widx = T("widx", [128, NT, 4], F32)
        wr = T("wr", [128, 8, 16], F32)
        rb = T("rb", [128, 16], F32)
        sinks = T("sinks", [128, 8], F32)
        gq = T("gq", [128, 256], F32)
        gkv = T("gkv", [128, 128], F32)
        sm = T("sm", [128, 256], F32)
        ARW = 22272
        arena = T("arena", [128, ARW], F32)
        psum = st.enter_context(nc.psum_tensor("psum", [128, 4096], F32))

        def bank(i):
            return psum[:, 512 * i:512 * (i + 1)]

        def bankb(i):
            return psum[:, 512 * i:512 * (i + 1)].bitcast(BF16)

        class Arena:
            def __init__(self):
                self.off = 0

            def f32(self, n):
                o = self.off
                self.off += n
                assert self.off <= ARW, self.off
                return arena[:, o:o + n]

            def bf16(self, n):
                w = (n + 1) // 2
                o = self.off
                self.off += w
                assert self.off <= ARW, self.off
                return arena[:, o:o + w].bitcast(BF16)

        A = Arena()
        WIN = A.bf16(8 * 768)
        WOUT = A.bf16(4 * 1024)
        HSB = [A.f32(768), A.f32(768)]
        phase_qkr_off = A.off
        QKR = [A.f32(768), A.f32(768)]
        SQ = A.f32(768)
        PB = [A.bf16(512), A.bf16(512)]
        PTB = [A.bf16(512), A.bf16(512)]
        ROPET = A.f32(640)
        MIXT = A.bf16(512)
        MIXTT = A.bf16(512)
        phase_base = A.off

        rot = [0]
        rot4 = [0]
        inpipe = [False]

        def rbank():
            i = rot[0]
            rot[0] = (i + 1) % 3
            return i

        def mbank():
            if inpipe[0]:
                return 3
            i = rot4[0]
            rot4[0] = (i + 1) % 4
            return i

        def mm(out, lhsT, rhs, start, stop, reads, writes, inc):
            P.op("pe", lambda e, o=out, l=lhsT, r=rhs, s0=start, s1=stop: e.matmul(o, lhsT=l, rhs=r, start=s0, stop=s1),
                 reads=reads, writes=writes, inc=inc)

        def tr(out, in_, ident, reads, writes, inc=True):
            P.op("pe", lambda e, o=out, i=in_, d=ident: e.transpose(out=o, in_=i, identity=d),
                 reads=reads, writes=writes, inc=inc)

        def act(out, in_, func, reads, writes, bias=None, scale=None, accum=None):
            kw = {}
            if bias is not None:
                kw["bias"] = bias
            if scale is not None:
                kw["scale"] = scale
            if accum is not None:
                kw["accum_out"] = accum
            P.op("act", lambda e, o=out, i=in_, f=func, kw=kw: e.activation(out=o, in_=i, func=f, **kw),
                 reads=reads, writes=writes)

        def tt(out, in0, in1, op, reads, writes, eng="dve"):
            P.op(eng, lambda e, o=out, a=in0, b=in1, p=op: e.tensor_tensor(out=o, in0=a, in1=b, op=p),
                 reads=reads, writes=writes)

        def ts(out, in0, s1, s2, op0, op1, reads, writes, accum=None, eng="dve"):
            def fn(e, o=out, a=in0, s1=s1, s2=s2, op0=op0, op1=op1, accum=accum):
                kw = {}
                if op1 is not None:
                    kw["op1"] = op1
                if accum is not None:
                    kw["accum_out"] = accum
                return e.tensor_scalar(out=o, in0=a, scalar1=s1, scalar2=s2, op0=op0, **kw)
            P.op(eng, fn, reads=reads, writes=writes)

        def stt(out, in0, scalar, in1, op0, op1, reads, writes, eng="dve"):
            P.op(eng, lambda e, o=out, a=in0, s=scalar, b=in1, p0=op0, p1=op1:
                 e.scalar_tensor_tensor(out=o, in0=a, scalar=s, in1=b, op0=p0, op1=p1),
                 reads=reads, writes=writes)

        def cp(eng, out, in_, reads, writes):
            if eng == "act":
                act(out, in_, AF.Copy, reads, writes)
            else:
                P.op(eng, lambda e, o=out, i=in_: e.tensor_copy(out=o, in_=i), reads=reads, writes=writes)

        def red(out, in_, op, reads, writes, absv=False):
            def fn(e, o=out, i=in_, p=op, a=absv):
                if a:
                    return e.tensor_reduce(out=o, in_=i, axis=AX.X, op=p, apply_absolute_value=True)
                return e.tensor_reduce(out=o, in_=i, axis=AX.X, op=p)
            P.op("dve", fn, reads=reads, writes=writes)

        def memset(ap, val, writes, eng="dve"):
            P.op(eng, lambda e, a=ap, v=val: e.memset(a, v), writes=writes)

        def recip(out, in_, reads, writes):
            P.op("dve", lambda e, o=out, i=in_: e.reciprocal(out=o, in_=i), reads=reads, writes=writes)

        bg = []

        def bg_run(k):
            for _ in range(k):
                if not bg:
                    return
                bg.pop(0)()

        def pipeline(items, s1, s2, s3, bg_per_iter=0):
            n = len(items)
            inpipe[0] = True
            for i in range(n + 2):
                if i < n:
                    s1(items[i])
                if 0 <= i - 1 < n:
                    s2(items[i - 1])
                if 0 <= i - 2 < n:
                    s3(items[i - 2])
                if bg_per_iter:
                    bg_run(bg_per_iter)
            bg_run(len(bg))
            inpipe[0] = False

        def run_tiles(Pf, Rf):
            Pf(0)
            for t in range(NT):
                if t + 1 < NT:
                    Pf(t + 1)
                Rf(t)

        def tcols(t):
            return slice(t * 128, (t + 1) * 128)

        PBs = [PB[0], PB[1], PTB[0]]
        pbrot = [0]

        def st1(it):
            b = rbank()
            it["b"] = b
            k = it["kind"]
            if k == "att":
                it["pi"] = pbrot[0]
                pbrot[0] = (pbrot[0] + 1) % 3
                sl = it["slots"]
                for j, s in enumerate(sl):
                    ka, kk = s["K"]
                    qa, qk = s["Q"]
                    mm(bank(b)[:, j * 128:(j + 1) * 128], ka, qa, True, True, kk + qk, [("ps", b)], inc=(j == len(sl) - 1))
            elif k == "idx":
                mm(bank(b)[:, 0:it["n"]], it["lhsT"], it["rhs"], True, True, it["keys"], [("ps", b)], True)
            elif k == "mskT":
                kts = it["kts"]
                for j, kt in enumerate(kts):
                    tr(bankb(b)[:, j * 128:(j + 1) * 128], it["src"][:, kt * 128:(kt + 1) * 128], identb[:],
                       [it["srckey"], "identb"], [("ps", b)], inc=(j == len(kts) - 1))

        def st2(it):
            b = it["b"]
            k = it["kind"]
            if k == "att":
                pi = it["pi"]
                n = 128 * len(it["slots"])
                act(PBs[pi][:, 0:n], bank(b)[:, 0:n], AF.Exp, [("ps", b), "negc"], [("PB", pi)], bias=it["negc"], scale=it["scale"])
                for (c0, c1, view, m_ap, mk) in it["masks"]:
                    pv = PBs[pi][:, c0:c1]
                    if view is not None:
                        pv = pv.rearrange(view[0], **view[1])
                    tt(pv, pv, m_ap, ALU.mult, [("PB", pi)] + mk, [("PB", pi)])
            elif k == "idx":
                ri, n, h, qb, k0 = it["ri"], it["n"], it["h"], it["qb"], it["k0"]
                sco, sk = it["sco"], it["scokey"]
                act(it["rb"][:, 0:n], bank(b)[:, 0:n], AF.Relu, [("ps", b)], ["RB%d" % ri])
                if h == 0:
                    ts(sco[:, k0:k0 + n], it["rb"][:, 0:n], widx[:, qb, 0:1], None, ALU.mult, None,
                       ["RB%d" % ri, ("widx", qb)], [sk], eng=ACC_ENG)
                elif ACC_ENG == "dve":
                    stt(sco[:, k0:k0 + n], it["rb"][:, 0:n], widx[:, qb, h:h + 1], sco[:, k0:k0 + n],
                        ALU.mult, ALU.add, ["RB%d" % ri, ("widx", qb), sk], [sk])
                else:
                    ts(it["rb"][:, 0:n], it["rb"][:, 0:n], widx[:, qb, h:h + 1], None, ALU.mult, None,
                       ["RB%d" % ri, ("widx", qb)], ["RB%d" % ri], eng=ACC_ENG)
                    tt(sco[:, k0:k0 + n], sco[:, k0:k0 + n], it["rb"][:, 0:n], ALU.add, ["RB%d" % ri, sk], [sk], eng=ACC_ENG)
            elif k == "mskT":
                kts = it["kts"]
                nj = len(kts)
                cp("act", it["dst"][:, kts[0]:kts[0] + nj, :],
                   bankb(b)[:, 0:nj * 128].rearrange("p (c n) -> p c n", c=nj), [("ps", b)], [it["dstkey"]])

        def st3(it):
            if it["kind"] != "att":
                return
            pi = it["pi"]
            sl = it["slots"]
            for j, s in enumerate(sl):
                va, vk = s["V"]
                ob = s["ob"]
                mm(bank(ob)[:, s["oreg"]:s["oreg"] + 65], PBs[pi][:, j * 128:(j + 1) * 128], va, s["start"], s["stop"],
                   [("PB", pi)] + vk, [("ps", ob)], inc=(j == len(sl) - 1 or sl[j + 1]["ob"] != ob))
            if it.get("fin") is not None:
                it["fin"]()

        def run_seq(seq, bg_per_iter=0):
            n = len(seq)
            inpipe[0] = True
            for i in range(n + 2):
                if i < n and seq[i][0] is not None:
                    st1(seq[i][0])
                if 0 <= i - 1 < n and seq[i - 1][0] is not None:
                    st2(seq[i - 1][0])
                if 0 <= i - 2 < n and seq[i - 2][0] is not None:
                    st3(seq[i - 2][0])
                if i < n:
                    for c in seq[i][1]:
                        c()
                if bg_per_iter:
                    bg_run(bg_per_iter)
            bg_run(len(bg))
            inpipe[0] = False

        def fin_generic(qb, ob, nheads, nchunks_w, mix_c0, epilogue):
            def fn():
                o3 = bank(ob)[:, 0:nheads * 65].rearrange("p (h d) -> p h d", h=nheads)
                rc = sm[:, 72:72 + nheads]
                recip(rc, o3[:, :, 64], [("ps", ob)], ["rc"])
                tt(MIXT[:, 0:nheads * 64].rearrange("p (h d) -> p h d", h=nheads), o3[:, :, 0:64],
                   rc.unsqueeze(2).to_broadcast([128, nheads, 64]), ALU.mult, [("ps", ob), "rc"], ["MIXT"])
                dbg_store(qb, mix_c0, nheads * 64)
                out_proj_partial(qb, nchunks_w, False, after=epilogue)
            return fn

        def tm(ap_d):
            return ap_d.rearrange("(t p) d -> p t d", p=128)

        P.dma("sp", cos64[:], tm(cos64_d), writes=["cos64"])
        P.dma("sp", sin64[:], tm(sin64_d), writes=["sin64"])
        P.dma("sp", cos32[:], tm(cos32_d), writes=["cos32"])
        P.dma("sp", sin32[:], tm(sin32_d), writes=["sin32"])
        P.dma("sp", identf[:], ident_d, writes=["identf"])
        P.dma("pool", maskA[:], maskA_d, writes=["maskA"])
        P.dma("pool", causalb[:], causal_d, writes=["causalb"])
        P.dma("pool", identb[:], ident_d, writes=["identb"])
        P.dma("pool", causalT[:], causalT_d, writes=["causalT"])
        P.dma("pool", prevT[:], prevT_d, writes=["prevT"])
        P.dma("pool", maskPC[:], maskPC_d, writes=["maskPC"])
        P.dma("sp", negmask[:], negmask_d, writes=["negmask"])
        P.dma("sp", wr[:], w_router_d.rearrange("(c p) n -> p c n", p=128), writes=["wr"])
        P.dma("sp", rb[:], rbias_d.partition_broadcast(128), writes=["rb"])
        xv = tm(x_d)
        for t4 in range(4):
            P.dma("sp", X[:, 4 * t4:4 * t4 + 4, :], xv[:, 4 * t4:4 * t4 + 4, :],
                  writes=[("X", t) for t in range(4 * t4, 4 * t4 + 4)])
        ts(nsin64[:], sin64[:], -1.0, None, ALU.mult, None, ["sin64"], ["nsin64"])
        ts(nsin32[:], sin32[:], -1.0, None, ALU.mult, None, ["sin32"], ["nsin32"])
        memset(ones1[:], 1.0, ["ones1"])

        def build_xT(t):
            for half in range(2):
                b = mbank()
                for j in range(4):
                    c = half * 4 + j
                    tr(bank(b)[:, j * 128:(j + 1) * 128], X[:, t, c * 128:(c + 1) * 128], identf[:],
                       [("X", t), "identf"], [("ps", b)], inc=(j == 3))
                cp("act" if half == 0 else "dve",
                   xT[:, half * 4:half * 4 + 4, tcols(t)],
                   bank(b).rearrange("p (c n) -> p c n", c=4),
                   [("ps", b)], [("xT", t)])

        def in_proj(t, ncols, wview, hs):
            n0 = 0
            while n0 < ncols:
                n1 = min(ncols, n0 + 512)
                b = mbank()
                for c in range(8):
                    mm(bank(b)[:, 0:n1 - n0], xT[:, c, tcols(t)], wview[:, c, n0:n1], c == 0, c == 7,
                       [("xT", t), "WIN"], [("ps", b)], inc=(c == 7))
                cp("act", hs[:, n0:n1], bank(b)[:, 0:n1 - n0], [("ps", b)], ["HSB%d" % (t % 2)])
                n0 = n1

        def rope(src, dst, nh, hd, t, cosT, sinT, nsinT, rk, wk, tmp=None):
            h2 = hd // 2
            cb = cosT[:, t, :].unsqueeze(1).unsqueeze(1).to_broadcast([128, nh, 2, h2])
            sb = sinT[:, t, :].unsqueeze(1).to_broadcast([128, nh, h2])
            nb = nsinT[:, t, :].unsqueeze(1).to_broadcast([128, nh, h2])
            tv = ROPET[:, 0:nh * hd].rearrange("p (h t d) -> p h t d", h=nh, t=2)
            rk = rk + ["cos64", "sin64", "nsin64", "cos32", "sin32", "nsin32"]
            tt(dst, src, cb, ALU.mult, rk, wk)
            tt(tv[:, :, 0, :], src[:, :, 1, :], nb, ALU.mult, rk, ["ropetmp"])
            tt(tv[:, :, 1, :], src[:, :, 0, :], sb, ALU.mult, rk, ["ropetmp"])
            tt(dst, dst, tv, ALU.add, wk + ["ropetmp"], wk)

        def global_bound(mt, nh, qsl, ksl, scale, negc):
            b = mbank()
            tr(bank(b)[0:nh, 0:128], mt, identf[:], ["mt", "identf"], [("ps", b)])
            red(sm[0:nh, 0:1], bank(b)[0:nh, 0:128], ALU.max, [("ps", b)], ["gb1"])
            b2 = mbank()
            tr(bank(b2)[0:1, 0:nh], sm[0:nh, 0:1], identf[0:nh, 0:nh], ["gb1", "identf"], [("ps", b2)])
            red(sm[0:1, 1:2], bank(b2)[0:1, qsl], ALU.max, [("ps", b2)], ["gb2"])
            red(sm[0:1, 2:3], bank(b2)[0:1, ksl], ALU.max, [("ps", b2)], ["gb3"])
            tt(sm[0:1, 3:4], sm[0:1, 1:2], sm[0:1, 2:3], ALU.mult, ["gb2", "gb3"], ["gb4"])
            act(sm[0:1, 4:5], sm[0:1, 3:4], AF.Ln, ["gb4"], ["gb5"])
            act(sm[0:1, 5:6], sm[0:1, 4:5], AF.Exp, ["gb5"], ["gb6"], scale=0.5)
            ts(sm[0:1, 6:7], sm[0:1, 5:6], -scale, None, ALU.mult, None, ["gb6"], ["gb7"])
            b3 = mbank()
            mm(bank(b3)[:, 0:1], ones1[0:1, :], sm[0:1, 6:7], True, True, ["gb7", "ones1"], [("ps", b3)], True)
            cp("dve", negc, bank(b3)[:, 0:1], [("ps", b3)], ["negc"])

        def head_sumsq(src, ncols, nh, mt, first, rk):
            act(SQ[:, 0:ncols], src, AF.Square, rk, ["SQ"])
            hd = ncols // nh
            if first:
                red(mt, SQ[:, 0:ncols].rearrange("p (h d) -> p h d", h=nh), ALU.add, ["SQ"], ["mt"])
            else:
                red(sm[:, 16:16 + nh], SQ[:, 0:ncols].rearrange("p (h d) -> p h d", h=nh), ALU.add, ["SQ"], ["hs"])
                tt(mt, mt, sm[:, 16:16 + nh], ALU.max, ["mt", "hs"], ["mt"])

        def out_proj_T(qb, nchunks):
            b = mbank()
            for c in range(nchunks):
                tr(bankb(b)[:, c * 128:(c + 1) * 128], MIXT[:, c * 128:(c + 1) * 128], identb[:],
                   ["MIXT", "identb"], [("ps", b)], inc=(c == nchunks - 1))
            cp("act", MIXTT[:, 0:nchunks * 128], bankb(b)[:, 0:nchunks * 128], [("ps", b)], ["MIXTT"])

        def out_proj_M(qb, nchunks, first):
            wv = WOUT.rearrange("p (c n) -> p c n", n=1024)
            for half in range(2):
                yb = 6 + half
                for c in range(nchunks):
                    mm(bank(yb), MIXTT[:, c * 128:(c + 1) * 128], wv[:, c, half * 512:(half + 1) * 512],
                       c == 0, c == nchunks - 1, ["MIXTT", "WOUT"], [("ps", yb)], inc=(c == nchunks - 1))
                xs = X[:, qb, half * 512:(half + 1) * 512]
                if first:
                    stt(xs, xs, ALPHA, bank(yb), ALU.mult, ALU.add, [("X", qb), ("ps", yb)], [("X", qb)])
                else:
                    tt(xs, xs, bank(yb), ALU.add, [("X", qb), ("ps", yb)], [("X", qb)])

        def out_proj_partial(qb, nchunks, first, after=None):
            bg.append(lambda: out_proj_T(qb, nchunks))

            def part2():
                out_proj_M(qb, nchunks, first)
                if after is not None:
                    after(qb)
            bg.append(part2)

        def dbg_store(qb, c0, ncols):
            if dbg_d is None:
                return
            cp("dve", ROPET[:, 0:ncols], MIXT[:, 0:ncols], ["MIXT"], ["ropetmp"])
            P.dma("sp", tm(dbg_d)[:, qb, c0:c0 + ncols], ROPET[:, 0:ncols], reads=["ropetmp"], final=True)

        def ln_stats(t, k):
            o = 32 + 16 * k
            ks = "ln%d" % k
            P.op("dve", lambda e, o_=sm[:, o:o + 6], i=X[:, t, 0:512]: e.bn_stats(out=o_, in_=i), reads=[("X", t)], writes=[ks + "a"])
            P.op("dve", lambda e, o_=sm[:, o + 6:o + 12], i=X[:, t, 512:1024]: e.bn_stats(out=o_, in_=i), reads=[("X", t)], writes=[ks + "b"])
            P.op("dve", lambda e, o_=sm[:, o + 12:o + 14], i=sm[:, o:o + 12]: e.bn_aggr(out=o_, in_=i), reads=[ks + "a", ks + "b"], writes=[ks + "mv"])
            act(sm[:, o + 14:o + 15], sm[:, o + 13:o + 14], AF.Ln, [ks + "mv"], [ks + "lv"], bias=1e-5)
            act(sm[:, o + 15:o + 16], sm[:, o + 14:o + 15], AF.Exp, [ks + "lv"], [ks + "rs"], scale=-0.5)

        def ln_apply(t, k):
            o = 32 + 16 * k
            ks = "ln%d" % k
            xs = X[:, t, :]
            ts(xs, xs, sm[:, o + 12:o + 13], sm[:, o + 15:o + 16], ALU.subtract, ALU.mult, [("X", t), ks + "mv", ks + "rs"], [("X", t)])
            tt(xs, xs, lnp[:, 0, :], ALU.mult, [("X", t), "lnp"], [("X", t)], eng=LNP_ENG)
            tt(xs, xs, lnp[:, 1, :], ALU.add, [("X", t), "lnp"], [("X", t)], eng=LNP_ENG)

        def layer_norm(t, k=0):
            ln_stats(t, k)
            ln_apply(t, k)

        def load_ln(g_d, b_d, l):
            P.dma("sp", lnp[:, 0, :], g_d[l].partition_broadcast(128), writes=["lnp"])
            P.dma("sp", lnp[:, 1, :], b_d[l].partition_broadcast(128), writes=["lnp"])

        def phase_A(l):
            A.off = phase_base
            FM = A.bf16(6 * S).rearrange("p (c n) -> p c n", c=6)
            VA = A.bf16(NT * 2 * 65).rearrange("p (t g d) -> p t g d", t=NT, g=2)
            mt = A.f32(16)[:, 0:10]
            negc = A.f32(2)[:, 0:1]
            esink = A.f32(8)
            den = A.f32(8)
            wv = WIN[:, 0:8 * 768].rearrange("p (c n) -> p c n", c=8)
            P.dma("pool", wv, w_in_d[l].rearrange("(c p) n -> p c n", p=128)[:, :, 0:768], writes=["WIN"])
            P.dma("pool", WOUT[:, 0:4096].rearrange("p (c n) -> p c n", c=4),
                  w_out_d[l, 0:512, :].rearrange("(c p) n -> p c n", p=128), writes=["WOUT"])
            P.dma("sp", sinks[:], sinks_d[l].partition_broadcast(128), writes=["sinks"])
            memset(VA[:, :, :, 64:65], 1.0, ["VAones"])
            def Pf(t):
                in_proj(t, 768, wv, HSB[t % 2])

            def Rf(t):
                hs = HSB[t % 2]
                qk = QKR[t % 2]
                hk = "HSB%d" % (t % 2)
                qkk = "QKR%d" % (t % 2)
                head_sumsq(hs[:, 0:640], 640, 10, mt, t == 0, [hk])
                rope(hs[:, 0:512].rearrange("p (h t d) -> p h t d", h=8, t=2),
                     qk[:, 0:512].rearrange("p (h t d) -> p h t d", h=8, t=2),
                     8, 64, t, cos64, sin64, nsin64, [hk], [qkk])
                kdst = qk[:, 512:768].rearrange("p (g r d) -> p g r d", g=2, r=2)
                rope(hs[:, 512:640].rearrange("p (h t d) -> p h t d", h=2, t=2),
                     kdst[:, :, 0, :].rearrange("p g (t d) -> p g t d", t=2),
                     2, 64, t, cos64, sin64, nsin64, [hk], [qkk])
                cp("dve", kdst[:, :, 1, :], kdst[:, :, 0, :], [qkk], [qkk])
                cp("act", VA[:, t, :, 0:64], hs[:, 640:768].rearrange("p (g d) -> p g d", g=2), [hk], [("VA", t)])
                for part, (c0, nchk) in enumerate(((0, 4), (4, 2))):
                    b = mbank()
                    for j in range(nchk):
                        c = c0 + j
                        tr(bank(b)[:, j * 128:(j + 1) * 128], qk[:, c * 128:(c + 1) * 128], identf[:],
                           [qkk, "identf"], [("ps", b)], inc=(j == nchk - 1))
                    cp("act" if part == 0 else "dve", FM[:, c0:c0 + nchk, tcols(t)],
                       bank(b)[:, 0:nchk * 128].rearrange("p (c n) -> p c n", c=nchk),
                       [("ps", b)], [("FM", t)])

            run_tiles(Pf, Rf)
            if stop_after == "A1":
                return
            global_bound(mt, 10, slice(0, 8), slice(8, 10), SC64, negc)
            act(esink, sinks[:], AF.Exp, ["sinks", "negc"], ["esink"], bias=negc)
            if stop_after == "A2":
                return

            def finA(qb, g):
                def fn():
                    ob = 4 + g
                    o3 = bank(ob)[:, 0:260].rearrange("p (h d) -> p h d", h=4)
                    tt(den[:, 4 * g:4 * g + 4], o3[:, :, 64], esink[:, 4 * g:4 * g + 4], ALU.add,
                       [("ps", ob), "esink"], ["den"])
                    recip(den[:, 4 * g:4 * g + 4], den[:, 4 * g:4 * g + 4], ["den"], ["den"])
                    tt(MIXT[:, g * 256:(g + 1) * 256].rearrange("p (h d) -> p h d", h=4), o3[:, :, 0:64],
                       den[:, 4 * g:4 * g + 4].unsqueeze(2).to_broadcast([128, 4, 64]), ALU.mult,
                       [("ps", ob), "den"], ["MIXT"])
                    if g == 1:
                        dbg_store(qb, 0, 512)
                        out_proj_partial(qb, 4, True)
                return fn

            seq = []
            for qb in range(NT):
                kts = [kt for kt in (qb - 1, qb) if kt >= 0]
                nk_ = len(kts)
                for g in range(2):
                    for par in range(2):
                        po = 64 * par
                        slots = []
                        for h in (4 * g + par, 4 * g + par + 2):
                            for kt in kts:
                                slots.append({"K": (FM[po:po + 64, 4 + g, tcols(kt)], [("FM", kt)]),
                                              "Q": (FM[po:po + 64, h // 2, tcols(qb)], [("FM", qb)]),
                                              "V": (VA[:, kt, g, :], [("VA", kt), "VAones"]),
                                              "ob": 4 + g, "oreg": (h % 4) * 65,
                                              "start": kt == kts[0], "stop": kt == kts[-1]})
                        if nk_ == 2:
                            masks = [(0, 512, ("p (h n) -> p h n", {"h": 2}),
                                      maskPC[:].unsqueeze(1).to_broadcast([128, 2, 256]), ["maskPC"])]
                        else:
                            masks = [(0, 256, ("p (h n) -> p h n", {"h": 2}),
                                      maskPC[:, 128:256].unsqueeze(1).to_broadcast([128, 2, 128]), ["maskPC"])]
                        seq.append(({"kind": "att", "slots": slots, "scale": SC64, "negc": negc, "masks": masks,
                                     "fin": finA(qb, g) if par == 1 else None}, []))
            run_seq(seq, bg_per_iter=1)

        def phase_B(l):
            A.off = phase_base
            FM = A.bf16(6 * S).rearrange("p (c n) -> p c n", c=6)
            VB = A.bf16(NT * 65 + 1)[:, 0:NT * 65].rearrange("p (t d) -> p t d", t=NT)
            SCO = A.f32(S)
            MSK = A.bf16(S)
            RB = [HSB[0][:, 0:512], HSB[1][:, 0:512]]
            mt = A.f32(16)[:, 0:5]
            negc = A.f32(2)[:, 0:1]
            bis = A.f32(8)
            wv = WIN[:, 0:8 * 708].rearrange("p (c n) -> p c n", c=8)
            P.dma("pool", wv, w_in_d[l].rearrange("(c p) n -> p c n", p=128)[:, :, 768:1476], writes=["WIN"])
            P.dma("pool", WOUT[:, 0:2048].rearrange("p (c n) -> p c n", c=2),
                  w_out_d[l, 512:768, :].rearrange("(c p) n -> p c n", p=128), writes=["WOUT"])
            memset(VB[:, :, 64:65], 1.0, ["VBones"])
            def Pf(t):
                in_proj(t, 708, wv, HSB[t % 2])

            def Rf(t):
                hs = HSB[t % 2]
                qk = QKR[t % 2]
                hk = "HSB%d" % (t % 2)
                qkk = "QKR%d" % (t % 2)
                head_sumsq(hs[:, 0:320], 320, 5, mt, t == 0, [hk])
                for (s0, d0) in ((0, 0), (384, 384)):
                    rope(hs[:, s0:s0 + 320].rearrange("p (h t d) -> p h t d", h=5, t=2),
                         qk[:, d0:d0 + 320].rearrange("p (h t d) -> p h t d", h=5, t=2),
                         5, 64, t, cos64, sin64, nsin64, [hk], [qkk])
                    cp("dve", qk[:, d0 + 320:d0 + 384], qk[:, d0 + 256:d0 + 320], [qkk], [qkk])
                cp("act", VB[:, t, 0:64], hs[:, 320:384], [hk], [("VB", t)])
                ts(widx[:, t, :], hs[:, 704:708], IDXW, None, ALU.mult, None, [hk], [("widx", t)])
                for part, (c0, nchk) in enumerate(((0, 4), (4, 2))):
                    b = mbank()
                    for j in range(nchk):
                        c = c0 + j
                        tr(bank(b)[:, j * 128:(j + 1) * 128], qk[:, c * 128:(c + 1) * 128], identf[:],
                           [qkk, "identf"], [("ps", b)], inc=(j == nchk - 1))
                    cp("act" if part == 0 else "dve", FM[:, c0:c0 + nchk, tcols(t)],
                       bank(b)[:, 0:nchk * 128].rearrange("p (c n) -> p c n", c=nchk),
                       [("ps", b)], [("FM", t)])

            run_tiles(Pf, Rf)
            global_bound(mt, 5, slice(0, 4), slice(4, 5), SC64, negc)

            P.barrier()
            SCOs = [SCO, arena[:, 0:S]]
            a_qkr = phase_qkr_off
            MSKT = [arena[:, a_qkr + 1024 * i:a_qkr + 1024 * (i + 1)].bitcast(BF16).rearrange("p (t n) -> p t n", t=NT)
                    for i in range(2)]

            def idx_items(qb):
                nk = (qb + 1) * 128
                par = qb % 2
                out = []
                for k0 in range(0, nk, 512):
                    n = min(512, nk - k0)
                    for h in range(4):
                        po = 64 * (h % 2)
                        ri = (len(out)) % 2
                        out.append({"kind": "idx", "n": n, "h": h, "qb": qb, "k0": k0, "ri": ri, "rb": RB[ri],
                                    "lhsT": FM[po:po + 64, 3 + h // 2, tcols(qb)], "rhs": FM[po:po + 64, 5, k0:k0 + n],
                                    "keys": [("FM", t) for t in range(qb + 1)],
                                    "sco": SCOs[par], "scokey": "SCO%d" % par})
                return out

            def bis_closures(qb):
                nk = (qb + 1) * 128
                par = qb % 2
                sco = SCOs[par]
                sk = "SCO%d" % par
                cl = []

                def init():
                    if qb >= 2:
                        red(bis[:, 0:1], sco[:, 0:nk], ALU.max, [sk], ["bnd"], absv=True)
                    tt(sco[:, qb * 128:nk], sco[:, qb * 128:nk], negmask[:], ALU.add, [sk, "negmask"], [sk])
                    if qb >= 2:
                        ts(bis[:, 1:2], bis[:, 0:1], -1.0, -1.0, ALU.mult, ALU.add, ["bnd"], ["lo"])
                        ts(bis[:, 2:3], bis[:, 0:1], 2.0, 2.0, ALU.mult, ALU.add, ["bnd"], ["w0"])
                    else:
                        ts(MSK[:, 0:nk], sco[:, 0:nk], -1.0e29, None, ALU.is_ge, None, [sk], ["MSK"])
                cl.append(init)
                if qb >= 2:
                    def it_fn(k):
                        def fn():
                            f = 2.0 ** -(k + 1)
                            stt(bis[:, 3:4], bis[:, 2:3], f, bis[:, 1:2], ALU.mult, ALU.add, ["w0", "lo"], ["mid"])
                            ts(MSK[:, 0:nk], sco[:, 0:nk], bis[:, 3:4], None, ALU.is_ge, ALU.add, [sk, "mid"],
                               ["MSK", "cnt"], accum=bis[:, 4:5])
                            ts(bis[:, 5:6], bis[:, 4:5], float(TOPK), bis[:, 2:3], ALU.is_ge, ALU.mult,
                               ["cnt", "w0"], ["stp"])
                            stt(bis[:, 1:2], bis[:, 5:6], f, bis[:, 1:2], ALU.mult, ALU.add, ["stp", "lo"], ["lo"])
                        return fn
                    for k in range(NBIS):
                        cl.append(it_fn(k))
                    cl.append(lambda: ts(MSK[:, 0:nk], sco[:, 0:nk], bis[:, 1:2], None, ALU.is_ge, None, [sk, "lo"], ["MSK"]))
                return cl

            def mskT_items(qb):
                par = qb % 2
                out = []
                for kt0 in range(0, qb + 1, 4):
                    kts = list(range(kt0, min(kt0 + 4, qb + 1)))
                    out.append({"kind": "mskT", "kts": kts, "src": MSK, "srckey": "MSK",
                                "dst": MSKT[par], "dstkey": ("MSKT", par)})
                return out

            def att_items(qb):
                par = qb % 2
                out = []
                for h in range(4):
                    po = 64 * (h % 2)
                    for kt0 in range(0, qb + 1, 4):
                        kts = list(range(kt0, min(kt0 + 4, qb + 1)))
                        slots = [{"K": (FM[po:po + 64, 2, tcols(kt)], [("FM", kt)]),
                                  "Q": (FM[po:po + 64, h // 2, tcols(qb)], [("FM", qb)]),
                                  "V": (VB[:, kt, :], [("VB", kt), "VBones"]),
                                  "ob": 4 + par, "oreg": h * 65, "start": kt == 0, "stop": kt == qb} for kt in kts]
                        ns = len(kts)
                        masks = [(0, 128 * ns, ("p (c n) -> p c n", {"c": ns}), MSKT[par][:, kt0:kt0 + ns, :], [("MSKT", par)])]
                        last = (h == 3 and kts[-1] == qb)
                        out.append({"kind": "att", "slots": slots, "scale": SC64, "negc": negc, "masks": masks,
                                    "fin": fin_generic(qb, 4 + par, 4, 2, 512, None) if last else None})
                return out

            def merge(a, b):
                out = []
                na, nb = len(a), len(b)
                ia = ib = 0
                while ia < na or ib < nb:
                    if ib >= nb or (ia < na and ia * nb <= ib * na):
                        out.append(a[ia])
                        ia += 1
                    else:
                        out.append(b[ib])
                        ib += 1
                return out

            seq = []
            for r in range(-2, NT):
                ia = att_items(r) if r >= 0 else []
                ii = idx_items(r + 2) if r + 2 < NT else []
                cl = bis_closures(r + 1) if 0 <= r + 1 < NT else []
                im = mskT_items(r + 1) if 0 <= r + 1 < NT else []
                ents = [[it, []] for it in merge(ia, ii)]
                if not ents:
                    ents = [[None, []]]
                ne = len(ents)
                for ci, c in enumerate(cl):
                    ents[min(ne - 1, (ci * ne) // max(1, len(cl)))][1].append(c)
                seq.extend((e[0], e[1]) for e in ents)
                seq.extend((it, []) for it in im)
            run_seq(seq, bg_per_iter=1)

        def phase_C(l):
            A.off = phase_base
            FQ = A.bf16(4 * S).rearrange("p (c n) -> p c n", c=4)
            FK = A.bf16(4 * S).rearrange("p (c n) -> p c n", c=4)
            VC = A.bf16(NT * 4 * 65).rearrange("p (t h d) -> p t h d", t=NT, h=4)
            QCF = QKR[0]
            KCF = QKR[1]
            CQN = A.bf16(256)
            CKN = A.bf16(128)
            CQT = A.bf16(256)
            CKT = A.bf16(128)
            mt = A.f32(16)[:, 0:8]
            negc = A.f32(2)[:, 0:1]
            rt = A.f32(128)
            wv = WIN[:, 0:8 * 416].rearrange("p (c n) -> p c n", c=8)
            WUQ = WIN[:, 8 * 416:8 * 416 + 768].rearrange("p (c n) -> p c n", c=2)
            WUKV = WIN[:, 8 * 416 + 768:8 * 416 + 768 + 512]
            P.dma("pool", wv, w_in_d[l].rearrange("(c p) n -> p c n", p=128)[:, :, 1476:1892], writes=["WIN"])
            P.dma("pool", WUQ, w_uq_d[l].rearrange("(c p) n -> p c n", p=128), writes=["WIN"])
            P.dma("pool", WUKV, w_ukv_d[l], writes=["WIN"])
            P.dma("pool", WOUT[:, 0:2048].rearrange("p (c n) -> p c n", c=2),
                  w_out_d[l, 768:1024, :].rearrange("(c p) n -> p c n", p=128), writes=["WOUT"])
            P.dma("sp", gq[:], gq_d[l].partition_broadcast(128), writes=["gq"])
            P.dma("sp", gkv[:], gkv_d[l].partition_broadcast(128), writes=["gkv"])
            load_ln(ln1g_d, ln1b_d, l)
            memset(VC[:, :, :, 64:65], 1.0, ["VCones"])
            def Pf(t):
                in_proj(t, 416, wv, HSB[t % 2])

            def Rf(t):
                hs = HSB[t % 2]
                hk = "HSB%d" % (t % 2)
                act(SQ[:, 0:256], hs[:, 0:256], AF.Square, [hk], ["SQ"], accum=sm[:, 64:65])
                act(SQ[:, 256:384], hs[:, 256:384], AF.Square, [hk], ["SQ2"], accum=sm[:, 65:66])
                act(sm[:, 66:67], sm[:, 64:65], AF.Ln, ["SQ"], ["ln1"], scale=1.0 / 256, bias=1e-6)
                act(sm[:, 67:68], sm[:, 65:66], AF.Ln, ["SQ2"], ["ln2"], scale=1.0 / 128, bias=1e-6)
                act(sm[:, 68:69], sm[:, 66:67], AF.Exp, ["ln1"], ["rs1"], scale=-0.5)
                act(sm[:, 69:70], sm[:, 67:68], AF.Exp, ["ln2"], ["rs2"], scale=-0.5)
                stt(CQN[:, 0:256], hs[:, 0:256], sm[:, 68:69], gq[:], ALU.mult, ALU.mult, [hk, "rs1", "gq"], ["CQN"])
                stt(CKN[:, 0:128], hs[:, 256:384], sm[:, 69:70], gkv[:], ALU.mult, ALU.mult, [hk, "rs2", "gkv"], ["CKN"])
                b = mbank()
                tr(bankb(b)[:, 0:128], CQN[:, 0:128], identb[:], ["CQN", "identb"], [("ps", b)], inc=False)
                tr(bankb(b)[:, 128:256], CQN[:, 128:256], identb[:], ["CQN", "identb"], [("ps", b)], inc=False)
                tr(bankb(b)[:, 256:384], CKN[:, 0:128], identb[:], ["CKN", "identb"], [("ps", b)], inc=True)
                cp("act", CQT[:, 0:256], bankb(b)[:, 0:256], [("ps", b)], ["CQT"])
                cp("dve", CKT[:, 0:128], bankb(b)[:, 256:384], [("ps", b)], ["CKT"])
                bq = mbank()
                mm(bank(bq)[:, 0:384], CQT[:, 0:128], WUQ[:, 0, :], True, False, ["CQT", "WIN"], [("ps", bq)], False)
                mm(bank(bq)[:, 0:384], CQT[:, 128:256], WUQ[:, 1, :], False, True, ["CQT", "WIN"], [("ps", bq)], True)
                bk = mbank()
                mm(bank(bk)[:, 0:512], CKT[:, 0:128], WUKV, True, True, ["CKT", "WIN"], [("ps", bk)], True)
                cp("act", QCF[:, 0:384], bank(bq)[:, 0:384], [("ps", bq)], ["QKR0"])
                q4 = QCF[:, 0:384].rearrange("p (h d) -> p h d", h=4)
                qr = q4[:, :, 64:96].rearrange("p h (t d) -> p h t d", t=2)
                cp("dve", rt[:, 0:128].rearrange("p (h d) -> p h d", h=4), q4[:, :, 64:96], ["QKR0"], ["rt"])
                rope(rt[:, 0:128].rearrange("p (h t d) -> p h t d", h=4, t=2), qr, 4, 32, t,
                     cos32, sin32, nsin32, ["rt"], ["QKR0"])
                kv4 = bank(bk)[:, 0:512].rearrange("p (h d) -> p h d", h=4)
                k4 = KCF[:, 0:384].rearrange("p (h d) -> p h d", h=4)
                cp("act", k4[:, :, 0:64], kv4[:, :, 0:64], [("ps", bk)], ["QKR1"])
                cp("dve", VC[:, t, :, 0:64], kv4[:, :, 64:128], [("ps", bk)], [("VC", t)])
                rope(hs[:, 384:416].rearrange("p (h t d) -> p h t d", h=1, t=2),
                     rt[:, 0:32].rearrange("p (h t d) -> p h t d", h=1, t=2), 1, 32, t,
                     cos32, sin32, nsin32, [hk], ["rt"])
                cp("dve", k4[:, :, 64:96], rt[:, 0:32].unsqueeze(1).to_broadcast([128, 4, 32]), ["rt"], ["QKR1"])
                act(SQ[:, 0:384], QCF[:, 0:384], AF.Square, ["QKR0"], ["SQ"])
                act(SQ[:, 384:768], KCF[:, 0:384], AF.Square, ["QKR1"], ["SQ"])
                if t == 0:
                    red(mt, SQ[:, 0:768].rearrange("p (h d) -> p h d", h=8), ALU.add, ["SQ"], ["mt"])
                else:
                    red(sm[:, 16:24], SQ[:, 0:768].rearrange("p (h d) -> p h d", h=8), ALU.add, ["SQ"], ["hs"])
                    tt(mt, mt, sm[:, 16:24], ALU.max, ["mt", "hs"], ["mt"])
                for (src, dstF, key, eng) in ((QCF, FQ, "QKR0", "act"), (KCF, FK, "QKR1", "dve")):
                    b = mbank()
                    for h in range(4):
                        tr(bank(b)[0:96, h * 128:(h + 1) * 128], src[:, h * 96:(h + 1) * 96], identf[:],
                           [key, "identf"], [("ps", b)], inc=(h == 3))
                    cp(eng, dstF[0:96, :, tcols(t)], bank(b)[0:96, :].rearrange("p (c n) -> p c n", c=4),
                       [("ps", b)], [("F" + key, t)])

            run_tiles(Pf, Rf)
            global_bound(mt, 8, slice(0, 4), slice(4, 8), SC96, negc)

            P.barrier()
            RT = arena[:, phase_qkr_off:phase_qkr_off + 2304]

            def epilogue(qb):
                k = qb % 2
                bg.append(lambda: ln_stats(qb, k))
                bg.append(lambda: ln_apply(qb, k))

                def xpose(half):
                    b = mbank()
                    for j in range(4):
                        c = half * 4 + j
                        tr(bank(b)[:, j * 128:(j + 1) * 128], X[:, qb, c * 128:(c + 1) * 128], identf[:],
                           [("X", qb), "identf"], [("ps", b)], inc=(j == 3))
                    cp("act", xT[:, half * 4:half * 4 + 4, tcols(qb)], bank(b).rearrange("p (c n) -> p c n", c=4),
                       [("ps", b)], [("xT", qb)])
                    cp("dve", HSB[half][:, 0:512], bank(b), [("ps", b)], ["HSB%d" % half])

                bg.append(lambda: xpose(0))
                bg.append(lambda: xpose(1))
                def router_a():
                    b = mbank()
                    for c in range(8):
                        mm(bank(b)[:, 0:16], HSB[c // 4][:, (c % 4) * 128:(c % 4 + 1) * 128], wr[:, c, :], c == 0, c == 7,
                           ["HSB0", "HSB1", "wr"], [("ps", b)], inc=(c == 7))
                    act(RT[:, qb * 16:(qb + 1) * 16], bank(b)[:, 0:16], AF.Exp, [("ps", b)], [("r_sc", qb)], scale=-1.0)

                bg.append(router_a)

            seq = []
            for qb in range(NT):
                par = qb % 2
                for h in range(4):
                    for kt0 in range(0, qb + 1, 4):
                        kts = list(range(kt0, min(kt0 + 4, qb + 1)))
                        slots = [{"K": (FK[0:96, h, tcols(kt)], [("FQKR1", kt)]),
                                  "Q": (FQ[0:96, h, tcols(qb)], [("FQKR0", qb)]),
                                  "V": (VC[:, kt, h, :], [("VC", kt), "VCones"]),
                                  "ob": 4 + par, "oreg": h * 65, "start": kt == 0, "stop": kt == qb} for kt in kts]
                        ns = len(kts)
                        masks = []
                        if kts[-1] == qb:
                            masks = [(128 * (ns - 1), 128 * ns, None, causalT[:], ["causalT"])]
                        last = (h == 3 and kts[-1] == qb)
                        seq.append(({"kind": "att", "slots": slots, "scale": SC96, "negc": negc, "masks": masks,
                                     "fin": fin_generic(qb, 4 + par, 4, 2, 768, epilogue) if last else None}, []))
            run_seq(seq, bg_per_iter=2)

            sc = RT[:, 0:256]
            bi = RT[:, 256:512]
            m1 = RT[:, 512:576]
            eq = RT[:, 576:832]
            msk = RT[:, 832:1088]
            m2 = RT[:, 1088:1152]
            gs = RT[:, 1152:1216]
            gm = RT[:, 1216:1232]
            gsel = RT[:, 1232:1296]
            t2 = RT[:, 1296:1552]
            wgt = RT[:, 1552:1808]
            ws = RT[:, 1808:1824]
            rsk = [("r_sc", t) for t in range(NT)]

            def v3(ap, a):
                return ap.rearrange("p (a b) -> p a b", a=a)

            ts(sc, sc, 1.0, None, ALU.add, None, rsk, ["r_s"])
            recip(sc, sc, ["r_s"], ["r_s"])
            tt(v3(bi, 16), v3(sc, 16), rb[:].unsqueeze(1).to_broadcast([128, 16, 16]), ALU.add, ["r_s", "rb"], ["r_bi"])
            red(m1, v3(bi, 64), ALU.max, ["r_bi"], ["r_m1"])
            tt(v3(eq, 64), v3(bi, 64), m1.unsqueeze(2).to_broadcast([128, 64, 4]), ALU.is_equal, ["r_bi", "r_m1"], ["r_eq"])
            stt(msk, eq, NEG, bi, ALU.mult, ALU.add, ["r_eq", "r_bi"], ["r_msk"])
            red(m2, v3(msk, 64), ALU.max, ["r_msk"], ["r_m2"])
            tt(gs, m1, m2, ALU.add, ["r_m1", "r_m2"], ["r_gs"])
            red(gm, v3(gs, 16), ALU.max, ["r_gs"], ["r_gm"])
            tt(v3(gsel, 16), v3(gs, 16), gm.unsqueeze(2).to_broadcast([128, 16, 4]), ALU.is_equal, ["r_gs", "r_gm"], ["r_gsel"])
            tt(v3(t2, 64), v3(bi, 64), m2.unsqueeze(2).to_broadcast([128, 64, 4]), ALU.is_ge, ["r_bi", "r_m2"], ["r_t2"])
            tt(v3(t2, 64), v3(t2, 64), gsel.unsqueeze(2).to_broadcast([128, 64, 4]), ALU.mult, ["r_t2", "r_gsel"], ["r_t2"])
            tt(wgt, sc, t2, ALU.mult, ["r_s", "r_t2"], ["r_w"])
            red(ws, v3(wgt, 16), ALU.add, ["r_w"], ["r_ws"])
            recip(ws, ws, ["r_ws"], ["r_ws"])
            tt(gates[:], v3(wgt, 16), ws.unsqueeze(2).to_broadcast([128, 16, 16]), ALU.mult, ["r_w", "r_ws"],
               [("gates", t) for t in range(NT)])

        def phase_moe(l, last_layer):
            A.off = 0
            NSLOT = 7
            WGU = [A.bf16(8 * 512).rearrange("p (c n) -> p c n", c=8) for _ in range(NSLOT)]
            WD = [A.bf16(2 * 1024).rearrange("p (c n) -> p c n", c=2) for _ in range(NSLOT)]
            SB = [A.bf16(256) for _ in range(2)]
            TB = [A.bf16(256) for _ in range(2)]
            TTB = [A.bf16(256) for _ in range(2)]
            load_ln(ln2g_d, ln2b_d, l)
            def load_expert(e):
                s = e % NSLOT
                P.dma("pool", WGU[s][:, :, 0:256], w_gate_d[l, e].rearrange("(c p) f -> p c f", p=128), writes=[("WGU", s)])
                P.dma("pool", WGU[s][:, :, 256:512], w_up_d[l, e].rearrange("(c p) f -> p c f", p=128), writes=[("WGU", s)])
                P.dma("pool", WD[s], w_down_d[l, e].rearrange("(c p) d -> p c d", p=128), writes=[("WD", s)])

            for e in range(NSLOT):
                load_expert(e)
            items = [(G, t, e) for G in range(4) for t in range(NT) for e in range(4 * G, 4 * G + 4)]
            sd = {}
            cnt = [0]

            def s1(it):
                G, t, e = it
                s = e % NSLOT
                b = rbank()
                sd[it] = {"b": b, "i": cnt[0] % 2}
                cnt[0] += 1
                for c in range(8):
                    mm(bank(b), xT[:, c, tcols(t)], WGU[s][:, c, :], c == 0, c == 7,
                       [("xT", t), ("WGU", s)], [("ps", b)], inc=(c == 7))

            def s2(it):
                G, t, e = it
                d = sd[it]
                b, i = d["b"], d["i"]
                act(SB[i][:, 0:256], bank(b)[:, 0:256], AF.Silu, [("ps", b)], [("SB", i)])
                stt(TB[i][:, 0:256], bank(b)[:, 256:512], gates[:, t, e:e + 1], SB[i][:, 0:256], ALU.mult, ALU.mult,
                    [("ps", b), ("gates", t), ("SB", i)], [("TB", i)])
                b2 = rbank()
                d["b2"] = b2
                for fc in range(2):
                    tr(bankb(b2)[:, fc * 128:(fc + 1) * 128], TB[i][:, fc * 128:(fc + 1) * 128], identb[:],
                       [("TB", i), "identb"], [("ps", b2)], inc=(fc == 1))

            def s3(it):
                G, t, e = it
                d = sd[it]
                b2, i = d["b2"], d["i"]
                s = e % NSLOT
                cp("act", TTB[i][:, 0:256], bankb(b2)[:, 0:256], [("ps", b2)], [("TTB", i)])
                first = (e % 4 == 0)
                lastx = (e % 4 == 3)
                for half in range(2):
                    yb = 4 + 2 * (t % 2) + half
                    for fc in range(2):
                        mm(bank(yb), TTB[i][:, fc * 128:(fc + 1) * 128], WD[s][:, fc, half * 512:(half + 1) * 512],
                           first and fc == 0, lastx and fc == 1, [("TTB", i), ("WD", s)], [("ps", yb)],
                           inc=(fc == 1))
                if t == NT - 1 and e + NSLOT < 16:
                    load_expert(e + NSLOT)
                if lastx:
                    for half in range(2):
                        yb = 4 + 2 * (t % 2) + half
                        xs = X[:, t, half * 512:(half + 1) * 512]
                        if G == 0:
                            stt(xs, xs, ALPHA, bank(yb), ALU.mult, ALU.add, [("X", t), ("ps", yb)], [("X", t)])
                        else:
                            tt(xs, xs, bank(yb), ALU.add, [("X", t), ("ps", yb)], [("X", t)])
                    if G == 3:
                        ln_stats(t, t % 2)

                        def fin(t=t):
                            ln_apply(t, t % 2)
                            if last_layer:
                                P.dma("sp", tm(out_d)[:, t, :], X[:, t, :], reads=[("X", t)], final=True)
                        bg.append(lambda: None)
                        bg.append(fin)

            pipeline(items, s1, s2, s3, bg_per_iter=1)

        def run_layers():
            for l in range(nlayers):
                for t in range(NT):
                    build_xT(t)
                if stop_after == "xT":
                    return False
                phase_A(l)
                P.barrier()
                if stop_after in ("A", "A1", "A2"):
                    return False
                phase_B(l)
                P.barrier()
                if stop_after == "B":
                    return False
                phase_C(l)
                P.barrier()
                if stop_after == "C":
                    return False
                phase_moe(l, l == nlayers - 1)
                P.barrier()
            return True

        if not run_layers():
            for t in range(NT):
                P.dma("sp", tm(out_d)[:, t, :], X[:, t, :], reads=[("X", t)], final=True)

        P.emit(nc, sems)
    return nc


_CACHE = {}


def kernel(**inputs):
    consts = _consts()
    if "nc" not in _CACHE:
        _CACHE["nc"] = build_program()
    nc = _CACHE["nc"]
    x = np.ascontiguousarray(np.asarray(inputs["x"], dtype=np.float32))
    shared = {}
    for k, v in inputs.items():
        if k == "x":
            continue
        shared[k] = np.ascontiguousarray(np.asarray(v, dtype=np.float32))
    shared.update(consts)
    in_maps = []
    for c in range(NCORES):
        m = dict(shared)
        m["x"] = x[c]
        in_maps.append(m)
    res = run_bass_kernel_spmd(nc, in_maps, core_ids=list(range(NCORES)))
    out = np.stack([np.asarray(r["out"], dtype=np.float32) for r in res.results], axis=0)
    return out
```

```python
import contextlib
import numpy as np
import concourse.bass as bass
import concourse.mybir as mybir
from concourse.bass_utils import run_bass_kernel_spmd

F32 = mybir.dt.float32
BF16 = mybir.dt.bfloat16
AF = mybir.ActivationFunctionType
ALU = mybir.AluOpType
AX = mybir.AxisListType

S = 2048
D = 1024
NT = 16
NCORES = 8
DEPTH = 2
ALPHA = float((2 * DEPTH) ** 0.25)
IDXW = float((4 * 64) ** -0.5)
SC64 = float(64 ** -0.5)
SC96 = float(96 ** -0.5)
TOPK = 256
NBIS = 14
NEG = -1.0e30
NDMA_SEMS = 8
ACC_ENG = "dve"
LNP_ENG = "dve"


class Prog:
    ENGS = ("pe", "act", "dve", "pool", "sp")

    def __init__(self):
        self.ops = {e: [] for e in self.ENGS}
        self.cnt = {e: 0 for e in self.ENGS}
        self.last_w = {}
        self.readers = {}
        self.waited = {e: {} for e in self.ENGS}
        self.dma_val = {}
        self.dma_rr = {"sp": 0, "pool": 0}
        self.final_tokens = []

    def _deps(self, eng, reads, writes):
        deps = {}

        def add(tok, raw):
            src, val = tok
            if src == eng and eng == "pe":
                return
            if deps.get(src, 0) < val:
                deps[src] = val

        for k in reads:
            if k in self.last_w:
                add(self.last_w[k], True)
            if isinstance(k, tuple) and k[0] == "ps":
                for r in self.readers.get(k, ()):
                    if r[0] != eng:
                        add(r, False)
        for k in writes:
            if k in self.last_w:
                add(self.last_w[k], False)
            for r in self.readers.get(k, ()):
                add(r, False)
        waits = []
        for src, val in deps.items():
            if self.waited[eng].get(src, 0) >= val:
                continue
            self.waited[eng][src] = val
            waits.append((src, val))
        return waits

    def _record(self, tok, reads, writes):
        for k in writes:
            self.last_w[k] = tok
            self.readers[k] = []
        for k in reads:
            self.readers.setdefault(k, []).append(tok)

    def op(self, eng, fn, reads=(), writes=(), inc=True):
        waits = self._deps(eng, reads, writes)
        if inc:
            self.cnt[eng] += 1
            idx = self.cnt[eng]
        else:
            idx = self.cnt[eng] + 1
        tok = (eng, idx)
        self._record(tok, reads, writes)
        self.ops[eng].append((waits, fn, ("eng", eng) if inc else None))
        return tok

    def dma(self, q, out_ap, in_ap, reads=(), writes=(), final=False):
        i = self.dma_rr[q]
        self.dma_rr[q] = (i + 1) % NDMA_SEMS
        src = ("dma", q, i)
        prev = self.dma_val.get(src, 0)
        waits = self._deps(q, reads, writes)
        if prev and self.waited[q].get(src, 0) < prev:
            self.waited[q][src] = prev
            waits.append((src, prev))
        val = prev + 16
        self.dma_val[src] = val
        tok = (src, val)
        self._record(tok, reads, writes)

        def fn(e, out_ap=out_ap, in_ap=in_ap):
            return e.dma_start(out=out_ap, in_=in_ap)

        self.ops[q].append((waits, fn, ("dma", src)))
        if final:
            self.final_tokens.append(tok)
        return tok

    def barrier(self):
        snap = [(e, self.cnt[e]) for e in self.ENGS if self.cnt[e] > 0]
        snap += [(src, v) for src, v in self.dma_val.items()]
        for e in self.ENGS:
            waits = []
            for src, val in snap:
                if src == e:
                    continue
                if self.waited[e].get(src, 0) >= val:
                    continue
                self.waited[e][src] = val
                waits.append((src, val))
            if waits:
                self.ops[e].append((waits, None, None))

    def emit(self, nc, sems):
        fin = list(self.final_tokens)

        def replay(eng, e):
            for waits, fn, inc in self.ops[eng]:
                for src, val in waits:
                    e.wait_ge(sems[src], val)
                if fn is None:
                    continue
                ins = fn(e)
                if inc is not None:
                    if inc[0] == "eng":
                        ins.then_inc(sems[inc[1]], 1)
                    else:
                        ins.then_inc(sems[inc[1]], 16)
            if eng == "sp":
                for src, val in fin:
                    e.wait_ge(sems[src], val)

        with nc.Block() as block:
            @block.tensor
            def _(e):
                replay("pe", e)

            @block.scalar
            def _(e):
                replay("act", e)

            @block.vector
            def _(e):
                replay("dve", e)

            @block.gpsimd
            def _(e):
                replay("pool", e)

            @block.sync
            def _(e):
                replay("sp", e)


def _consts():
    pos = np.arange(S, dtype=np.float64)
    c = {}
    for dim, nm in ((64, "64"), (32, "32")):
        inv = 1.0 / (10000.0 ** (np.arange(0, dim, 2, dtype=np.float64) / dim))
        inv = inv.astype(np.float32).astype(np.float64)
        ang = (pos.astype(np.float32)[:, None] * inv.astype(np.float32)[None, :]).astype(np.float32)
        c["cos" + nm] = np.cos(ang.astype(np.float64)).astype(np.float32)
        c["sin" + nm] = np.sin(ang.astype(np.float64)).astype(np.float32)
    c["ident"] = np.eye(128, dtype=np.float32)
    qi = np.arange(128)[:, None]
    kj = np.arange(256)[None, :]
    diff = qi + 128 - kj
    kk = np.arange(128)[None, :]
    c["negmask"] = np.where(kk <= qi, 0.0, NEG).astype(np.float32)
    c["causalT"] = (qi <= kk).astype(np.float32)
    c["maskPC"] = np.concatenate([(qi > kk).astype(np.float32), c["causalT"]], axis=1)
    return c


def build_program(nlayers=DEPTH, debug_mix=False, stop_after=None):
    nc = bass.Bass("TRN2", target_bir_lowering=False)

    def din(name, shape):
        return nc.dram_tensor(name, list(shape), F32, kind="ExternalInput").ap()

    x_d = din("x", [S, D])
    w_in_d = din("w_in", [DEPTH, D, 1892])
    sinks_d = din("attn_sinks", [DEPTH, 8])
    gq_d = din("c_q_norm_g", [DEPTH, 256])
    gkv_d = din("c_kv_norm_g", [DEPTH, 128])
    w_uq_d = din("w_uq", [DEPTH, 256, 384])
    w_ukv_d = din("w_ukv", [DEPTH, 128, 512])
    w_out_d = din("w_out", [DEPTH, D, D])
    ln1g_d = din("ln1_g", [DEPTH, D])
    ln1b_d = din("ln1_b", [DEPTH, D])
    w_router_d = din("w_router", [D, 16])
    rbias_d = din("router_bias", [16])
    w_gate_d = din("w_gate", [DEPTH, 16, D, 256])
    w_up_d = din("w_up", [DEPTH, 16, D, 256])
    w_down_d = din("w_down", [DEPTH, 16, 256, D])
    ln2g_d = din("ln2_g", [DEPTH, D])
    ln2b_d = din("ln2_b", [DEPTH, D])
    cos64_d = din("cos64", [S, 32])
    sin64_d = din("sin64", [S, 32])
    cos32_d = din("cos32", [S, 16])
    sin32_d = din("sin32", [S, 16])
    ident_d = din("ident", [128, 128])
    negmask_d = din("negmask", [128, 128])
    causalT_d = din("causalT", [128, 128])
    maskPC_d = din("maskPC", [128, 256])
    out_d = nc.dram_tensor("out", [S, D], F32, kind="ExternalOutput").ap()
    dbg_d = None
    if debug_mix:
        dbg_d = nc.dram_tensor("dbg", [S, D], F32, kind="ExternalOutput").ap()

    P = Prog()
    st = contextlib.ExitStack()
    with st:
        sems = {}
        for e in Prog.ENGS:
            sems[e] = st.enter_context(nc.semaphore("s_" + e))
        for q in ("sp", "pool"):
            for i in range(NDMA_SEMS):
                sems[("dma", q, i)] = st.enter_context(nc.semaphore(f"d_{q}{i}"))

        def T(name, shape, dt):
            return st.enter_context(nc.sbuf_tensor("sb_" + name, list(shape), dt))

        X = T("X", [128, NT, D], F32)
        xT = T("xT", [128, 8, S], BF16)
        cos64 = T("cos64", [128, NT, 32], F32)
        sin64 = T("sin64", [128, NT, 32], F32)
        nsin64 = T("nsin64", [128, NT, 32], F32)
        cos32 = T("cos32", [128, NT, 16], F32)
        sin32 = T("sin32", [128, NT, 16], F32)
        nsin32 = T("nsin32", [128, NT, 16], F32)
        identf = T("identf", [128, 128], F32)
        identb = T("identb", [128, 128], BF16)
        negmask = T("negmask", [128, 128], F32)
        causalT = T("causalT", [128, 128], BF16)
        maskPC = T("maskPC", [128, 256], BF16)
        ones1 = T("ones1", [1, 128], F32)
        lnp = T("lnp", [128, 2, D], F32)
        gates = T("gates", [128, NT, 16], F32)
        widx = T("widx", [128, NT, 4], F32)
        wr = T("wr", [128, 8, 16], F32)
        rb = T("rb", [128, 16], F32)
        sinks = T("sinks", [128, 8], F32)
        gq = T("gq", [128, 256], F32)
        gkv = T("gkv", [128, 128], F32)
        sm = T("sm", [128, 256], F32)
        ARW = 22400
        arena = T("arena", [128, ARW], F32)
        psum = st.enter_context(nc.psum_tensor("psum", [128, 4096], F32))

        def bank(i):
            return psum[:, 512 * i:512 * (i + 1)]

        def bankb(i):
            return psum[:, 512 * i:512 * (i + 1)].bitcast(BF16)

        class Arena:
            def __init__(self):
                self.off = 0

            def f32(self, n):
                o = self.off
                self.off += n
                assert self.off <= ARW, self.off
                return arena[:, o:o + n]

            def bf16(self, n):
                w = (n + 1) // 2
                o = self.off
                self.off += w
                assert self.off <= ARW, self.off
                return arena[:, o:o + w].bitcast(BF16)

        A = Arena()
        WIN = A.bf16(8 * 768)
        WOUT = A.bf16(4 * 1024)
        HSB = [A.f32(768), A.f32(768)]
        phase_qkr_off = A.off
        QKR = [A.f32(768), A.f32(768)]
        SQ = A.f32(768)
        PB = [A.bf16(512), A.bf16(512)]
        PTB = [A.bf16(512), A.bf16(512)]
        ROPET = A.f32(640)
        mixt_off = A.off
        MIXT = A.bf16(512)
        MIXTT = A.bf16(512)
        MIXT_f32 = arena[:, mixt_off:mixt_off + 512]
        phase_base = A.off

        rot = [0]
        rot4 = [0]
        inpipe = [False]

        def rbank():
            i = rot[0]
            rot[0] = (i + 1) % 3
            return i

        def mbank():
            if inpipe[0]:
                return 3
            i = rot4[0]
            rot4[0] = (i + 1) % 4
            return i

        def mm(out, lhsT, rhs, start, stop, reads, writes, inc):
            P.op("pe", lambda e, o=out, l=lhsT, r=rhs, s0=start, s1=stop: e.matmul(o, lhsT=l, rhs=r, start=s0, stop=s1),
                 reads=reads, writes=writes, inc=inc)

        def tr(out, in_, ident, reads, writes, inc=True):
            P.op("pe", lambda e, o=out, i=in_, d=ident: e.transpose(out=o, in_=i, identity=d),
                 reads=reads, writes=writes, inc=inc)

        def act(out, in_, func, reads, writes, bias=None, scale=None, accum=None):
            kw = {}
            if bias is not None:
                kw["bias"] = bias
            if scale is not None:
                kw["scale"] = scale
            if accum is not None:
                kw["accum_out"] = accum
            P.op("act", lambda e, o=out, i=in_, f=func, kw=kw: e.activation(out=o, in_=i, func=f, **kw),
                 reads=reads, writes=writes)

        def tt(out, in0, in1, op, reads, writes, eng="dve"):
            P.op(eng, lambda e, o=out, a=in0, b=in1, p=op: e.tensor_tensor(out=o, in0=a, in1=b, op=p),
                 reads=reads, writes=writes)

        def ts(out, in0, s1, s2, op0, op1, reads, writes, accum=None, eng="dve"):
            def fn(e, o=out, a=in0, s1=s1, s2=s2, op0=op0, op1=op1, accum=accum):
                kw = {}
                if op1 is not None:
                    kw["op1"] = op1
                if accum is not None:
                    kw["accum_out"] = accum
                return e.tensor_scalar(out=o, in0=a, scalar1=s1, scalar2=s2, op0=op0, **kw)
            P.op(eng, fn, reads=reads, writes=writes)

        def stt(out, in0, scalar, in1, op0, op1, reads, writes, eng="dve"):
            P.op(eng, lambda e, o=out, a=in0, s=scalar, b=in1, p0=op0, p1=op1:
                 e.scalar_tensor_tensor(out=o, in0=a, scalar=s, in1=b, op0=p0, op1=p1),
                 reads=reads, writes=writes)

        def cp(eng, out, in_, reads, writes):
            if eng == "act":
                act(out, in_, AF.Copy, reads, writes)
            else:
                P.op(eng, lambda e, o=out, i=in_: e.tensor_copy(out=o, in_=i), reads=reads, writes=writes)

        def red(out, in_, op, reads, writes, absv=False):
            def fn(e, o=out, i=in_, p=op, a=absv):
                if a:
                    return e.tensor_reduce(out=o, in_=i, axis=AX.X, op=p, apply_absolute_value=True)
                return e.tensor_reduce(out=o, in_=i, axis=AX.X, op=p)
            P.op("dve", fn, reads=reads, writes=writes)

        def memset(ap, val, writes, eng="dve"):
            P.op(eng, lambda e, a=ap, v=val: e.memset(a, v), writes=writes)

        def recip(out, in_, reads, writes):
            P.op("dve", lambda e, o=out, i=in_: e.reciprocal(out=o, in_=i), reads=reads, writes=writes)

        bg = []

        def bg_run(k):
            for _ in range(k):
                if not bg:
                    return
                bg.pop(0)()

        def pipeline(items, s1, s2, s3, bg_per_iter=0):
            n = len(items)
            inpipe[0] = True
            for i in range(n + 2):
                if i < n:
                    s1(items[i])
                if 0 <= i - 1 < n:
                    s2(items[i - 1])
                if 0 <= i - 2 < n:
                    s3(items[i - 2])
                if bg_per_iter:
                    bg_run(bg_per_iter)
            bg_run(len(bg))
            inpipe[0] = False

        def run_tiles(Pf, Rf):
            Pf(0)
            for t in range(NT):
                if t + 1 < NT:
                    Pf(t + 1)
                Rf(t)

        def run_stages(Pf, stages):
            ns = len(stages)
            Pf(0)
            for i in range(NT + ns - 1):
                if i + 1 < NT:
                    Pf(i + 1)
                for k, st_ in enumerate(stages):
                    if 0 <= i - k < NT:
                        st_(i - k)

        def tcols(t):
            return slice(t * 128, (t + 1) * 128)

        PBs = [PB[0], PB[1], PTB[0]]
        pbrot = [0]

        def st1(it):
            b = rbank()
            it["b"] = b
            k = it["kind"]
            if k == "att":
                it["pi"] = pbrot[0]
                pbrot[0] = (pbrot[0] + 1) % 3
                sl = it["slots"]
                for j, s in enumerate(sl):
                    ka, kk = s["K"]
                    qa, qk = s["Q"]
                    mm(bank(b)[:, j * 128:(j + 1) * 128], ka, qa, True, True, kk + qk, [("ps", b)], inc=(j == len(sl) - 1))
            elif k == "idx":
                mm(bank(b)[:, 0:it["n"]], it["lhsT"], it["rhs"], True, True, it["keys"], [("ps", b)], True)
            elif k == "mskT":
                kts = it["kts"]
                for j, kt in enumerate(kts):
                    tr(bankb(b)[:, j * 128:(j + 1) * 128], it["src"][:, kt * 128:(kt + 1) * 128], identb[:],
                       [it["srckey"], "identb"], [("ps", b)], inc=(j == len(kts) - 1))

        def st2(it):
            b = it["b"]
            k = it["kind"]
            if k == "att":
                pi = it["pi"]
                n = 128 * len(it["slots"])
                act(PBs[pi][:, 0:n], bank(b)[:, 0:n], AF.Exp, [("ps", b), "negc"], [("PB", pi)], bias=it["negc"], scale=it["scale"])
                for (c0, c1, view, m_ap, mk) in it["masks"]:
                    pv = PBs[pi][:, c0:c1]
                    if view is not None:
                        pv = pv.rearrange(view[0], **view[1])
                    tt(pv, pv, m_ap, ALU.mult, [("PB", pi)] + mk, [("PB", pi)])
            elif k == "idx":
                ri, n, h, qb, k0 = it["ri"], it["n"], it["h"], it["qb"], it["k0"]
                sco, sk = it["sco"], it["scokey"]
                act(it["rb"][:, 0:n], bank(b)[:, 0:n], AF.Relu, [("ps", b)], ["RB%d" % ri])
                if h == 0:
                    ts(sco[:, k0:k0 + n], it["rb"][:, 0:n], widx[:, qb, 0:1], None, ALU.mult, None,
                       ["RB%d" % ri, ("widx", qb)], [sk], eng=ACC_ENG)
                elif ACC_ENG == "dve":
                    stt(sco[:, k0:k0 + n], it["rb"][:, 0:n], widx[:, qb, h:h + 1], sco[:, k0:k0 + n],
                        ALU.mult, ALU.add, ["RB%d" % ri, ("widx", qb), sk], [sk])
                else:
                    ts(it["rb"][:, 0:n], it["rb"][:, 0:n], widx[:, qb, h:h + 1], None, ALU.mult, None,
                       ["RB%d" % ri, ("widx", qb)], ["RB%d" % ri], eng=ACC_ENG)
                    tt(sco[:, k0:k0 + n], sco[:, k0:k0 + n], it["rb"][:, 0:n], ALU.add, ["RB%d" % ri, sk], [sk], eng=ACC_ENG)
            elif k == "mskT":
                kts = it["kts"]
                nj = len(kts)
                cp("act", it["dst"][:, kts[0]:kts[0] + nj, :],
                   bankb(b)[:, 0:nj * 128].rearrange("p (c n) -> p c n", c=nj), [("ps", b)], [it["dstkey"]])

        def st3(it):
            if it["kind"] != "att":
                return
            pi = it["pi"]
            sl = it["slots"]
            for j, s in enumerate(sl):
                va, vk = s["V"]
                ob = s["ob"]
                mm(bank(ob)[:, s["oreg"]:s["oreg"] + 65], PBs[pi][:, j * 128:(j + 1) * 128], va, s["start"], s["stop"],
                   [("PB", pi)] + vk, [("ps", ob)], inc=(j == len(sl) - 1 or sl[j + 1]["ob"] != ob))
            if it.get("fin") is not None:
                it["fin"]()

        def run_seq(seq, bg_per_iter=0):
            n = len(seq)
            inpipe[0] = True
            for i in range(n + 2):
                if i < n and seq[i][0] is not None:
                    st1(seq[i][0])
                if 0 <= i - 1 < n and seq[i - 1][0] is not None:
                    st2(seq[i - 1][0])
                if 0 <= i - 2 < n and seq[i - 2][0] is not None:
                    st3(seq[i - 2][0])
                if i < n:
                    for c in seq[i][1]:
                        c()
                if bg_per_iter:
                    bg_run(bg_per_iter)
            bg_run(len(bg))
            inpipe[0] = False

        def fin_generic(qb, ob, nheads, nchunks_w, mix_c0, epilogue):
            def fn():
                o3 = bank(ob)[:, 0:nheads * 65].rearrange("p (h d) -> p h d", h=nheads)
                rc = sm[:, 72:72 + nheads]
                recip(rc, o3[:, :, 64], [("ps", ob)], ["rc"])
                tt(MIXT[:, 0:nheads * 64].rearrange("p (h d) -> p h d", h=nheads), o3[:, :, 0:64],
                   rc.unsqueeze(2).to_broadcast([128, nheads, 64]), ALU.mult, [("ps", ob), "rc"], ["MIXT"])
                dbg_store(qb, mix_c0, nheads * 64)
                out_proj_partial(qb, nchunks_w, False, after=epilogue)
            return fn

        def tm(ap_d):
            return ap_d.rearrange("(t p) d -> p t d", p=128)

        P.dma("sp", cos64[:], tm(cos64_d), writes=["cos64"])
        P.dma("sp", sin64[:], tm(sin64_d), writes=["sin64"])
        P.dma("sp", cos32[:], tm(cos32_d), writes=["cos32"])
        P.dma("sp", sin32[:], tm(sin32_d), writes=["sin32"])
        P.dma("sp", identf[:], ident_d, writes=["identf"])
        P.dma("pool", identb[:], ident_d, writes=["identb"])
        P.dma("pool", causalT[:], causalT_d, writes=["causalT"])
        P.dma("pool", maskPC[:], maskPC_d, writes=["maskPC"])
        P.dma("sp", negmask[:], negmask_d, writes=["negmask"])
        P.dma("sp", wr[:], w_router_d.rearrange("(c p) n -> p c n", p=128), writes=["wr"])
        P.dma("sp", rb[:], rbias_d.partition_broadcast(128), writes=["rb"])
        xv = tm(x_d)
        for t4 in range(4):
            P.dma("sp", X[:, 4 * t4:4 * t4 + 4, :], xv[:, 4 * t4:4 * t4 + 4, :],
                  writes=[("X", t) for t in range(4 * t4, 4 * t4 + 4)])
        ts(nsin64[:], sin64[:], -1.0, None, ALU.mult, None, ["sin64"], ["nsin64"])
        ts(nsin32[:], sin32[:], -1.0, None, ALU.mult, None, ["sin32"], ["nsin32"])
        memset(ones1[:], 1.0, ["ones1"])

        def build_xT(t):
            for half in range(2):
                b = mbank()
                for j in range(4):
                    c = half * 4 + j
                    tr(bank(b)[:, j * 128:(j + 1) * 128], X[:, t, c * 128:(c + 1) * 128], identf[:],
                       [("X", t), "identf"], [("ps", b)], inc=(j == 3))
                cp("act" if half == 0 else "dve",
                   xT[:, half * 4:half * 4 + 4, tcols(t)],
                   bank(b).rearrange("p (c n) -> p c n", c=4),
                   [("ps", b)], [("xT", t)])

        def in_proj(t, ncols, wview, hs):
            n0 = 0
            while n0 < ncols:
                n1 = min(ncols, n0 + 512)
                b = mbank()
                for c in range(8):
                    mm(bank(b)[:, 0:n1 - n0], xT[:, c, tcols(t)], wview[:, c, n0:n1], c == 0, c == 7,
                       [("xT", t), "WIN"], [("ps", b)], inc=(c == 7))
                cp("act", hs[:, n0:n1], bank(b)[:, 0:n1 - n0], [("ps", b)], ["HSB%d" % (t % 2)])
                n0 = n1

        def rope(src, dst, nh, hd, t, cosT, sinT, nsinT, rk, wk, tmp=None):
            h2 = hd // 2
            cb = cosT[:, t, :].unsqueeze(1).unsqueeze(1).to_broadcast([128, nh, 2, h2])
            sb = sinT[:, t, :].unsqueeze(1).to_broadcast([128, nh, h2])
            nb = nsinT[:, t, :].unsqueeze(1).to_broadcast([128, nh, h2])
            tv = ROPET[:, 0:nh * hd].rearrange("p (h t d) -> p h t d", h=nh, t=2)
            rk = rk + ["cos64", "sin64", "nsin64", "cos32", "sin32", "nsin32"]
            tt(dst, src, cb, ALU.mult, rk, wk)
            tt(tv[:, :, 0, :], src[:, :, 1, :], nb, ALU.mult, rk, ["ropetmp"])
            tt(tv[:, :, 1, :], src[:, :, 0, :], sb, ALU.mult, rk, ["ropetmp"])
            tt(dst, dst, tv, ALU.add, wk + ["ropetmp"], wk)

        def global_bound(mt, nh, qsl, ksl, scale, negc):
            b = mbank()
            tr(bank(b)[0:nh, 0:128], mt, identf[:], ["mt", "identf"], [("ps", b)])
            red(sm[0:nh, 0:1], bank(b)[0:nh, 0:128], ALU.max, [("ps", b)], ["gb1"])
            b2 = mbank()
            tr(bank(b2)[0:1, 0:nh], sm[0:nh, 0:1], identf[0:nh, 0:nh], ["gb1", "identf"], [("ps", b2)])
            red(sm[0:1, 1:2], bank(b2)[0:1, qsl], ALU.max, [("ps", b2)], ["gb2"])
            red(sm[0:1, 2:3], bank(b2)[0:1, ksl], ALU.max, [("ps", b2)], ["gb3"])
            tt(sm[0:1, 3:4], sm[0:1, 1:2], sm[0:1, 2:3], ALU.mult, ["gb2", "gb3"], ["gb4"])
            act(sm[0:1, 4:5], sm[0:1, 3:4], AF.Ln, ["gb4"], ["gb5"])
            act(sm[0:1, 5:6], sm[0:1, 4:5], AF.Exp, ["gb5"], ["gb6"], scale=0.5)
            ts(sm[0:1, 6:7], sm[0:1, 5:6], -scale, None, ALU.mult, None, ["gb6"], ["gb7"])
            b3 = mbank()
            mm(bank(b3)[:, 0:1], ones1[0:1, :], sm[0:1, 6:7], True, True, ["gb7", "ones1"], [("ps", b3)], True)
            cp("dve", negc, bank(b3)[:, 0:1], [("ps", b3)], ["negc"])

        def head_sumsq(src, ncols, nh, mt, first, rk):
            act(SQ[:, 0:ncols], src, AF.Square, rk, ["SQ"])
            hd = ncols // nh
            if first:
                red(mt, SQ[:, 0:ncols].rearrange("p (h d) -> p h d", h=nh), ALU.add, ["SQ"], ["mt"])
            else:
                red(sm[:, 16:16 + nh], SQ[:, 0:ncols].rearrange("p (h d) -> p h d", h=nh), ALU.add, ["SQ"], ["hs"])
                tt(mt, mt, sm[:, 16:16 + nh], ALU.max, ["mt", "hs"], ["mt"])

        def out_proj_T(qb, nchunks):
            b = mbank()
            for c in range(nchunks):
                tr(bankb(b)[:, c * 128:(c + 1) * 128], MIXT[:, c * 128:(c + 1) * 128], identb[:],
                   ["MIXT", "identb"], [("ps", b)], inc=(c == nchunks - 1))
            cp("act", MIXTT[:, 0:nchunks * 128], bankb(b)[:, 0:nchunks * 128], [("ps", b)], ["MIXTT"])

        def out_proj_M(qb, nchunks, first):
            wv = WOUT.rearrange("p (c n) -> p c n", n=1024)
            for half in range(2):
                yb = 6 + half
                for c in range(nchunks):
                    mm(bank(yb), MIXTT[:, c * 128:(c + 1) * 128], wv[:, c, half * 512:(half + 1) * 512],
                       c == 0, c == nchunks - 1, ["MIXTT", "WOUT"], [("ps", yb)], inc=(c == nchunks - 1))
                xs = X[:, qb, half * 512:(half + 1) * 512]
                if first:
                    stt(xs, xs, ALPHA, bank(yb), ALU.mult, ALU.add, [("X", qb), ("ps", yb)], [("X", qb)])
                else:
                    tt(xs, xs, bank(yb), ALU.add, [("X", qb), ("ps", yb)], [("X", qb)])

        def out_proj_partial(qb, nchunks, first, after=None):
            bg.append(lambda: out_proj_T(qb, nchunks))

            def part2():
                out_proj_M(qb, nchunks, first)
                if after is not None:
                    after(qb)
            bg.append(part2)

        def dbg_store(qb, c0, ncols):
            if dbg_d is None:
                return
            cp("dve", ROPET[:, 0:ncols], MIXT[:, 0:ncols], ["MIXT"], ["ropetmp"])
            P.dma("sp", tm(dbg_d)[:, qb, c0:c0 + ncols], ROPET[:, 0:ncols], reads=["ropetmp"], final=True)

        def ln_stats(t, k):
            o = 32 + 16 * k
            ks = "ln%d" % k
            P.op("dve", lambda e, o_=sm[:, o:o + 6], i=X[:, t, 0:512]: e.bn_stats(out=o_, in_=i), reads=[("X", t)], writes=[ks + "a"])
            P.op("dve", lambda e, o_=sm[:, o + 6:o + 12], i=X[:, t, 512:1024]: e.bn_stats(out=o_, in_=i), reads=[("X", t)], writes=[ks + "b"])
            P.op("dve", lambda e, o_=sm[:, o + 12:o + 14], i=sm[:, o:o + 12]: e.bn_aggr(out=o_, in_=i), reads=[ks + "a", ks + "b"], writes=[ks + "mv"])
            act(sm[:, o + 14:o + 15], sm[:, o + 13:o + 14], AF.Ln, [ks + "mv"], [ks + "lv"], bias=1e-5)
            act(sm[:, o + 15:o + 16], sm[:, o + 14:o + 15], AF.Exp, [ks + "lv"], [ks + "rs"], scale=-0.5)

        def ln_apply(t, k):
            o = 32 + 16 * k
            ks = "ln%d" % k
            xs = X[:, t, :]
            ts(xs, xs, sm[:, o + 12:o + 13], sm[:, o + 15:o + 16], ALU.subtract, ALU.mult, [("X", t), ks + "mv", ks + "rs"], [("X", t)])
            tt(xs, xs, lnp[:, 0, :], ALU.mult, [("X", t), "lnp"], [("X", t)], eng=LNP_ENG)
            tt(xs, xs, lnp[:, 1, :], ALU.add, [("X", t), "lnp"], [("X", t)], eng=LNP_ENG)

        def layer_norm(t, k=0):
            ln_stats(t, k)
            ln_apply(t, k)

        def load_ln(g_d, b_d, l):
            P.dma("sp", lnp[:, 0, :], g_d[l].partition_broadcast(128), writes=["lnp"])
            P.dma("sp", lnp[:, 1, :], b_d[l].partition_broadcast(128), writes=["lnp"])

        def phase_A(l):
            A.off = phase_base
            FM = A.bf16(6 * S).rearrange("p (c n) -> p c n", c=6)
            VA = A.bf16(NT * 2 * 65).rearrange("p (t g d) -> p t g d", t=NT, g=2)
            mt = A.f32(16)[:, 0:10]
            negc = A.f32(2)[:, 0:1]
            esink = A.f32(8)
            den = A.f32(8)
            wv = WIN[:, 0:8 * 768].rearrange("p (c n) -> p c n", c=8)
            P.dma("pool", wv, w_in_d[l].rearrange("(c p) n -> p c n", p=128)[:, :, 0:768], writes=["WIN"])
            P.dma("pool", WOUT[:, 0:4096].rearrange("p (c n) -> p c n", c=4),
                  w_out_d[l, 0:512, :].rearrange("(c p) n -> p c n", p=128), writes=["WOUT"])
            P.dma("sp", sinks[:], sinks_d[l].partition_broadcast(128), writes=["sinks"])
            memset(VA[:, :, :, 64:65], 1.0, ["VAones"])
            def Pf(t):
                in_proj(t, 768, wv, HSB[t % 2])

            def Rf(t):
                hs = HSB[t % 2]
                qk = QKR[t % 2]
                hk = "HSB%d" % (t % 2)
                qkk = "QKR%d" % (t % 2)
                head_sumsq(hs[:, 0:640], 640, 10, mt, t == 0, [hk])
                rope(hs[:, 0:512].rearrange("p (h t d) -> p h t d", h=8, t=2),
                     qk[:, 0:512].rearrange("p (h t d) -> p h t d", h=8, t=2),
                     8, 64, t, cos64, sin64, nsin64, [hk], [qkk])
                kdst = qk[:, 512:768].rearrange("p (g r d) -> p g r d", g=2, r=2)
                rope(hs[:, 512:640].rearrange("p (h t d) -> p h t d", h=2, t=2),
                     kdst[:, :, 0, :].rearrange("p g (t d) -> p g t d", t=2),
                     2, 64, t, cos64, sin64, nsin64, [hk], [qkk])
                cp("dve", kdst[:, :, 1, :], kdst[:, :, 0, :], [qkk], [qkk])
                cp("act", VA[:, t, :, 0:64], hs[:, 640:768].rearrange("p (g d) -> p g d", g=2), [hk], [("VA", t)])

            def Rb(t):
                qk = QKR[t % 2]
                qkk = "QKR%d" % (t % 2)
                for part, (c0, nchk) in enumerate(((0, 4), (4, 2))):
                    b = mbank()
                    for j in range(nchk):
                        c = c0 + j
                        tr(bank(b)[:, j * 128:(j + 1) * 128], qk[:, c * 128:(c + 1) * 128], identf[:],
                           [qkk, "identf"], [("ps", b)], inc=(j == nchk - 1))
                    cp("act" if part == 0 else "dve", FM[:, c0:c0 + nchk, tcols(t)],
                       bank(b)[:, 0:nchk * 128].rearrange("p (c n) -> p c n", c=nchk),
                       [("ps", b)], [("FM", t)])

            run_stages(Pf, [Rf, Rb])
            if stop_after == "A1":
                return
            global_bound(mt, 10, slice(0, 8), slice(8, 10), SC64, negc)
            act(esink, sinks[:], AF.Exp, ["sinks", "negc"], ["esink"], bias=negc)
            if stop_after == "A2":
                return

            def finA(qb, g):
                def fn():
                    ob = 4 + g
                    o3 = bank(ob)[:, 0:260].rearrange("p (h d) -> p h d", h=4)
                    tt(den[:, 4 * g:4 * g + 4], o3[:, :, 64], esink[:, 4 * g:4 * g + 4], ALU.add,
                       [("ps", ob), "esink"], ["den"])
                    recip(den[:, 4 * g:4 * g + 4], den[:, 4 * g:4 * g + 4], ["den"], ["den"])
                    tt(MIXT[:, g * 256:(g + 1) * 256].rearrange("p (h d) -> p h d", h=4), o3[:, :, 0:64],
                       den[:, 4 * g:4 * g + 4].unsqueeze(2).to_broadcast([128, 4, 64]), ALU.mult,
                       [("ps", ob), "den"], ["MIXT"])
                    if g == 1:
                        dbg_store(qb, 0, 512)
                        out_proj_partial(qb, 4, True)
                return fn

            seq = []
            for qb in range(NT):
                kts = [kt for kt in (qb - 1, qb) if kt >= 0]
                nk_ = len(kts)
                for g in range(2):
                    for par in range(2):
                        po = 64 * par
                        slots = []
                        for h in (4 * g + par, 4 * g + par + 2):
                            for kt in kts:
                                slots.append({"K": (FM[po:po + 64, 4 + g, tcols(kt)], [("FM", kt)]),
                                              "Q": (FM[po:po + 64, h // 2, tcols(qb)], [("FM", qb)]),
                                              "V": (VA[:, kt, g, :], [("VA", kt), "VAones"]),
                                              "ob": 4 + g, "oreg": (h % 4) * 65,
                                              "start": kt == kts[0], "stop": kt == kts[-1]})
                        if nk_ == 2:
                            masks = [(0, 512, ("p (h n) -> p h n", {"h": 2}),
                                      maskPC[:].unsqueeze(1).to_broadcast([128, 2, 256]), ["maskPC"])]
                        else:
                            masks = [(0, 256, ("p (h n) -> p h n", {"h": 2}),
                                      maskPC[:, 128:256].unsqueeze(1).to_broadcast([128, 2, 128]), ["maskPC"])]
                        seq.append(({"kind": "att", "slots": slots, "scale": SC64, "negc": negc, "masks": masks,
                                     "fin": finA(qb, g) if par == 1 else None}, []))
            run_seq(seq, bg_per_iter=1)

        def phase_B(l):
            A.off = phase_base
            FM = A.bf16(6 * S).rearrange("p (c n) -> p c n", c=6)
            VB = A.bf16(NT * 65 + 1)[:, 0:NT * 65].rearrange("p (t d) -> p t d", t=NT)
            SCO = A.f32(S)
            MSK = A.bf16(S)
            RB = [HSB[0][:, 0:512], HSB[1][:, 0:512]]
            mt = A.f32(16)[:, 0:5]
            negc = A.f32(2)[:, 0:1]
            bis = A.f32(8)
            wv = WIN[:, 0:8 * 708].rearrange("p (c n) -> p c n", c=8)
            P.dma("pool", wv, w_in_d[l].rearrange("(c p) n -> p c n", p=128)[:, :, 768:1476], writes=["WIN"])
            P.dma("pool", WOUT[:, 0:2048].rearrange("p (c n) -> p c n", c=2),
                  w_out_d[l, 512:768, :].rearrange("(c p) n -> p c n", p=128), writes=["WOUT"])
            memset(VB[:, :, 64:65], 1.0, ["VBones"])
            def Pf(t):
                in_proj(t, 708, wv, HSB[t % 2])

            def Rf(t):
                hs = HSB[t % 2]
                qk = QKR[t % 2]
                hk = "HSB%d" % (t % 2)
                qkk = "QKR%d" % (t % 2)
                head_sumsq(hs[:, 0:320], 320, 5, mt, t == 0, [hk])
                for (s0, d0) in ((0, 0), (384, 384)):
                    rope(hs[:, s0:s0 + 320].rearrange("p (h t d) -> p h t d", h=5, t=2),
                         qk[:, d0:d0 + 320].rearrange("p (h t d) -> p h t d", h=5, t=2),
                         5, 64, t, cos64, sin64, nsin64, [hk], [qkk])
                    cp("dve", qk[:, d0 + 320:d0 + 384], qk[:, d0 + 256:d0 + 320], [qkk], [qkk])
                cp("act", VB[:, t, 0:64], hs[:, 320:384], [hk], [("VB", t)])
                ts(widx[:, t, :], hs[:, 704:708], IDXW, None, ALU.mult, None, [hk], [("widx", t)])

            def Rb(t):
                qk = QKR[t % 2]
                qkk = "QKR%d" % (t % 2)
                for part, (c0, nchk) in enumerate(((0, 4), (4, 2))):
                    b = mbank()
                    for j in range(nchk):
                        c = c0 + j
                        tr(bank(b)[:, j * 128:(j + 1) * 128], qk[:, c * 128:(c + 1) * 128], identf[:],
                           [qkk, "identf"], [("ps", b)], inc=(j == nchk - 1))
                    cp("act" if part == 0 else "dve", FM[:, c0:c0 + nchk, tcols(t)],
                       bank(b)[:, 0:nchk * 128].rearrange("p (c n) -> p c n", c=nchk),
                       [("ps", b)], [("FM", t)])

            run_stages(Pf, [Rf, Rb])
            global_bound(mt, 5, slice(0, 4), slice(4, 5), SC64, negc)

            P.barrier()
            SCOs = [SCO, arena[:, 0:S]]
            a_qkr = phase_qkr_off
            MSKT = [arena[:, a_qkr + 1024 * i:a_qkr + 1024 * (i + 1)].bitcast(BF16).rearrange("p (t n) -> p t n", t=NT)
                    for i in range(2)]

            def idx_items(qb):
                nk = (qb + 1) * 128
                par = qb % 2
                out = []
                for k0 in range(0, nk, 512):
                    n = min(512, nk - k0)
                    for h in range(4):
                        po = 64 * (h % 2)
                        ri = (len(out)) % 2
                        out.append({"kind": "idx", "n": n, "h": h, "qb": qb, "k0": k0, "ri": ri, "rb": RB[ri],
                                    "lhsT": FM[po:po + 64, 3 + h // 2, tcols(qb)], "rhs": FM[po:po + 64, 5, k0:k0 + n],
                                    "keys": [("FM", t) for t in range(qb + 1)],
                                    "sco": SCOs[par], "scokey": "SCO%d" % par})
                return out

            def bis_closures(qb):
                nk = (qb + 1) * 128
                par = qb % 2
                sco = SCOs[par]
                sk = "SCO%d" % par
                cl = []

                def init():
                    if qb >= 2:
                        red(bis[:, 0:1], sco[:, 0:nk], ALU.max, [sk], ["bnd"], absv=True)
                    tt(sco[:, qb * 128:nk], sco[:, qb * 128:nk], negmask[:], ALU.add, [sk, "negmask"], [sk])
                    if qb >= 2:
                        ts(bis[:, 1:2], bis[:, 0:1], -1.0, -1.0, ALU.mult, ALU.add, ["bnd"], ["lo"])
                        ts(bis[:, 2:3], bis[:, 0:1], 2.0, 2.0, ALU.mult, ALU.add, ["bnd"], ["w0"])
                    else:
                        ts(MSK[:, 0:nk], sco[:, 0:nk], -1.0e29, None, ALU.is_ge, None, [sk], ["MSK"])
                cl.append(init)
                if qb >= 2:
                    def it_fn(k):
                        def fn():
                            f = 2.0 ** -(k + 1)
                            stt(bis[:, 3:4], bis[:, 2:3], f, bis[:, 1:2], ALU.mult, ALU.add, ["w0", "lo"], ["mid"])
                            ts(MSK[:, 0:nk], sco[:, 0:nk], bis[:, 3:4], None, ALU.is_ge, ALU.add, [sk, "mid"],
                               ["MSK", "cnt"], accum=bis[:, 4:5])
                            ts(bis[:, 5:6], bis[:, 4:5], float(TOPK), bis[:, 2:3], ALU.is_ge, ALU.mult,
                               ["cnt", "w0"], ["stp"])
                            stt(bis[:, 1:2], bis[:, 5:6], f, bis[:, 1:2], ALU.mult, ALU.add, ["stp", "lo"], ["lo"])
                        return fn
                    for k in range(NBIS):
                        cl.append(it_fn(k))
                    cl.append(lambda: ts(MSK[:, 0:nk], sco[:, 0:nk], bis[:, 1:2], None, ALU.is_ge, None, [sk, "lo"], ["MSK"]))
                return cl

            def mskT_items(qb):
                par = qb % 2
                out = []
                for kt0 in range(0, qb + 1, 4):
                    kts = list(range(kt0, min(kt0 + 4, qb + 1)))
                    out.append({"kind": "mskT", "kts": kts, "src": MSK, "srckey": "MSK",
                                "dst": MSKT[par], "dstkey": ("MSKT", par)})
                return out

            def att_items(qb):
                par = qb % 2
                out = []
                for h in range(4):
                    po = 64 * (h % 2)
                    for kt0 in range(0, qb + 1, 4):
                        kts = list(range(kt0, min(kt0 + 4, qb + 1)))
                        slots = [{"K": (FM[po:po + 64, 2, tcols(kt)], [("FM", kt)]),
                                  "Q": (FM[po:po + 64, h // 2, tcols(qb)], [("FM", qb)]),
                                  "V": (VB[:, kt, :], [("VB", kt), "VBones"]),
                                  "ob": 4 + par, "oreg": h * 65, "start": kt == 0, "stop": kt == qb} for kt in kts]
                        ns = len(kts)
                        masks = [(0, 128 * ns, ("p (c n) -> p c n", {"c": ns}), MSKT[par][:, kt0:kt0 + ns, :], [("MSKT", par)])]
                        last = (h == 3 and kts[-1] == qb)
                        out.append({"kind": "att", "slots": slots, "scale": SC64, "negc": negc, "masks": masks,
                                    "fin": fin_generic(qb, 4 + par, 4, 2, 512, None) if last else None})
                return out

            def merge(a, b):
                out = []
                na, nb = len(a), len(b)
                ia = ib = 0
                while ia < na or ib < nb:
                    if ib >= nb or (ia < na and ia * nb <= ib * na):
                        out.append(a[ia])
                        ia += 1
                    else:
                        out.append(b[ib])
                        ib += 1
                return out

            seq = []
            for r in range(-2, NT):
                ia = att_items(r) if r >= 0 else []
                ii = idx_items(r + 2) if r + 2 < NT else []
                cl = bis_closures(r + 1) if 0 <= r + 1 < NT else []
                im = mskT_items(r + 1) if 0 <= r + 1 < NT else []
                ents = [[it, []] for it in merge(ia, ii)]
                if not ents:
                    ents = [[None, []]]
                ne = len(ents)
                for ci, c in enumerate(cl):
                    ents[min(ne - 1, (ci * ne) // max(1, len(cl)))][1].append(c)
                seq.extend((e[0], e[1]) for e in ents)
                seq.extend((it, []) for it in im)
            run_seq(seq, bg_per_iter=1)

        def phase_C(l):
            A.off = phase_base
            FQ = A.bf16(4 * S).rearrange("p (c n) -> p c n", c=4)
            FK = A.bf16(4 * S).rearrange("p (c n) -> p c n", c=4)
            VC = A.bf16(NT * 4 * 65).rearrange("p (t h d) -> p t h d", t=NT, h=4)
            QCF = [QKR[0][:, 0:384], QKR[0][:, 384:768]]
            KCF = [QKR[1][:, 0:384], QKR[1][:, 384:768]]
            CQN = [A.bf16(256), A.bf16(256)]
            CKN = [A.bf16(128), A.bf16(128)]
            CQT = [A.bf16(256), A.bf16(256)]
            CKT = [A.bf16(128), A.bf16(128)]
            mt = A.f32(16)[:, 0:8]
            negc = A.f32(2)[:, 0:1]
            rt = A.f32(128)
            rtk = [A.f32(32), A.f32(32)]
            SQJ = MIXT_f32
            wv = WIN[:, 0:8 * 416].rearrange("p (c n) -> p c n", c=8)
            WUQ = WIN[:, 8 * 416:8 * 416 + 768].rearrange("p (c n) -> p c n", c=2)
            WUKV = WIN[:, 8 * 416 + 768:8 * 416 + 768 + 512]
            P.dma("pool", wv, w_in_d[l].rearrange("(c p) n -> p c n", p=128)[:, :, 1476:1892], writes=["WIN"])
            P.dma("pool", WUQ, w_uq_d[l].rearrange("(c p) n -> p c n", p=128), writes=["WIN"])
            P.dma("pool", WUKV, w_ukv_d[l], writes=["WIN"])
            P.dma("pool", WOUT[:, 0:2048].rearrange("p (c n) -> p c n", c=2),
                  w_out_d[l, 768:1024, :].rearrange("(c p) n -> p c n", p=128), writes=["WOUT"])
            P.dma("sp", gq[:], gq_d[l].partition_broadcast(128), writes=["gq"])
            P.dma("sp", gkv[:], gkv_d[l].partition_broadcast(128), writes=["gkv"])
            load_ln(ln1g_d, ln1b_d, l)
            memset(VC[:, :, :, 64:65], 1.0, ["VCones"])
            def Pf(t):
                in_proj(t, 416, wv, HSB[t % 2])

            def c1(t):
                p = t % 2
                hs = HSB[p]
                hk = "HSB%d" % p
                o = 80 + 8 * p
                ck = "c1s%d" % p
                act(SQJ[:, 0:256], hs[:, 0:256], AF.Square, [hk], ["SQJ"], accum=sm[:, o:o + 1])
                act(SQJ[:, 256:384], hs[:, 256:384], AF.Square, [hk], ["SQJ2"], accum=sm[:, o + 1:o + 2])
                act(sm[:, o + 2:o + 3], sm[:, o:o + 1], AF.Ln, ["SQJ"], [ck + "l1"], scale=1.0 / 256, bias=1e-6)
                act(sm[:, o + 3:o + 4], sm[:, o + 1:o + 2], AF.Ln, ["SQJ2"], [ck + "l2"], scale=1.0 / 128, bias=1e-6)
                act(sm[:, o + 4:o + 5], sm[:, o + 2:o + 3], AF.Exp, [ck + "l1"], [ck + "r1"], scale=-0.5)
                act(sm[:, o + 5:o + 6], sm[:, o + 3:o + 4], AF.Exp, [ck + "l2"], [ck + "r2"], scale=-0.5)
                stt(CQN[p][:, 0:256], hs[:, 0:256], sm[:, o + 4:o + 5], gq[:], ALU.mult, ALU.mult, [hk, ck + "r1", "gq"], ["CQN%d" % p])
                stt(CKN[p][:, 0:128], hs[:, 256:384], sm[:, o + 5:o + 6], gkv[:], ALU.mult, ALU.mult, [hk, ck + "r2", "gkv"], ["CKN%d" % p])
                rope(hs[:, 384:416].rearrange("p (h t d) -> p h t d", h=1, t=2),
                     rtk[p][:, 0:32].rearrange("p (h t d) -> p h t d", h=1, t=2), 1, 32, t,
                     cos32, sin32, nsin32, [hk], ["rtk%d" % p])

            def c23(t):
                p = t % 2
                qck, kck = "QCF%d" % p, "KCF%d" % p
                b = mbank()
                tr(bankb(b)[:, 0:128], CQN[p][:, 0:128], identb[:], ["CQN%d" % p, "identb"], [("ps", b)], inc=False)
                tr(bankb(b)[:, 128:256], CQN[p][:, 128:256], identb[:], ["CQN%d" % p, "identb"], [("ps", b)], inc=False)
                tr(bankb(b)[:, 256:384], CKN[p][:, 0:128], identb[:], ["CKN%d" % p, "identb"], [("ps", b)], inc=True)
                cp("act", CQT[p][:, 0:256], bankb(b)[:, 0:256], [("ps", b)], ["CQT%d" % p])
                cp("dve", CKT[p][:, 0:128], bankb(b)[:, 256:384], [("ps", b)], ["CKT%d" % p])
                bq = mbank()
                mm(bank(bq)[:, 0:384], CQT[p][:, 0:128], WUQ[:, 0, :], True, False, ["CQT%d" % p, "WIN"], [("ps", bq)], False)
                mm(bank(bq)[:, 0:384], CQT[p][:, 128:256], WUQ[:, 1, :], False, True, ["CQT%d" % p, "WIN"], [("ps", bq)], True)
                bk = mbank()
                mm(bank(bk)[:, 0:512], CKT[p][:, 0:128], WUKV, True, True, ["CKT%d" % p, "WIN"], [("ps", bk)], True)
                cp("act", QCF[p], bank(bq)[:, 0:384], [("ps", bq)], [qck])
                q4 = QCF[p].rearrange("p (h d) -> p h d", h=4)
                qr = q4[:, :, 64:96].rearrange("p h (t d) -> p h t d", t=2)
                cp("dve", rt[:, 0:128].rearrange("p (h d) -> p h d", h=4), q4[:, :, 64:96], [qck], ["rt"])
                rope(rt[:, 0:128].rearrange("p (h t d) -> p h t d", h=4, t=2), qr, 4, 32, t,
                     cos32, sin32, nsin32, ["rt"], [qck])
                kv4 = bank(bk)[:, 0:512].rearrange("p (h d) -> p h d", h=4)
                k4 = KCF[p].rearrange("p (h d) -> p h d", h=4)
                cp("act", k4[:, :, 0:64], kv4[:, :, 0:64], [("ps", bk)], [kck])
                cp("dve", VC[:, t, :, 0:64], kv4[:, :, 64:128], [("ps", bk)], [("VC", t)])
                cp("dve", k4[:, :, 64:96], rtk[p][:, 0:32].unsqueeze(1).to_broadcast([128, 4, 32]), ["rtk%d" % p], [kck])
                act(SQ[:, 0:384], QCF[p], AF.Square, [qck], ["SQ"])
                act(SQ[:, 384:768], KCF[p], AF.Square, [kck], ["SQ"])
                if t == 0:
                    red(mt, SQ[:, 0:768].rearrange("p (h d) -> p h d", h=8), ALU.add, ["SQ"], ["mt"])
                else:
                    red(sm[:, 16:24], SQ[:, 0:768].rearrange("p (h d) -> p h d", h=8), ALU.add, ["SQ"], ["hs"])
                    tt(mt, mt, sm[:, 16:24], ALU.max, ["mt", "hs"], ["mt"])

            def c4(t):
                p = t % 2
                for (src_, dstF, key, fkey, eng) in ((QCF[p], FQ, "QCF%d" % p, "FQKR0", "act"), (KCF[p], FK, "KCF%d" % p, "FQKR1", "dve")):
                    b = mbank()
                    for h in range(4):
                        tr(bank(b)[0:96, h * 128:(h + 1) * 128], src_[:, h * 96:(h + 1) * 96], identf[:],
                           [key, "identf"], [("ps", b)], inc=(h == 3))
                    cp(eng, dstF[0:96, :, tcols(t)], bank(b)[0:96, :].rearrange("p (c n) -> p c n", c=4),
                       [("ps", b)], [(fkey, t)])

            run_stages(Pf, [c1, c23, c4])
            global_bound(mt, 8, slice(0, 4), slice(4, 8), SC96, negc)

            P.barrier()
            RT = arena[:, phase_qkr_off:phase_qkr_off + 2304]

            def epilogue(qb):
                k = qb % 2
                bg.append(lambda: ln_stats(qb, k))
                bg.append(lambda: ln_apply(qb, k))

                def xpose(half):
                    b = mbank()
                    for j in range(4):
                        c = half * 4 + j
                        tr(bank(b)[:, j * 128:(j + 1) * 128], X[:, qb, c * 128:(c + 1) * 128], identf[:],
                           [("X", qb), "identf"], [("ps", b)], inc=(j == 3))
                    cp("act", xT[:, half * 4:half * 4 + 4, tcols(qb)], bank(b).rearrange("p (c n) -> p c n", c=4),
                       [("ps", b)], [("xT", qb)])
                    cp("dve", HSB[half][:, 0:512], bank(b), [("ps", b)], ["HSB%d" % half])

                bg.append(lambda: xpose(0))
                bg.append(lambda: xpose(1))
                def router_a():
                    b = mbank()
                    for c in range(8):
                        mm(bank(b)[:, 0:16], HSB[c // 4][:, (c % 4) * 128:(c % 4 + 1) * 128], wr[:, c, :], c == 0, c == 7,
                           ["HSB0", "HSB1", "wr"], [("ps", b)], inc=(c == 7))
                    act(RT[:, qb * 16:(qb + 1) * 16], bank(b)[:, 0:16], AF.Exp, [("ps", b)], [("r_sc", qb)], scale=-1.0)

                bg.append(router_a)

            seq = []
            for qb in range(NT):
                par = qb % 2
                for h in range(4):
                    for kt0 in range(0, qb + 1, 4):
                        kts = list(range(kt0, min(kt0 + 4, qb + 1)))
                        slots = [{"K": (FK[0:96, h, tcols(kt)], [("FQKR1", kt)]),
                                  "Q": (FQ[0:96, h, tcols(qb)], [("FQKR0", qb)]),
                                  "V": (VC[:, kt, h, :], [("VC", kt), "VCones"]),
                                  "ob": 4 + par, "oreg": h * 65, "start": kt == 0, "stop": kt == qb} for kt in kts]
                        ns = len(kts)
                        masks = []
                        if kts[-1] == qb:
                            masks = [(128 * (ns - 1), 128 * ns, None, causalT[:], ["causalT"])]
                        last = (h == 3 and kts[-1] == qb)
                        seq.append(({"kind": "att", "slots": slots, "scale": SC96, "negc": negc, "masks": masks,
                                     "fin": fin_generic(qb, 4 + par, 4, 2, 768, epilogue) if last else None}, []))
            run_seq(seq, bg_per_iter=2)

            sc = RT[:, 0:256]
            bi = RT[:, 256:512]
            m1 = RT[:, 512:576]
            eq = RT[:, 576:832]
            msk = RT[:, 832:1088]
            m2 = RT[:, 1088:1152]
            gs = RT[:, 1152:1216]
            gm = RT[:, 1216:1232]
            gsel = RT[:, 1232:1296]
            t2 = RT[:, 1296:1552]
            wgt = RT[:, 1552:1808]
            ws = RT[:, 1808:1824]
            rsk = [("r_sc", t) for t in range(NT)]

            def v3(ap, a):
                return ap.rearrange("p (a b) -> p a b", a=a)

            ts(sc, sc, 1.0, None, ALU.add, None, rsk, ["r_s"])
            recip(sc, sc, ["r_s"], ["r_s"])
            tt(v3(bi, 16), v3(sc, 16), rb[:].unsqueeze(1).to_broadcast([128, 16, 16]), ALU.add, ["r_s", "rb"], ["r_bi"])
            red(m1, v3(bi, 64), ALU.max, ["r_bi"], ["r_m1"])
            tt(v3(eq, 64), v3(bi, 64), m1.unsqueeze(2).to_broadcast([128, 64, 4]), ALU.is_equal, ["r_bi", "r_m1"], ["r_eq"])
            stt(msk, eq, NEG, bi, ALU.mult, ALU.add, ["r_eq", "r_bi"], ["r_msk"])
            red(m2, v3(msk, 64), ALU.max, ["r_msk"], ["r_m2"])
            tt(gs, m1, m2, ALU.add, ["r_m1", "r_m2"], ["r_gs"])
            red(gm, v3(gs, 16), ALU.max, ["r_gs"], ["r_gm"])
            tt(v3(gsel, 16), v3(gs, 16), gm.unsqueeze(2).to_broadcast([128, 16, 4]), ALU.is_equal, ["r_gs", "r_gm"], ["r_gsel"])
            tt(v3(t2, 64), v3(bi, 64), m2.unsqueeze(2).to_broadcast([128, 64, 4]), ALU.is_ge, ["r_bi", "r_m2"], ["r_t2"])
            tt(v3(t2, 64), v3(t2, 64), gsel.unsqueeze(2).to_broadcast([128, 64, 4]), ALU.mult, ["r_t2", "r_gsel"], ["r_t2"])
            tt(wgt, sc, t2, ALU.mult, ["r_s", "r_t2"], ["r_w"])
            red(ws, v3(wgt, 16), ALU.add, ["r_w"], ["r_ws"])
            recip(ws, ws, ["r_ws"], ["r_ws"])
            tt(gates[:], v3(wgt, 16), ws.unsqueeze(2).to_broadcast([128, 16, 16]), ALU.mult, ["r_w", "r_ws"],
               [("gates", t) for t in range(NT)])

        def phase_moe(l, last_layer):
            A.off = 0
            NSLOT = 7
            WGU = [A.bf16(8 * 512).rearrange("p (c n) -> p c n", c=8) for _ in range(NSLOT)]
            WD = [A.bf16(2 * 1024).rearrange("p (c n) -> p c n", c=2) for _ in range(NSLOT)]
            SB = [A.bf16(256) for _ in range(2)]
            TB = [A.bf16(256) for _ in range(2)]
            TTB = [A.bf16(256) for _ in range(2)]
            load_ln(ln2g_d, ln2b_d, l)
            def load_expert(e):
                s = e % NSLOT
                P.dma("pool", WGU[s][:, :, 0:256], w_gate_d[l, e].rearrange("(c p) f -> p c f", p=128), writes=[("WGU", s)])
                P.dma("pool", WGU[s][:, :, 256:512], w_up_d[l, e].rearrange("(c p) f -> p c f", p=128), writes=[("WGU", s)])
                P.dma("pool", WD[s], w_down_d[l, e].rearrange("(c p) d -> p c d", p=128), writes=[("WD", s)])

            for e in range(NSLOT):
                load_expert(e)
            items = [(G, t, e) for G in range(4) for t in range(NT) for e in range(4 * G, 4 * G + 4)]
            sd = {}
            cnt = [0]

            def s1(it):
                G, t, e = it
                s = e % NSLOT
                b = rbank()
                sd[it] = {"b": b, "i": cnt[0] % 2}
                cnt[0] += 1
                for c in range(8):
                    mm(bank(b), xT[:, c, tcols(t)], WGU[s][:, c, :], c == 0, c == 7,
                       [("xT", t), ("WGU", s)], [("ps", b)], inc=(c == 7))

            def s2(it):
                G, t, e = it
                d = sd[it]
                b, i = d["b"], d["i"]
                act(SB[i][:, 0:256], bank(b)[:, 0:256], AF.Silu, [("ps", b)], [("SB", i)])
                stt(TB[i][:, 0:256], bank(b)[:, 256:512], gates[:, t, e:e + 1], SB[i][:, 0:256], ALU.mult, ALU.mult,
                    [("ps", b), ("gates", t), ("SB", i)], [("TB", i)])
                b2 = rbank()
                d["b2"] = b2
                for fc in range(2):
                    tr(bankb(b2)[:, fc * 128:(fc + 1) * 128], TB[i][:, fc * 128:(fc + 1) * 128], identb[:],
                       [("TB", i), "identb"], [("ps", b2)], inc=(fc == 1))

            def s3(it):
                G, t, e = it
                d = sd[it]
                b2, i = d["b2"], d["i"]
                s = e % NSLOT
                cp("act", TTB[i][:, 0:256], bankb(b2)[:, 0:256], [("ps", b2)], [("TTB", i)])
                first = (e % 4 == 0)
                lastx = (e % 4 == 3)
                for half in range(2):
                    yb = 4 + 2 * (t % 2) + half
                    for fc in range(2):
                        mm(bank(yb), TTB[i][:, fc * 128:(fc + 1) * 128], WD[s][:, fc, half * 512:(half + 1) * 512],
                           first and fc == 0, lastx and fc == 1, [("TTB", i), ("WD", s)], [("ps", yb)],
                           inc=(fc == 1))
                if t == NT - 1 and e + NSLOT < 16:
                    load_expert(e + NSLOT)
                if lastx:
                    for half in range(2):
                        yb = 4 + 2 * (t % 2) + half
                        xs = X[:, t, half * 512:(half + 1) * 512]
                        if G == 0:
                            stt(xs, xs, ALPHA, bank(yb), ALU.mult, ALU.add, [("X", t), ("ps", yb)], [("X", t)])
                        else:
                            tt(xs, xs, bank(yb), ALU.add, [("X", t), ("ps", yb)], [("X", t)])
                    if G == 3:
                        ln_stats(t, t % 2)

                        def fin(t=t):
                            ln_apply(t, t % 2)
                            if last_layer:
                                P.dma("sp", tm(out_d)[:, t, :], X[:, t, :], reads=[("X", t)], final=True)
                        bg.append(lambda: None)
                        bg.append(fin)

            pipeline(items, s1, s2, s3, bg_per_iter=1)

        def run_layers():
            for l in range(nlayers):
                for t in range(NT):
                    build_xT(t)
                if stop_after == "xT":
                    return False
                phase_A(l)
                P.barrier()
                if stop_after in ("A", "A1", "A2"):
                    return False
                phase_B(l)
                P.barrier()
                if stop_after == "B":
                    return False
                phase_C(l)
                P.barrier()
                if stop_after == "C":
                    return False
                phase_moe(l, l == nlayers - 1)
                P.barrier()
            return True

        if not run_layers():
            for t in range(NT):
                P.dma("sp", tm(out_d)[:, t, :], X[:, t, :], reads=[("X", t)], final=True)

        P.emit(nc, sems)
    return nc


_CACHE = {}


def kernel(**inputs):
    consts = _consts()
    if "nc" not in _CACHE:
        _CACHE["nc"] = build_program()
    nc = _CACHE["nc"]
    x = np.ascontiguousarray(np.asarray(inputs["x"], dtype=np.float32))
    shared = {}
    for k, v in inputs.items():
        if k == "x":
            continue
        shared[k] = np.ascontiguousarray(np.asarray(v, dtype=np.float32))
    shared.update(consts)
    in_maps = []
    for c in range(NCORES):
        m = dict(shared)
        m["x"] = x[c]
        in_maps.append(m)
    res = run_bass_kernel_spmd(nc, in_maps, core_ids=list(range(NCORES)))
    out = np.stack([np.asarray(r["out"], dtype=np.float32) for r in res.results], axis=0)
    return out
```

```python
import contextlib
import numpy as np
import concourse.bass as bass
import concourse.mybir as mybir
from concourse.bass_utils import run_bass_kernel_spmd

F32 = mybir.dt.float32
BF16 = mybir.dt.bfloat16
AF = mybir.ActivationFunctionType
ALU = mybir.AluOpType
AX = mybir.AxisListType

S = 2048
D = 1024
NT = 16
NCORES = 8
DEPTH = 2
ALPHA = float((2 * DEPTH) ** 0.25)
IDXW = float((4 * 64) ** -0.5)
SC64 = float(64 ** -0.5)
SC96 = float(96 ** -0.5)
TOPK = 256
NBIS = 14
NEG = -1.0e30
NDMA_SEMS = 8
ACC_ENG = "dve"
LNP_ENG = "dve"


class Prog:
    ENGS = ("pe", "act", "dve", "pool", "sp")

    def __init__(self):
        self.ops = {e: [] for e in self.ENGS}
        self.cnt = {e: 0 for e in self.ENGS}
        self.last_w = {}
        self.readers = {}
        self.waited = {e: {} for e in self.ENGS}
        self.dma_val = {}
        self.dma_rr = {"sp": 0, "pool": 0}
        self.final_tokens = []

    def _deps(self, eng, reads, writes):
        deps = {}

        def add(tok, raw):
            src, val = tok
            if src == eng and eng == "pe":
                return
            if deps.get(src, 0) < val:
                deps[src] = val

        for k in reads:
            if k in self.last_w:
                add(self.last_w[k], True)
            if isinstance(k, tuple) and k[0] == "ps":
                for r in self.readers.get(k, ()):
                    if r[0] != eng:
                        add(r, False)
        for k in writes:
            if k in self.last_w:
                add(self.last_w[k], False)
            for r in self.readers.get(k, ()):
                add(r, False)
        waits = []
        for src, val in deps.items():
            if self.waited[eng].get(src, 0) >= val:
                continue
            self.waited[eng][src] = val
            waits.append((src, val))
        return waits

    def _record(self, tok, reads, writes):
        for k in writes:
            self.last_w[k] = tok
            self.readers[k] = []
        for k in reads:
            self.readers.setdefault(k, []).append(tok)

    def op(self, eng, fn, reads=(), writes=(), inc=True):
        waits = self._deps(eng, reads, writes)
        if inc:
            self.cnt[eng] += 1
            idx = self.cnt[eng]
        else:
            idx = self.cnt[eng] + 1
        tok = (eng, idx)
        self._record(tok, reads, writes)
        self.ops[eng].append((waits, fn, ("eng", eng) if inc else None))
        return tok

    def dma(self, q, out_ap, in_ap, reads=(), writes=(), final=False):
        i = self.dma_rr[q]
        self.dma_rr[q] = (i + 1) % NDMA_SEMS
        src = ("dma", q, i)
        prev = self.dma_val.get(src, 0)
        waits = self._deps(q, reads, writes)
        if prev and self.waited[q].get(src, 0) < prev:
            self.waited[q][src] = prev
            waits.append((src, prev))
        val = prev + 16
        self.dma_val[src] = val
        tok = (src, val)
        self._record(tok, reads, writes)

        def fn(e, out_ap=out_ap, in_ap=in_ap):
            return e.dma_start(out=out_ap, in_=in_ap)

        self.ops[q].append((waits, fn, ("dma", src)))
        if final:
            self.final_tokens.append(tok)
        return tok

    def barrier(self):
        snap = [(e, self.cnt[e]) for e in self.ENGS if self.cnt[e] > 0]
        snap += [(src, v) for src, v in self.dma_val.items()]
        for e in self.ENGS:
            waits = []
            for src, val in snap:
                if src == e:
                    continue
                if self.waited[e].get(src, 0) >= val:
                    continue
                self.waited[e][src] = val
                waits.append((src, val))
            if waits:
                self.ops[e].append((waits, None, None))

    def emit(self, nc, sems):
        fin = list(self.final_tokens)

        def replay(eng, e):
            for waits, fn, inc in self.ops[eng]:
                for src, val in waits:
                    e.wait_ge(sems[src], val)
                if fn is None:
                    continue
                ins = fn(e)
                if inc is not None:
                    if inc[0] == "eng":
                        ins.then_inc(sems[inc[1]], 1)
                    else:
                        ins.then_inc(sems[inc[1]], 16)
            if eng == "sp":
                for src, val in fin:
                    e.wait_ge(sems[src], val)

        with nc.Block() as block:
            @block.tensor
            def _(e):
                replay("pe", e)

            @block.scalar
            def _(e):
                replay("act", e)

            @block.vector
            def _(e):
                replay("dve", e)

            @block.gpsimd
            def _(e):
                replay("pool", e)

            @block.sync
            def _(e):
                replay("sp", e)


def _consts():
    pos = np.arange(S, dtype=np.float64)
    c = {}
    for dim, nm in ((64, "64"), (32, "32")):
        inv = 1.0 / (10000.0 ** (np.arange(0, dim, 2, dtype=np.float64) / dim))
        inv = inv.astype(np.float32).astype(np.float64)
        ang = (pos.astype(np.float32)[:, None] * inv.astype(np.float32)[None, :]).astype(np.float32)
        c["cos" + nm] = np.cos(ang.astype(np.float64)).astype(np.float32)
        c["sin" + nm] = np.sin(ang.astype(np.float64)).astype(np.float32)
    c["ident"] = np.eye(128, dtype=np.float32)
    fv = np.concatenate([2.0 ** -(np.arange(16) + 1.0), 2.0 ** -np.arange(16).astype(np.float64)])
    c["fvec"] = np.tile(fv.astype(np.float32)[None, :], (128, 1))
    qi = np.arange(128)[:, None]
    kj = np.arange(256)[None, :]
    diff = qi + 128 - kj
    kk = np.arange(128)[None, :]
    c["negmask"] = np.where(kk <= qi, 0.0, NEG).astype(np.float32)
    c["causalT"] = (qi <= kk).astype(np.float32)
    c["maskPC"] = np.concatenate([(qi > kk).astype(np.float32), c["causalT"]], axis=1)
    return c


def build_program(nlayers=DEPTH, debug_mix=False, stop_after=None):
    nc = bass.Bass("TRN2", target_bir_lowering=False)

    def din(name, shape):
        return nc.dram_tensor(name, list(shape), F32, kind="ExternalInput").ap()

    x_d = din("x", [S, D])
    w_in_d = din("w_in", [DEPTH, D, 1892])
    sinks_d = din("attn_sinks", [DEPTH, 8])
    gq_d = din("c_q_norm_g", [DEPTH, 256])
    gkv_d = din("c_kv_norm_g", [DEPTH, 128])
    w_uq_d = din("w_uq", [DEPTH, 256, 384])
    w_ukv_d = din("w_ukv", [DEPTH, 128, 512])
    w_out_d = din("w_out", [DEPTH, D, D])
    ln1g_d = din("ln1_g", [DEPTH, D])
    ln1b_d = din("ln1_b", [DEPTH, D])
    w_router_d = din("w_router", [D, 16])
    rbias_d = din("router_bias", [16])
    w_gate_d = din("w_gate", [DEPTH, 16, D, 256])
    w_up_d = din("w_up", [DEPTH, 16, D, 256])
    w_down_d = din("w_down", [DEPTH, 16, 256, D])
    ln2g_d = din("ln2_g", [DEPTH, D])
    ln2b_d = din("ln2_b", [DEPTH, D])
    cos64_d = din("cos64", [S, 32])
    sin64_d = din("sin64", [S, 32])
    cos32_d = din("cos32", [S, 16])
    sin32_d = din("sin32", [S, 16])
    ident_d = din("ident", [128, 128])
    fvec_d = din("fvec", [128, 32])
    negmask_d = din("negmask", [128, 128])
    causalT_d = din("causalT", [128, 128])
    maskPC_d = din("maskPC", [128, 256])
    out_d = nc.dram_tensor("out", [S, D], F32, kind="ExternalOutput").ap()
    dbg_d = None
    if debug_mix:
        dbg_d = nc.dram_tensor("dbg", [S, D], F32, kind="ExternalOutput").ap()

    P = Prog()
    st = contextlib.ExitStack()
    with st:
        sems = {}
        for e in Prog.ENGS:
            sems[e] = st.enter_context(nc.semaphore("s_" + e))
        for q in ("sp", "pool"):
            for i in range(NDMA_SEMS):
                sems[("dma", q, i)] = st.enter_context(nc.semaphore(f"d_{q}{i}"))

        def T(name, shape, dt):
            return st.enter_context(nc.sbuf_tensor("sb_" + name, list(shape), dt))

        X = T("X", [128, NT, D], F32)
        xT = T("xT", [128, 8, S], BF16)
        cos64 = T("cos64", [128, NT, 32], F32)
        sin64 = T("sin64", [128, NT, 32], F32)
        nsin64 = T("nsin64", [128, NT, 32], F32)
        cos32 = T("cos32", [128, NT, 16], F32)
        sin32 = T("sin32", [128, NT, 16], F32)
        nsin32 = T("nsin32", [128, NT, 16], F32)
        identf = T("identf", [128, 128], F32)
        fvec = T("fvec", [128, 32], F32)
        identb = T("identb", [128, 128], BF16)
        negmask = T("negmask", [128, 128], F32)
        causalT = T("causalT", [128, 128], BF16)
        maskPC = T("maskPC", [128, 256], BF16)
        ones1 = T("ones1", [1, 128], F32)
        lnp = T("lnp", [128, 2, D], F32)
        gates = T("gates", [128, NT, 16], F32)
        widx = T("widx", [128, NT, 4], F32)
        wr = T("wr", [128, 8, 16], F32)
        rb = T("rb", [128, 16], F32)
        sinks = T("sinks", [128, 8], F32)
        gq = T("gq", [128, 256], F32)
        gkv = T("gkv", [128, 128], F32)
        sm = T("sm", [128, 256], F32)
        ARW = 22400
        arena = T("arena", [128, ARW], F32)
        psum = st.enter_context(nc.psum_tensor("psum", [128, 4096], F32))

        def bank(i):
            return psum[:, 512 * i:512 * (i + 1)]

        def bankb(i):
            return psum[:, 512 * i:512 * (i + 1)].bitcast(BF16)

        class Arena:
            def __init__(self):
                self.off = 0

            def f32(self, n):
                o = self.off
                self.off += n
                assert self.off <= ARW, self.off
                return arena[:, o:o + n]

            def bf16(self, n):
                w = (n + 1) // 2
                o = self.off
                self.off += w
                assert self.off <= ARW, self.off
                return arena[:, o:o + w].bitcast(BF16)

        A = Arena()
        WIN = A.bf16(8 * 768)
        WOUT = A.bf16(4 * 1024)
        HSB = [A.f32(768), A.f32(768)]
        phase_qkr_off = A.off
        QKR = [A.f32(768), A.f32(768)]
        SQ = A.f32(768)
        PB = [A.bf16(512), A.bf16(512)]
        PTB = [A.bf16(512), A.bf16(512)]
        ROPET = A.f32(640)
        mixt_off = A.off
        MIXT = A.bf16(512)
        MIXTT = A.bf16(512)
        MIXT_f32 = arena[:, mixt_off:mixt_off + 512]
        phase_base = A.off

        rot = [0]
        rot4 = [0]
        inpipe = [False]

        def rbank():
            i = rot[0]
            rot[0] = (i + 1) % 3
            return i

        def mbank():
            if inpipe[0]:
                return 3
            i = rot4[0]
            rot4[0] = (i + 1) % 4
            return i

        def mm(out, lhsT, rhs, start, stop, reads, writes, inc):
            P.op("pe", lambda e, o=out, l=lhsT, r=rhs, s0=start, s1=stop: e.matmul(o, lhsT=l, rhs=r, start=s0, stop=s1),
                 reads=reads, writes=writes, inc=inc)

        def tr(out, in_, ident, reads, writes, inc=True):
            P.op("pe", lambda e, o=out, i=in_, d=ident: e.transpose(out=o, in_=i, identity=d),
                 reads=reads, writes=writes, inc=inc)

        def act(out, in_, func, reads, writes, bias=None, scale=None, accum=None):
            kw = {}
            if bias is not None:
                kw["bias"] = bias
            if scale is not None:
                kw["scale"] = scale
            if accum is not None:
                kw["accum_out"] = accum
            P.op("act", lambda e, o=out, i=in_, f=func, kw=kw: e.activation(out=o, in_=i, func=f, **kw),
                 reads=reads, writes=writes)

        def tt(out, in0, in1, op, reads, writes, eng="dve"):
            P.op(eng, lambda e, o=out, a=in0, b=in1, p=op: e.tensor_tensor(out=o, in0=a, in1=b, op=p),
                 reads=reads, writes=writes)

        def ts(out, in0, s1, s2, op0, op1, reads, writes, accum=None, eng="dve"):
            def fn(e, o=out, a=in0, s1=s1, s2=s2, op0=op0, op1=op1, accum=accum):
                kw = {}
                if op1 is not None:
                    kw["op1"] = op1
                if accum is not None:
                    kw["accum_out"] = accum
                return e.tensor_scalar(out=o, in0=a, scalar1=s1, scalar2=s2, op0=op0, **kw)
            P.op(eng, fn, reads=reads, writes=writes)

        def stt(out, in0, scalar, in1, op0, op1, reads, writes, eng="dve"):
            P.op(eng, lambda e, o=out, a=in0, s=scalar, b=in1, p0=op0, p1=op1:
                 e.scalar_tensor_tensor(out=o, in0=a, scalar=s, in1=b, op0=p0, op1=p1),
                 reads=reads, writes=writes)

        def cp(eng, out, in_, reads, writes):
            if eng == "act":
                act(out, in_, AF.Copy, reads, writes)
            else:
                P.op(eng, lambda e, o=out, i=in_: e.tensor_copy(out=o, in_=i), reads=reads, writes=writes)

        def red(out, in_, op, reads, writes, absv=False):
            def fn(e, o=out, i=in_, p=op, a=absv):
                if a:
                    return e.tensor_reduce(out=o, in_=i, axis=AX.X, op=p, apply_absolute_value=True)
                return e.tensor_reduce(out=o, in_=i, axis=AX.X, op=p)
            P.op("dve", fn, reads=reads, writes=writes)

        def memset(ap, val, writes, eng="dve"):
            P.op(eng, lambda e, a=ap, v=val: e.memset(a, v), writes=writes)

        def recip(out, in_, reads, writes):
            P.op("dve", lambda e, o=out, i=in_: e.reciprocal(out=o, in_=i), reads=reads, writes=writes)

        bg = []

        def bg_run(k):
            for _ in range(k):
                if not bg:
                    return
                bg.pop(0)()

        def pipeline(items, s1, s2, s3, bg_per_iter=0):
            n = len(items)
            inpipe[0] = True
            for i in range(n + 2):
                if i < n:
                    s1(items[i])
                if 0 <= i - 1 < n:
                    s2(items[i - 1])
                if 0 <= i - 2 < n:
                    s3(items[i - 2])
                if bg_per_iter:
                    bg_run(bg_per_iter)
            bg_run(len(bg))
            inpipe[0] = False

        def run_tiles(Pf, Rf):
            Pf(0)
            for t in range(NT):
                if t + 1 < NT:
                    Pf(t + 1)
                Rf(t)

        def run_stages(Pf, stages):
            ns = len(stages)
            Pf(0)
            for i in range(NT + ns - 1):
                if i + 1 < NT:
                    Pf(i + 1)
                for k, st_ in enumerate(stages):
                    if 0 <= i - k < NT:
                        st_(i - k)

        def tcols(t):
            return slice(t * 128, (t + 1) * 128)

        PBs = [PB[0], PB[1], PTB[0]]
        pbrot = [0]

        def st1(it):
            b = rbank()
            it["b"] = b
            k = it["kind"]
            if k == "att":
                it["pi"] = pbrot[0]
                pbrot[0] = (pbrot[0] + 1) % 3
                sl = it["slots"]
                for j, s in enumerate(sl):
                    ka, kk = s["K"]
                    qa, qk = s["Q"]
                    mm(bank(b)[:, j * 128:(j + 1) * 128], ka, qa, True, True, kk + qk, [("ps", b)], inc=(j == len(sl) - 1))
            elif k == "idx":
                mm(bank(b)[:, 0:it["n"]], it["lhsT"], it["rhs"], True, True, it["keys"], [("ps", b)], True)
            elif k == "mskT":
                kts = it["kts"]
                for j, kt in enumerate(kts):
                    tr(bankb(b)[:, j * 128:(j + 1) * 128], it["src"][:, kt * 128:(kt + 1) * 128], identb[:],
                       [it["srckey"], "identb"], [("ps", b)], inc=(j == len(kts) - 1))

        def st2(it):
            b = it["b"]
            k = it["kind"]
            if k == "att":
                pi = it["pi"]
                n = 128 * len(it["slots"])
                act(PBs[pi][:, 0:n], bank(b)[:, 0:n], AF.Exp, [("ps", b), "negc"], [("PB", pi)], bias=it["negc"], scale=it["scale"])
                for (c0, c1, view, m_ap, mk) in it["masks"]:
                    pv = PBs[pi][:, c0:c1]
                    if view is not None:
                        pv = pv.rearrange(view[0], **view[1])
                    tt(pv, pv, m_ap, ALU.mult, [("PB", pi)] + mk, [("PB", pi)])
            elif k == "idx":
                ri, n, h, qb, k0 = it["ri"], it["n"], it["h"], it["qb"], it["k0"]
                sco, sk = it["sco"], it["scokey"]
                act(it["rb"][:, 0:n], bank(b)[:, 0:n], AF.Relu, [("ps", b)], ["RB%d" % ri])
                if h == 0:
                    ts(sco[:, k0:k0 + n], it["rb"][:, 0:n], widx[:, qb, 0:1], None, ALU.mult, None,
                       ["RB%d" % ri, ("widx", qb)], [sk], eng=ACC_ENG)
                elif ACC_ENG == "dve":
                    stt(sco[:, k0:k0 + n], it["rb"][:, 0:n], widx[:, qb, h:h + 1], sco[:, k0:k0 + n],
                        ALU.mult, ALU.add, ["RB%d" % ri, ("widx", qb), sk], [sk])
                else:
                    ts(it["rb"][:, 0:n], it["rb"][:, 0:n], widx[:, qb, h:h + 1], None, ALU.mult, None,
                       ["RB%d" % ri, ("widx", qb)], ["RB%d" % ri], eng=ACC_ENG)
                    tt(sco[:, k0:k0 + n], sco[:, k0:k0 + n], it["rb"][:, 0:n], ALU.add, ["RB%d" % ri, sk], [sk], eng=ACC_ENG)
            elif k == "mskT":
                kts = it["kts"]
                nj = len(kts)
                cp("act", it["dst"][:, kts[0]:kts[0] + nj, :],
                   bankb(b)[:, 0:nj * 128].rearrange("p (c n) -> p c n", c=nj), [("ps", b)], [it["dstkey"]])

        def st3(it):
            if it["kind"] != "att":
                return
            pi = it["pi"]
            sl = it["slots"]
            for j, s in enumerate(sl):
                va, vk = s["V"]
                ob = s["ob"]
                mm(bank(ob)[:, s["oreg"]:s["oreg"] + 65], PBs[pi][:, j * 128:(j + 1) * 128], va, s["start"], s["stop"],
                   [("PB", pi)] + vk, [("ps", ob)], inc=(j == len(sl) - 1 or sl[j + 1]["ob"] != ob))
            if it.get("fin") is not None:
                it["fin"]()

        def run_seq(seq, bg_per_iter=0):
            n = len(seq)
            inpipe[0] = True
            for i in range(n + 2):
                if i < n and seq[i][0] is not None:
                    st1(seq[i][0])
                if 0 <= i - 1 < n and seq[i - 1][0] is not None:
                    st2(seq[i - 1][0])
                if 0 <= i - 2 < n and seq[i - 2][0] is not None:
                    st3(seq[i - 2][0])
                if i < n:
                    for c in seq[i][1]:
                        c()
                if bg_per_iter:
                    bg_run(bg_per_iter)
            bg_run(len(bg))
            inpipe[0] = False

        def fin_generic(qb, ob, nheads, nchunks_w, mix_c0, epilogue):
            def fn():
                o3 = bank(ob)[:, 0:nheads * 65].rearrange("p (h d) -> p h d", h=nheads)
                rc = sm[:, 72:72 + nheads]
                recip(rc, o3[:, :, 64], [("ps", ob)], ["rc"])
                tt(MIXT[:, 0:nheads * 64].rearrange("p (h d) -> p h d", h=nheads), o3[:, :, 0:64],
                   rc.unsqueeze(2).to_broadcast([128, nheads, 64]), ALU.mult, [("ps", ob), "rc"], ["MIXT"])
                dbg_store(qb, mix_c0, nheads * 64)
                out_proj_partial(qb, nchunks_w, False, after=epilogue)
            return fn

        def tm(ap_d):
            return ap_d.rearrange("(t p) d -> p t d", p=128)

        P.dma("sp", cos64[:], tm(cos64_d), writes=["cos64"])
        P.dma("sp", sin64[:], tm(sin64_d), writes=["sin64"])
        P.dma("sp", cos32[:], tm(cos32_d), writes=["cos32"])
        P.dma("sp", sin32[:], tm(sin32_d), writes=["sin32"])
        P.dma("sp", identf[:], ident_d, writes=["identf"])
        P.dma("sp", fvec[:], fvec_d, writes=["fvec"])
        P.dma("pool", identb[:], ident_d, writes=["identb"])
        P.dma("pool", causalT[:], causalT_d, writes=["causalT"])
        P.dma("pool", maskPC[:], maskPC_d, writes=["maskPC"])
        P.dma("sp", negmask[:], negmask_d, writes=["negmask"])
        P.dma("sp", wr[:], w_router_d.rearrange("(c p) n -> p c n", p=128), writes=["wr"])
        P.dma("sp", rb[:], rbias_d.partition_broadcast(128), writes=["rb"])
        xv = tm(x_d)
        for t4 in range(4):
            P.dma("sp", X[:, 4 * t4:4 * t4 + 4, :], xv[:, 4 * t4:4 * t4 + 4, :],
                  writes=[("X", t) for t in range(4 * t4, 4 * t4 + 4)])
        ts(nsin64[:], sin64[:], -1.0, None, ALU.mult, None, ["sin64"], ["nsin64"])
        ts(nsin32[:], sin32[:], -1.0, None, ALU.mult, None, ["sin32"], ["nsin32"])
        memset(ones1[:], 1.0, ["ones1"])

        def build_xT(t):
            for half in range(2):
                b = mbank()
                for j in range(4):
                    c = half * 4 + j
                    tr(bank(b)[:, j * 128:(j + 1) * 128], X[:, t, c * 128:(c + 1) * 128], identf[:],
                       [("X", t), "identf"], [("ps", b)], inc=(j == 3))
                cp("act" if half == 0 else "dve",
                   xT[:, half * 4:half * 4 + 4, tcols(t)],
                   bank(b).rearrange("p (c n) -> p c n", c=4),
                   [("ps", b)], [("xT", t)])

        def in_proj(t, ncols, wview, hs):
            n0 = 0
            while n0 < ncols:
                n1 = min(ncols, n0 + 512)
                b = mbank()
                for c in range(8):
                    mm(bank(b)[:, 0:n1 - n0], xT[:, c, tcols(t)], wview[:, c, n0:n1], c == 0, c == 7,
                       [("xT", t), "WIN"], [("ps", b)], inc=(c == 7))
                cp("act", hs[:, n0:n1], bank(b)[:, 0:n1 - n0], [("ps", b)], ["HSB%d" % (t % 2)])
                n0 = n1

        def rope(src, dst, nh, hd, t, cosT, sinT, nsinT, rk, wk, tmp=None):
            h2 = hd // 2
            cb = cosT[:, t, :].unsqueeze(1).unsqueeze(1).to_broadcast([128, nh, 2, h2])
            sb = sinT[:, t, :].unsqueeze(1).to_broadcast([128, nh, h2])
            nb = nsinT[:, t, :].unsqueeze(1).to_broadcast([128, nh, h2])
            tv = ROPET[:, 0:nh * hd].rearrange("p (h t d) -> p h t d", h=nh, t=2)
            rk = rk + ["cos64", "sin64", "nsin64", "cos32", "sin32", "nsin32"]
            tt(dst, src, cb, ALU.mult, rk, wk)
            tt(tv[:, :, 0, :], src[:, :, 1, :], nb, ALU.mult, rk, ["ropetmp"])
            tt(tv[:, :, 1, :], src[:, :, 0, :], sb, ALU.mult, rk, ["ropetmp"])
            tt(dst, dst, tv, ALU.add, wk + ["ropetmp"], wk)

        def global_bound(mt, nh, qsl, ksl, scale, negc):
            b = mbank()
            tr(bank(b)[0:nh, 0:128], mt, identf[:], ["mt", "identf"], [("ps", b)])
            red(sm[0:nh, 0:1], bank(b)[0:nh, 0:128], ALU.max, [("ps", b)], ["gb1"])
            b2 = mbank()
            tr(bank(b2)[0:1, 0:nh], sm[0:nh, 0:1], identf[0:nh, 0:nh], ["gb1", "identf"], [("ps", b2)])
            red(sm[0:1, 1:2], bank(b2)[0:1, qsl], ALU.max, [("ps", b2)], ["gb2"])
            red(sm[0:1, 2:3], bank(b2)[0:1, ksl], ALU.max, [("ps", b2)], ["gb3"])
            tt(sm[0:1, 3:4], sm[0:1, 1:2], sm[0:1, 2:3], ALU.mult, ["gb2", "gb3"], ["gb4"])
            act(sm[0:1, 4:5], sm[0:1, 3:4], AF.Ln, ["gb4"], ["gb5"])
            act(sm[0:1, 5:6], sm[0:1, 4:5], AF.Exp, ["gb5"], ["gb6"], scale=0.5)
            ts(sm[0:1, 6:7], sm[0:1, 5:6], -scale, None, ALU.mult, None, ["gb6"], ["gb7"])
            b3 = mbank()
            mm(bank(b3)[:, 0:1], ones1[0:1, :], sm[0:1, 6:7], True, True, ["gb7", "ones1"], [("ps", b3)], True)
            cp("dve", negc, bank(b3)[:, 0:1], [("ps", b3)], ["negc"])

        def head_sumsq(src, ncols, nh, mt, first, rk):
            act(SQ[:, 0:ncols], src, AF.Square, rk, ["SQ"])
            hd = ncols // nh
            if first:
                red(mt, SQ[:, 0:ncols].rearrange("p (h d) -> p h d", h=nh), ALU.add, ["SQ"], ["mt"])
            else:
                red(sm[:, 16:16 + nh], SQ[:, 0:ncols].rearrange("p (h d) -> p h d", h=nh), ALU.add, ["SQ"], ["hs"])
                tt(mt, mt, sm[:, 16:16 + nh], ALU.max, ["mt", "hs"], ["mt"])

        def out_proj_T(qb, nchunks):
            b = mbank()
            for c in range(nchunks):
                tr(bankb(b)[:, c * 128:(c + 1) * 128], MIXT[:, c * 128:(c + 1) * 128], identb[:],
                   ["MIXT", "identb"], [("ps", b)], inc=(c == nchunks - 1))
            cp("act", MIXTT[:, 0:nchunks * 128], bankb(b)[:, 0:nchunks * 128], [("ps", b)], ["MIXTT"])

        def out_proj_M(qb, nchunks, first):
            wv = WOUT.rearrange("p (c n) -> p c n", n=1024)
            for half in range(2):
                yb = 6 + half
                for c in range(nchunks):
                    mm(bank(yb), MIXTT[:, c * 128:(c + 1) * 128], wv[:, c, half * 512:(half + 1) * 512],
                       c == 0, c == nchunks - 1, ["MIXTT", "WOUT"], [("ps", yb)], inc=(c == nchunks - 1))
                xs = X[:, qb, half * 512:(half + 1) * 512]
                if first:
                    stt(xs, xs, ALPHA, bank(yb), ALU.mult, ALU.add, [("X", qb), ("ps", yb)], [("X", qb)])
                else:
                    tt(xs, xs, bank(yb), ALU.add, [("X", qb), ("ps", yb)], [("X", qb)])

        def out_proj_partial(qb, nchunks, first, after=None):
            bg.append(lambda: out_proj_T(qb, nchunks))

            def part2():
                out_proj_M(qb, nchunks, first)
                if after is not None:
                    after(qb)
            bg.append(part2)

        def dbg_store(qb, c0, ncols):
            if dbg_d is None:
                return
            cp("dve", ROPET[:, 0:ncols], MIXT[:, 0:ncols], ["MIXT"], ["ropetmp"])
            P.dma("sp", tm(dbg_d)[:, qb, c0:c0 + ncols], ROPET[:, 0:ncols], reads=["ropetmp"], final=True)

        def ln_stats(t, k):
            o = 32 + 16 * k
            ks = "ln%d" % k
            P.op("dve", lambda e, o_=sm[:, o:o + 6], i=X[:, t, 0:512]: e.bn_stats(out=o_, in_=i), reads=[("X", t)], writes=[ks + "a"])
            P.op("dve", lambda e, o_=sm[:, o + 6:o + 12], i=X[:, t, 512:1024]: e.bn_stats(out=o_, in_=i), reads=[("X", t)], writes=[ks + "b"])
            P.op("dve", lambda e, o_=sm[:, o + 12:o + 14], i=sm[:, o:o + 12]: e.bn_aggr(out=o_, in_=i), reads=[ks + "a", ks + "b"], writes=[ks + "mv"])
            act(sm[:, o + 14:o + 15], sm[:, o + 13:o + 14], AF.Ln, [ks + "mv"], [ks + "lv"], bias=1e-5)
            act(sm[:, o + 15:o + 16], sm[:, o + 14:o + 15], AF.Exp, [ks + "lv"], [ks + "rs"], scale=-0.5)

        def ln_apply(t, k):
            o = 32 + 16 * k
            ks = "ln%d" % k
            xs = X[:, t, :]
            ts(xs, xs, sm[:, o + 12:o + 13], sm[:, o + 15:o + 16], ALU.subtract, ALU.mult, [("X", t), ks + "mv", ks + "rs"], [("X", t)])
            tt(xs, xs, lnp[:, 0, :], ALU.mult, [("X", t), "lnp"], [("X", t)], eng=LNP_ENG)
            tt(xs, xs, lnp[:, 1, :], ALU.add, [("X", t), "lnp"], [("X", t)], eng=LNP_ENG)

        def layer_norm(t, k=0):
            ln_stats(t, k)
            ln_apply(t, k)

        def load_ln(g_d, b_d, l):
            P.dma("sp", lnp[:, 0, :], g_d[l].partition_broadcast(128), writes=["lnp"])
            P.dma("sp", lnp[:, 1, :], b_d[l].partition_broadcast(128), writes=["lnp"])

        def phase_A(l):
            A.off = phase_base
            FM = A.bf16(6 * S).rearrange("p (c n) -> p c n", c=6)
            VA = A.bf16(NT * 2 * 65).rearrange("p (t g d) -> p t g d", t=NT, g=2)
            mt = A.f32(16)[:, 0:10]
            negc = A.f32(2)[:, 0:1]
            esink = A.f32(8)
            den = A.f32(8)
            wv = WIN[:, 0:8 * 768].rearrange("p (c n) -> p c n", c=8)
            P.dma("pool", wv, w_in_d[l].rearrange("(c p) n -> p c n", p=128)[:, :, 0:768], writes=["WIN"])
            P.dma("pool", WOUT[:, 0:4096].rearrange("p (c n) -> p c n", c=4),
                  w_out_d[l, 0:512, :].rearrange("(c p) n -> p c n", p=128), writes=["WOUT"])
            P.dma("sp", sinks[:], sinks_d[l].partition_broadcast(128), writes=["sinks"])
            memset(VA[:, :, :, 64:65], 1.0, ["VAones"])
            def Pf(t):
                in_proj(t, 768, wv, HSB[t % 2])

            def Rf(t):
                hs = HSB[t % 2]
                qk = QKR[t % 2]
                hk = "HSB%d" % (t % 2)
                qkk = "QKR%d" % (t % 2)
                head_sumsq(hs[:, 0:640], 640, 10, mt, t == 0, [hk])
                rope(hs[:, 0:512].rearrange("p (h t d) -> p h t d", h=8, t=2),
                     qk[:, 0:512].rearrange("p (h t d) -> p h t d", h=8, t=2),
                     8, 64, t, cos64, sin64, nsin64, [hk], [qkk])
                kdst = qk[:, 512:768].rearrange("p (g r d) -> p g r d", g=2, r=2)
                rope(hs[:, 512:640].rearrange("p (h t d) -> p h t d", h=2, t=2),
                     kdst[:, :, 0, :].rearrange("p g (t d) -> p g t d", t=2),
                     2, 64, t, cos64, sin64, nsin64, [hk], [qkk])
                cp("dve", kdst[:, :, 1, :], kdst[:, :, 0, :], [qkk], [qkk])
                cp("act", VA[:, t, :, 0:64], hs[:, 640:768].rearrange("p (g d) -> p g d", g=2), [hk], [("VA", t)])

            def Rb(t):
                qk = QKR[t % 2]
                qkk = "QKR%d" % (t % 2)
                for part, (c0, nchk) in enumerate(((0, 4), (4, 2))):
                    b = mbank()
                    for j in range(nchk):
                        c = c0 + j
                        tr(bank(b)[:, j * 128:(j + 1) * 128], qk[:, c * 128:(c + 1) * 128], identf[:],
                           [qkk, "identf"], [("ps", b)], inc=(j == nchk - 1))
                    cp("act" if part == 0 else "dve", FM[:, c0:c0 + nchk, tcols(t)],
                       bank(b)[:, 0:nchk * 128].rearrange("p (c n) -> p c n", c=nchk),
                       [("ps", b)], [("FM", t)])

            run_stages(Pf, [Rf, Rb])
            if stop_after == "A1":
                return
            global_bound(mt, 10, slice(0, 8), slice(8, 10), SC64, negc)
            act(esink, sinks[:], AF.Exp, ["sinks", "negc"], ["esink"], bias=negc)
            if stop_after == "A2":
                return

            def finA(qb, g):
                def fn():
                    ob = 4 + g
                    o3 = bank(ob)[:, 0:260].rearrange("p (h d) -> p h d", h=4)
                    tt(den[:, 4 * g:4 * g + 4], o3[:, :, 64], esink[:, 4 * g:4 * g + 4], ALU.add,
                       [("ps", ob), "esink"], ["den"])
                    recip(den[:, 4 * g:4 * g + 4], den[:, 4 * g:4 * g + 4], ["den"], ["den"])
                    tt(MIXT[:, g * 256:(g + 1) * 256].rearrange("p (h d) -> p h d", h=4), o3[:, :, 0:64],
                       den[:, 4 * g:4 * g + 4].unsqueeze(2).to_broadcast([128, 4, 64]), ALU.mult,
                       [("ps", ob), "den"], ["MIXT"])
                    if g == 1:
                        dbg_store(qb, 0, 512)
                        out_proj_partial(qb, 4, True)
                return fn

            seq = []
            for qb in range(NT):
                kts = [kt for kt in (qb - 1, qb) if kt >= 0]
                nk_ = len(kts)
                for g in range(2):
                    for par in range(2):
                        po = 64 * par
                        slots = []
                        for h in (4 * g + par, 4 * g + par + 2):
                            for kt in kts:
                                slots.append({"K": (FM[po:po + 64, 4 + g, tcols(kt)], [("FM", kt)]),
                                              "Q": (FM[po:po + 64, h // 2, tcols(qb)], [("FM", qb)]),
                                              "V": (VA[:, kt, g, :], [("VA", kt), "VAones"]),
                                              "ob": 4 + g, "oreg": (h % 4) * 65,
                                              "start": kt == kts[0], "stop": kt == kts[-1]})
                        if nk_ == 2:
                            masks = [(0, 512, ("p (h n) -> p h n", {"h": 2}),
                                      maskPC[:].unsqueeze(1).to_broadcast([128, 2, 256]), ["maskPC"])]
                        else:
                            masks = [(0, 256, ("p (h n) -> p h n", {"h": 2}),
                                      maskPC[:, 128:256].unsqueeze(1).to_broadcast([128, 2, 128]), ["maskPC"])]
                        seq.append(({"kind": "att", "slots": slots, "scale": SC64, "negc": negc, "masks": masks,
                                     "fin": finA(qb, g) if par == 1 else None}, []))
            run_seq(seq, bg_per_iter=1)

        def phase_B(l):
            A.off = phase_base
            FM = A.bf16(6 * S).rearrange("p (c n) -> p c n", c=6)
            VB = A.bf16(NT * 65 + 1)[:, 0:NT * 65].rearrange("p (t d) -> p t d", t=NT)
            SCO = A.f32(S)
            MSK = A.bf16(S)
            RB = [HSB[0][:, 0:512], HSB[1][:, 0:512]]
            mt = A.f32(16)[:, 0:5]
            negc = A.f32(2)[:, 0:1]
            bis = A.f32(8)
            btab = A.f32(32)
            wv = WIN[:, 0:8 * 708].rearrange("p (c n) -> p c n", c=8)
            P.dma("pool", wv, w_in_d[l].rearrange("(c p) n -> p c n", p=128)[:, :, 768:1476], writes=["WIN"])
            P.dma("pool", WOUT[:, 0:2048].rearrange("p (c n) -> p c n", c=2),
                  w_out_d[l, 512:768, :].rearrange("(c p) n -> p c n", p=128), writes=["WOUT"])
            memset(VB[:, :, 64:65], 1.0, ["VBones"])
            def Pf(t):
                in_proj(t, 708, wv, HSB[t % 2])

            def Rf(t):
                hs = HSB[t % 2]
                qk = QKR[t % 2]
                hk = "HSB%d" % (t % 2)
                qkk = "QKR%d" % (t % 2)
                head_sumsq(hs[:, 0:320], 320, 5, mt, t == 0, [hk])
                for (s0, d0) in ((0, 0), (384, 384)):
                    rope(hs[:, s0:s0 + 320].rearrange("p (h t d) -> p h t d", h=5, t=2),
                         qk[:, d0:d0 + 320].rearrange("p (h t d) -> p h t d", h=5, t=2),
                         5, 64, t, cos64, sin64, nsin64, [hk], [qkk])
                    cp("dve", qk[:, d0 + 320:d0 + 384], qk[:, d0 + 256:d0 + 320], [qkk], [qkk])
                cp("act", VB[:, t, 0:64], hs[:, 320:384], [hk], [("VB", t)])
                ts(widx[:, t, :], hs[:, 704:708], IDXW, None, ALU.mult, None, [hk], [("widx", t)])

            def Rb(t):
                qk = QKR[t % 2]
                qkk = "QKR%d" % (t % 2)
                for part, (c0, nchk) in enumerate(((0, 4), (4, 2))):
                    b = mbank()
                    for j in range(nchk):
                        c = c0 + j
                        tr(bank(b)[:, j * 128:(j + 1) * 128], qk[:, c * 128:(c + 1) * 128], identf[:],
                           [qkk, "identf"], [("ps", b)], inc=(j == nchk - 1))
                    cp("act" if part == 0 else "dve", FM[:, c0:c0 + nchk, tcols(t)],
                       bank(b)[:, 0:nchk * 128].rearrange("p (c n) -> p c n", c=nchk),
                       [("ps", b)], [("FM", t)])

            run_stages(Pf, [Rf, Rb])
            global_bound(mt, 5, slice(0, 4), slice(4, 5), SC64, negc)

            P.barrier()
            SCOs = [SCO, arena[:, 0:S]]
            a_qkr = phase_qkr_off
            MSKT = [arena[:, a_qkr + 1024 * i:a_qkr + 1024 * (i + 1)].bitcast(BF16).rearrange("p (t n) -> p t n", t=NT)
                    for i in range(2)]

            def idx_items(qb):
                nk = (qb + 1) * 128
                par = qb % 2
                out = []
                for k0 in range(0, nk, 512):
                    n = min(512, nk - k0)
                    for h in range(4):
                        po = 64 * (h % 2)
                        ri = (len(out)) % 2
                        out.append({"kind": "idx", "n": n, "h": h, "qb": qb, "k0": k0, "ri": ri, "rb": RB[ri],
                                    "lhsT": FM[po:po + 64, 3 + h // 2, tcols(qb)], "rhs": FM[po:po + 64, 5, k0:k0 + n],
                                    "keys": [("FM", t) for t in range(qb + 1)],
                                    "sco": SCOs[par], "scokey": "SCO%d" % par})
                return out

            def bis_closures(qb):
                nk = (qb + 1) * 128
                par = qb % 2
                sco = SCOs[par]
                sk = "SCO%d" % par
                cl = []

                def init():
                    if qb >= 2:
                        red(bis[:, 0:1], sco[:, 0:nk], ALU.max, [sk], ["bnd"], absv=True)
                    tt(sco[:, qb * 128:nk], sco[:, qb * 128:nk], negmask[:], ALU.add, [sk, "negmask"], [sk])
                    if qb >= 2:
                        ts(bis[:, 2:3], bis[:, 0:1], 2.0, 2.0, ALU.mult, ALU.add, ["bnd"], ["w0"])
                        ts(btab[:], fvec[:], bis[:, 2:3], None, ALU.mult, None, ["w0", "fvec"], ["btab"])
                        memset(bis[:, 3:4], 0.0, ["mid"])
                    else:
                        ts(MSK[:, 0:nk], sco[:, 0:nk], -1.0e29, None, ALU.is_ge, None, [sk], ["MSK"])
                cl.append(init)
                if qb >= 2:
                    def it_fn(k):
                        def fn():
                            ts(MSK[:, 0:nk], sco[:, 0:nk], bis[:, 3:4], None, ALU.is_ge, ALU.add, [sk, "mid"],
                               ["MSK", "cnt"], accum=bis[:, 4:5])
                            if k < NBIS - 1:
                                ts(bis[:, 5:6], bis[:, 4:5], float(TOPK), btab[:, 16 + k + 1:16 + k + 2], ALU.is_ge, ALU.mult,
                                   ["cnt", "btab"], ["stp"])
                                stt(bis[:, 3:4], bis[:, 5:6], btab[:, k + 1:k + 2], bis[:, 3:4], ALU.subtract, ALU.add,
                                    ["stp", "btab", "mid"], ["mid"])
                            else:
                                ts(bis[:, 5:6], bis[:, 4:5], float(TOPK), btab[:, k:k + 1], ALU.is_ge, ALU.mult,
                                   ["cnt", "btab"], ["stp"])
                                stt(bis[:, 1:2], bis[:, 5:6], btab[:, k:k + 1], bis[:, 3:4], ALU.subtract, ALU.add,
                                    ["stp", "btab", "mid"], ["lo"])
                        return fn
                    for k in range(NBIS):
                        cl.append(it_fn(k))
                    cl.append(lambda: ts(MSK[:, 0:nk], sco[:, 0:nk], bis[:, 1:2], None, ALU.is_ge, None, [sk, "lo"], ["MSK"]))
                return cl

            def mskT_items(qb):
                par = qb % 2
                out = []
                for kt0 in range(0, qb + 1, 4):
                    kts = list(range(kt0, min(kt0 + 4, qb + 1)))
                    out.append({"kind": "mskT", "kts": kts, "src": MSK, "srckey": "MSK",
                                "dst": MSKT[par], "dstkey": ("MSKT", par)})
                return out

            def att_items(qb):
                par = qb % 2
                out = []
                for h in range(4):
                    po = 64 * (h % 2)
                    for kt0 in range(0, qb + 1, 4):
                        kts = list(range(kt0, min(kt0 + 4, qb + 1)))
                        slots = [{"K": (FM[po:po + 64, 2, tcols(kt)], [("FM", kt)]),
                                  "Q": (FM[po:po + 64, h // 2, tcols(qb)], [("FM", qb)]),
                                  "V": (VB[:, kt, :], [("VB", kt), "VBones"]),
                                  "ob": 4 + par, "oreg": h * 65, "start": kt == 0, "stop": kt == qb} for kt in kts]
                        ns = len(kts)
                        masks = [(0, 128 * ns, ("p (c n) -> p c n", {"c": ns}), MSKT[par][:, kt0:kt0 + ns, :], [("MSKT", par)])]
                        last = (h == 3 and kts[-1] == qb)
                        out.append({"kind": "att", "slots": slots, "scale": SC64, "negc": negc, "masks": masks,
                                    "fin": fin_generic(qb, 4 + par, 4, 2, 512, None) if last else None})
                return out

            def merge(a, b):
                out = []
                na, nb = len(a), len(b)
                ia = ib = 0
                while ia < na or ib < nb:
                    if ib >= nb or (ia < na and ia * nb <= ib * na):
                        out.append(a[ia])
                        ia += 1
                    else:
                        out.append(b[ib])
                        ib += 1
                return out

            seq = []
            for r in range(-2, NT):
                ia = att_items(r) if r >= 0 else []
                ii = idx_items(r + 2) if r + 2 < NT else []
                cl = bis_closures(r + 1) if 0 <= r + 1 < NT else []
                im = mskT_items(r + 1) if 0 <= r + 1 < NT else []
                ents = [[it, []] for it in merge(ia, ii)]
                if not ents:
                    ents = [[None, []]]
                ne = len(ents)
                for ci, c in enumerate(cl):
                    ents[min(ne - 1, (ci * ne) // max(1, len(cl)))][1].append(c)
                seq.extend((e[0], e[1]) for e in ents)
                seq.extend((it, []) for it in im)
            run_seq(seq, bg_per_iter=1)

        def phase_C(l):
            A.off = phase_base
            FQ = A.bf16(4 * S).rearrange("p (c n) -> p c n", c=4)
            FK = A.bf16(4 * S).rearrange("p (c n) -> p c n", c=4)
            VC = A.bf16(NT * 4 * 65).rearrange("p (t h d) -> p t h d", t=NT, h=4)
            QCF = [QKR[0][:, 0:384], QKR[0][:, 384:768]]
            KCF = [QKR[1][:, 0:384], QKR[1][:, 384:768]]
            CQN = [A.bf16(256), A.bf16(256)]
            CKN = [A.bf16(128), A.bf16(128)]
            CQT = [A.bf16(256), A.bf16(256)]
            CKT = [A.bf16(128), A.bf16(128)]
            mt = A.f32(16)[:, 0:8]
            negc = A.f32(2)[:, 0:1]
            rt = A.f32(128)
            rtk = [A.f32(32), A.f32(32)]
            SQJ = MIXT_f32
            wv = WIN[:, 0:8 * 416].rearrange("p (c n) -> p c n", c=8)
            WUQ = WIN[:, 8 * 416:8 * 416 + 768].rearrange("p (c n) -> p c n", c=2)
            WUKV = WIN[:, 8 * 416 + 768:8 * 416 + 768 + 512]
            P.dma("pool", wv, w_in_d[l].rearrange("(c p) n -> p c n", p=128)[:, :, 1476:1892], writes=["WIN"])
            P.dma("pool", WUQ, w_uq_d[l].rearrange("(c p) n -> p c n", p=128), writes=["WIN"])
            P.dma("pool", WUKV, w_ukv_d[l], writes=["WIN"])
            P.dma("pool", WOUT[:, 0:2048].rearrange("p (c n) -> p c n", c=2),
                  w_out_d[l, 768:1024, :].rearrange("(c p) n -> p c n", p=128), writes=["WOUT"])
            P.dma("sp", gq[:], gq_d[l].partition_broadcast(128), writes=["gq"])
            P.dma("sp", gkv[:], gkv_d[l].partition_broadcast(128), writes=["gkv"])
            load_ln(ln1g_d, ln1b_d, l)
            memset(VC[:, :, :, 64:65], 1.0, ["VCones"])
            def Pf(t):
                in_proj(t, 416, wv, HSB[t % 2])

            def c1(t):
                p = t % 2
                hs = HSB[p]
                hk = "HSB%d" % p
                o = 80 + 8 * p
                ck = "c1s%d" % p
                act(SQJ[:, 0:256], hs[:, 0:256], AF.Square, [hk], ["SQJ"], accum=sm[:, o:o + 1])
                act(SQJ[:, 256:384], hs[:, 256:384], AF.Square, [hk], ["SQJ2"], accum=sm[:, o + 1:o + 2])
                act(sm[:, o + 2:o + 3], sm[:, o:o + 1], AF.Ln, ["SQJ"], [ck + "l1"], scale=1.0 / 256, bias=1e-6)
                act(sm[:, o + 3:o + 4], sm[:, o + 1:o + 2], AF.Ln, ["SQJ2"], [ck + "l2"], scale=1.0 / 128, bias=1e-6)
                act(sm[:, o + 4:o + 5], sm[:, o + 2:o + 3], AF.Exp, [ck + "l1"], [ck + "r1"], scale=-0.5)
                act(sm[:, o + 5:o + 6], sm[:, o + 3:o + 4], AF.Exp, [ck + "l2"], [ck + "r2"], scale=-0.5)
                stt(CQN[p][:, 0:256], hs[:, 0:256], sm[:, o + 4:o + 5], gq[:], ALU.mult, ALU.mult, [hk, ck + "r1", "gq"], ["CQN%d" % p])
                stt(CKN[p][:, 0:128], hs[:, 256:384], sm[:, o + 5:o + 6], gkv[:], ALU.mult, ALU.mult, [hk, ck + "r2", "gkv"], ["CKN%d" % p])
                rope(hs[:, 384:416].rearrange("p (h t d) -> p h t d", h=1, t=2),
                     rtk[p][:, 0:32].rearrange("p (h t d) -> p h t d", h=1, t=2), 1, 32, t,
                     cos32, sin32, nsin32, [hk], ["rtk%d" % p])

            def c23(t):
                p = t % 2
                qck, kck = "QCF%d" % p, "KCF%d" % p
                b = mbank()
                tr(bankb(b)[:, 0:128], CQN[p][:, 0:128], identb[:], ["CQN%d" % p, "identb"], [("ps", b)], inc=False)
                tr(bankb(b)[:, 128:256], CQN[p][:, 128:256], identb[:], ["CQN%d" % p, "identb"], [("ps", b)], inc=False)
                tr(bankb(b)[:, 256:384], CKN[p][:, 0:128], identb[:], ["CKN%d" % p, "identb"], [("ps", b)], inc=True)
                cp("act", CQT[p][:, 0:256], bankb(b)[:, 0:256], [("ps", b)], ["CQT%d" % p])
                cp("dve", CKT[p][:, 0:128], bankb(b)[:, 256:384], [("ps", b)], ["CKT%d" % p])
                bq = mbank()
                mm(bank(bq)[:, 0:384], CQT[p][:, 0:128], WUQ[:, 0, :], True, False, ["CQT%d" % p, "WIN"], [("ps", bq)], False)
                mm(bank(bq)[:, 0:384], CQT[p][:, 128:256], WUQ[:, 1, :], False, True, ["CQT%d" % p, "WIN"], [("ps", bq)], True)
                bk = mbank()
                mm(bank(bk)[:, 0:512], CKT[p][:, 0:128], WUKV, True, True, ["CKT%d" % p, "WIN"], [("ps", bk)], True)
                cp("act", QCF[p], bank(bq)[:, 0:384], [("ps", bq)], [qck])
                q4 = QCF[p].rearrange("p (h d) -> p h d", h=4)
                qr = q4[:, :, 64:96].rearrange("p h (t d) -> p h t d", t=2)
                cp("dve", rt[:, 0:128].rearrange("p (h d) -> p h d", h=4), q4[:, :, 64:96], [qck], ["rt"])
                rope(rt[:, 0:128].rearrange("p (h t d) -> p h t d", h=4, t=2), qr, 4, 32, t,
                     cos32, sin32, nsin32, ["rt"], [qck])
                kv4 = bank(bk)[:, 0:512].rearrange("p (h d) -> p h d", h=4)
                k4 = KCF[p].rearrange("p (h d) -> p h d", h=4)
                cp("act", k4[:, :, 0:64], kv4[:, :, 0:64], [("ps", bk)], [kck])
                cp("dve", VC[:, t, :, 0:64], kv4[:, :, 64:128], [("ps", bk)], [("VC", t)])
                cp("dve", k4[:, :, 64:96], rtk[p][:, 0:32].unsqueeze(1).to_broadcast([128, 4, 32]), ["rtk%d" % p], [kck])
                act(SQ[:, 0:384], QCF[p], AF.Square, [qck], ["SQ"])
                act(SQ[:, 384:768], KCF[p], AF.Square, [kck], ["SQ"])
                if t == 0:
                    red(mt, SQ[:, 0:768].rearrange("p (h d) -> p h d", h=8), ALU.add, ["SQ"], ["mt"])
                else:
                    red(sm[:, 16:24], SQ[:, 0:768].rearrange("p (h d) -> p h d", h=8), ALU.add, ["SQ"], ["hs"])
                    tt(mt, mt, sm[:, 16:24], ALU.max, ["mt", "hs"], ["mt"])

            def c4(t):
                p = t % 2
                for (src_, dstF, key, fkey, eng) in ((QCF[p], FQ, "QCF%d" % p, "FQKR0", "act"), (KCF[p], FK, "KCF%d" % p, "FQKR1", "dve")):
                    b = mbank()
                    for h in range(4):
                        tr(bank(b)[0:96, h * 128:(h + 1) * 128], src_[:, h * 96:(h + 1) * 96], identf[:],
                           [key, "identf"], [("ps", b)], inc=(h == 3))
                    cp(eng, dstF[0:96, :, tcols(t)], bank(b)[0:96, :].rearrange("p (c n) -> p c n", c=4),
                       [("ps", b)], [(fkey, t)])

            run_stages(Pf, [c1, c23, c4])
            global_bound(mt, 8, slice(0, 4), slice(4, 8), SC96, negc)

            P.barrier()
            RT = arena[:, phase_qkr_off:phase_qkr_off + 2304]

            def epilogue(qb):
                k = qb % 2
                bg.append(lambda: ln_stats(qb, k))
                bg.append(lambda: ln_apply(qb, k))

                def xpose(half):
                    b = mbank()
                    for j in range(4):
                        c = half * 4 + j
                        tr(bank(b)[:, j * 128:(j + 1) * 128], X[:, qb, c * 128:(c + 1) * 128], identf[:],
                           [("X", qb), "identf"], [("ps", b)], inc=(j == 3))
                    cp("act", xT[:, half * 4:half * 4 + 4, tcols(qb)], bank(b).rearrange("p (c n) -> p c n", c=4),
                       [("ps", b)], [("xT", qb)])
                    cp("dve", HSB[half][:, 0:512], bank(b), [("ps", b)], ["HSB%d" % half])

                bg.append(lambda: xpose(0))
                bg.append(lambda: xpose(1))
                def router_a():
                    b = mbank()
                    for c in range(8):
                        mm(bank(b)[:, 0:16], HSB[c // 4][:, (c % 4) * 128:(c % 4 + 1) * 128], wr[:, c, :], c == 0, c == 7,
                           ["HSB0", "HSB1", "wr"], [("ps", b)], inc=(c == 7))
                    act(RT[:, qb * 16:(qb + 1) * 16], bank(b)[:, 0:16], AF.Exp, [("ps", b)], [("r_sc", qb)], scale=-1.0)

                bg.append(router_a)

            seq = []
            for qb in range(NT):
                par = qb % 2
                for h in range(4):
                    for kt0 in range(0, qb + 1, 4):
                        kts = list(range(kt0, min(kt0 + 4, qb + 1)))
                        slots = [{"K": (FK[0:96, h, tcols(kt)], [("FQKR1", kt)]),
                                  "Q": (FQ[0:96, h, tcols(qb)], [("FQKR0", qb)]),
                                  "V": (VC[:, kt, h, :], [("VC", kt), "VCones"]),
                                  "ob": 4 + par, "oreg": h * 65, "start": kt == 0, "stop": kt == qb} for kt in kts]
                        ns = len(kts)
                        masks = []
                        if kts[-1] == qb:
                            masks = [(128 * (ns - 1), 128 * ns, None, causalT[:], ["causalT"])]
                        last = (h == 3 and kts[-1] == qb)
                        seq.append(({"kind": "att", "slots": slots, "scale": SC96, "negc": negc, "masks": masks,
                                     "fin": fin_generic(qb, 4 + par, 4, 2, 768, epilogue) if last else None}, []))
            run_seq(seq, bg_per_iter=2)

            sc = RT[:, 0:256]
            bi = RT[:, 256:512]
            m1 = RT[:, 512:576]
            eq = RT[:, 576:832]
            msk = RT[:, 832:1088]
            m2 = RT[:, 1088:1152]
            gs = RT[:, 1152:1216]
            gm = RT[:, 1216:1232]
            gsel = RT[:, 1232:1296]
            t2 = RT[:, 1296:1552]
            wgt = RT[:, 1552:1808]
            ws = RT[:, 1808:1824]
            rsk = [("r_sc", t) for t in range(NT)]

            def v3(ap, a):
                return ap.rearrange("p (a b) -> p a b", a=a)

            ts(sc, sc, 1.0, None, ALU.add, None, rsk, ["r_s"])
            recip(sc, sc, ["r_s"], ["r_s"])
            tt(v3(bi, 16), v3(sc, 16), rb[:].unsqueeze(1).to_broadcast([128, 16, 16]), ALU.add, ["r_s", "rb"], ["r_bi"])
            red(m1, v3(bi, 64), ALU.max, ["r_bi"], ["r_m1"])
            tt(v3(eq, 64), v3(bi, 64), m1.unsqueeze(2).to_broadcast([128, 64, 4]), ALU.is_equal, ["r_bi", "r_m1"], ["r_eq"])
            stt(msk, eq, NEG, bi, ALU.mult, ALU.add, ["r_eq", "r_bi"], ["r_msk"])
            red(m2, v3(msk, 64), ALU.max, ["r_msk"], ["r_m2"])
            tt(gs, m1, m2, ALU.add, ["r_m1", "r_m2"], ["r_gs"])
            red(gm, v3(gs, 16), ALU.max, ["r_gs"], ["r_gm"])
            tt(v3(gsel, 16), v3(gs, 16), gm.unsqueeze(2).to_broadcast([128, 16, 4]), ALU.is_equal, ["r_gs", "r_gm"], ["r_gsel"])
            tt(v3(t2, 64), v3(bi, 64), m2.unsqueeze(2).to_broadcast([128, 64, 4]), ALU.is_ge, ["r_bi", "r_m2"], ["r_t2"])
            tt(v3(t2, 64), v3(t2, 64), gsel.unsqueeze(2).to_broadcast([128, 64, 4]), ALU.mult, ["r_t2", "r_gsel"], ["r_t2"])
            tt(wgt, sc, t2, ALU.mult, ["r_s", "r_t2"], ["r_w"])
            red(ws, v3(wgt, 16), ALU.add, ["r_w"], ["r_ws"])
            recip(ws, ws, ["r_ws"], ["r_ws"])
            tt(gates[:], v3(wgt, 16), ws.unsqueeze(2).to_broadcast([128, 16, 16]), ALU.mult, ["r_w", "r_ws"],
               [("gates", t) for t in range(NT)])

        def phase_moe(l, last_layer):
            A.off = 0
            NSLOT = 7
            WGU = [A.bf16(8 * 512).rearrange("p (c n) -> p c n", c=8) for _ in range(NSLOT)]
            WD = [A.bf16(2 * 1024).rearrange("p (c n) -> p c n", c=2) for _ in range(NSLOT)]
            SB = [A.bf16(256) for _ in range(2)]
            TB = [A.bf16(256) for _ in range(2)]
            TTB = [A.bf16(256) for _ in range(2)]
            load_ln(ln2g_d, ln2b_d, l)
            def load_expert(e):
                s = e % NSLOT
                P.dma("pool", WGU[s][:, :, 0:256], w_gate_d[l, e].rearrange("(c p) f -> p c f", p=128), writes=[("WGU", s)])
                P.dma("pool", WGU[s][:, :, 256:512], w_up_d[l, e].rearrange("(c p) f -> p c f", p=128), writes=[("WGU", s)])
                P.dma("pool", WD[s], w_down_d[l, e].rearrange("(c p) d -> p c d", p=128), writes=[("WD", s)])

            for e in range(NSLOT):
                load_expert(e)
            items = [(G, t, e) for G in range(4) for t in range(NT) for e in range(4 * G, 4 * G + 4)]
            sd = {}
            cnt = [0]

            def s1(it):
                G, t, e = it
                s = e % NSLOT
                b = rbank()
                sd[it] = {"b": b, "i": cnt[0] % 2}
                cnt[0] += 1
                for c in range(8):
                    mm(bank(b), xT[:, c, tcols(t)], WGU[s][:, c, :], c == 0, c == 7,
                       [("xT", t), ("WGU", s)], [("ps", b)], inc=(c == 7))

            def s2(it):
                G, t, e = it
                d = sd[it]
                b, i = d["b"], d["i"]
                act(SB[i][:, 0:256], bank(b)[:, 0:256], AF.Silu, [("ps", b)], [("SB", i)])
                stt(TB[i][:, 0:256], bank(b)[:, 256:512], gates[:, t, e:e + 1], SB[i][:, 0:256], ALU.mult, ALU.mult,
                    [("ps", b), ("gates", t), ("SB", i)], [("TB", i)])
                b2 = rbank()
                d["b2"] = b2
                for fc in range(2):
                    tr(bankb(b2)[:, fc * 128:(fc + 1) * 128], TB[i][:, fc * 128:(fc + 1) * 128], identb[:],
                       [("TB", i), "identb"], [("ps", b2)], inc=(fc == 1))

            def s3(it):
                G, t, e = it
                d = sd[it]
                b2, i = d["b2"], d["i"]
                s = e % NSLOT
                cp("act", TTB[i][:, 0:256], bankb(b2)[:, 0:256], [("ps", b2)], [("TTB", i)])
                first = (e % 4 == 0)
                lastx = (e % 4 == 3)
                for half in range(2):
                    yb = 4 + 2 * (t % 2) + half
                    for fc in range(2):
                        mm(bank(yb), TTB[i][:, fc * 128:(fc + 1) * 128], WD[s][:, fc, half * 512:(half + 1) * 512],
                           first and fc == 0, lastx and fc == 1, [("TTB", i), ("WD", s)], [("ps", yb)],
                           inc=(fc == 1))
                if t == NT - 1 and e + NSLOT < 16:
                    load_expert(e + NSLOT)
                if lastx:
                    for half in range(2):
                        yb = 4 + 2 * (t % 2) + half
                        xs = X[:, t, half * 512:(half + 1) * 512]
                        if G == 0:
                            stt(xs, xs, ALPHA, bank(yb), ALU.mult, ALU.add, [("X", t), ("ps", yb)], [("X", t)])
                        else:
                            tt(xs, xs, bank(yb), ALU.add, [("X", t), ("ps", yb)], [("X", t)])
                    if G == 3:
                        k = t % 2
                        o = 32 + 16 * k
                        ks = "ln%d" % k

                        def st_a(t=t, o=o, ks=ks):
                            P.op("dve", lambda e, o_=sm[:, o:o + 6], i=X[:, t, 0:512]: e.bn_stats(out=o_, in_=i),
                                 reads=[("X", t)], writes=[ks + "a"])

                        def st_b(t=t, o=o, ks=ks):
                            P.op("dve", lambda e, o_=sm[:, o + 6:o + 12], i=X[:, t, 512:1024]: e.bn_stats(out=o_, in_=i),
                                 reads=[("X", t)], writes=[ks + "b"])
                            P.op("dve", lambda e, o_=sm[:, o + 12:o + 14], i=sm[:, o:o + 12]: e.bn_aggr(out=o_, in_=i),
                                 reads=[ks + "a", ks + "b"], writes=[ks + "mv"])
                            act(sm[:, o + 14:o + 15], sm[:, o + 13:o + 14], AF.Ln, [ks + "mv"], [ks + "lv"], bias=1e-5)
                            act(sm[:, o + 15:o + 16], sm[:, o + 14:o + 15], AF.Exp, [ks + "lv"], [ks + "rs"], scale=-0.5)

                        bg.append(st_a)
                        bg.append(st_b)
                        for half in range(2):
                            hsl = slice(half * 512, (half + 1) * 512)

                            def ap1(t=t, o=o, ks=ks, hsl=hsl):
                                xs = X[:, t, hsl]
                                ts(xs, xs, sm[:, o + 12:o + 13], sm[:, o + 15:o + 16], ALU.subtract, ALU.mult,
                                   [("X", t), ks + "mv", ks + "rs"], [("X", t)])

                            def ap2(t=t, hsl=hsl):
                                xs = X[:, t, hsl]
                                tt(xs, xs, lnp[:, 0, hsl], ALU.mult, [("X", t), "lnp"], [("X", t)])

                            def ap3(t=t, hsl=hsl, half=half):
                                xs = X[:, t, hsl]
                                tt(xs, xs, lnp[:, 1, hsl], ALU.add, [("X", t), "lnp"], [("X", t)])
                                if last_layer and half == 1:
                                    P.dma("sp", tm(out_d)[:, t, :], X[:, t, :], reads=[("X", t)], final=True)
                            bg.append(ap1)
                            bg.append(ap2)
                            bg.append(ap3)

            pipeline(items, s1, s2, s3, bg_per_iter=2)

        def run_layers():
            for l in range(nlayers):
                for t in range(NT):
                    build_xT(t)
                if stop_after == "xT":
                    return False
                phase_A(l)
                P.barrier()
                if stop_after in ("A", "A1", "A2"):
                    return False
                phase_B(l)
                P.barrier()
                if stop_after == "B":
                    return False
                phase_C(l)
                P.barrier()
                if stop_after == "C":
                    return False
                phase_moe(l, l == nlayers - 1)
                P.barrier()
            return True

        if not run_layers():
            for t in range(NT):
                P.dma("sp", tm(out_d)[:, t, :], X[:, t, :], reads=[("X", t)], final=True)

        P.emit(nc, sems)
    return nc


_CACHE = {}


def kernel(**inputs):
    consts = _consts()
    if "nc" not in _CACHE:
        _CACHE["nc"] = build_program()
    nc = _CACHE["nc"]
    x = np.ascontiguousarray(np.asarray(inputs["x"], dtype=np.float32))
    shared = {}
    for k, v in inputs.items():
        if k == "x":
            continue
        shared[k] = np.ascontiguousarray(np.asarray(v, dtype=np.float32))
    shared.update(consts)
    in_maps = []
    for c in range(NCORES):
        m = dict(shared)
        m["x"] = x[c]
        in_maps.append(m)
    res = run_bass_kernel_spmd(nc, in_maps, core_ids=list(range(NCORES)))
    out = np.stack([np.asarray(r["out"], dtype=np.float32) for r in res.results], axis=0)
    return out
```

```python
import contextlib
import numpy as np
import concourse.bass as bass
import concourse.mybir as mybir
from concourse.bass_utils import run_bass_kernel_spmd

F32 = mybir.dt.float32
BF16 = mybir.dt.bfloat16
AF = mybir.ActivationFunctionType
ALU = mybir.AluOpType
AX = mybir.AxisListType

S = 2048
D = 1024
NT = 16
NCORES = 8
DEPTH = 2
ALPHA = float((2 * DEPTH) ** 0.25)
IDXW = float((4 * 64) ** -0.5)
SC64 = float(64 ** -0.5)
SC96 = float(96 ** -0.5)
TOPK = 256
NBIS = 14
NEG = -1.0e30
NDMA_SEMS = 8
ACC_ENG = "dve"
LNP_ENG = "dve"


class Prog:
    ENGS = ("pe", "act", "dve", "pool", "sp")

    def __init__(self):
        self.ops = {e: [] for e in self.ENGS}
        self.cnt = {e: 0 for e in self.ENGS}
        self.last_w = {}
        self.readers = {}
        self.waited = {e: {} for e in self.ENGS}
        self.dma_val = {}
        self.dma_rr = {"sp": 0, "pool": 0}
        self.final_tokens = []

    def _deps(self, eng, reads, writes):
        deps = {}

        def add(tok, raw):
            src, val = tok
            if src == eng and eng == "pe":
                return
            if deps.get(src, 0) < val:
                deps[src] = val

        for k in reads:
            if k in self.last_w:
                add(self.last_w[k], True)
            if isinstance(k, tuple) and k[0] == "ps":
                for r in self.readers.get(k, ()):
                    if r[0] != eng:
                        add(r, False)
        for k in writes:
            if k in self.last_w:
                add(self.last_w[k], False)
            for r in self.readers.get(k, ()):
                add(r, False)
        waits = []
        for src, val in deps.items():
            if self.waited[eng].get(src, 0) >= val:
                continue
            self.waited[eng][src] = val
            waits.append((src, val))
        return waits

    def _record(self, tok, reads, writes):
        for k in writes:
            self.last_w[k] = tok
            self.readers[k] = []
        for k in reads:
            self.readers.setdefault(k, []).append(tok)

    def op(self, eng, fn, reads=(), writes=(), inc=True):
        waits = self._deps(eng, reads, writes)
        if inc:
            self.cnt[eng] += 1
            idx = self.cnt[eng]
        else:
            idx = self.cnt[eng] + 1
        tok = (eng, idx)
        self._record(tok, reads, writes)
        self.ops[eng].append((waits, fn, ("eng", eng) if inc else None))
        return tok

    def dma(self, q, out_ap, in_ap, reads=(), writes=(), final=False):
        i = self.dma_rr[q]
        self.dma_rr[q] = (i + 1) % NDMA_SEMS
        src = ("dma", q, i)
        prev = self.dma_val.get(src, 0)
        waits = self._deps(q, reads, writes)
        if prev and self.waited[q].get(src, 0) < prev:
            self.waited[q][src] = prev
            waits.append((src, prev))
        val = prev + 16
        self.dma_val[src] = val
        tok = (src, val)
        self._record(tok, reads, writes)

        def fn(e, out_ap=out_ap, in_ap=in_ap):
            return e.dma_start(out=out_ap, in_=in_ap)

        self.ops[q].append((waits, fn, ("dma", src)))
        if final:
            self.final_tokens.append(tok)
        return tok

    def barrier(self):
        snap = [(e, self.cnt[e]) for e in self.ENGS if self.cnt[e] > 0]
        snap += [(src, v) for src, v in self.dma_val.items()]
        for e in self.ENGS:
            waits = []
            for src, val in snap:
                if src == e:
                    continue
                if self.waited[e].get(src, 0) >= val:
                    continue
                self.waited[e][src] = val
                waits.append((src, val))
            if waits:
                self.ops[e].append((waits, None, None))

    def emit(self, nc, sems):
        fin = list(self.final_tokens)

        def replay(eng, e):
            for waits, fn, inc in self.ops[eng]:
                for src, val in waits:
                    e.wait_ge(sems[src], val)
                if fn is None:
                    continue
                ins = fn(e)
                if inc is not None:
                    if inc[0] == "eng":
                        ins.then_inc(sems[inc[1]], 1)
                    else:
                        ins.then_inc(sems[inc[1]], 16)
            if eng == "sp":
                for src, val in fin:
                    e.wait_ge(sems[src], val)

        with nc.Block() as block:
            @block.tensor
            def _(e):
                replay("pe", e)

            @block.scalar
            def _(e):
                replay("act", e)

            @block.vector
            def _(e):
                replay("dve", e)

            @block.gpsimd
            def _(e):
                replay("pool", e)

            @block.sync
            def _(e):
                replay("sp", e)


def _consts():
    pos = np.arange(S, dtype=np.float64)
    c = {}
    for dim, nm in ((64, "64"), (32, "32")):
        inv = 1.0 / (10000.0 ** (np.arange(0, dim, 2, dtype=np.float64) / dim))
        inv = inv.astype(np.float32).astype(np.float64)
        ang = (pos.astype(np.float32)[:, None] * inv.astype(np.float32)[None, :]).astype(np.float32)
        c["cos" + nm] = np.cos(ang.astype(np.float64)).astype(np.float32)
        c["sin" + nm] = np.sin(ang.astype(np.float64)).astype(np.float32)
    c["ident"] = np.eye(128, dtype=np.float32)
    fv = np.concatenate([2.0 ** -(np.arange(16) + 1.0), 2.0 ** -np.arange(16).astype(np.float64)])
    c["fvec"] = np.tile(fv.astype(np.float32)[None, :], (128, 1))
    qi = np.arange(128)[:, None]
    kj = np.arange(256)[None, :]
    diff = qi + 128 - kj
    kk = np.arange(128)[None, :]
    c["negmask"] = np.where(kk <= qi, 0.0, NEG).astype(np.float32)
    c["causalT"] = (qi <= kk).astype(np.float32)
    c["maskPC"] = np.concatenate([(qi > kk).astype(np.float32), c["causalT"]], axis=1)
    return c


def build_program(nlayers=DEPTH, debug_mix=False, stop_after=None):
    nc = bass.Bass("TRN2", target_bir_lowering=False)

    def din(name, shape):
        return nc.dram_tensor(name, list(shape), F32, kind="ExternalInput").ap()

    x_d = din("x", [S, D])
    w_in_d = din("w_in", [DEPTH, D, 1892])
    sinks_d = din("attn_sinks", [DEPTH, 8])
    gq_d = din("c_q_norm_g", [DEPTH, 256])
    gkv_d = din("c_kv_norm_g", [DEPTH, 128])
    w_uq_d = din("w_uq", [DEPTH, 256, 384])
    w_ukv_d = din("w_ukv", [DEPTH, 128, 512])
    w_out_d = din("w_out", [DEPTH, D, D])
    ln1g_d = din("ln1_g", [DEPTH, D])
    ln1b_d = din("ln1_b", [DEPTH, D])
    w_router_d = din("w_router", [D, 16])
    rbias_d = din("router_bias", [16])
    w_gate_d = din("w_gate", [DEPTH, 16, D, 256])
    w_up_d = din("w_up", [DEPTH, 16, D, 256])
    w_down_d = din("w_down", [DEPTH, 16, 256, D])
    ln2g_d = din("ln2_g", [DEPTH, D])
    ln2b_d = din("ln2_b", [DEPTH, D])
    cos64_d = din("cos64", [S, 32])
    sin64_d = din("sin64", [S, 32])
    cos32_d = din("cos32", [S, 16])
    sin32_d = din("sin32", [S, 16])
    ident_d = din("ident", [128, 128])
    fvec_d = din("fvec", [128, 32])
    negmask_d = din("negmask", [128, 128])
    causalT_d = din("causalT", [128, 128])
    maskPC_d = din("maskPC", [128, 256])
    out_d = nc.dram_tensor("out", [S, D], F32, kind="ExternalOutput").ap()
    dbg_d = None
    if debug_mix:
        dbg_d = nc.dram_tensor("dbg", [S, D], F32, kind="ExternalOutput").ap()

    P = Prog()
    st = contextlib.ExitStack()
    with st:
        sems = {}
        for e in Prog.ENGS:
            sems[e] = st.enter_context(nc.semaphore("s_" + e))
        for q in ("sp", "pool"):
            for i in range(NDMA_SEMS):
                sems[("dma", q, i)] = st.enter_context(nc.semaphore(f"d_{q}{i}"))

        def T(name, shape, dt):
            return st.enter_context(nc.sbuf_tensor("sb_" + name, list(shape), dt))

        X = T("X", [128, NT, D], F32)
        xT = T("xT", [128, 8, S], BF16)
        cos64 = T("cos64", [128, NT, 32], F32)
        sin64 = T("sin64", [128, NT, 32], F32)
        nsin64 = T("nsin64", [128, NT, 32], F32)
        cos32 = T("cos32", [128, NT, 16], F32)
        sin32 = T("sin32", [128, NT, 16], F32)
        nsin32 = T("nsin32", [128, NT, 16], F32)
        identf = T("identf", [128, 128], F32)
        fvec = T("fvec", [128, 32], F32)
        identb = T("identb", [128, 128], BF16)
        negmask = T("negmask", [128, 128], F32)
        causalT = T("causalT", [128, 128], BF16)
        maskPC = T("maskPC", [128, 256], BF16)
        ones1 = T("ones1", [1, 128], F32)
        lnp = T("lnp", [128, 2, D], F32)
        gates = T("gates", [128, NT, 16], F32)
        widx = T("widx", [128, NT, 4], F32)
        wr = T("wr", [128, 8, 16], F32)
        rb = T("rb", [128, 16], F32)
        sinks = T("sinks", [128, 8], F32)
        gq = T("gq", [128, 256], F32)
        gkv = T("gkv", [128, 128], F32)
        sm = T("sm", [128, 256], F32)
        ARW = 22400
        arena = T("arena", [128, ARW], F32)
        psum = st.enter_context(nc.psum_tensor("psum", [128, 4096], F32))

        def bank(i):
            return psum[:, 512 * i:512 * (i + 1)]

        def bankb(i):
            return psum[:, 512 * i:512 * (i + 1)].bitcast(BF16)

        class Arena:
            def __init__(self):
                self.off = 0

            def f32(self, n):
                o = self.off
                self.off += n
                assert self.off <= ARW, self.off
                return arena[:, o:o + n]

            def bf16(self, n):
                w = (n + 1) // 2
                o = self.off
                self.off += w
                assert self.off <= ARW, self.off
                return arena[:, o:o + w].bitcast(BF16)

        A = Arena()
        WIN = A.bf16(8 * 768)
        WOUT = A.bf16(4 * 1024)
        HSB = [A.f32(768), A.f32(768)]
        phase_qkr_off = A.off
        QKR = [A.f32(768), A.f32(768)]
        SQ = A.f32(768)
        PB = [A.bf16(512), A.bf16(512)]
        PTB = [A.bf16(512), A.bf16(512)]
        ROPET = A.f32(640)
        mixt_off = A.off
        MIXT = A.bf16(512)
        MIXTT = A.bf16(512)
        MIXT_f32 = arena[:, mixt_off:mixt_off + 512]
        phase_base = A.off

        rot = [0]
        rot4 = [0]
        inpipe = [False]

        def rbank():
            i = rot[0]
            rot[0] = (i + 1) % 3
            return i

        def mbank():
            if inpipe[0]:
                return 3
            i = rot4[0]
            rot4[0] = (i + 1) % 4
            return i

        def mm(out, lhsT, rhs, start, stop, reads, writes, inc):
            P.op("pe", lambda e, o=out, l=lhsT, r=rhs, s0=start, s1=stop: e.matmul(o, lhsT=l, rhs=r, start=s0, stop=s1),
                 reads=reads, writes=writes, inc=inc)

        def tr(out, in_, ident, reads, writes, inc=True):
            P.op("pe", lambda e, o=out, i=in_, d=ident: e.transpose(out=o, in_=i, identity=d),
                 reads=reads, writes=writes, inc=inc)

        def act(out, in_, func, reads, writes, bias=None, scale=None, accum=None):
            kw = {}
            if bias is not None:
                kw["bias"] = bias
            if scale is not None:
                kw["scale"] = scale
            if accum is not None:
                kw["accum_out"] = accum
            P.op("act", lambda e, o=out, i=in_, f=func, kw=kw: e.activation(out=o, in_=i, func=f, **kw),
                 reads=reads, writes=writes)

        def tt(out, in0, in1, op, reads, writes, eng="dve"):
            P.op(eng, lambda e, o=out, a=in0, b=in1, p=op: e.tensor_tensor(out=o, in0=a, in1=b, op=p),
                 reads=reads, writes=writes)

        def ts(out, in0, s1, s2, op0, op1, reads, writes, accum=None, eng="dve"):
            def fn(e, o=out, a=in0, s1=s1, s2=s2, op0=op0, op1=op1, accum=accum):
                kw = {}
                if op1 is not None:
                    kw["op1"] = op1
                if accum is not None:
                    kw["accum_out"] = accum
                return e.tensor_scalar(out=o, in0=a, scalar1=s1, scalar2=s2, op0=op0, **kw)
            P.op(eng, fn, reads=reads, writes=writes)

        def stt(out, in0, scalar, in1, op0, op1, reads, writes, eng="dve"):
            P.op(eng, lambda e, o=out, a=in0, s=scalar, b=in1, p0=op0, p1=op1:
                 e.scalar_tensor_tensor(out=o, in0=a, scalar=s, in1=b, op0=p0, op1=p1),
                 reads=reads, writes=writes)

        def cp(eng, out, in_, reads, writes):
            if eng == "act":
                act(out, in_, AF.Copy, reads, writes)
            else:
                P.op(eng, lambda e, o=out, i=in_: e.tensor_copy(out=o, in_=i), reads=reads, writes=writes)

        def red(out, in_, op, reads, writes, absv=False):
            def fn(e, o=out, i=in_, p=op, a=absv):
                if a:
                    return e.tensor_reduce(out=o, in_=i, axis=AX.X, op=p, apply_absolute_value=True)
                return e.tensor_reduce(out=o, in_=i, axis=AX.X, op=p)
            P.op("dve", fn, reads=reads, writes=writes)

        def memset(ap, val, writes, eng="dve"):
            P.op(eng, lambda e, a=ap, v=val: e.memset(a, v), writes=writes)

        def recip(out, in_, reads, writes):
            P.op("dve", lambda e, o=out, i=in_: e.reciprocal(out=o, in_=i), reads=reads, writes=writes)

        bg = []

        def bg_run(k):
            for _ in range(k):
                if not bg:
                    return
                bg.pop(0)()

        def pipeline(items, s1, s2, s3, bg_per_iter=0):
            n = len(items)
            inpipe[0] = True
            for i in range(n + 2):
                if i < n:
                    s1(items[i])
                if 0 <= i - 1 < n:
                    s2(items[i - 1])
                if 0 <= i - 2 < n:
                    s3(items[i - 2])
                if bg_per_iter:
                    bg_run(bg_per_iter)
            bg_run(len(bg))
            inpipe[0] = False

        def run_tiles(Pf, Rf):
            Pf(0)
            for t in range(NT):
                if t + 1 < NT:
                    Pf(t + 1)
                Rf(t)

        def run_stages(Pf, stages):
            ns = len(stages)
            Pf(0)
            for i in range(NT + ns - 1):
                if i + 1 < NT:
                    Pf(i + 1)
                for k, st_ in enumerate(stages):
                    if 0 <= i - k < NT:
                        st_(i - k)

        def tcols(t):
            return slice(t * 128, (t + 1) * 128)

        PBs = [PB[0], PB[1], PTB[0]]
        pbrot = [0]

        def st1(it):
            b = rbank()
            it["b"] = b
            k = it["kind"]
            if k == "att":
                it["pi"] = pbrot[0]
                pbrot[0] = (pbrot[0] + 1) % 3
                sl = it["slots"]
                for j, s in enumerate(sl):
                    ka, kk = s["K"]
                    qa, qk = s["Q"]
                    mm(bank(b)[:, j * 128:(j + 1) * 128], ka, qa, True, True, kk + qk, [("ps", b)], inc=(j == len(sl) - 1))
            elif k == "idx":
                mm(bank(b)[:, 0:it["n"]], it["lhsT"], it["rhs"], True, True, it["keys"], [("ps", b)], True)
            elif k == "mskT":
                kts = it["kts"]
                for j, kt in enumerate(kts):
                    tr(bankb(b)[:, j * 128:(j + 1) * 128], it["src"][:, kt * 128:(kt + 1) * 128], identb[:],
                       [it["srckey"], "identb"], [("ps", b)], inc=(j == len(kts) - 1))

        def st2(it):
            b = it["b"]
            k = it["kind"]
            if k == "att":
                pi = it["pi"]
                n = 128 * len(it["slots"])
                act(PBs[pi][:, 0:n], bank(b)[:, 0:n], AF.Exp, [("ps", b), "negc"], [("PB", pi)], bias=it["negc"], scale=it["scale"])
                for (c0, c1, view, m_ap, mk) in it["masks"]:
                    pv = PBs[pi][:, c0:c1]
                    if view is not None:
                        pv = pv.rearrange(view[0], **view[1])
                    tt(pv, pv, m_ap, ALU.mult, [("PB", pi)] + mk, [("PB", pi)])
            elif k == "idx":
                ri, n, h, qb, k0 = it["ri"], it["n"], it["h"], it["qb"], it["k0"]
                sco, sk = it["sco"], it["scokey"]
                act(it["rb"][:, 0:n], bank(b)[:, 0:n], AF.Relu, [("ps", b)], ["RB%d" % ri])
                if h == 0:
                    ts(sco[:, k0:k0 + n], it["rb"][:, 0:n], widx[:, qb, 0:1], None, ALU.mult, None,
                       ["RB%d" % ri, ("widx", qb)], [sk], eng=ACC_ENG)
                elif ACC_ENG == "dve":
                    stt(sco[:, k0:k0 + n], it["rb"][:, 0:n], widx[:, qb, h:h + 1], sco[:, k0:k0 + n],
                        ALU.mult, ALU.add, ["RB%d" % ri, ("widx", qb), sk], [sk])
                else:
                    ts(it["rb"][:, 0:n], it["rb"][:, 0:n], widx[:, qb, h:h + 1], None, ALU.mult, None,
                       ["RB%d" % ri, ("widx", qb)], ["RB%d" % ri], eng=ACC_ENG)
                    tt(sco[:, k0:k0 + n], sco[:, k0:k0 + n], it["rb"][:, 0:n], ALU.add, ["RB%d" % ri, sk], [sk], eng=ACC_ENG)
            elif k == "mskT":
                kts = it["kts"]
                nj = len(kts)
                cp("act", it["dst"][:, kts[0]:kts[0] + nj, :],
                   bankb(b)[:, 0:nj * 128].rearrange("p (c n) -> p c n", c=nj), [("ps", b)], [it["dstkey"]])

        def st3(it):
            if it["kind"] != "att":
                return
            pi = it["pi"]
            sl = it["slots"]
            for j, s in enumerate(sl):
                va, vk = s["V"]
                ob = s["ob"]
                mm(bank(ob)[:, s["oreg"]:s["oreg"] + 65], PBs[pi][:, j * 128:(j + 1) * 128], va, s["start"], s["stop"],
                   [("PB", pi)] + vk, [("ps", ob)], inc=(j == len(sl) - 1 or sl[j + 1]["ob"] != ob))
            if it.get("fin") is not None:
                it["fin"]()

        def run_seq(seq, bg_per_iter=0):
            n = len(seq)
            inpipe[0] = True
            for i in range(n + 2):
                if i < n and seq[i][0] is not None:
                    st1(seq[i][0])
                if 0 <= i - 1 < n and seq[i - 1][0] is not None:
                    st2(seq[i - 1][0])
                if 0 <= i - 2 < n and seq[i - 2][0] is not None:
                    st3(seq[i - 2][0])
                if i < n:
                    for c in seq[i][1]:
                        c()
                if bg_per_iter:
                    bg_run(bg_per_iter)
            bg_run(len(bg))
            inpipe[0] = False

        def fin_generic(qb, ob, nheads, nchunks_w, mix_c0, epilogue):
            def fn():
                o3 = bank(ob)[:, 0:nheads * 65].rearrange("p (h d) -> p h d", h=nheads)
                rc = sm[:, 72:72 + nheads]
                recip(rc, o3[:, :, 64], [("ps", ob)], ["rc"])
                tt(MIXT[:, 0:nheads * 64].rearrange("p (h d) -> p h d", h=nheads), o3[:, :, 0:64],
                   rc.unsqueeze(2).to_broadcast([128, nheads, 64]), ALU.mult, [("ps", ob), "rc"], ["MIXT"])
                dbg_store(qb, mix_c0, nheads * 64)
                out_proj_partial(qb, nchunks_w, False, after=epilogue)
            return fn

        def tm(ap_d):
            return ap_d.rearrange("(t p) d -> p t d", p=128)

        P.dma("sp", cos64[:], tm(cos64_d), writes=["cos64"])
        P.dma("sp", sin64[:], tm(sin64_d), writes=["sin64"])
        P.dma("sp", cos32[:], tm(cos32_d), writes=["cos32"])
        P.dma("sp", sin32[:], tm(sin32_d), writes=["sin32"])
        P.dma("sp", identf[:], ident_d, writes=["identf"])
        P.dma("sp", fvec[:], fvec_d, writes=["fvec"])
        P.dma("pool", identb[:], ident_d, writes=["identb"])
        P.dma("pool", causalT[:], causalT_d, writes=["causalT"])
        P.dma("pool", maskPC[:], maskPC_d, writes=["maskPC"])
        P.dma("sp", negmask[:], negmask_d, writes=["negmask"])
        P.dma("sp", wr[:], w_router_d.rearrange("(c p) n -> p c n", p=128), writes=["wr"])
        P.dma("sp", rb[:], rbias_d.partition_broadcast(128), writes=["rb"])
        xv = tm(x_d)
        for t4 in range(4):
            P.dma("sp", X[:, 4 * t4:4 * t4 + 4, :], xv[:, 4 * t4:4 * t4 + 4, :],
                  writes=[("X", t) for t in range(4 * t4, 4 * t4 + 4)])
        ts(nsin64[:], sin64[:], -1.0, None, ALU.mult, None, ["sin64"], ["nsin64"])
        ts(nsin32[:], sin32[:], -1.0, None, ALU.mult, None, ["sin32"], ["nsin32"])
        memset(ones1[:], 1.0, ["ones1"])

        def build_xT(t):
            for half in range(2):
                b = mbank()
                for j in range(4):
                    c = half * 4 + j
                    tr(bank(b)[:, j * 128:(j + 1) * 128], X[:, t, c * 128:(c + 1) * 128], identf[:],
                       [("X", t), "identf"], [("ps", b)], inc=(j == 3))
                cp("act" if half == 0 else "dve",
                   xT[:, half * 4:half * 4 + 4, tcols(t)],
                   bank(b).rearrange("p (c n) -> p c n", c=4),
                   [("ps", b)], [("xT", t)])

        def in_proj(t, ncols, wview, hs):
            n0 = 0
            while n0 < ncols:
                n1 = min(ncols, n0 + 512)
                b = mbank()
                for c in range(8):
                    mm(bank(b)[:, 0:n1 - n0], xT[:, c, tcols(t)], wview[:, c, n0:n1], c == 0, c == 7,
                       [("xT", t), "WIN"], [("ps", b)], inc=(c == 7))
                cp("act", hs[:, n0:n1], bank(b)[:, 0:n1 - n0], [("ps", b)], ["HSB%d" % (t % 2)])
                n0 = n1

        def rope(src, dst, nh, hd, t, cosT, sinT, nsinT, rk, wk, tmp=None):
            h2 = hd // 2
            cb = cosT[:, t, :].unsqueeze(1).unsqueeze(1).to_broadcast([128, nh, 2, h2])
            sb = sinT[:, t, :].unsqueeze(1).to_broadcast([128, nh, h2])
            nb = nsinT[:, t, :].unsqueeze(1).to_broadcast([128, nh, h2])
            tv = ROPET[:, 0:nh * hd].rearrange("p (h t d) -> p h t d", h=nh, t=2)
            rk = rk + ["cos64", "sin64", "nsin64", "cos32", "sin32", "nsin32"]
            tt(dst, src, cb, ALU.mult, rk, wk)
            tt(tv[:, :, 0, :], src[:, :, 1, :], nb, ALU.mult, rk, ["ropetmp"])
            tt(tv[:, :, 1, :], src[:, :, 0, :], sb, ALU.mult, rk, ["ropetmp"])
            tt(dst, dst, tv, ALU.add, wk + ["ropetmp"], wk)

        def global_bound(mt, nh, qsl, ksl, scale, negc):
            b = mbank()
            tr(bank(b)[0:nh, 0:128], mt, identf[:], ["mt", "identf"], [("ps", b)])
            red(sm[0:nh, 0:1], bank(b)[0:nh, 0:128], ALU.max, [("ps", b)], ["gb1"])
            b2 = mbank()
            tr(bank(b2)[0:1, 0:nh], sm[0:nh, 0:1], identf[0:nh, 0:nh], ["gb1", "identf"], [("ps", b2)])
            red(sm[0:1, 1:2], bank(b2)[0:1, qsl], ALU.max, [("ps", b2)], ["gb2"])
            red(sm[0:1, 2:3], bank(b2)[0:1, ksl], ALU.max, [("ps", b2)], ["gb3"])
            tt(sm[0:1, 3:4], sm[0:1, 1:2], sm[0:1, 2:3], ALU.mult, ["gb2", "gb3"], ["gb4"])
            act(sm[0:1, 4:5], sm[0:1, 3:4], AF.Ln, ["gb4"], ["gb5"])
            act(sm[0:1, 5:6], sm[0:1, 4:5], AF.Exp, ["gb5"], ["gb6"], scale=0.5)
            ts(sm[0:1, 6:7], sm[0:1, 5:6], -scale, None, ALU.mult, None, ["gb6"], ["gb7"])
            b3 = mbank()
            mm(bank(b3)[:, 0:1], ones1[0:1, :], sm[0:1, 6:7], True, True, ["gb7", "ones1"], [("ps", b3)], True)
            cp("dve", negc, bank(b3)[:, 0:1], [("ps", b3)], ["negc"])

        def head_sumsq(src, ncols, nh, mt, first, rk):
            act(SQ[:, 0:ncols], src, AF.Square, rk, ["SQ"])
            hd = ncols // nh
            if first:
                red(mt, SQ[:, 0:ncols].rearrange("p (h d) -> p h d", h=nh), ALU.add, ["SQ"], ["mt"])
            else:
                red(sm[:, 16:16 + nh], SQ[:, 0:ncols].rearrange("p (h d) -> p h d", h=nh), ALU.add, ["SQ"], ["hs"])
                tt(mt, mt, sm[:, 16:16 + nh], ALU.max, ["mt", "hs"], ["mt"])

        def out_proj_T(qb, nchunks):
            b = mbank()
            for c in range(nchunks):
                tr(bankb(b)[:, c * 128:(c + 1) * 128], MIXT[:, c * 128:(c + 1) * 128], identb[:],
                   ["MIXT", "identb"], [("ps", b)], inc=(c == nchunks - 1))
            cp("act", MIXTT[:, 0:nchunks * 128], bankb(b)[:, 0:nchunks * 128], [("ps", b)], ["MIXTT"])

        def out_proj_M(qb, nchunks, first):
            wv = WOUT.rearrange("p (c n) -> p c n", n=1024)
            for half in range(2):
                yb = 6 + half
                for c in range(nchunks):
                    mm(bank(yb), MIXTT[:, c * 128:(c + 1) * 128], wv[:, c, half * 512:(half + 1) * 512],
                       c == 0, c == nchunks - 1, ["MIXTT", "WOUT"], [("ps", yb)], inc=(c == nchunks - 1))
                xs = X[:, qb, half * 512:(half + 1) * 512]
                if first:
                    stt(xs, xs, ALPHA, bank(yb), ALU.mult, ALU.add, [("X", qb), ("ps", yb)], [("X", qb)])
                else:
                    tt(xs, xs, bank(yb), ALU.add, [("X", qb), ("ps", yb)], [("X", qb)])

        def out_proj_partial(qb, nchunks, first, after=None):
            bg.append(lambda: out_proj_T(qb, nchunks))

            def part2():
                out_proj_M(qb, nchunks, first)
                if after is not None:
                    after(qb)
            bg.append(part2)

        def dbg_store(qb, c0, ncols):
            if dbg_d is None:
                return
            cp("dve", ROPET[:, 0:ncols], MIXT[:, 0:ncols], ["MIXT"], ["ropetmp"])
            P.dma("sp", tm(dbg_d)[:, qb, c0:c0 + ncols], ROPET[:, 0:ncols], reads=["ropetmp"], final=True)

        def ln_stats(t, k):
            o = 32 + 16 * k
            ks = "ln%d" % k
            P.op("dve", lambda e, o_=sm[:, o:o + 6], i=X[:, t, 0:512]: e.bn_stats(out=o_, in_=i), reads=[("X", t)], writes=[ks + "a"])
            P.op("dve", lambda e, o_=sm[:, o + 6:o + 12], i=X[:, t, 512:1024]: e.bn_stats(out=o_, in_=i), reads=[("X", t)], writes=[ks + "b"])
            P.op("dve", lambda e, o_=sm[:, o + 12:o + 14], i=sm[:, o:o + 12]: e.bn_aggr(out=o_, in_=i), reads=[ks + "a", ks + "b"], writes=[ks + "mv"])
            act(sm[:, o + 14:o + 15], sm[:, o + 13:o + 14], AF.Ln, [ks + "mv"], [ks + "lv"], bias=1e-5)
            act(sm[:, o + 15:o + 16], sm[:, o + 14:o + 15], AF.Exp, [ks + "lv"], [ks + "rs"], scale=-0.5)

        def ln_apply(t, k):
            o = 32 + 16 * k
            ks = "ln%d" % k
            xs = X[:, t, :]
            ts(xs, xs, sm[:, o + 12:o + 13], sm[:, o + 15:o + 16], ALU.subtract, ALU.mult, [("X", t), ks + "mv", ks + "rs"], [("X", t)])
            tt(xs, xs, lnp[:, 0, :], ALU.mult, [("X", t), "lnp"], [("X", t)], eng=LNP_ENG)
            tt(xs, xs, lnp[:, 1, :], ALU.add, [("X", t), "lnp"], [("X", t)], eng=LNP_ENG)

        def layer_norm(t, k=0):
            ln_stats(t, k)
            ln_apply(t, k)

        def load_ln(g_d, b_d, l):
            P.dma("sp", lnp[:, 0, :], g_d[l].partition_broadcast(128), writes=["lnp"])
            P.dma("sp", lnp[:, 1, :], b_d[l].partition_broadcast(128), writes=["lnp"])

        def phase_A(l):
            A.off = phase_base
            FM = A.bf16(6 * S).rearrange("p (c n) -> p c n", c=6)
            VA = A.bf16(NT * 2 * 65).rearrange("p (t g d) -> p t g d", t=NT, g=2)
            mt = A.f32(16)[:, 0:10]
            negc = A.f32(2)[:, 0:1]
            esink = A.f32(8)
            den = A.f32(8)
            wv = WIN[:, 0:8 * 768].rearrange("p (c n) -> p c n", c=8)
            P.dma("pool", wv, w_in_d[l].rearrange("(c p) n -> p c n", p=128)[:, :, 0:768], writes=["WIN"])
            P.dma("pool", WOUT[:, 0:4096].rearrange("p (c n) -> p c n", c=4),
                  w_out_d[l, 0:512, :].rearrange("(c p) n -> p c n", p=128), writes=["WOUT"])
            P.dma("sp", sinks[:], sinks_d[l].partition_broadcast(128), writes=["sinks"])
            memset(VA[:, :, :, 64:65], 1.0, ["VAones"])
            def Pf(t):
                in_proj(t, 768, wv, HSB[t % 2])

            def Rf(t):
                hs = HSB[t % 2]
                qk = QKR[t % 2]
                hk = "HSB%d" % (t % 2)
                qkk = "QKR%d" % (t % 2)
                head_sumsq(hs[:, 0:640], 640, 10, mt, t == 0, [hk])
                rope(hs[:, 0:512].rearrange("p (h t d) -> p h t d", h=8, t=2),
                     qk[:, 0:512].rearrange("p (h t d) -> p h t d", h=8, t=2),
                     8, 64, t, cos64, sin64, nsin64, [hk], [qkk])
                kdst = qk[:, 512:768].rearrange("p (g r d) -> p g r d", g=2, r=2)
                rope(hs[:, 512:640].rearrange("p (h t d) -> p h t d", h=2, t=2),
                     kdst[:, :, 0, :].rearrange("p g (t d) -> p g t d", t=2),
                     2, 64, t, cos64, sin64, nsin64, [hk], [qkk])
                cp("dve", kdst[:, :, 1, :], kdst[:, :, 0, :], [qkk], [qkk])
                cp("act", VA[:, t, :, 0:64], hs[:, 640:768].rearrange("p (g d) -> p g d", g=2), [hk], [("VA", t)])

            def Rb(t):
                qk = QKR[t % 2]
                qkk = "QKR%d" % (t % 2)
                for part, (c0, nchk) in enumerate(((0, 4), (4, 2))):
                    b = mbank()
                    for j in range(nchk):
                        c = c0 + j
                        tr(bank(b)[:, j * 128:(j + 1) * 128], qk[:, c * 128:(c + 1) * 128], identf[:],
                           [qkk, "identf"], [("ps", b)], inc=(j == nchk - 1))
                    cp("act" if part == 0 else "dve", FM[:, c0:c0 + nchk, tcols(t)],
                       bank(b)[:, 0:nchk * 128].rearrange("p (c n) -> p c n", c=nchk),
                       [("ps", b)], [("FM", t)])

            run_stages(Pf, [Rf, Rb])
            if stop_after == "A1":
                return
            global_bound(mt, 10, slice(0, 8), slice(8, 10), SC64, negc)
            act(esink, sinks[:], AF.Exp, ["sinks", "negc"], ["esink"], bias=negc)
            if stop_after == "A2":
                return

            def finA(qb, g):
                def fn():
                    ob = 4 + g
                    o3 = bank(ob)[:, 0:260].rearrange("p (h d) -> p h d", h=4)
                    tt(den[:, 4 * g:4 * g + 4], o3[:, :, 64], esink[:, 4 * g:4 * g + 4], ALU.add,
                       [("ps", ob), "esink"], ["den"])
                    recip(den[:, 4 * g:4 * g + 4], den[:, 4 * g:4 * g + 4], ["den"], ["den"])
                    tt(MIXT[:, g * 256:(g + 1) * 256].rearrange("p (h d) -> p h d", h=4), o3[:, :, 0:64],
                       den[:, 4 * g:4 * g + 4].unsqueeze(2).to_broadcast([128, 4, 64]), ALU.mult,
                       [("ps", ob), "den"], ["MIXT"])
                    if g == 1:
                        dbg_store(qb, 0, 512)
                        out_proj_partial(qb, 4, True)
                return fn

            seq = []
            for qb in range(NT):
                kts = [kt for kt in (qb - 1, qb) if kt >= 0]
                nk_ = len(kts)
                for g in range(2):
                    for par in range(2):
                        po = 64 * par
                        slots = []
                        for h in (4 * g + par, 4 * g + par + 2):
                            for kt in kts:
                                slots.append({"K": (FM[po:po + 64, 4 + g, tcols(kt)], [("FM", kt)]),
                                              "Q": (FM[po:po + 64, h // 2, tcols(qb)], [("FM", qb)]),
                                              "V": (VA[:, kt, g, :], [("VA", kt), "VAones"]),
                                              "ob": 4 + g, "oreg": (h % 4) * 65,
                                              "start": kt == kts[0], "stop": kt == kts[-1]})
                        if nk_ == 2:
                            masks = [(0, 512, ("p (h n) -> p h n", {"h": 2}),
                                      maskPC[:].unsqueeze(1).to_broadcast([128, 2, 256]), ["maskPC"])]
                        else:
                            masks = [(0, 256, ("p (h n) -> p h n", {"h": 2}),
                                      maskPC[:, 128:256].unsqueeze(1).to_broadcast([128, 2, 128]), ["maskPC"])]
                        seq.append(({"kind": "att", "slots": slots, "scale": SC64, "negc": negc, "masks": masks,
                                     "fin": finA(qb, g) if par == 1 else None}, []))
            run_seq(seq, bg_per_iter=1)

        def phase_B(l):
            A.off = phase_base
            FM = A.bf16(6 * S).rearrange("p (c n) -> p c n", c=6)
            VB = A.bf16(NT * 65 + 1)[:, 0:NT * 65].rearrange("p (t d) -> p t d", t=NT)
            SCO = A.f32(S)
            MSK = A.bf16(S)
            RB = [HSB[0][:, 0:512], HSB[1][:, 0:512]]
            mt = A.f32(16)[:, 0:5]
            negc = A.f32(2)[:, 0:1]
            bis = A.f32(8)
            btab = A.f32(32)
            wv = WIN[:, 0:8 * 708].rearrange("p (c n) -> p c n", c=8)
            P.dma("pool", wv, w_in_d[l].rearrange("(c p) n -> p c n", p=128)[:, :, 768:1476], writes=["WIN"])
            P.dma("pool", WOUT[:, 0:2048].rearrange("p (c n) -> p c n", c=2),
                  w_out_d[l, 512:768, :].rearrange("(c p) n -> p c n", p=128), writes=["WOUT"])
            memset(VB[:, :, 64:65], 1.0, ["VBones"])
            def Pf(t):
                in_proj(t, 708, wv, HSB[t % 2])

            def Rf(t):
                hs = HSB[t % 2]
                qk = QKR[t % 2]
                hk = "HSB%d" % (t % 2)
                qkk = "QKR%d" % (t % 2)
                head_sumsq(hs[:, 0:320], 320, 5, mt, t == 0, [hk])
                for (s0, d0) in ((0, 0), (384, 384)):
                    rope(hs[:, s0:s0 + 320].rearrange("p (h t d) -> p h t d", h=5, t=2),
                         qk[:, d0:d0 + 320].rearrange("p (h t d) -> p h t d", h=5, t=2),
                         5, 64, t, cos64, sin64, nsin64, [hk], [qkk])
                    cp("dve", qk[:, d0 + 320:d0 + 384], qk[:, d0 + 256:d0 + 320], [qkk], [qkk])
                cp("act", VB[:, t, 0:64], hs[:, 320:384], [hk], [("VB", t)])
                ts(widx[:, t, :], hs[:, 704:708], IDXW, None, ALU.mult, None, [hk], [("widx", t)])

            def Rb(t):
                qk = QKR[t % 2]
                qkk = "QKR%d" % (t % 2)
                for part, (c0, nchk) in enumerate(((0, 4), (4, 2))):
                    b = mbank()
                    for j in range(nchk):
                        c = c0 + j
                        tr(bank(b)[:, j * 128:(j + 1) * 128], qk[:, c * 128:(c + 1) * 128], identf[:],
                           [qkk, "identf"], [("ps", b)], inc=(j == nchk - 1))
                    cp("act" if part == 0 else "dve", FM[:, c0:c0 + nchk, tcols(t)],
                       bank(b)[:, 0:nchk * 128].rearrange("p (c n) -> p c n", c=nchk),
                       [("ps", b)], [("FM", t)])

            run_stages(Pf, [Rf, Rb])
            global_bound(mt, 5, slice(0, 4), slice(4, 5), SC64, negc)

            P.barrier()
            SCOs = [SCO, arena[:, 0:S]]
            a_qkr = phase_qkr_off
            MSKT = [arena[:, a_qkr + 1024 * i:a_qkr + 1024 * (i + 1)].bitcast(BF16).rearrange("p (t n) -> p t n", t=NT)
                    for i in range(2)]

            def idx_items(qb):
                nk = (qb + 1) * 128
                par = qb % 2
                out = []
                for k0 in range(0, nk, 512):
                    n = min(512, nk - k0)
                    for h in range(4):
                        po = 64 * (h % 2)
                        ri = (len(out)) % 2
                        out.append({"kind": "idx", "n": n, "h": h, "qb": qb, "k0": k0, "ri": ri, "rb": RB[ri],
                                    "lhsT": FM[po:po + 64, 3 + h // 2, tcols(qb)], "rhs": FM[po:po + 64, 5, k0:k0 + n],
                                    "keys": [("FM", t) for t in range(qb + 1)],
                                    "sco": SCOs[par], "scokey": "SCO%d" % par})
                return out

            def bis_closures(qb):
                nk = (qb + 1) * 128
                par = qb % 2
                sco = SCOs[par]
                sk = "SCO%d" % par
                cl = []

                def init():
                    if qb >= 2:
                        red(bis[:, 0:1], sco[:, 0:nk], ALU.max, [sk], ["bnd"], absv=True)
                    tt(sco[:, qb * 128:nk], sco[:, qb * 128:nk], negmask[:], ALU.add, [sk, "negmask"], [sk])
                    if qb >= 2:
                        ts(bis[:, 2:3], bis[:, 0:1], 2.0, 2.0, ALU.mult, ALU.add, ["bnd"], ["w0"])
                        ts(btab[:], fvec[:], bis[:, 2:3], None, ALU.mult, None, ["w0", "fvec"], ["btab"])
                        memset(bis[:, 3:4], 0.0, ["mid"])
                    else:
                        ts(MSK[:, 0:nk], sco[:, 0:nk], -1.0e29, None, ALU.is_ge, None, [sk], ["MSK"])
                cl.append(init)
                if qb >= 2:
                    def it_fn(k):
                        def fn():
                            ts(MSK[:, 0:nk], sco[:, 0:nk], bis[:, 3:4], None, ALU.is_ge, ALU.add, [sk, "mid"],
                               ["MSK", "cnt"], accum=bis[:, 4:5])
                            if k < NBIS - 1:
                                ts(bis[:, 5:6], bis[:, 4:5], float(TOPK), btab[:, 16 + k + 1:16 + k + 2], ALU.is_ge, ALU.mult,
                                   ["cnt", "btab"], ["stp"])
                                stt(bis[:, 3:4], bis[:, 5:6], btab[:, k + 1:k + 2], bis[:, 3:4], ALU.subtract, ALU.add,
                                    ["stp", "btab", "mid"], ["mid"])
                            else:
                                ts(bis[:, 5:6], bis[:, 4:5], float(TOPK), btab[:, k:k + 1], ALU.is_ge, ALU.mult,
                                   ["cnt", "btab"], ["stp"])
                                stt(bis[:, 1:2], bis[:, 5:6], btab[:, k:k + 1], bis[:, 3:4], ALU.subtract, ALU.add,
                                    ["stp", "btab", "mid"], ["lo"])
                        return fn
                    for k in range(NBIS):
                        cl.append(it_fn(k))
                    cl.append(lambda: ts(MSK[:, 0:nk], sco[:, 0:nk], bis[:, 1:2], None, ALU.is_ge, None, [sk, "lo"], ["MSK"]))
                return cl

            def mskT_items(qb):
                par = qb % 2
                out = []
                for kt0 in range(0, qb + 1, 4):
                    kts = list(range(kt0, min(kt0 + 4, qb + 1)))
                    out.append({"kind": "mskT", "kts": kts, "src": MSK, "srckey": "MSK",
                                "dst": MSKT[par], "dstkey": ("MSKT", par)})
                return out

            def att_items(qb):
                par = qb % 2
                out = []
                for h in range(4):
                    po = 64 * (h % 2)
                    for kt0 in range(0, qb + 1, 4):
                        kts = list(range(kt0, min(kt0 + 4, qb + 1)))
                        slots = [{"K": (FM[po:po + 64, 2, tcols(kt)], [("FM", kt)]),
                                  "Q": (FM[po:po + 64, h // 2, tcols(qb)], [("FM", qb)]),
                                  "V": (VB[:, kt, :], [("VB", kt), "VBones"]),
                                  "ob": 4 + par, "oreg": h * 65, "start": kt == 0, "stop": kt == qb} for kt in kts]
                        ns = len(kts)
                        masks = [(0, 128 * ns, ("p (c n) -> p c n", {"c": ns}), MSKT[par][:, kt0:kt0 + ns, :], [("MSKT", par)])]
                        last = (h == 3 and kts[-1] == qb)
                        out.append({"kind": "att", "slots": slots, "scale": SC64, "negc": negc, "masks": masks,
                                    "fin": fin_generic(qb, 4 + par, 4, 2, 512, None) if last else None})
                return out

            def merge(a, b):
                out = []
                na, nb = len(a), len(b)
                ia = ib = 0
                while ia < na or ib < nb:
                    if ib >= nb or (ia < na and ia * nb <= ib * na):
                        out.append(a[ia])
                        ia += 1
                    else:
                        out.append(b[ib])
                        ib += 1
                return out

            seq = []
            for r in range(-2, NT):
                ia = att_items(r) if r >= 0 else []
                ii = idx_items(r + 2) if r + 2 < NT else []
                cl = bis_closures(r + 1) if 0 <= r + 1 < NT else []
                im = mskT_items(r + 1) if 0 <= r + 1 < NT else []
                ents = [[it, []] for it in merge(ia, ii)]
                if not ents:
                    ents = [[None, []]]
                ne = len(ents)
                for ci, c in enumerate(cl):
                    ents[min(ne - 1, (ci * ne) // max(1, len(cl)))][1].append(c)
                seq.extend((e[0], e[1]) for e in ents)
                seq.extend((it, []) for it in im)
            run_seq(seq, bg_per_iter=1)

        def phase_C(l):
            A.off = phase_base
            FQ = A.bf16(4 * S).rearrange("p (c n) -> p c n", c=4)
            FK = A.bf16(4 * S).rearrange("p (c n) -> p c n", c=4)
            VC = A.bf16(NT * 4 * 65).rearrange("p (t h d) -> p t h d", t=NT, h=4)
            QCF = [QKR[0][:, 0:384], QKR[0][:, 384:768]]
            KCF = [QKR[1][:, 0:384], QKR[1][:, 384:768]]
            CQN = [A.bf16(256), A.bf16(256)]
            CKN = [A.bf16(128), A.bf16(128)]
            CQT = [A.bf16(256), A.bf16(256)]
            CKT = [A.bf16(128), A.bf16(128)]
            mt = A.f32(16)[:, 0:8]
            negc = A.f32(2)[:, 0:1]
            rt = A.f32(128)
            rtk = [A.f32(32), A.f32(32)]
            SQJ = MIXT_f32
            wv = WIN[:, 0:8 * 416].rearrange("p (c n) -> p c n", c=8)
            WUQ = WIN[:, 8 * 416:8 * 416 + 768].rearrange("p (c n) -> p c n", c=2)
            WUKV = WIN[:, 8 * 416 + 768:8 * 416 + 768 + 512]
            P.dma("pool", wv, w_in_d[l].rearrange("(c p) n -> p c n", p=128)[:, :, 1476:1892], writes=["WIN"])
            P.dma("pool", WUQ, w_uq_d[l].rearrange("(c p) n -> p c n", p=128), writes=["WIN"])
            P.dma("pool", WUKV, w_ukv_d[l], writes=["WIN"])
            P.dma("pool", WOUT[:, 0:2048].rearrange("p (c n) -> p c n", c=2),
                  w_out_d[l, 768:1024, :].rearrange("(c p) n -> p c n", p=128), writes=["WOUT"])
            P.dma("sp", gq[:], gq_d[l].partition_broadcast(128), writes=["gq"])
            P.dma("sp", gkv[:], gkv_d[l].partition_broadcast(128), writes=["gkv"])
            load_ln(ln1g_d, ln1b_d, l)
            memset(VC[:, :, :, 64:65], 1.0, ["VCones"])
            def Pf(t):
                in_proj(t, 416, wv, HSB[t % 2])

            def c1(t):
                p = t % 2
                hs = HSB[p]
                hk = "HSB%d" % p
                o = 80 + 8 * p
                ck = "c1s%d" % p
                act(SQJ[:, 0:256], hs[:, 0:256], AF.Square, [hk], ["SQJ"], accum=sm[:, o:o + 1])
                act(SQJ[:, 256:384], hs[:, 256:384], AF.Square, [hk], ["SQJ2"], accum=sm[:, o + 1:o + 2])
                act(sm[:, o + 2:o + 3], sm[:, o:o + 1], AF.Ln, ["SQJ"], [ck + "l1"], scale=1.0 / 256, bias=1e-6)
                act(sm[:, o + 3:o + 4], sm[:, o + 1:o + 2], AF.Ln, ["SQJ2"], [ck + "l2"], scale=1.0 / 128, bias=1e-6)
                act(sm[:, o + 4:o + 5], sm[:, o + 2:o + 3], AF.Exp, [ck + "l1"], [ck + "r1"], scale=-0.5)
                act(sm[:, o + 5:o + 6], sm[:, o + 3:o + 4], AF.Exp, [ck + "l2"], [ck + "r2"], scale=-0.5)
                stt(CQN[p][:, 0:256], hs[:, 0:256], sm[:, o + 4:o + 5], gq[:], ALU.mult, ALU.mult, [hk, ck + "r1", "gq"], ["CQN%d" % p])
                stt(CKN[p][:, 0:128], hs[:, 256:384], sm[:, o + 5:o + 6], gkv[:], ALU.mult, ALU.mult, [hk, ck + "r2", "gkv"], ["CKN%d" % p])
                rope(hs[:, 384:416].rearrange("p (h t d) -> p h t d", h=1, t=2),
                     rtk[p][:, 0:32].rearrange("p (h t d) -> p h t d", h=1, t=2), 1, 32, t,
                     cos32, sin32, nsin32, [hk], ["rtk%d" % p])

            def c23(t):
                p = t % 2
                qck, kck = "QCF%d" % p, "KCF%d" % p
                b = mbank()
                tr(bankb(b)[:, 0:128], CQN[p][:, 0:128], identb[:], ["CQN%d" % p, "identb"], [("ps", b)], inc=False)
                tr(bankb(b)[:, 128:256], CQN[p][:, 128:256], identb[:], ["CQN%d" % p, "identb"], [("ps", b)], inc=False)
                tr(bankb(b)[:, 256:384], CKN[p][:, 0:128], identb[:], ["CKN%d" % p, "identb"], [("ps", b)], inc=True)
                cp("act", CQT[p][:, 0:256], bankb(b)[:, 0:256], [("ps", b)], ["CQT%d" % p])
                cp("dve", CKT[p][:, 0:128], bankb(b)[:, 256:384], [("ps", b)], ["CKT%d" % p])
                bq = mbank()
                mm(bank(bq)[:, 0:384], CQT[p][:, 0:128], WUQ[:, 0, :], True, False, ["CQT%d" % p, "WIN"], [("ps", bq)], False)
                mm(bank(bq)[:, 0:384], CQT[p][:, 128:256], WUQ[:, 1, :], False, True, ["CQT%d" % p, "WIN"], [("ps", bq)], True)
                bk = mbank()
                mm(bank(bk)[:, 0:512], CKT[p][:, 0:128], WUKV, True, True, ["CKT%d" % p, "WIN"], [("ps", bk)], True)
                cp("act", QCF[p], bank(bq)[:, 0:384], [("ps", bq)], [qck])
                q4 = QCF[p].rearrange("p (h d) -> p h d", h=4)
                qr = q4[:, :, 64:96].rearrange("p h (t d) -> p h t d", t=2)
                cp("dve", rt[:, 0:128].rearrange("p (h d) -> p h d", h=4), q4[:, :, 64:96], [qck], ["rt"])
                rope(rt[:, 0:128].rearrange("p (h t d) -> p h t d", h=4, t=2), qr, 4, 32, t,
                     cos32, sin32, nsin32, ["rt"], [qck])
                kv4 = bank(bk)[:, 0:512].rearrange("p (h d) -> p h d", h=4)
                k4 = KCF[p].rearrange("p (h d) -> p h d", h=4)
                cp("act", k4[:, :, 0:64], kv4[:, :, 0:64], [("ps", bk)], [kck])
                cp("dve", VC[:, t, :, 0:64], kv4[:, :, 64:128], [("ps", bk)], [("VC", t)])
                cp("dve", k4[:, :, 64:96], rtk[p][:, 0:32].unsqueeze(1).to_broadcast([128, 4, 32]), ["rtk%d" % p], [kck])
                act(SQ[:, 0:384], QCF[p], AF.Square, [qck], ["SQ"])
                act(SQ[:, 384:768], KCF[p], AF.Square, [kck], ["SQ"])
                if t == 0:
                    red(mt, SQ[:, 0:768].rearrange("p (h d) -> p h d", h=8), ALU.add, ["SQ"], ["mt"])
                else:
                    red(sm[:, 16:24], SQ[:, 0:768].rearrange("p (h d) -> p h d", h=8), ALU.add, ["SQ"], ["hs"])
                    tt(mt, mt, sm[:, 16:24], ALU.max, ["mt", "hs"], ["mt"])

            def c4(t):
                p = t % 2
                for (src_, dstF, key, fkey, eng) in ((QCF[p], FQ, "QCF%d" % p, "FQKR0", "act"), (KCF[p], FK, "KCF%d" % p, "FQKR1", "dve")):
                    b = mbank()
                    for h in range(4):
                        tr(bank(b)[0:96, h * 128:(h + 1) * 128], src_[:, h * 96:(h + 1) * 96], identf[:],
                           [key, "identf"], [("ps", b)], inc=(h == 3))
                    cp(eng, dstF[0:96, :, tcols(t)], bank(b)[0:96, :].rearrange("p (c n) -> p c n", c=4),
                       [("ps", b)], [(fkey, t)])

            run_stages(Pf, [c1, c23, c4])
            global_bound(mt, 8, slice(0, 4), slice(4, 8), SC96, negc)

            P.barrier()
            RT = arena[:, phase_qkr_off:phase_qkr_off + 2304]

            def epilogue(qb):
                k = qb % 2
                bg.append(lambda: ln_stats(qb, k))
                bg.append(lambda: ln_apply(qb, k))

                def xpose(half):
                    b = mbank()
                    for j in range(4):
                        c = half * 4 + j
                        tr(bank(b)[:, j * 128:(j + 1) * 128], X[:, qb, c * 128:(c + 1) * 128], identf[:],
                           [("X", qb), "identf"], [("ps", b)], inc=(j == 3))
                    cp("act", xT[:, half * 4:half * 4 + 4, tcols(qb)], bank(b).rearrange("p (c n) -> p c n", c=4),
                       [("ps", b)], [("xT", qb)])
                    cp("dve", HSB[half][:, 0:512], bank(b), [("ps", b)], ["HSB%d" % half])

                bg.append(lambda: xpose(0))
                bg.append(lambda: xpose(1))
                def router_a():
                    b = mbank()
                    for c in range(8):
                        mm(bank(b)[:, 0:16], HSB[c // 4][:, (c % 4) * 128:(c % 4 + 1) * 128], wr[:, c, :], c == 0, c == 7,
                           ["HSB0", "HSB1", "wr"], [("ps", b)], inc=(c == 7))
                    act(RT[:, qb * 16:(qb + 1) * 16], bank(b)[:, 0:16], AF.Exp, [("ps", b)], [("r_sc", qb)], scale=-1.0)

                bg.append(router_a)

            seq = []
            for qb in range(NT):
                par = qb % 2
                for h in range(4):
                    for kt0 in range(0, qb + 1, 4):
                        kts = list(range(kt0, min(kt0 + 4, qb + 1)))
                        slots = [{"K": (FK[0:96, h, tcols(kt)], [("FQKR1", kt)]),
                                  "Q": (FQ[0:96, h, tcols(qb)], [("FQKR0", qb)]),
                                  "V": (VC[:, kt, h, :], [("VC", kt), "VCones"]),
                                  "ob": 4 + par, "oreg": h * 65, "start": kt == 0, "stop": kt == qb} for kt in kts]
                        ns = len(kts)
                        masks = []
                        if kts[-1] == qb:
                            masks = [(128 * (ns - 1), 128 * ns, None, causalT[:], ["causalT"])]
                        last = (h == 3 and kts[-1] == qb)
                        seq.append(({"kind": "att", "slots": slots, "scale": SC96, "negc": negc, "masks": masks,
                                     "fin": fin_generic(qb, 4 + par, 4, 2, 768, epilogue) if last else None}, []))
            run_seq(seq, bg_per_iter=2)

            sc = RT[:, 0:256]
            bi = RT[:, 256:512]
            m1 = RT[:, 512:576]
            eq = RT[:, 576:832]
            msk = RT[:, 832:1088]
            m2 = RT[:, 1088:1152]
            gs = RT[:, 1152:1216]
            gm = RT[:, 1216:1232]
            gsel = RT[:, 1232:1296]
            t2 = RT[:, 1296:1552]
            wgt = RT[:, 1552:1808]
            ws = RT[:, 1808:1824]
            rsk = [("r_sc", t) for t in range(NT)]

            def v3(ap, a):
                return ap.rearrange("p (a b) -> p a b", a=a)

            ts(sc, sc, 1.0, None, ALU.add, None, rsk, ["r_s"])
            recip(sc, sc, ["r_s"], ["r_s"])
            tt(v3(bi, 16), v3(sc, 16), rb[:].unsqueeze(1).to_broadcast([128, 16, 16]), ALU.add, ["r_s", "rb"], ["r_bi"])
            red(m1, v3(bi, 64), ALU.max, ["r_bi"], ["r_m1"])
            tt(v3(eq, 64), v3(bi, 64), m1.unsqueeze(2).to_broadcast([128, 64, 4]), ALU.is_equal, ["r_bi", "r_m1"], ["r_eq"])
            stt(msk, eq, NEG, bi, ALU.mult, ALU.add, ["r_eq", "r_bi"], ["r_msk"])
            red(m2, v3(msk, 64), ALU.max, ["r_msk"], ["r_m2"])
            tt(gs, m1, m2, ALU.add, ["r_m1", "r_m2"], ["r_gs"])
            red(gm, v3(gs, 16), ALU.max, ["r_gs"], ["r_gm"])
            tt(v3(gsel, 16), v3(gs, 16), gm.unsqueeze(2).to_broadcast([128, 16, 4]), ALU.is_equal, ["r_gs", "r_gm"], ["r_gsel"])
            tt(v3(t2, 64), v3(bi, 64), m2.unsqueeze(2).to_broadcast([128, 64, 4]), ALU.is_ge, ["r_bi", "r_m2"], ["r_t2"])
            tt(v3(t2, 64), v3(t2, 64), gsel.unsqueeze(2).to_broadcast([128, 64, 4]), ALU.mult, ["r_t2", "r_gsel"], ["r_t2"])
            tt(wgt, sc, t2, ALU.mult, ["r_s", "r_t2"], ["r_w"])
            red(ws, v3(wgt, 16), ALU.add, ["r_w"], ["r_ws"])
            recip(ws, ws, ["r_ws"], ["r_ws"])
            tt(gates[:], v3(wgt, 16), ws.unsqueeze(2).to_broadcast([128, 16, 16]), ALU.mult, ["r_w", "r_ws"],
               [("gates", t) for t in range(NT)])

        def phase_moe(l, last_layer):
            A.off = 0
            NSLOT = 7
            WGU = [A.bf16(8 * 512).rearrange("p (c n) -> p c n", c=8) for _ in range(NSLOT)]
            WD = [A.bf16(2 * 1024).rearrange("p (c n) -> p c n", c=2) for _ in range(NSLOT)]
            SB = [A.bf16(256) for _ in range(2)]
            TB = [A.bf16(256) for _ in range(2)]
            TTB = [A.bf16(256) for _ in range(2)]
            load_ln(ln2g_d, ln2b_d, l)
            def load_expert(e):
                s = e % NSLOT
                P.dma("pool", WGU[s][:, :, 0:256], w_gate_d[l, e].rearrange("(c p) f -> p c f", p=128), writes=[("WGU", s)])
                P.dma("pool", WGU[s][:, :, 256:512], w_up_d[l, e].rearrange("(c p) f -> p c f", p=128), writes=[("WGU", s)])
                P.dma("pool", WD[s], w_down_d[l, e].rearrange("(c p) d -> p c d", p=128), writes=[("WD", s)])

            for e in range(NSLOT):
                load_expert(e)
            items = [(G, t, e) for G in range(4) for t in range(NT) for e in range(4 * G, 4 * G + 4)]
            sd = {}
            cnt = [0]

            def s1(it):
                G, t, e = it
                s = e % NSLOT
                b = rbank()
                sd[it] = {"b": b, "i": cnt[0] % 2}
                cnt[0] += 1
                for c in range(8):
                    mm(bank(b), xT[:, c, tcols(t)], WGU[s][:, c, :], c == 0, c == 7,
                       [("xT", t), ("WGU", s)], [("ps", b)], inc=(c == 7))

            def s2(it):
                G, t, e = it
                d = sd[it]
                b, i = d["b"], d["i"]
                act(SB[i][:, 0:256], bank(b)[:, 0:256], AF.Silu, [("ps", b)], [("SB", i)])
                stt(TB[i][:, 0:256], bank(b)[:, 256:512], gates[:, t, e:e + 1], SB[i][:, 0:256], ALU.mult, ALU.mult,
                    [("ps", b), ("gates", t), ("SB", i)], [("TB", i)])
                b2 = rbank()
                d["b2"] = b2
                for fc in range(2):
                    tr(bankb(b2)[:, fc * 128:(fc + 1) * 128], TB[i][:, fc * 128:(fc + 1) * 128], identb[:],
                       [("TB", i), "identb"], [("ps", b2)], inc=(fc == 1))

            def s3(it):
                G, t, e = it
                d = sd[it]
                b2, i = d["b2"], d["i"]
                s = e % NSLOT
                cp("act", TTB[i][:, 0:256], bankb(b2)[:, 0:256], [("ps", b2)], [("TTB", i)])
                first = (e % 4 == 0)
                lastx = (e % 4 == 3)
                for half in range(2):
                    yb = 4 + 2 * (t % 2) + half
                    for fc in range(2):
                        mm(bank(yb), TTB[i][:, fc * 128:(fc + 1) * 128], WD[s][:, fc, half * 512:(half + 1) * 512],
                           first and fc == 0, lastx and fc == 1, [("TTB", i), ("WD", s)], [("ps", yb)],
                           inc=(fc == 1))
                if t == NT - 1 and e + NSLOT < 16:
                    load_expert(e + NSLOT)
                if lastx:
                    for half in range(2):
                        yb = 4 + 2 * (t % 2) + half
                        xs = X[:, t, half * 512:(half + 1) * 512]
                        if G == 0:
                            stt(xs, xs, ALPHA, bank(yb), ALU.mult, ALU.add, [("X", t), ("ps", yb)], [("X", t)])
                        else:
                            tt(xs, xs, bank(yb), ALU.add, [("X", t), ("ps", yb)], [("X", t)])
                    if G == 3:
                        k = t % 2
                        o = 32 + 16 * k
                        mvo = 96 + 2 * t

                        def st_a(t=t, o=o, k=k):
                            P.op("dve", lambda e, o_=sm[:, o:o + 6], i=X[:, t, 0:512]: e.bn_stats(out=o_, in_=i),
                                 reads=[("X", t)], writes=["bs%da" % k])

                        def st_b(t=t, o=o, k=k, mvo=mvo):
                            P.op("dve", lambda e, o_=sm[:, o + 6:o + 12], i=X[:, t, 512:1024]: e.bn_stats(out=o_, in_=i),
                                 reads=[("X", t)], writes=["bs%db" % k])
                            P.op("dve", lambda e, o_=sm[:, mvo:mvo + 2], i=sm[:, o:o + 12]: e.bn_aggr(out=o_, in_=i),
                                 reads=["bs%da" % k, "bs%db" % k], writes=[("mv2", t)])

                        bg.append(st_a)
                        bg.append(st_b)
                        if t % 8 == 7:
                            t0 = t - 7

                            def rstd8(t0=t0):
                                var8 = sm[:, 96 + 2 * t0:96 + 2 * t0 + 16].rearrange("p (t two) -> p t two", two=2)[:, :, 1]
                                rs8 = sm[:, 128 + t0:128 + t0 + 8]
                                act(rs8, var8, AF.Ln, [("mv2", u) for u in range(t0, t0 + 8)], [("rs2", t0)], bias=1e-5)
                                act(rs8, rs8, AF.Exp, [("rs2", t0)], [("rs2", t0)], scale=-0.5)
                            bg.append(rstd8)
                            for u in range(t0, t0 + 8):
                                for half in range(2):
                                    hsl = slice(half * 512, (half + 1) * 512)

                                    def ap1(u=u, t0=t0, hsl=hsl):
                                        xs = X[:, u, hsl]
                                        ts(xs, xs, sm[:, 96 + 2 * u:96 + 2 * u + 1], sm[:, 128 + u:128 + u + 1], ALU.subtract, ALU.mult,
                                           [("X", u), ("mv2", u), ("rs2", t0)], [("X", u)])

                                    def ap2(u=u, hsl=hsl):
                                        xs = X[:, u, hsl]
                                        tt(xs, xs, lnp[:, 0, hsl], ALU.mult, [("X", u), "lnp"], [("X", u)])

                                    def ap3(u=u, hsl=hsl, half=half):
                                        xs = X[:, u, hsl]
                                        tt(xs, xs, lnp[:, 1, hsl], ALU.add, [("X", u), "lnp"], [("X", u)])
                                        if last_layer and half == 1:
                                            P.dma("sp", tm(out_d)[:, u, :], X[:, u, :], reads=[("X", u)], final=True)
                                    bg.append(ap1)
                                    bg.append(ap2)
                                    bg.append(ap3)

            pipeline(items, s1, s2, s3, bg_per_iter=2)

        def run_layers():
            for l in range(nlayers):
                for t in range(NT):
                    build_xT(t)
                if stop_after == "xT":
                    return False
                phase_A(l)
                P.barrier()
                if stop_after in ("A", "A1", "A2"):
                    return False
                phase_B(l)
                P.barrier()
                if stop_after == "B":
                    return False
                phase_C(l)
                P.barrier()
                if stop_after == "C":
                    return False
                phase_moe(l, l == nlayers - 1)
                P.barrier()
            return True

        if not run_layers():
            for t in range(NT):
                P.dma("sp", tm(out_d)[:, t, :], X[:, t, :], reads=[("X", t)], final=True)

        P.emit(nc, sems)
    return nc


_CACHE = {}


def kernel(**inputs):
    consts = _consts()
    if "nc" not in _CACHE:
        _CACHE["nc"] = build_program()
    nc = _CACHE["nc"]
    x = np.ascontiguousarray(np.asarray(inputs["x"], dtype=np.float32))
    shared = {}
    for k, v in inputs.items():
        if k == "x":
            continue
        shared[k] = np.ascontiguousarray(np.asarray(v, dtype=np.float32))
    shared.update(consts)
    in_maps = []
    for c in range(NCORES):
        m = dict(shared)
        m["x"] = x[c]
        in_maps.append(m)
    res = run_bass_kernel_spmd(nc, in_maps, core_ids=list(range(NCORES)))
    out = np.stack([np.asarray(r["out"], dtype=np.float32) for r in res.results], axis=0)
    return out
```

```python
import contextlib
import numpy as np
import concourse.bass as bass
import concourse.mybir as mybir
from concourse.bass_utils import run_bass_kernel_spmd

F32 = mybir.dt.float32
BF16 = mybir.dt.bfloat16
AF = mybir.ActivationFunctionType
ALU = mybir.AluOpType
AX = mybir.AxisListType

S = 2048
D = 1024
NT = 16
NCORES = 8
DEPTH = 2
ALPHA = float((2 * DEPTH) ** 0.25)
IDXW = float((4 * 64) ** -0.5)
SC64 = float(64 ** -0.5)
SC96 = float(96 ** -0.5)
TOPK = 256
NBIS = 14
NEG = -1.0e30
NDMA_SEMS = 8
ACC_ENG = "dve"
LNP_ENG = "dve"


class Prog:
    ENGS = ("pe", "act", "dve", "pool", "sp")

    def __init__(self):
        self.ops = {e: [] for e in self.ENGS}
        self.cnt = {e: 0 for e in self.ENGS}
        self.last_w = {}
        self.readers = {}
        self.waited = {e: {} for e in self.ENGS}
        self.dma_val = {}
        self.dma_rr = {"sp": 0, "pool": 0}
        self.final_tokens = []

    def _deps(self, eng, reads, writes):
        deps = {}

        def add(tok, raw):
            src, val = tok
            if src == eng and eng == "pe":
                return
            if deps.get(src, 0) < val:
                deps[src] = val

        for k in reads:
            if k in self.last_w:
                add(self.last_w[k], True)
            if isinstance(k, tuple) and k[0] == "ps":
                for r in self.readers.get(k, ()):
                    if r[0] != eng:
                        add(r, False)
        for k in writes:
            if k in self.last_w:
                add(self.last_w[k], False)
            for r in self.readers.get(k, ()):
                add(r, False)
        waits = []
        for src, val in deps.items():
            if self.waited[eng].get(src, 0) >= val:
                continue
            self.waited[eng][src] = val
            waits.append((src, val))
        return waits

    def _record(self, tok, reads, writes):
        for k in writes:
            self.last_w[k] = tok
            self.readers[k] = []
        for k in reads:
            self.readers.setdefault(k, []).append(tok)

    def op(self, eng, fn, reads=(), writes=(), inc=True):
        waits = self._deps(eng, reads, writes)
        if inc:
            self.cnt[eng] += 1
            idx = self.cnt[eng]
        else:
            idx = self.cnt[eng] + 1
        tok = (eng, idx)
        self._record(tok, reads, writes)
        self.ops[eng].append((waits, fn, ("eng", eng) if inc else None))
        return tok

    def dma(self, q, out_ap, in_ap, reads=(), writes=(), final=False):
        i = self.dma_rr[q]
        self.dma_rr[q] = (i + 1) % NDMA_SEMS
        src = ("dma", q, i)
        prev = self.dma_val.get(src, 0)
        waits = self._deps(q, reads, writes)
        if prev and self.waited[q].get(src, 0) < prev:
            self.waited[q][src] = prev
            waits.append((src, prev))
        val = prev + 16
        self.dma_val[src] = val
        tok = (src, val)
        self._record(tok, reads, writes)

        def fn(e, out_ap=out_ap, in_ap=in_ap):
            return e.dma_start(out=out_ap, in_=in_ap)

        self.ops[q].append((waits, fn, ("dma", src)))
        if final:
            self.final_tokens.append(tok)
        return tok

    def barrier(self):
        snap = [(e, self.cnt[e]) for e in self.ENGS if self.cnt[e] > 0]
        snap += [(src, v) for src, v in self.dma_val.items()]
        for e in self.ENGS:
            waits = []
            for src, val in snap:
                if src == e:
                    continue
                if self.waited[e].get(src, 0) >= val:
                    continue
                self.waited[e][src] = val
                waits.append((src, val))
            if waits:
                self.ops[e].append((waits, None, None))

    def emit(self, nc, sems):
        fin = list(self.final_tokens)

        def replay(eng, e):
            for waits, fn, inc in self.ops[eng]:
                for src, val in waits:
                    e.wait_ge(sems[src], val)
                if fn is None:
                    continue
                ins = fn(e)
                if inc is not None:
                    if inc[0] == "eng":
                        ins.then_inc(sems[inc[1]], 1)
                    else:
                        ins.then_inc(sems[inc[1]], 16)
            if eng == "sp":
                for src, val in fin:
                    e.wait_ge(sems[src], val)

        with nc.Block() as block:
            @block.tensor
            def _(e):
                replay("pe", e)

            @block.scalar
            def _(e):
                replay("act", e)

            @block.vector
            def _(e):
                replay("dve", e)

            @block.gpsimd
            def _(e):
                replay("pool", e)

            @block.sync
            def _(e):
                replay("sp", e)


def _consts():
    pos = np.arange(S, dtype=np.float64)
    c = {}
    for dim, nm in ((64, "64"), (32, "32")):
        inv = 1.0 / (10000.0 ** (np.arange(0, dim, 2, dtype=np.float64) / dim))
        inv = inv.astype(np.float32).astype(np.float64)
        ang = (pos.astype(np.float32)[:, None] * inv.astype(np.float32)[None, :]).astype(np.float32)
        c["cos" + nm] = np.cos(ang.astype(np.float64)).astype(np.float32)
        c["sin" + nm] = np.sin(ang.astype(np.float64)).astype(np.float32)
    c["ident"] = np.eye(128, dtype=np.float32)
    fv = np.concatenate([2.0 ** -(np.arange(16) + 1.0), 2.0 ** -np.arange(16).astype(np.float64)])
    c["fvec"] = np.tile(fv.astype(np.float32)[None, :], (128, 1))
    qi = np.arange(128)[:, None]
    kj = np.arange(256)[None, :]
    diff = qi + 128 - kj
    kk = np.arange(128)[None, :]
    c["negmask"] = np.where(kk <= qi, 0.0, NEG).astype(np.float32)
    c["causalT"] = (qi <= kk).astype(np.float32)
    c["maskPC"] = np.concatenate([(qi > kk).astype(np.float32), c["causalT"]], axis=1)
    return c


def build_program(nlayers=DEPTH, debug_mix=False, stop_after=None):
    nc = bass.Bass("TRN2", target_bir_lowering=False)

    def din(name, shape):
        return nc.dram_tensor(name, list(shape), F32, kind="ExternalInput").ap()

    x_d = din("x", [S, D])
    w_in_d = din("w_in", [DEPTH, D, 1892])
    sinks_d = din("attn_sinks", [DEPTH, 8])
    gq_d = din("c_q_norm_g", [DEPTH, 256])
    gkv_d = din("c_kv_norm_g", [DEPTH, 128])
    w_uq_d = din("w_uq", [DEPTH, 256, 384])
    w_ukv_d = din("w_ukv", [DEPTH, 128, 512])
    w_out_d = din("w_out", [DEPTH, D, D])
    ln1g_d = din("ln1_g", [DEPTH, D])
    ln1b_d = din("ln1_b", [DEPTH, D])
    w_router_d = din("w_router", [D, 16])
    rbias_d = din("router_bias", [16])
    w_gate_d = din("w_gate", [DEPTH, 16, D, 256])
    w_up_d = din("w_up", [DEPTH, 16, D, 256])
    w_down_d = din("w_down", [DEPTH, 16, 256, D])
    ln2g_d = din("ln2_g", [DEPTH, D])
    ln2b_d = din("ln2_b", [DEPTH, D])
    cos64_d = din("cos64", [S, 32])
    sin64_d = din("sin64", [S, 32])
    cos32_d = din("cos32", [S, 16])
    sin32_d = din("sin32", [S, 16])
    ident_d = din("ident", [128, 128])
    fvec_d = din("fvec", [128, 32])
    negmask_d = din("negmask", [128, 128])
    causalT_d = din("causalT", [128, 128])
    maskPC_d = din("maskPC", [128, 256])
    out_d = nc.dram_tensor("out", [S, D], F32, kind="ExternalOutput").ap()
    dbg_d = None
    if debug_mix:
        dbg_d = nc.dram_tensor("dbg", [S, D], F32, kind="ExternalOutput").ap()

    P = Prog()
    st = contextlib.ExitStack()
    with st:
        sems = {}
        for e in Prog.ENGS:
            sems[e] = st.enter_context(nc.semaphore("s_" + e))
        for q in ("sp", "pool"):
            for i in range(NDMA_SEMS):
                sems[("dma", q, i)] = st.enter_context(nc.semaphore(f"d_{q}{i}"))

        def T(name, shape, dt):
            return st.enter_context(nc.sbuf_tensor("sb_" + name, list(shape), dt))

        X = T("X", [128, NT, D], F32)
        xT = T("xT", [128, 8, S], BF16)
        cos64 = T("cos64", [128, NT, 32], F32)
        sin64 = T("sin64", [128, NT, 32], F32)
        nsin64 = T("nsin64", [128, NT, 32], F32)
        cos32 = T("cos32", [128, NT, 16], F32)
        sin32 = T("sin32", [128, NT, 16], F32)
        nsin32 = T("nsin32", [128, NT, 16], F32)
        identf = T("identf", [128, 128], F32)
        fvec = T("fvec", [128, 32], F32)
        identb = T("identb", [128, 128], BF16)
        negmask = T("negmask", [128, 128], F32)
        causalT = T("causalT", [128, 128], BF16)
        maskPC = T("maskPC", [128, 256], BF16)
        ones1 = T("ones1", [1, 128], F32)
        lnp = T("lnp", [128, 2, D], F32)
        gates = T("gates", [128, NT, 16], F32)
        widx = T("widx", [128, NT, 4], F32)
        wr = T("wr", [128, 8, 16], F32)
        rb = T("rb", [128, 16], F32)
        sinks = T("sinks", [128, 8], F32)
        gq = T("gq", [128, 256], F32)
        gkv = T("gkv", [128, 128], F32)
        sm = T("sm", [128, 256], F32)
        ARW = 22400
        arena = T("arena", [128, ARW], F32)
        psum = st.enter_context(nc.psum_tensor("psum", [128, 4096], F32))

        def bank(i):
            return psum[:, 512 * i:512 * (i + 1)]

        def bankb(i):
            return psum[:, 512 * i:512 * (i + 1)].bitcast(BF16)

        class Arena:
            def __init__(self):
                self.off = 0

            def f32(self, n):
                o = self.off
                self.off += n
                assert self.off <= ARW, self.off
                return arena[:, o:o + n]

            def bf16(self, n):
                w = (n + 1) // 2
                o = self.off
                self.off += w
                assert self.off <= ARW, self.off
                return arena[:, o:o + w].bitcast(BF16)

        A = Arena()
        WIN = A.bf16(8 * 768)
        WOUT = A.bf16(4 * 1024)
        HSB = [A.f32(768), A.f32(768)]
        phase_qkr_off = A.off
        QKR = [A.f32(768), A.f32(768)]
        SQ = A.f32(768)
        PB = [A.bf16(512), A.bf16(512)]
        PTB = [A.bf16(512), A.bf16(512)]
        ROPET = A.f32(640)
        mixt_off = A.off
        MIXT = A.bf16(512)
        MIXTT = A.bf16(512)
        MIXT_f32 = arena[:, mixt_off:mixt_off + 512]
        phase_base = A.off

        rot = [0]
        rot4 = [0]
        inpipe = [False]

        def rbank():
            i = rot[0]
            rot[0] = (i + 1) % 3
            return i

        def mbank():
            if inpipe[0]:
                return 3
            i = rot4[0]
            rot4[0] = (i + 1) % 4
            return i

        def mm(out, lhsT, rhs, start, stop, reads, writes, inc):
            P.op("pe", lambda e, o=out, l=lhsT, r=rhs, s0=start, s1=stop: e.matmul(o, lhsT=l, rhs=r, start=s0, stop=s1),
                 reads=reads, writes=writes, inc=inc)

        def tr(out, in_, ident, reads, writes, inc=True):
            P.op("pe", lambda e, o=out, i=in_, d=ident: e.transpose(out=o, in_=i, identity=d),
                 reads=reads, writes=writes, inc=inc)

        def act(out, in_, func, reads, writes, bias=None, scale=None, accum=None):
            kw = {}
            if bias is not None:
                kw["bias"] = bias
            if scale is not None:
                kw["scale"] = scale
            if accum is not None:
                kw["accum_out"] = accum
            P.op("act", lambda e, o=out, i=in_, f=func, kw=kw: e.activation(out=o, in_=i, func=f, **kw),
                 reads=reads, writes=writes)

        def tt(out, in0, in1, op, reads, writes, eng="dve"):
            P.op(eng, lambda e, o=out, a=in0, b=in1, p=op: e.tensor_tensor(out=o, in0=a, in1=b, op=p),
                 reads=reads, writes=writes)

        def ts(out, in0, s1, s2, op0, op1, reads, writes, accum=None, eng="dve"):
            def fn(e, o=out, a=in0, s1=s1, s2=s2, op0=op0, op1=op1, accum=accum):
                kw = {}
                if op1 is not None:
                    kw["op1"] = op1
                if accum is not None:
                    kw["accum_out"] = accum
                return e.tensor_scalar(out=o, in0=a, scalar1=s1, scalar2=s2, op0=op0, **kw)
            P.op(eng, fn, reads=reads, writes=writes)

        def stt(out, in0, scalar, in1, op0, op1, reads, writes, eng="dve"):
            P.op(eng, lambda e, o=out, a=in0, s=scalar, b=in1, p0=op0, p1=op1:
                 e.scalar_tensor_tensor(out=o, in0=a, scalar=s, in1=b, op0=p0, op1=p1),
                 reads=reads, writes=writes)

        def cp(eng, out, in_, reads, writes):
            if eng == "act":
                act(out, in_, AF.Copy, reads, writes)
            else:
                P.op(eng, lambda e, o=out, i=in_: e.tensor_copy(out=o, in_=i), reads=reads, writes=writes)

        def red(out, in_, op, reads, writes, absv=False):
            def fn(e, o=out, i=in_, p=op, a=absv):
                if a:
                    return e.tensor_reduce(out=o, in_=i, axis=AX.X, op=p, apply_absolute_value=True)
                return e.tensor_reduce(out=o, in_=i, axis=AX.X, op=p)
            P.op("dve", fn, reads=reads, writes=writes)

        def memset(ap, val, writes, eng="dve"):
            P.op(eng, lambda e, a=ap, v=val: e.memset(a, v), writes=writes)

        def recip(out, in_, reads, writes):
            P.op("dve", lambda e, o=out, i=in_: e.reciprocal(out=o, in_=i), reads=reads, writes=writes)

        bg = []

        def bg_run(k):
            for _ in range(k):
                if not bg:
                    return
                bg.pop(0)()

        def pipeline(items, s1, s2, s3, bg_per_iter=0):
            n = len(items)
            inpipe[0] = True
            for i in range(n + 2):
                if i < n:
                    s1(items[i])
                if 0 <= i - 1 < n:
                    s2(items[i - 1])
                if 0 <= i - 2 < n:
                    s3(items[i - 2])
                if bg_per_iter:
                    bg_run(bg_per_iter)
            bg_run(len(bg))
            inpipe[0] = False

        def run_tiles(Pf, Rf):
            Pf(0)
            for t in range(NT):
                if t + 1 < NT:
                    Pf(t + 1)
                Rf(t)

        def run_stages(Pf, stages):
            ns = len(stages)
            Pf(0)
            for i in range(NT + ns - 1):
                if i + 1 < NT:
                    Pf(i + 1)
                for k, st_ in enumerate(stages):
                    if 0 <= i - k < NT:
                        st_(i - k)

        def tcols(t):
            return slice(t * 128, (t + 1) * 128)

        PBs = [PB[0], PB[1], PTB[0]]
        pbrot = [0]

        def st1(it):
            b = rbank()
            it["b"] = b
            k = it["kind"]
            if k == "att":
                it["pi"] = pbrot[0]
                pbrot[0] = (pbrot[0] + 1) % 3
                sl = it["slots"]
                for j, s in enumerate(sl):
                    ka, kk = s["K"]
                    qa, qk = s["Q"]
                    mm(bank(b)[:, j * 128:(j + 1) * 128], ka, qa, True, True, kk + qk, [("ps", b)], inc=(j == len(sl) - 1))
            elif k == "idx":
                mm(bank(b)[:, 0:it["n"]], it["lhsT"], it["rhs"], True, True, it["keys"], [("ps", b)], True)
            elif k == "mskT":
                kts = it["kts"]
                for j, kt in enumerate(kts):
                    tr(bankb(b)[:, j * 128:(j + 1) * 128], it["src"][:, kt * 128:(kt + 1) * 128], identb[:],
                       [it["srckey"], "identb"], [("ps", b)], inc=(j == len(kts) - 1))

        def st2(it):
            b = it["b"]
            k = it["kind"]
            if k == "att":
                pi = it["pi"]
                n = 128 * len(it["slots"])
                act(PBs[pi][:, 0:n], bank(b)[:, 0:n], AF.Exp, [("ps", b), "negc"], [("PB", pi)], bias=it["negc"], scale=it["scale"])
                for (c0, c1, view, m_ap, mk) in it["masks"]:
                    pv = PBs[pi][:, c0:c1]
                    if view is not None:
                        pv = pv.rearrange(view[0], **view[1])
                    tt(pv, pv, m_ap, ALU.mult, [("PB", pi)] + mk, [("PB", pi)])
            elif k == "idx":
                ri, n, h, qb, k0 = it["ri"], it["n"], it["h"], it["qb"], it["k0"]
                sco, sk = it["sco"], it["scokey"]
                act(it["rb"][:, 0:n], bank(b)[:, 0:n], AF.Relu, [("ps", b)], ["RB%d" % ri])
                if h == 0:
                    ts(sco[:, k0:k0 + n], it["rb"][:, 0:n], widx[:, qb, 0:1], None, ALU.mult, None,
                       ["RB%d" % ri, ("widx", qb)], [sk], eng=ACC_ENG)
                elif ACC_ENG == "dve":
                    stt(sco[:, k0:k0 + n], it["rb"][:, 0:n], widx[:, qb, h:h + 1], sco[:, k0:k0 + n],
                        ALU.mult, ALU.add, ["RB%d" % ri, ("widx", qb), sk], [sk])
                else:
                    ts(it["rb"][:, 0:n], it["rb"][:, 0:n], widx[:, qb, h:h + 1], None, ALU.mult, None,
                       ["RB%d" % ri, ("widx", qb)], ["RB%d" % ri], eng=ACC_ENG)
                    tt(sco[:, k0:k0 + n], sco[:, k0:k0 + n], it["rb"][:, 0:n], ALU.add, ["RB%d" % ri, sk], [sk], eng=ACC_ENG)
            elif k == "mskT":
                kts = it["kts"]
                nj = len(kts)
                cp("act", it["dst"][:, kts[0]:kts[0] + nj, :],
                   bankb(b)[:, 0:nj * 128].rearrange("p (c n) -> p c n", c=nj), [("ps", b)], [it["dstkey"]])

        def st3(it):
            if it["kind"] != "att":
                return
            pi = it["pi"]
            sl = it["slots"]
            for j, s in enumerate(sl):
                va, vk = s["V"]
                ob = s["ob"]
                mm(bank(ob)[:, s["oreg"]:s["oreg"] + 65], PBs[pi][:, j * 128:(j + 1) * 128], va, s["start"], s["stop"],
                   [("PB", pi)] + vk, [("ps", ob)], inc=(j == len(sl) - 1 or sl[j + 1]["ob"] != ob))
            if it.get("fin") is not None:
                it["fin"]()

        def run_seq(seq, bg_per_iter=0):
            n = len(seq)
            inpipe[0] = True
            for i in range(n + 2):
                if i < n and seq[i][0] is not None:
                    st1(seq[i][0])
                if 0 <= i - 1 < n and seq[i - 1][0] is not None:
                    st2(seq[i - 1][0])
                if 0 <= i - 2 < n and seq[i - 2][0] is not None:
                    st3(seq[i - 2][0])
                if i < n:
                    for c in seq[i][1]:
                        c()
                if bg_per_iter:
                    bg_run(bg_per_iter)
            bg_run(len(bg))
            inpipe[0] = False

        def fin_generic(qb, ob, nheads, nchunks_w, mix_c0, epilogue):
            def fn():
                o3 = bank(ob)[:, 0:nheads * 65].rearrange("p (h d) -> p h d", h=nheads)
                rc = sm[:, 72:72 + nheads]
                recip(rc, o3[:, :, 64], [("ps", ob)], ["rc"])
                tt(MIXT[:, 0:nheads * 64].rearrange("p (h d) -> p h d", h=nheads), o3[:, :, 0:64],
                   rc.unsqueeze(2).to_broadcast([128, nheads, 64]), ALU.mult, [("ps", ob), "rc"], ["MIXT"])
                dbg_store(qb, mix_c0, nheads * 64)
                out_proj_partial(qb, nchunks_w, False, after=epilogue)
            return fn

        def tm(ap_d):
            return ap_d.rearrange("(t p) d -> p t d", p=128)

        P.dma("sp", cos64[:], tm(cos64_d), writes=["cos64"])
        P.dma("sp", sin64[:], tm(sin64_d), writes=["sin64"])
        P.dma("sp", cos32[:], tm(cos32_d), writes=["cos32"])
        P.dma("sp", sin32[:], tm(sin32_d), writes=["sin32"])
        P.dma("sp", identf[:], ident_d, writes=["identf"])
        P.dma("sp", fvec[:], fvec_d, writes=["fvec"])
        P.dma("pool", identb[:], ident_d, writes=["identb"])
        P.dma("pool", causalT[:], causalT_d, writes=["causalT"])
        P.dma("pool", maskPC[:], maskPC_d, writes=["maskPC"])
        P.dma("sp", negmask[:], negmask_d, writes=["negmask"])
        P.dma("sp", wr[:], w_router_d.rearrange("(c p) n -> p c n", p=128), writes=["wr"])
        P.dma("sp", rb[:], rbias_d.partition_broadcast(128), writes=["rb"])
        xv = tm(x_d)
        for t4 in range(4):
            P.dma("sp", X[:, 4 * t4:4 * t4 + 4, :], xv[:, 4 * t4:4 * t4 + 4, :],
                  writes=[("X", t) for t in range(4 * t4, 4 * t4 + 4)])
        ts(nsin64[:], sin64[:], -1.0, None, ALU.mult, None, ["sin64"], ["nsin64"])
        ts(nsin32[:], sin32[:], -1.0, None, ALU.mult, None, ["sin32"], ["nsin32"])
        memset(ones1[:], 1.0, ["ones1"])

        def build_xT(t):
            for half in range(2):
                b = mbank()
                for j in range(4):
                    c = half * 4 + j
                    tr(bank(b)[:, j * 128:(j + 1) * 128], X[:, t, c * 128:(c + 1) * 128], identf[:],
                       [("X", t), "identf"], [("ps", b)], inc=(j == 3))
                cp("act" if half == 0 else "dve",
                   xT[:, half * 4:half * 4 + 4, tcols(t)],
                   bank(b).rearrange("p (c n) -> p c n", c=4),
                   [("ps", b)], [("xT", t)])

        def in_proj(t, ncols, wview, hs):
            n0 = 0
            while n0 < ncols:
                n1 = min(ncols, n0 + 512)
                b = mbank()
                for c in range(8):
                    mm(bank(b)[:, 0:n1 - n0], xT[:, c, tcols(t)], wview[:, c, n0:n1], c == 0, c == 7,
                       [("xT", t), "WIN"], [("ps", b)], inc=(c == 7))
                cp("act", hs[:, n0:n1], bank(b)[:, 0:n1 - n0], [("ps", b)], ["HSB%d" % (t % 2)])
                n0 = n1

        def rope(src, dst, nh, hd, t, cosT, sinT, nsinT, rk, wk, tmp=None):
            h2 = hd // 2
            cb = cosT[:, t, :].unsqueeze(1).unsqueeze(1).to_broadcast([128, nh, 2, h2])
            sb = sinT[:, t, :].unsqueeze(1).to_broadcast([128, nh, h2])
            nb = nsinT[:, t, :].unsqueeze(1).to_broadcast([128, nh, h2])
            tv = ROPET[:, 0:nh * hd].rearrange("p (h t d) -> p h t d", h=nh, t=2)
            rk = rk + ["cos64", "sin64", "nsin64", "cos32", "sin32", "nsin32"]
            tt(dst, src, cb, ALU.mult, rk, wk)
            tt(tv[:, :, 0, :], src[:, :, 1, :], nb, ALU.mult, rk, ["ropetmp"])
            tt(tv[:, :, 1, :], src[:, :, 0, :], sb, ALU.mult, rk, ["ropetmp"])
            tt(dst, dst, tv, ALU.add, wk + ["ropetmp"], wk)

        def global_bound(mt, nh, qsl, ksl, scale, negc):
            b = mbank()
            tr(bank(b)[0:nh, 0:128], mt, identf[:], ["mt", "identf"], [("ps", b)])
            red(sm[0:nh, 0:1], bank(b)[0:nh, 0:128], ALU.max, [("ps", b)], ["gb1"])
            b2 = mbank()
            tr(bank(b2)[0:1, 0:nh], sm[0:nh, 0:1], identf[0:nh, 0:nh], ["gb1", "identf"], [("ps", b2)])
            red(sm[0:1, 1:2], bank(b2)[0:1, qsl], ALU.max, [("ps", b2)], ["gb2"])
            red(sm[0:1, 2:3], bank(b2)[0:1, ksl], ALU.max, [("ps", b2)], ["gb3"])
            tt(sm[0:1, 3:4], sm[0:1, 1:2], sm[0:1, 2:3], ALU.mult, ["gb2", "gb3"], ["gb4"])
            act(sm[0:1, 4:5], sm[0:1, 3:4], AF.Ln, ["gb4"], ["gb5"])
            act(sm[0:1, 5:6], sm[0:1, 4:5], AF.Exp, ["gb5"], ["gb6"], scale=0.5)
            ts(sm[0:1, 6:7], sm[0:1, 5:6], -scale, None, ALU.mult, None, ["gb6"], ["gb7"])
            b3 = mbank()
            mm(bank(b3)[:, 0:1], ones1[0:1, :], sm[0:1, 6:7], True, True, ["gb7", "ones1"], [("ps", b3)], True)
            cp("dve", negc, bank(b3)[:, 0:1], [("ps", b3)], ["negc"])

        def head_sumsq(src, ncols, nh, mt, first, rk):
            act(SQ[:, 0:ncols], src, AF.Square, rk, ["SQ"])
            hd = ncols // nh
            if first:
                red(mt, SQ[:, 0:ncols].rearrange("p (h d) -> p h d", h=nh), ALU.add, ["SQ"], ["mt"])
            else:
                red(sm[:, 16:16 + nh], SQ[:, 0:ncols].rearrange("p (h d) -> p h d", h=nh), ALU.add, ["SQ"], ["hs"])
                tt(mt, mt, sm[:, 16:16 + nh], ALU.max, ["mt", "hs"], ["mt"])

        def out_proj_T(qb, nchunks):
            b = mbank()
            for c in range(nchunks):
                tr(bankb(b)[:, c * 128:(c + 1) * 128], MIXT[:, c * 128:(c + 1) * 128], identb[:],
                   ["MIXT", "identb"], [("ps", b)], inc=(c == nchunks - 1))
            cp("act", MIXTT[:, 0:nchunks * 128], bankb(b)[:, 0:nchunks * 128], [("ps", b)], ["MIXTT"])

        def out_proj_M(qb, nchunks, first):
            wv = WOUT.rearrange("p (c n) -> p c n", n=1024)
            for half in range(2):
                yb = 6 + half
                for c in range(nchunks):
                    mm(bank(yb), MIXTT[:, c * 128:(c + 1) * 128], wv[:, c, half * 512:(half + 1) * 512],
                       c == 0, c == nchunks - 1, ["MIXTT", "WOUT"], [("ps", yb)], inc=(c == nchunks - 1))
                xs = X[:, qb, half * 512:(half + 1) * 512]
                if first:
                    stt(xs, xs, ALPHA, bank(yb), ALU.mult, ALU.add, [("X", qb), ("ps", yb)], [("X", qb)])
                else:
                    tt(xs, xs, bank(yb), ALU.add, [("X", qb), ("ps", yb)], [("X", qb)])

        def out_proj_partial(qb, nchunks, first, after=None):
            bg.append(lambda: out_proj_T(qb, nchunks))

            def part2():
                out_proj_M(qb, nchunks, first)
                if after is not None:
                    after(qb)
            bg.append(part2)

        def dbg_store(qb, c0, ncols):
            if dbg_d is None:
                return
            cp("dve", ROPET[:, 0:ncols], MIXT[:, 0:ncols], ["MIXT"], ["ropetmp"])
            P.dma("sp", tm(dbg_d)[:, qb, c0:c0 + ncols], ROPET[:, 0:ncols], reads=["ropetmp"], final=True)

        def ln_stats(t, k):
            o = 32 + 16 * k
            ks = "ln%d" % k
            P.op("dve", lambda e, o_=sm[:, o:o + 6], i=X[:, t, 0:512]: e.bn_stats(out=o_, in_=i), reads=[("X", t)], writes=[ks + "a"])
            P.op("dve", lambda e, o_=sm[:, o + 6:o + 12], i=X[:, t, 512:1024]: e.bn_stats(out=o_, in_=i), reads=[("X", t)], writes=[ks + "b"])
            P.op("dve", lambda e, o_=sm[:, o + 12:o + 14], i=sm[:, o:o + 12]: e.bn_aggr(out=o_, in_=i), reads=[ks + "a", ks + "b"], writes=[ks + "mv"])
            act(sm[:, o + 14:o + 15], sm[:, o + 13:o + 14], AF.Ln, [ks + "mv"], [ks + "lv"], bias=1e-5)
            act(sm[:, o + 15:o + 16], sm[:, o + 14:o + 15], AF.Exp, [ks + "lv"], [ks + "rs"], scale=-0.5)

        def ln_apply(t, k):
            o = 32 + 16 * k
            ks = "ln%d" % k
            xs = X[:, t, :]
            ts(xs, xs, sm[:, o + 12:o + 13], sm[:, o + 15:o + 16], ALU.subtract, ALU.mult, [("X", t), ks + "mv", ks + "rs"], [("X", t)])
            tt(xs, xs, lnp[:, 0, :], ALU.mult, [("X", t), "lnp"], [("X", t)], eng=LNP_ENG)
            tt(xs, xs, lnp[:, 1, :], ALU.add, [("X", t), "lnp"], [("X", t)], eng=LNP_ENG)

        def layer_norm(t, k=0):
            ln_stats(t, k)
            ln_apply(t, k)

        def load_ln(g_d, b_d, l):
            P.dma("sp", lnp[:, 0, :], g_d[l].partition_broadcast(128), writes=["lnp"])
            P.dma("sp", lnp[:, 1, :], b_d[l].partition_broadcast(128), writes=["lnp"])

        def phase_A(l):
            A.off = phase_base
            FM = A.bf16(6 * S).rearrange("p (c n) -> p c n", c=6)
            VA = A.bf16(NT * 2 * 65).rearrange("p (t g d) -> p t g d", t=NT, g=2)
            mt = A.f32(16)[:, 0:10]
            negc = A.f32(2)[:, 0:1]
            esink = A.f32(8)
            den = A.f32(8)
            wv = WIN[:, 0:8 * 768].rearrange("p (c n) -> p c n", c=8)
            P.dma("pool", wv, w_in_d[l].rearrange("(c p) n -> p c n", p=128)[:, :, 0:768], writes=["WIN"])
            P.dma("pool", WOUT[:, 0:4096].rearrange("p (c n) -> p c n", c=4),
                  w_out_d[l, 0:512, :].rearrange("(c p) n -> p c n", p=128), writes=["WOUT"])
            P.dma("sp", sinks[:], sinks_d[l].partition_broadcast(128), writes=["sinks"])
            memset(VA[:, :, :, 64:65], 1.0, ["VAones"])
            def Pf(t):
                in_proj(t, 768, wv, HSB[t % 2])

            def Rf(t):
                hs = HSB[t % 2]
                qk = QKR[t % 2]
                hk = "HSB%d" % (t % 2)
                qkk = "QKR%d" % (t % 2)
                head_sumsq(hs[:, 0:640], 640, 10, mt, t == 0, [hk])
                rope(hs[:, 0:512].rearrange("p (h t d) -> p h t d", h=8, t=2),
                     qk[:, 0:512].rearrange("p (h t d) -> p h t d", h=8, t=2),
                     8, 64, t, cos64, sin64, nsin64, [hk], [qkk])
                kdst = qk[:, 512:768].rearrange("p (g r d) -> p g r d", g=2, r=2)
                rope(hs[:, 512:640].rearrange("p (h t d) -> p h t d", h=2, t=2),
                     kdst[:, :, 0, :].rearrange("p g (t d) -> p g t d", t=2),
                     2, 64, t, cos64, sin64, nsin64, [hk], [qkk])
                cp("dve", kdst[:, :, 1, :], kdst[:, :, 0, :], [qkk], [qkk])
                cp("act", VA[:, t, :, 0:64], hs[:, 640:768].rearrange("p (g d) -> p g d", g=2), [hk], [("VA", t)])

            def Rb(t):
                qk = QKR[t % 2]
                qkk = "QKR%d" % (t % 2)
                for part, (c0, nchk) in enumerate(((0, 4), (4, 2))):
                    b = mbank()
                    for j in range(nchk):
                        c = c0 + j
                        tr(bank(b)[:, j * 128:(j + 1) * 128], qk[:, c * 128:(c + 1) * 128], identf[:],
                           [qkk, "identf"], [("ps", b)], inc=(j == nchk - 1))
                    cp("act" if part == 0 else "dve", FM[:, c0:c0 + nchk, tcols(t)],
                       bank(b)[:, 0:nchk * 128].rearrange("p (c n) -> p c n", c=nchk),
                       [("ps", b)], [("FM", t)])

            run_stages(Pf, [Rf, Rb])
            if stop_after == "A1":
                return
            global_bound(mt, 10, slice(0, 8), slice(8, 10), SC64, negc)
            act(esink, sinks[:], AF.Exp, ["sinks", "negc"], ["esink"], bias=negc)
            if stop_after == "A2":
                return

            def finA(qb, g):
                def fn():
                    ob = 4 + g
                    o3 = bank(ob)[:, 0:260].rearrange("p (h d) -> p h d", h=4)
                    tt(den[:, 4 * g:4 * g + 4], o3[:, :, 64], esink[:, 4 * g:4 * g + 4], ALU.add,
                       [("ps", ob), "esink"], ["den"])
                    recip(den[:, 4 * g:4 * g + 4], den[:, 4 * g:4 * g + 4], ["den"], ["den"])
                    tt(MIXT[:, g * 256:(g + 1) * 256].rearrange("p (h d) -> p h d", h=4), o3[:, :, 0:64],
                       den[:, 4 * g:4 * g + 4].unsqueeze(2).to_broadcast([128, 4, 64]), ALU.mult,
                       [("ps", ob), "den"], ["MIXT"])
                    if g == 1:
                        dbg_store(qb, 0, 512)
                        out_proj_partial(qb, 4, True)
                return fn

            seq = []
            for qb in range(NT):
                kts = [kt for kt in (qb - 1, qb) if kt >= 0]
                nk_ = len(kts)
                for g in range(2):
                    for par in range(2):
                        po = 64 * par
                        slots = []
                        for h in (4 * g + par, 4 * g + par + 2):
                            for kt in kts:
                                slots.append({"K": (FM[po:po + 64, 4 + g, tcols(kt)], [("FM", kt)]),
                                              "Q": (FM[po:po + 64, h // 2, tcols(qb)], [("FM", qb)]),
                                              "V": (VA[:, kt, g, :], [("VA", kt), "VAones"]),
                                              "ob": 4 + g, "oreg": (h % 4) * 65,
                                              "start": kt == kts[0], "stop": kt == kts[-1]})
                        if nk_ == 2:
                            masks = [(0, 512, ("p (h n) -> p h n", {"h": 2}),
                                      maskPC[:].unsqueeze(1).to_broadcast([128, 2, 256]), ["maskPC"])]
                        else:
                            masks = [(0, 256, ("p (h n) -> p h n", {"h": 2}),
                                      maskPC[:, 128:256].unsqueeze(1).to_broadcast([128, 2, 128]), ["maskPC"])]
                        seq.append(({"kind": "att", "slots": slots, "scale": SC64, "negc": negc, "masks": masks,
                                     "fin": finA(qb, g) if par == 1 else None}, []))
            run_seq(seq, bg_per_iter=1)

        def phase_B(l):
            A.off = phase_base
            FM = A.bf16(6 * S).rearrange("p (c n) -> p c n", c=6)
            VB = A.bf16(NT * 65 + 1)[:, 0:NT * 65].rearrange("p (t d) -> p t d", t=NT)
            SCO = A.f32(S)
            MSK = A.bf16(S)
            RB = [HSB[0][:, 0:512], HSB[1][:, 0:512]]
            mt = A.f32(16)[:, 0:5]
            negc = A.f32(2)[:, 0:1]
            bis = A.f32(8)
            btab = A.f32(32)
            wv = WIN[:, 0:8 * 708].rearrange("p (c n) -> p c n", c=8)
            P.dma("pool", wv, w_in_d[l].rearrange("(c p) n -> p c n", p=128)[:, :, 768:1476], writes=["WIN"])
            P.dma("pool", WOUT[:, 0:2048].rearrange("p (c n) -> p c n", c=2),
                  w_out_d[l, 512:768, :].rearrange("(c p) n -> p c n", p=128), writes=["WOUT"])
            memset(VB[:, :, 64:65], 1.0, ["VBones"])
            def Pf(t):
                in_proj(t, 708, wv, HSB[t % 2])

            def Rf(t):
                hs = HSB[t % 2]
                qk = QKR[t % 2]
                hk = "HSB%d" % (t % 2)
                qkk = "QKR%d" % (t % 2)
                head_sumsq(hs[:, 0:320], 320, 5, mt, t == 0, [hk])
                for (s0, d0) in ((0, 0), (384, 384)):
                    rope(hs[:, s0:s0 + 320].rearrange("p (h t d) -> p h t d", h=5, t=2),
                         qk[:, d0:d0 + 320].rearrange("p (h t d) -> p h t d", h=5, t=2),
                         5, 64, t, cos64, sin64, nsin64, [hk], [qkk])
                    cp("dve", qk[:, d0 + 320:d0 + 384], qk[:, d0 + 256:d0 + 320], [qkk], [qkk])
                cp("act", VB[:, t, 0:64], hs[:, 320:384], [hk], [("VB", t)])
                ts(widx[:, t, :], hs[:, 704:708], IDXW, None, ALU.mult, None, [hk], [("widx", t)])

            def Rb(t):
                qk = QKR[t % 2]
                qkk = "QKR%d" % (t % 2)
                for part, (c0, nchk) in enumerate(((0, 4), (4, 2))):
                    b = mbank()
                    for j in range(nchk):
                        c = c0 + j
                        tr(bank(b)[:, j * 128:(j + 1) * 128], qk[:, c * 128:(c + 1) * 128], identf[:],
                           [qkk, "identf"], [("ps", b)], inc=(j == nchk - 1))
                    cp("act" if part == 0 else "dve", FM[:, c0:c0 + nchk, tcols(t)],
                       bank(b)[:, 0:nchk * 128].rearrange("p (c n) -> p c n", c=nchk),
                       [("ps", b)], [("FM", t)])

            run_stages(Pf, [Rf, Rb])
            global_bound(mt, 5, slice(0, 4), slice(4, 5), SC64, negc)

            P.barrier()
            SCOs = [SCO, arena[:, 0:S]]
            a_qkr = phase_qkr_off
            MSKT = [arena[:, a_qkr + 1024 * i:a_qkr + 1024 * (i + 1)].bitcast(BF16).rearrange("p (t n) -> p t n", t=NT)
                    for i in range(2)]

            def idx_items(qb):
                nk = (qb + 1) * 128
                par = qb % 2
                out = []
                for k0 in range(0, nk, 512):
                    n = min(512, nk - k0)
                    for h in range(4):
                        po = 64 * (h % 2)
                        ri = (len(out)) % 2
                        out.append({"kind": "idx", "n": n, "h": h, "qb": qb, "k0": k0, "ri": ri, "rb": RB[ri],
                                    "lhsT": FM[po:po + 64, 3 + h // 2, tcols(qb)], "rhs": FM[po:po + 64, 5, k0:k0 + n],
                                    "keys": [("FM", t) for t in range(qb + 1)],
                                    "sco": SCOs[par], "scokey": "SCO%d" % par})
                return out

            def bis_closures(qb):
                nk = (qb + 1) * 128
                par = qb % 2
                sco = SCOs[par]
                sk = "SCO%d" % par
                cl = []

                def init():
                    if qb >= 2:
                        red(bis[:, 0:1], sco[:, 0:nk], ALU.max, [sk], ["bnd"], absv=True)
                    tt(sco[:, qb * 128:nk], sco[:, qb * 128:nk], negmask[:], ALU.add, [sk, "negmask"], [sk])
                    if qb >= 2:
                        ts(bis[:, 2:3], bis[:, 0:1], 2.0, 2.0, ALU.mult, ALU.add, ["bnd"], ["w0"])
                        ts(btab[:], fvec[:], bis[:, 2:3], None, ALU.mult, None, ["w0", "fvec"], ["btab"])
                        memset(bis[:, 3:4], 0.0, ["mid"])
                    else:
                        ts(MSK[:, 0:nk], sco[:, 0:nk], -1.0e29, None, ALU.is_ge, None, [sk], ["MSK"])
                cl.append(init)
                if qb >= 2:
                    def it_fn(k):
                        def fn():
                            ts(MSK[:, 0:nk], sco[:, 0:nk], bis[:, 3:4], None, ALU.is_ge, ALU.add, [sk, "mid"],
                               ["MSK", "cnt"], accum=bis[:, 4:5])
                            if k < NBIS - 1:
                                ts(bis[:, 5:6], bis[:, 4:5], float(TOPK), btab[:, 16 + k + 1:16 + k + 2], ALU.is_ge, ALU.mult,
                                   ["cnt", "btab"], ["stp"])
                                stt(bis[:, 3:4], bis[:, 5:6], btab[:, k + 1:k + 2], bis[:, 3:4], ALU.subtract, ALU.add,
                                    ["stp", "btab", "mid"], ["mid"])
                            else:
                                ts(bis[:, 5:6], bis[:, 4:5], float(TOPK), btab[:, k:k + 1], ALU.is_ge, ALU.mult,
                                   ["cnt", "btab"], ["stp"])
                                stt(bis[:, 1:2], bis[:, 5:6], btab[:, k:k + 1], bis[:, 3:4], ALU.subtract, ALU.add,
                                    ["stp", "btab", "mid"], ["lo"])
                        return fn
                    for k in range(NBIS):
                        cl.append(it_fn(k))
                    cl.append(lambda: ts(MSK[:, 0:nk], sco[:, 0:nk], bis[:, 1:2], None, ALU.is_ge, None, [sk, "lo"], ["MSK"]))
                return cl

            def mskT_items(qb):
                par = qb % 2
                out = []
                for kt0 in range(0, qb + 1, 4):
                    kts = list(range(kt0, min(kt0 + 4, qb + 1)))
                    out.append({"kind": "mskT", "kts": kts, "src": MSK, "srckey": "MSK",
                                "dst": MSKT[par], "dstkey": ("MSKT", par)})
                return out

            def att_items(qb):
                par = qb % 2
                out = []
                for h in range(4):
                    po = 64 * (h % 2)
                    for kt0 in range(0, qb + 1, 4):
                        kts = list(range(kt0, min(kt0 + 4, qb + 1)))
                        slots = [{"K": (FM[po:po + 64, 2, tcols(kt)], [("FM", kt)]),
                                  "Q": (FM[po:po + 64, h // 2, tcols(qb)], [("FM", qb)]),
                                  "V": (VB[:, kt, :], [("VB", kt), "VBones"]),
                                  "ob": 4 + par, "oreg": h * 65, "start": kt == 0, "stop": kt == qb} for kt in kts]
                        ns = len(kts)
                        masks = [(0, 128 * ns, ("p (c n) -> p c n", {"c": ns}), MSKT[par][:, kt0:kt0 + ns, :], [("MSKT", par)])]
                        last = (h == 3 and kts[-1] == qb)
                        out.append({"kind": "att", "slots": slots, "scale": SC64, "negc": negc, "masks": masks,
                                    "fin": fin_generic(qb, 4 + par, 4, 2, 512, None) if last else None})
                return out

            def merge(a, b):
                out = []
                na, nb = len(a), len(b)
                ia = ib = 0
                while ia < na or ib < nb:
                    if ib >= nb or (ia < na and ia * nb <= ib * na):
                        out.append(a[ia])
                        ia += 1
                    else:
                        out.append(b[ib])
                        ib += 1
                return out

            seq = []
            for r in range(-2, NT):
                ia = att_items(r) if r >= 0 else []
                ii = idx_items(r + 2) if r + 2 < NT else []
                cl = bis_closures(r + 1) if 0 <= r + 1 < NT else []
                im = mskT_items(r + 1) if 0 <= r + 1 < NT else []
                ents = [[it, []] for it in merge(ia, ii)]
                if not ents:
                    ents = [[None, []]]
                ne = len(ents)
                for ci, c in enumerate(cl):
                    ents[min(ne - 1, (ci * ne) // max(1, len(cl)))][1].append(c)
                seq.extend((e[0], e[1]) for e in ents)
                seq.extend((it, []) for it in im)
            run_seq(seq, bg_per_iter=1)

        def phase_C(l):
            A.off = phase_base
            FQ = A.bf16(4 * S).rearrange("p (c n) -> p c n", c=4)
            FK = A.bf16(4 * S).rearrange("p (c n) -> p c n", c=4)
            VC = A.bf16(NT * 4 * 65).rearrange("p (t h d) -> p t h d", t=NT, h=4)
            QCF = [QKR[0][:, 0:384], QKR[0][:, 384:768]]
            KCF = [QKR[1][:, 0:384], QKR[1][:, 384:768]]
            CQN = [A.bf16(256), A.bf16(256)]
            CKN = [A.bf16(128), A.bf16(128)]
            CQT = [A.bf16(256), A.bf16(256)]
            CKT = [A.bf16(128), A.bf16(128)]
            mt = A.f32(16)[:, 0:8]
            negc = A.f32(2)[:, 0:1]
            rt = A.f32(128)
            rtk = [A.f32(32), A.f32(32)]
            SQJ = MIXT_f32
            wv = WIN[:, 0:8 * 416].rearrange("p (c n) -> p c n", c=8)
            WUQ = WIN[:, 8 * 416:8 * 416 + 768].rearrange("p (c n) -> p c n", c=2)
            WUKV = WIN[:, 8 * 416 + 768:8 * 416 + 768 + 512]
            P.dma("pool", wv, w_in_d[l].rearrange("(c p) n -> p c n", p=128)[:, :, 1476:1892], writes=["WIN"])
            P.dma("pool", WUQ, w_uq_d[l].rearrange("(c p) n -> p c n", p=128), writes=["WIN"])
            P.dma("pool", WUKV, w_ukv_d[l], writes=["WIN"])
            P.dma("pool", WOUT[:, 0:2048].rearrange("p (c n) -> p c n", c=2),
                  w_out_d[l, 768:1024, :].rearrange("(c p) n -> p c n", p=128), writes=["WOUT"])
            P.dma("sp", gq[:], gq_d[l].partition_broadcast(128), writes=["gq"])
            P.dma("sp", gkv[:], gkv_d[l].partition_broadcast(128), writes=["gkv"])
            load_ln(ln1g_d, ln1b_d, l)
            memset(VC[:, :, :, 64:65], 1.0, ["VCones"])
            def Pf(t):
                in_proj(t, 416, wv, HSB[t % 2])

            def c1(t):
                p = t % 2
                hs = HSB[p]
                hk = "HSB%d" % p
                o = 80 + 8 * p
                ck = "c1s%d" % p
                act(SQJ[:, 0:256], hs[:, 0:256], AF.Square, [hk], ["SQJ"], accum=sm[:, o:o + 1])
                act(SQJ[:, 256:384], hs[:, 256:384], AF.Square, [hk], ["SQJ2"], accum=sm[:, o + 1:o + 2])
                act(sm[:, o + 2:o + 3], sm[:, o:o + 1], AF.Ln, ["SQJ"], [ck + "l1"], scale=1.0 / 256, bias=1e-6)
                act(sm[:, o + 3:o + 4], sm[:, o + 1:o + 2], AF.Ln, ["SQJ2"], [ck + "l2"], scale=1.0 / 128, bias=1e-6)
                act(sm[:, o + 4:o + 5], sm[:, o + 2:o + 3], AF.Exp, [ck + "l1"], [ck + "r1"], scale=-0.5)
                act(sm[:, o + 5:o + 6], sm[:, o + 3:o + 4], AF.Exp, [ck + "l2"], [ck + "r2"], scale=-0.5)
                stt(CQN[p][:, 0:256], hs[:, 0:256], sm[:, o + 4:o + 5], gq[:], ALU.mult, ALU.mult, [hk, ck + "r1", "gq"], ["CQN%d" % p])
                stt(CKN[p][:, 0:128], hs[:, 256:384], sm[:, o + 5:o + 6], gkv[:], ALU.mult, ALU.mult, [hk, ck + "r2", "gkv"], ["CKN%d" % p])
                rope(hs[:, 384:416].rearrange("p (h t d) -> p h t d", h=1, t=2),
                     rtk[p][:, 0:32].rearrange("p (h t d) -> p h t d", h=1, t=2), 1, 32, t,
                     cos32, sin32, nsin32, [hk], ["rtk%d" % p])

            def c23(t):
                p = t % 2
                qck, kck = "QCF%d" % p, "KCF%d" % p
                b = mbank()
                tr(bankb(b)[:, 0:128], CQN[p][:, 0:128], identb[:], ["CQN%d" % p, "identb"], [("ps", b)], inc=False)
                tr(bankb(b)[:, 128:256], CQN[p][:, 128:256], identb[:], ["CQN%d" % p, "identb"], [("ps", b)], inc=False)
                tr(bankb(b)[:, 256:384], CKN[p][:, 0:128], identb[:], ["CKN%d" % p, "identb"], [("ps", b)], inc=True)
                cp("act", CQT[p][:, 0:256], bankb(b)[:, 0:256], [("ps", b)], ["CQT%d" % p])
                cp("dve", CKT[p][:, 0:128], bankb(b)[:, 256:384], [("ps", b)], ["CKT%d" % p])
                bq = mbank()
                mm(bank(bq)[:, 0:384], CQT[p][:, 0:128], WUQ[:, 0, :], True, False, ["CQT%d" % p, "WIN"], [("ps", bq)], False)
                mm(bank(bq)[:, 0:384], CQT[p][:, 128:256], WUQ[:, 1, :], False, True, ["CQT%d" % p, "WIN"], [("ps", bq)], True)
                bk = mbank()
                mm(bank(bk)[:, 0:512], CKT[p][:, 0:128], WUKV, True, True, ["CKT%d" % p, "WIN"], [("ps", bk)], True)
                cp("act", QCF[p], bank(bq)[:, 0:384], [("ps", bq)], [qck])
                q4 = QCF[p].rearrange("p (h d) -> p h d", h=4)
                qr = q4[:, :, 64:96].rearrange("p h (t d) -> p h t d", t=2)
                cp("dve", rt[:, 0:128].rearrange("p (h d) -> p h d", h=4), q4[:, :, 64:96], [qck], ["rt"])
                rope(rt[:, 0:128].rearrange("p (h t d) -> p h t d", h=4, t=2), qr, 4, 32, t,
                     cos32, sin32, nsin32, ["rt"], [qck])
                kv4 = bank(bk)[:, 0:512].rearrange("p (h d) -> p h d", h=4)
                k4 = KCF[p].rearrange("p (h d) -> p h d", h=4)
                cp("act", k4[:, :, 0:64], kv4[:, :, 0:64], [("ps", bk)], [kck])
                cp("dve", VC[:, t, :, 0:64], kv4[:, :, 64:128], [("ps", bk)], [("VC", t)])
                cp("dve", k4[:, :, 64:96], rtk[p][:, 0:32].unsqueeze(1).to_broadcast([128, 4, 32]), ["rtk%d" % p], [kck])
                act(SQ[:, 0:384], QCF[p], AF.Square, [qck], ["SQ"])
                act(SQ[:, 384:768], KCF[p], AF.Square, [kck], ["SQ"])
                if t == 0:
                    red(mt, SQ[:, 0:768].rearrange("p (h d) -> p h d", h=8), ALU.add, ["SQ"], ["mt"])
                else:
                    red(sm[:, 16:24], SQ[:, 0:768].rearrange("p (h d) -> p h d", h=8), ALU.add, ["SQ"], ["hs"])
                    tt(mt, mt, sm[:, 16:24], ALU.max, ["mt", "hs"], ["mt"])

            def c4(t):
                p = t % 2
                for (src_, dstF, key, fkey, eng) in ((QCF[p], FQ, "QCF%d" % p, "FQKR0", "act"), (KCF[p], FK, "KCF%d" % p, "FQKR1", "dve")):
                    b = mbank()
                    for h in range(4):
                        tr(bank(b)[0:96, h * 128:(h + 1) * 128], src_[:, h * 96:(h + 1) * 96], identf[:],
                           [key, "identf"], [("ps", b)], inc=(h == 3))
                    cp(eng, dstF[0:96, :, tcols(t)], bank(b)[0:96, :].rearrange("p (c n) -> p c n", c=4),
                       [("ps", b)], [(fkey, t)])

            run_stages(Pf, [c1, c23, c4])
            global_bound(mt, 8, slice(0, 4), slice(4, 8), SC96, negc)

            P.barrier()
            RT = arena[:, phase_qkr_off:phase_qkr_off + 2304]

            def xpose(qb, half):
                b = mbank()
                for j in range(4):
                    c = half * 4 + j
                    tr(bank(b)[:, j * 128:(j + 1) * 128], X[:, qb, c * 128:(c + 1) * 128], identf[:],
                       [("X", qb), "identf"], [("ps", b)], inc=(j == 3))
                cp("act", xT[:, half * 4:half * 4 + 4, tcols(qb)], bank(b).rearrange("p (c n) -> p c n", c=4),
                   [("ps", b)], [("xT", qb)])
                cp("dve", HSB[half][:, 0:512], bank(b), [("ps", b)], ["HSB%d" % half])

            def router_a(qb):
                b = mbank()
                for c in range(8):
                    mm(bank(b)[:, 0:16], HSB[c // 4][:, (c % 4) * 128:(c % 4 + 1) * 128], wr[:, c, :], c == 0, c == 7,
                       ["HSB0", "HSB1", "wr"], [("ps", b)], inc=(c == 7))
                act(RT[:, qb * 16:(qb + 1) * 16], bank(b)[:, 0:16], AF.Exp, [("ps", b)], [("r_sc", qb)], scale=-1.0)

            def finC(qb, ob):
                def fn():
                    o3 = bank(ob)[:, 0:260].rearrange("p (h d) -> p h d", h=4)
                    rc = sm[:, 72:76]
                    recip(rc, o3[:, :, 64], [("ps", ob)], ["rc"])
                    tt(MIXT[:, 0:256].rearrange("p (h d) -> p h d", h=4), o3[:, :, 0:64],
                       rc.unsqueeze(2).to_broadcast([128, 4, 64]), ALU.mult, [("ps", ob), "rc"], ["MIXT"])
                    dbg_store(qb, 768, 256)
                    pv = qb - 1
                    if pv >= 0:
                        bg.append(lambda: xpose(pv, 0))
                    bg.append(lambda: out_proj_T(qb, 2))
                    if pv >= 0:
                        bg.append(lambda: xpose(pv, 1))
                    bg.append(lambda: out_proj_M(qb, 2, False))
                    if pv >= 0:
                        bg.append(lambda: router_a(pv))
                    bg.append(lambda: ln_stats(qb, qb % 2))
                    bg.append(lambda: ln_apply(qb, qb % 2))
                return fn

            seq = []
            for qb in range(NT):
                par = qb % 2
                for h in range(4):
                    for kt0 in range(0, qb + 1, 4):
                        kts = list(range(kt0, min(kt0 + 4, qb + 1)))
                        slots = [{"K": (FK[0:96, h, tcols(kt)], [("FQKR1", kt)]),
                                  "Q": (FQ[0:96, h, tcols(qb)], [("FQKR0", qb)]),
                                  "V": (VC[:, kt, h, :], [("VC", kt), "VCones"]),
                                  "ob": 4 + par, "oreg": h * 65, "start": kt == 0, "stop": kt == qb} for kt in kts]
                        ns = len(kts)
                        masks = []
                        if kts[-1] == qb:
                            masks = [(128 * (ns - 1), 128 * ns, None, causalT[:], ["causalT"])]
                        last = (h == 3 and kts[-1] == qb)
                        seq.append(({"kind": "att", "slots": slots, "scale": SC96, "negc": negc, "masks": masks,
                                     "fin": finC(qb, 4 + par) if last else None}, []))
            run_seq(seq, bg_per_iter=2)
            xpose(NT - 1, 0)
            xpose(NT - 1, 1)
            router_a(NT - 1)

            sc = RT[:, 0:256]
            bi = RT[:, 256:512]
            m1 = RT[:, 512:576]
            eq = RT[:, 576:832]
            msk = RT[:, 832:1088]
            m2 = RT[:, 1088:1152]
            gs = RT[:, 1152:1216]
            gm = RT[:, 1216:1232]
            gsel = RT[:, 1232:1296]
            t2 = RT[:, 1296:1552]
            wgt = RT[:, 1552:1808]
            ws = RT[:, 1808:1824]
            rsk = [("r_sc", t) for t in range(NT)]

            def v3(ap, a):
                return ap.rearrange("p (a b) -> p a b", a=a)

            ts(sc, sc, 1.0, None, ALU.add, None, rsk, ["r_s"])
            recip(sc, sc, ["r_s"], ["r_s"])
            tt(v3(bi, 16), v3(sc, 16), rb[:].unsqueeze(1).to_broadcast([128, 16, 16]), ALU.add, ["r_s", "rb"], ["r_bi"])
            red(m1, v3(bi, 64), ALU.max, ["r_bi"], ["r_m1"])
            tt(v3(eq, 64), v3(bi, 64), m1.unsqueeze(2).to_broadcast([128, 64, 4]), ALU.is_equal, ["r_bi", "r_m1"], ["r_eq"])
            stt(msk, eq, NEG, bi, ALU.mult, ALU.add, ["r_eq", "r_bi"], ["r_msk"])
            red(m2, v3(msk, 64), ALU.max, ["r_msk"], ["r_m2"])
            tt(gs, m1, m2, ALU.add, ["r_m1", "r_m2"], ["r_gs"])
            red(gm, v3(gs, 16), ALU.max, ["r_gs"], ["r_gm"])
            tt(v3(gsel, 16), v3(gs, 16), gm.unsqueeze(2).to_broadcast([128, 16, 4]), ALU.is_equal, ["r_gs", "r_gm"], ["r_gsel"])
            tt(v3(t2, 64), v3(bi, 64), m2.unsqueeze(2).to_broadcast([128, 64, 4]), ALU.is_ge, ["r_bi", "r_m2"], ["r_t2"])
            tt(v3(t2, 64), v3(t2, 64), gsel.unsqueeze(2).to_broadcast([128, 64, 4]), ALU.mult, ["r_t2", "r_gsel"], ["r_t2"])
            tt(wgt, sc, t2, ALU.mult, ["r_s", "r_t2"], ["r_w"])
            red(ws, v3(wgt, 16), ALU.add, ["r_w"], ["r_ws"])
            recip(ws, ws, ["r_ws"], ["r_ws"])
            tt(gates[:], v3(wgt, 16), ws.unsqueeze(2).to_broadcast([128, 16, 16]), ALU.mult, ["r_w", "r_ws"],
               [("gates", t) for t in range(NT)])

        def phase_moe(l, last_layer):
            A.off = 0
            NSLOT = 7
            WGU = [A.bf16(8 * 512).rearrange("p (c n) -> p c n", c=8) for _ in range(NSLOT)]
            WD = [A.bf16(2 * 1024).rearrange("p (c n) -> p c n", c=2) for _ in range(NSLOT)]
            SB = [A.bf16(256) for _ in range(2)]
            TB = [A.bf16(256) for _ in range(2)]
            TTB = [A.bf16(256) for _ in range(2)]
            load_ln(ln2g_d, ln2b_d, l)
            def load_expert(e):
                s = e % NSLOT
                P.dma("pool", WGU[s][:, :, 0:256], w_gate_d[l, e].rearrange("(c p) f -> p c f", p=128), writes=[("WGU", s)])
                P.dma("pool", WGU[s][:, :, 256:512], w_up_d[l, e].rearrange("(c p) f -> p c f", p=128), writes=[("WGU", s)])
                P.dma("pool", WD[s], w_down_d[l, e].rearrange("(c p) d -> p c d", p=128), writes=[("WD", s)])

            for e in range(NSLOT):
                load_expert(e)
            items = [(0, t, e) for e in range(4) for t in range(NT)]
            items += [(G, t, e) for G in range(1, 4) for t in range(NT) for e in range(4 * G, 4 * G + 4)]
            sd = {}
            cnt = [0]

            def s1(it):
                G, t, e = it
                s = e % NSLOT
                b = rbank()
                sd[it] = {"b": b, "i": cnt[0] % 2}
                cnt[0] += 1
                for c in range(8):
                    mm(bank(b), xT[:, c, tcols(t)], WGU[s][:, c, :], c == 0, c == 7,
                       [("xT", t), ("WGU", s)], [("ps", b)], inc=(c == 7))

            def s2(it):
                G, t, e = it
                d = sd[it]
                b, i = d["b"], d["i"]
                act(SB[i][:, 0:256], bank(b)[:, 0:256], AF.Silu, [("ps", b)], [("SB", i)])
                stt(TB[i][:, 0:256], bank(b)[:, 256:512], gates[:, t, e:e + 1], SB[i][:, 0:256], ALU.mult, ALU.mult,
                    [("ps", b), ("gates", t), ("SB", i)], [("TB", i)])
                b2 = rbank()
                d["b2"] = b2
                for fc in range(2):
                    tr(bankb(b2)[:, fc * 128:(fc + 1) * 128], TB[i][:, fc * 128:(fc + 1) * 128], identb[:],
                       [("TB", i), "identb"], [("ps", b2)], inc=(fc == 1))

            def s3(it):
                G, t, e = it
                d = sd[it]
                b2, i = d["b2"], d["i"]
                s = e % NSLOT
                cp("act", TTB[i][:, 0:256], bankb(b2)[:, 0:256], [("ps", b2)], [("TTB", i)])
                first = (e % 4 == 0) or G == 0
                lastx = (e % 4 == 3) or G == 0
                for half in range(2):
                    yb = 4 + 2 * (t % 2) + half
                    for fc in range(2):
                        mm(bank(yb), TTB[i][:, fc * 128:(fc + 1) * 128], WD[s][:, fc, half * 512:(half + 1) * 512],
                           first and fc == 0, lastx and fc == 1, [("TTB", i), ("WD", s)], [("ps", yb)],
                           inc=(fc == 1))
                if t == NT - 1 and e + NSLOT < 16:
                    load_expert(e + NSLOT)
                if lastx:
                    for half in range(2):
                        yb = 4 + 2 * (t % 2) + half
                        xs = X[:, t, half * 512:(half + 1) * 512]
                        if e == 0:
                            stt(xs, xs, ALPHA, bank(yb), ALU.mult, ALU.add, [("X", t), ("ps", yb)], [("X", t)])
                        else:
                            tt(xs, xs, bank(yb), ALU.add, [("X", t), ("ps", yb)], [("X", t)])
                    if G == 3:
                        k = t % 2
                        o = 32 + 16 * k
                        mvo = 96 + 2 * t

                        def st_a(t=t, o=o, k=k):
                            P.op("dve", lambda e, o_=sm[:, o:o + 6], i=X[:, t, 0:512]: e.bn_stats(out=o_, in_=i),
                                 reads=[("X", t)], writes=["bs%da" % k])

                        def st_b(t=t, o=o, k=k, mvo=mvo):
                            P.op("dve", lambda e, o_=sm[:, o + 6:o + 12], i=X[:, t, 512:1024]: e.bn_stats(out=o_, in_=i),
                                 reads=[("X", t)], writes=["bs%db" % k])
                            P.op("dve", lambda e, o_=sm[:, mvo:mvo + 2], i=sm[:, o:o + 12]: e.bn_aggr(out=o_, in_=i),
                                 reads=["bs%da" % k, "bs%db" % k], writes=[("mv2", t)])

                        bg.append(st_a)
                        bg.append(st_b)
                        if t % 8 == 7:
                            t0 = t - 7

                            def rstd8(t0=t0):
                                var8 = sm[:, 96 + 2 * t0:96 + 2 * t0 + 16].rearrange("p (t two) -> p t two", two=2)[:, :, 1]
                                rs8 = sm[:, 128 + t0:128 + t0 + 8]
                                act(rs8, var8, AF.Ln, [("mv2", u) for u in range(t0, t0 + 8)], [("rs2", t0)], bias=1e-5)
                                act(rs8, rs8, AF.Exp, [("rs2", t0)], [("rs2", t0)], scale=-0.5)
                            bg.append(rstd8)
                            for u in range(t0, t0 + 8):
                                for half in range(2):
                                    hsl = slice(half * 512, (half + 1) * 512)

                                    def ap1(u=u, t0=t0, hsl=hsl):
                                        xs = X[:, u, hsl]
                                        ts(xs, xs, sm[:, 96 + 2 * u:96 + 2 * u + 1], sm[:, 128 + u:128 + u + 1], ALU.subtract, ALU.mult,
                                           [("X", u), ("mv2", u), ("rs2", t0)], [("X", u)])

                                    def ap2(u=u, hsl=hsl):
                                        xs = X[:, u, hsl]
                                        tt(xs, xs, lnp[:, 0, hsl], ALU.mult, [("X", u), "lnp"], [("X", u)])

                                    def ap3(u=u, hsl=hsl, half=half):
                                        xs = X[:, u, hsl]
                                        tt(xs, xs, lnp[:, 1, hsl], ALU.add, [("X", u), "lnp"], [("X", u)])
                                        if last_layer and half == 1:
                                            P.dma("sp", tm(out_d)[:, u, :], X[:, u, :], reads=[("X", u)], final=True)
                                    bg.append(ap1)
                                    bg.append(ap2)
                                    bg.append(ap3)

            pipeline(items, s1, s2, s3, bg_per_iter=2)

        def run_layers():
            for l in range(nlayers):
                for t in range(NT):
                    build_xT(t)
                if stop_after == "xT":
                    return False
                phase_A(l)
                P.barrier()
                if stop_after in ("A", "A1", "A2"):
                    return False
                phase_B(l)
                P.barrier()
                if stop_after == "B":
                    return False
                phase_C(l)
                P.barrier()
                if stop_after == "C":
                    return False
                phase_moe(l, l == nlayers - 1)
                P.barrier()
            return True

        if not run_layers():
            for t in range(NT):
                P.dma("sp", tm(out_d)[:, t, :], X[:, t, :], reads=[("X", t)], final=True)

        P.emit(nc, sems)
    return nc


_CACHE = {}


def kernel(**inputs):
    consts = _consts()
    if "nc" not in _CACHE:
        _CACHE["nc"] = build_program()
    nc = _CACHE["nc"]
    x = np.ascontiguousarray(np.asarray(inputs["x"], dtype=np.float32))
    shared = {}
    for k, v in inputs.items():
        if k == "x":
            continue
        shared[k] = np.ascontiguousarray(np.asarray(v, dtype=np.float32))
    shared.update(consts)
    in_maps = []
    for c in range(NCORES):
        m = dict(shared)
        m["x"] = x[c]
        in_maps.append(m)
    res = run_bass_kernel_spmd(nc, in_maps, core_ids=list(range(NCORES)))
    out = np.stack([np.asarray(r["out"], dtype=np.float32) for r in res.results], axis=0)
    return out
```

```python
import contextlib
import numpy as np
import concourse.bass as bass
import concourse.mybir as mybir
from concourse.bass_utils import run_bass_kernel_spmd

F32 = mybir.dt.float32
BF16 = mybir.dt.bfloat16
AF = mybir.ActivationFunctionType
ALU = mybir.AluOpType
AX = mybir.AxisListType

S = 2048
D = 1024
NT = 16
NCORES = 8
DEPTH = 2
ALPHA = float((2 * DEPTH) ** 0.25)
IDXW = float((4 * 64) ** -0.5)
SC64 = float(64 ** -0.5)
SC96 = float(96 ** -0.5)
TOPK = 256
NBIS = 14
NEG = -1.0e30
NDMA_SEMS = 8
ACC_ENG = "dve"
LNP_ENG = "dve"


class Prog:
    ENGS = ("pe", "act", "dve", "pool", "sp")

    def __init__(self):
        self.ops = {e: [] for e in self.ENGS}
        self.cnt = {e: 0 for e in self.ENGS}
        self.last_w = {}
        self.readers = {}
        self.waited = {e: {} for e in self.ENGS}
        self.dma_val = {}
        self.dma_rr = {"sp": 0, "pool": 0}
        self.final_tokens = []

    def _deps(self, eng, reads, writes):
        deps = {}

        def add(tok, raw):
            src, val = tok
            if src == eng and eng == "pe":
                return
            if deps.get(src, 0) < val:
                deps[src] = val

        for k in reads:
            if k in self.last_w:
                add(self.last_w[k], True)
            if isinstance(k, tuple) and k[0] == "ps":
                for r in self.readers.get(k, ()):
                    if r[0] != eng:
                        add(r, False)
        for k in writes:
            if k in self.last_w:
                add(self.last_w[k], False)
            for r in self.readers.get(k, ()):
                add(r, False)
        waits = []
        for src, val in deps.items():
            if self.waited[eng].get(src, 0) >= val:
                continue
            self.waited[eng][src] = val
            waits.append((src, val))
        return waits

    def _record(self, tok, reads, writes):
        for k in writes:
            self.last_w[k] = tok
            self.readers[k] = []
        for k in reads:
            self.readers.setdefault(k, []).append(tok)

    def op(self, eng, fn, reads=(), writes=(), inc=True):
        waits = self._deps(eng, reads, writes)
        if inc:
            self.cnt[eng] += 1
            idx = self.cnt[eng]
        else:
            idx = self.cnt[eng] + 1
        tok = (eng, idx)
        self._record(tok, reads, writes)
        self.ops[eng].append((waits, fn, ("eng", eng) if inc else None))
        return tok

    def dma(self, q, out_ap, in_ap, reads=(), writes=(), final=False):
        i = self.dma_rr[q]
        self.dma_rr[q] = (i + 1) % NDMA_SEMS
        src = ("dma", q, i)
        prev = self.dma_val.get(src, 0)
        waits = self._deps(q, reads, writes)
        if prev and self.waited[q].get(src, 0) < prev:
            self.waited[q][src] = prev
            waits.append((src, prev))
        val = prev + 16
        self.dma_val[src] = val
        tok = (src, val)
        self._record(tok, reads, writes)

        def fn(e, out_ap=out_ap, in_ap=in_ap):
            return e.dma_start(out=out_ap, in_=in_ap)

        self.ops[q].append((waits, fn, ("dma", src)))
        if final:
            self.final_tokens.append(tok)
        return tok

    def barrier(self):
        snap = [(e, self.cnt[e]) for e in self.ENGS if self.cnt[e] > 0]
        snap += [(src, v) for src, v in self.dma_val.items()]
        for e in self.ENGS:
            waits = []
            for src, val in snap:
                if src == e:
                    continue
                if self.waited[e].get(src, 0) >= val:
                    continue
                self.waited[e][src] = val
                waits.append((src, val))
            if waits:
                self.ops[e].append((waits, None, None))

    def emit(self, nc, sems):
        fin = list(self.final_tokens)

        def replay(eng, e):
            for waits, fn, inc in self.ops[eng]:
                for src, val in waits:
                    e.wait_ge(sems[src], val)
                if fn is None:
                    continue
                ins = fn(e)
                if inc is not None:
                    if inc[0] == "eng":
                        ins.then_inc(sems[inc[1]], 1)
                    else:
                        ins.then_inc(sems[inc[1]], 16)
            if eng == "sp":
                for src, val in fin:
                    e.wait_ge(sems[src], val)

        with nc.Block() as block:
            @block.tensor
            def _(e):
                replay("pe", e)

            @block.scalar
            def _(e):
                replay("act", e)

            @block.vector
            def _(e):
                replay("dve", e)

            @block.gpsimd
            def _(e):
                replay("pool", e)

            @block.sync
            def _(e):
                replay("sp", e)


def _consts():
    pos = np.arange(S, dtype=np.float64)
    c = {}
    for dim, nm in ((64, "64"), (32, "32")):
        inv = 1.0 / (10000.0 ** (np.arange(0, dim, 2, dtype=np.float64) / dim))
        inv = inv.astype(np.float32).astype(np.float64)
        ang = (pos.astype(np.float32)[:, None] * inv.astype(np.float32)[None, :]).astype(np.float32)
        c["cos" + nm] = np.cos(ang.astype(np.float64)).astype(np.float32)
        c["sin" + nm] = np.sin(ang.astype(np.float64)).astype(np.float32)
    c["ident"] = np.eye(128, dtype=np.float32)
    fv = np.concatenate([2.0 ** -(np.arange(16) + 1.0), 2.0 ** -np.arange(16).astype(np.float64)])
    c["fvec"] = np.tile(fv.astype(np.float32)[None, :], (128, 1))
    qi = np.arange(128)[:, None]
    kj = np.arange(256)[None, :]
    diff = qi + 128 - kj
    kk = np.arange(128)[None, :]
    c["negmask"] = np.where(kk <= qi, 0.0, NEG).astype(np.float32)
    c["causalT"] = (qi <= kk).astype(np.float32)
    c["maskPC"] = np.concatenate([(qi > kk).astype(np.float32), c["causalT"]], axis=1)
    return c


def build_program(nlayers=DEPTH, debug_mix=False, stop_after=None):
    nc = bass.Bass("TRN2", target_bir_lowering=False)

    def din(name, shape):
        return nc.dram_tensor(name, list(shape), F32, kind="ExternalInput").ap()

    x_d = din("x", [S, D])
    w_in_d = din("w_in", [DEPTH, D, 1892])
    sinks_d = din("attn_sinks", [DEPTH, 8])
    gq_d = din("c_q_norm_g", [DEPTH, 256])
    gkv_d = din("c_kv_norm_g", [DEPTH, 128])
    w_uq_d = din("w_uq", [DEPTH, 256, 384])
    w_ukv_d = din("w_ukv", [DEPTH, 128, 512])
    w_out_d = din("w_out", [DEPTH, D, D])
    ln1g_d = din("ln1_g", [DEPTH, D])
    ln1b_d = din("ln1_b", [DEPTH, D])
    w_router_d = din("w_router", [D, 16])
    rbias_d = din("router_bias", [16])
    w_gate_d = din("w_gate", [DEPTH, 16, D, 256])
    w_up_d = din("w_up", [DEPTH, 16, D, 256])
    w_down_d = din("w_down", [DEPTH, 16, 256, D])
    ln2g_d = din("ln2_g", [DEPTH, D])
    ln2b_d = din("ln2_b", [DEPTH, D])
    cos64_d = din("cos64", [S, 32])
    sin64_d = din("sin64", [S, 32])
    cos32_d = din("cos32", [S, 16])
    sin32_d = din("sin32", [S, 16])
    ident_d = din("ident", [128, 128])
    fvec_d = din("fvec", [128, 32])
    negmask_d = din("negmask", [128, 128])
    causalT_d = din("causalT", [128, 128])
    maskPC_d = din("maskPC", [128, 256])
    out_d = nc.dram_tensor("out", [S, D], F32, kind="ExternalOutput").ap()
    dbg_d = None
    if debug_mix:
        dbg_d = nc.dram_tensor("dbg", [S, D], F32, kind="ExternalOutput").ap()

    P = Prog()
    st = contextlib.ExitStack()
    with st:
        sems = {}
        for e in Prog.ENGS:
            sems[e] = st.enter_context(nc.semaphore("s_" + e))
        for q in ("sp", "pool"):
            for i in range(NDMA_SEMS):
                sems[("dma", q, i)] = st.enter_context(nc.semaphore(f"d_{q}{i}"))

        def T(name, shape, dt):
            return st.enter_context(nc.sbuf_tensor("sb_" + name, list(shape), dt))

        X = T("X", [128, NT, D], F32)
        xT = T("xT", [128, 8, S], BF16)
        cos64 = T("cos64", [128, NT, 32], F32)
        sin64 = T("sin64", [128, NT, 32], F32)
        nsin64 = T("nsin64", [128, NT, 32], F32)
        cos32 = T("cos32", [128, NT, 16], F32)
        sin32 = T("sin32", [128, NT, 16], F32)
        nsin32 = T("nsin32", [128, NT, 16], F32)
        identf = T("identf", [128, 128], F32)
        fvec = T("fvec", [128, 32], F32)
        identb = T("identb", [128, 128], BF16)
        negmask = T("negmask", [128, 128], F32)
        causalT = T("causalT", [128, 128], BF16)
        maskPC = T("maskPC", [128, 256], BF16)
        ones1 = T("ones1", [1, 128], F32)
        lnp = T("lnp", [128, 2, D], F32)
        gates = T("gates", [128, NT, 16], F32)
        widx = T("widx", [128, NT, 4], F32)
        wr = T("wr", [128, 8, 16], F32)
        rb = T("rb", [128, 16], F32)
        sinks = T("sinks", [128, 8], F32)
        gq = T("gq", [128, 256], F32)
        gkv = T("gkv", [128, 128], F32)
        sm = T("sm", [128, 256], F32)
        ARW = 22400
        arena = T("arena", [128, ARW], F32)
        psum = st.enter_context(nc.psum_tensor("psum", [128, 4096], F32))

        def bank(i):
            return psum[:, 512 * i:512 * (i + 1)]

        def bankb(i):
            return psum[:, 512 * i:512 * (i + 1)].bitcast(BF16)

        class Arena:
            def __init__(self):
                self.off = 0

            def f32(self, n):
                o = self.off
                self.off += n
                assert self.off <= ARW, self.off
                return arena[:, o:o + n]

            def bf16(self, n):
                w = (n + 1) // 2
                o = self.off
                self.off += w
                assert self.off <= ARW, self.off
                return arena[:, o:o + w].bitcast(BF16)

        A = Arena()
        WIN = A.bf16(8 * 768)
        WOUT = A.bf16(4 * 1024)
        HSB = [A.f32(768), A.f32(768)]
        phase_qkr_off = A.off
        QKR = [A.f32(768), A.f32(768)]
        SQ = A.f32(768)
        PB = [A.bf16(512), A.bf16(512)]
        PTB = [A.bf16(512), A.bf16(512)]
        ROPET = A.f32(640)
        mixt_off = A.off
        MIXT = A.bf16(512)
        MIXTT = A.bf16(512)
        MIXT_f32 = arena[:, mixt_off:mixt_off + 512]
        phase_base = A.off

        rot = [0]
        rot4 = [0]
        inpipe = [False]

        def rbank():
            i = rot[0]
            rot[0] = (i + 1) % 3
            return i

        def mbank():
            if inpipe[0]:
                return 3
            i = rot4[0]
            rot4[0] = (i + 1) % 4
            return i

        def mm(out, lhsT, rhs, start, stop, reads, writes, inc):
            P.op("pe", lambda e, o=out, l=lhsT, r=rhs, s0=start, s1=stop: e.matmul(o, lhsT=l, rhs=r, start=s0, stop=s1),
                 reads=reads, writes=writes, inc=inc)

        def tr(out, in_, ident, reads, writes, inc=True):
            P.op("pe", lambda e, o=out, i=in_, d=ident: e.transpose(out=o, in_=i, identity=d),
                 reads=reads, writes=writes, inc=inc)

        def act(out, in_, func, reads, writes, bias=None, scale=None, accum=None):
            kw = {}
            if bias is not None:
                kw["bias"] = bias
            if scale is not None:
                kw["scale"] = scale
            if accum is not None:
                kw["accum_out"] = accum
            P.op("act", lambda e, o=out, i=in_, f=func, kw=kw: e.activation(out=o, in_=i, func=f, **kw),
                 reads=reads, writes=writes)

        def tt(out, in0, in1, op, reads, writes, eng="dve"):
            P.op(eng, lambda e, o=out, a=in0, b=in1, p=op: e.tensor_tensor(out=o, in0=a, in1=b, op=p),
                 reads=reads, writes=writes)

        def ts(out, in0, s1, s2, op0, op1, reads, writes, accum=None, eng="dve"):
            def fn(e, o=out, a=in0, s1=s1, s2=s2, op0=op0, op1=op1, accum=accum):
                kw = {}
                if op1 is not None:
                    kw["op1"] = op1
                if accum is not None:
                    kw["accum_out"] = accum
                return e.tensor_scalar(out=o, in0=a, scalar1=s1, scalar2=s2, op0=op0, **kw)
            P.op(eng, fn, reads=reads, writes=writes)

        def stt(out, in0, scalar, in1, op0, op1, reads, writes, eng="dve"):
            P.op(eng, lambda e, o=out, a=in0, s=scalar, b=in1, p0=op0, p1=op1:
                 e.scalar_tensor_tensor(out=o, in0=a, scalar=s, in1=b, op0=p0, op1=p1),
                 reads=reads, writes=writes)

        def cp(eng, out, in_, reads, writes):
            if eng == "act":
                act(out, in_, AF.Copy, reads, writes)
            else:
                P.op(eng, lambda e, o=out, i=in_: e.tensor_copy(out=o, in_=i), reads=reads, writes=writes)

        def red(out, in_, op, reads, writes, absv=False):
            def fn(e, o=out, i=in_, p=op, a=absv):
                if a:
                    return e.tensor_reduce(out=o, in_=i, axis=AX.X, op=p, apply_absolute_value=True)
                return e.tensor_reduce(out=o, in_=i, axis=AX.X, op=p)
            P.op("dve", fn, reads=reads, writes=writes)

        def memset(ap, val, writes, eng="dve"):
            P.op(eng, lambda e, a=ap, v=val: e.memset(a, v), writes=writes)

        def recip(out, in_, reads, writes):
            P.op("dve", lambda e, o=out, i=in_: e.reciprocal(out=o, in_=i), reads=reads, writes=writes)

        bg = []

        def bg_run(k):
            for _ in range(k):
                if not bg:
                    return
                bg.pop(0)()

        def pipeline(items, s1, s2, s3, bg_per_iter=0):
            n = len(items)
            inpipe[0] = True
            for i in range(n + 2):
                if i < n:
                    s1(items[i])
                if 0 <= i - 1 < n:
                    s2(items[i - 1])
                if 0 <= i - 2 < n:
                    s3(items[i - 2])
                if bg_per_iter:
                    bg_run(bg_per_iter)
            bg_run(len(bg))
            inpipe[0] = False

        def run_tiles(Pf, Rf):
            Pf(0)
            for t in range(NT):
                if t + 1 < NT:
                    Pf(t + 1)
                Rf(t)

        def run_stages(Pf, stages):
            ns = len(stages)
            Pf(0)
            for i in range(NT + ns - 1):
                if i + 1 < NT:
                    Pf(i + 1)
                for k, st_ in enumerate(stages):
                    if 0 <= i - k < NT:
                        st_(i - k)

        def tcols(t):
            return slice(t * 128, (t + 1) * 128)

        PBs = [PB[0], PB[1], PTB[0]]
        pbrot = [0]

        def st1(it):
            b = rbank()
            it["b"] = b
            k = it["kind"]
            if k == "att":
                it["pi"] = pbrot[0]
                pbrot[0] = (pbrot[0] + 1) % 3
                sl = it["slots"]
                for j, s in enumerate(sl):
                    ka, kk = s["K"]
                    qa, qk = s["Q"]
                    mm(bank(b)[:, j * 128:(j + 1) * 128], ka, qa, True, True, kk + qk, [("ps", b)], inc=(j == len(sl) - 1))
            elif k == "idx":
                mm(bank(b)[:, 0:it["n"]], it["lhsT"], it["rhs"], True, True, it["keys"], [("ps", b)], True)
            elif k == "mskT":
                kts = it["kts"]
                for j, kt in enumerate(kts):
                    tr(bankb(b)[:, j * 128:(j + 1) * 128], it["src"][:, kt * 128:(kt + 1) * 128], identb[:],
                       [it["srckey"], "identb"], [("ps", b)], inc=(j == len(kts) - 1))

        def st2(it):
            b = it["b"]
            k = it["kind"]
            if k == "att":
                pi = it["pi"]
                n = 128 * len(it["slots"])
                act(PBs[pi][:, 0:n], bank(b)[:, 0:n], AF.Exp, [("ps", b), "negc"], [("PB", pi)], bias=it["negc"], scale=it["scale"])
                for (c0, c1, view, m_ap, mk) in it["masks"]:
                    pv = PBs[pi][:, c0:c1]
                    if view is not None:
                        pv = pv.rearrange(view[0], **view[1])
                    tt(pv, pv, m_ap, ALU.mult, [("PB", pi)] + mk, [("PB", pi)])
            elif k == "idx":
                ri, n, h, qb, k0 = it["ri"], it["n"], it["h"], it["qb"], it["k0"]
                sco, sk = it["sco"], it["scokey"]
                act(it["rb"][:, 0:n], bank(b)[:, 0:n], AF.Relu, [("ps", b)], ["RB%d" % ri])
                if h == 0:
                    ts(sco[:, k0:k0 + n], it["rb"][:, 0:n], widx[:, qb, 0:1], None, ALU.mult, None,
                       ["RB%d" % ri, ("widx", qb)], [sk], eng=ACC_ENG)
                elif ACC_ENG == "dve":
                    stt(sco[:, k0:k0 + n], it["rb"][:, 0:n], widx[:, qb, h:h + 1], sco[:, k0:k0 + n],
                        ALU.mult, ALU.add, ["RB%d" % ri, ("widx", qb), sk], [sk])
                else:
                    ts(it["rb"][:, 0:n], it["rb"][:, 0:n], widx[:, qb, h:h + 1], None, ALU.mult, None,
                       ["RB%d" % ri, ("widx", qb)], ["RB%d" % ri], eng=ACC_ENG)
                    tt(sco[:, k0:k0 + n], sco[:, k0:k0 + n], it["rb"][:, 0:n], ALU.add, ["RB%d" % ri, sk], [sk], eng=ACC_ENG)
            elif k == "mskT":
                kts = it["kts"]
                nj = len(kts)
                cp("act", it["dst"][:, kts[0]:kts[0] + nj, :],
                   bankb(b)[:, 0:nj * 128].rearrange("p (c n) -> p c n", c=nj), [("ps", b)], [it["dstkey"]])

        def st3(it):
            if it["kind"] != "att":
                return
            pi = it["pi"]
            sl = it["slots"]
            for j, s in enumerate(sl):
                va, vk = s["V"]
                ob = s["ob"]
                mm(bank(ob)[:, s["oreg"]:s["oreg"] + 65], PBs[pi][:, j * 128:(j + 1) * 128], va, s["start"], s["stop"],
                   [("PB", pi)] + vk, [("ps", ob)], inc=(j == len(sl) - 1 or sl[j + 1]["ob"] != ob))
            if it.get("fin") is not None:
                it["fin"]()

        def run_seq(seq, bg_per_iter=0):
            n = len(seq)
            inpipe[0] = True
            for i in range(n + 2):
                if i < n and seq[i][0] is not None:
                    st1(seq[i][0])
                if 0 <= i - 1 < n and seq[i - 1][0] is not None:
                    st2(seq[i - 1][0])
                if 0 <= i - 2 < n and seq[i - 2][0] is not None:
                    st3(seq[i - 2][0])
                if i < n:
                    for c in seq[i][1]:
                        c()
                if bg_per_iter:
                    bg_run(bg_per_iter)
            bg_run(len(bg))
            inpipe[0] = False

        def fin_generic(qb, ob, nheads, nchunks_w, mix_c0, epilogue):
            def fn():
                o3 = bank(ob)[:, 0:nheads * 65].rearrange("p (h d) -> p h d", h=nheads)
                rc = sm[:, 72:72 + nheads]
                recip(rc, o3[:, :, 64], [("ps", ob)], ["rc"])
                tt(MIXT[:, 0:nheads * 64].rearrange("p (h d) -> p h d", h=nheads), o3[:, :, 0:64],
                   rc.unsqueeze(2).to_broadcast([128, nheads, 64]), ALU.mult, [("ps", ob), "rc"], ["MIXT"])
                dbg_store(qb, mix_c0, nheads * 64)
                out_proj_partial(qb, nchunks_w, False, after=epilogue)
            return fn

        def tm(ap_d):
            return ap_d.rearrange("(t p) d -> p t d", p=128)

        P.dma("sp", cos64[:], tm(cos64_d), writes=["cos64"])
        P.dma("sp", sin64[:], tm(sin64_d), writes=["sin64"])
        P.dma("sp", cos32[:], tm(cos32_d), writes=["cos32"])
        P.dma("sp", sin32[:], tm(sin32_d), writes=["sin32"])
        P.dma("sp", identf[:], ident_d, writes=["identf"])
        P.dma("sp", fvec[:], fvec_d, writes=["fvec"])
        P.dma("pool", identb[:], ident_d, writes=["identb"])
        P.dma("pool", causalT[:], causalT_d, writes=["causalT"])
        P.dma("pool", maskPC[:], maskPC_d, writes=["maskPC"])
        P.dma("sp", negmask[:], negmask_d, writes=["negmask"])
        P.dma("sp", wr[:], w_router_d.rearrange("(c p) n -> p c n", p=128), writes=["wr"])
        P.dma("sp", rb[:], rbias_d.partition_broadcast(128), writes=["rb"])
        xv = tm(x_d)
        for t4 in range(4):
            P.dma("sp", X[:, 4 * t4:4 * t4 + 4, :], xv[:, 4 * t4:4 * t4 + 4, :],
                  writes=[("X", t) for t in range(4 * t4, 4 * t4 + 4)])
        ts(nsin64[:], sin64[:], -1.0, None, ALU.mult, None, ["sin64"], ["nsin64"])
        ts(nsin32[:], sin32[:], -1.0, None, ALU.mult, None, ["sin32"], ["nsin32"])
        memset(ones1[:], 1.0, ["ones1"])

        def build_xT(t):
            for half in range(2):
                b = mbank()
                for j in range(4):
                    c = half * 4 + j
                    tr(bank(b)[:, j * 128:(j + 1) * 128], X[:, t, c * 128:(c + 1) * 128], identf[:],
                       [("X", t), "identf"], [("ps", b)], inc=(j == 3))
                cp("act" if half == 0 else "dve",
                   xT[:, half * 4:half * 4 + 4, tcols(t)],
                   bank(b).rearrange("p (c n) -> p c n", c=4),
                   [("ps", b)], [("xT", t)])

        def in_proj(t, ncols, wview, hs):
            n0 = 0
            while n0 < ncols:
                n1 = min(ncols, n0 + 512)
                b = mbank()
                for c in range(8):
                    mm(bank(b)[:, 0:n1 - n0], xT[:, c, tcols(t)], wview[:, c, n0:n1], c == 0, c == 7,
                       [("xT", t), "WIN"], [("ps", b)], inc=(c == 7))
                cp("act", hs[:, n0:n1], bank(b)[:, 0:n1 - n0], [("ps", b)], ["HSB%d" % (t % 2)])
                n0 = n1

        def rope(src, dst, nh, hd, t, cosT, sinT, nsinT, rk, wk, tmp=None):
            h2 = hd // 2
            cb = cosT[:, t, :].unsqueeze(1).unsqueeze(1).to_broadcast([128, nh, 2, h2])
            sb = sinT[:, t, :].unsqueeze(1).to_broadcast([128, nh, h2])
            nb = nsinT[:, t, :].unsqueeze(1).to_broadcast([128, nh, h2])
            tv = ROPET[:, 0:nh * hd].rearrange("p (h t d) -> p h t d", h=nh, t=2)
            rk = rk + ["cos64", "sin64", "nsin64", "cos32", "sin32", "nsin32"]
            tt(dst, src, cb, ALU.mult, rk, wk)
            tt(tv[:, :, 0, :], src[:, :, 1, :], nb, ALU.mult, rk, ["ropetmp"])
            tt(tv[:, :, 1, :], src[:, :, 0, :], sb, ALU.mult, rk, ["ropetmp"])
            tt(dst, dst, tv, ALU.add, wk + ["ropetmp"], wk)

        def global_bound(mt, nh, qsl, ksl, scale, negc):
            b = mbank()
            tr(bank(b)[0:nh, 0:128], mt, identf[:], ["mt", "identf"], [("ps", b)])
            red(sm[0:nh, 0:1], bank(b)[0:nh, 0:128], ALU.max, [("ps", b)], ["gb1"])
            b2 = mbank()
            tr(bank(b2)[0:1, 0:nh], sm[0:nh, 0:1], identf[0:nh, 0:nh], ["gb1", "identf"], [("ps", b2)])
            red(sm[0:1, 1:2], bank(b2)[0:1, qsl], ALU.max, [("ps", b2)], ["gb2"])
            red(sm[0:1, 2:3], bank(b2)[0:1, ksl], ALU.max, [("ps", b2)], ["gb3"])
            tt(sm[0:1, 3:4], sm[0:1, 1:2], sm[0:1, 2:3], ALU.mult, ["gb2", "gb3"], ["gb4"])
            act(sm[0:1, 4:5], sm[0:1, 3:4], AF.Ln, ["gb4"], ["gb5"])
            act(sm[0:1, 5:6], sm[0:1, 4:5], AF.Exp, ["gb5"], ["gb6"], scale=0.5)
            ts(sm[0:1, 6:7], sm[0:1, 5:6], -scale, None, ALU.mult, None, ["gb6"], ["gb7"])
            b3 = mbank()
            mm(bank(b3)[:, 0:1], ones1[0:1, :], sm[0:1, 6:7], True, True, ["gb7", "ones1"], [("ps", b3)], True)
            cp("dve", negc, bank(b3)[:, 0:1], [("ps", b3)], ["negc"])

        def head_sumsq(src, ncols, nh, mt, first, rk):
            act(SQ[:, 0:ncols], src, AF.Square, rk, ["SQ"])
            hd = ncols // nh
            if first:
                red(mt, SQ[:, 0:ncols].rearrange("p (h d) -> p h d", h=nh), ALU.add, ["SQ"], ["mt"])
            else:
                red(sm[:, 16:16 + nh], SQ[:, 0:ncols].rearrange("p (h d) -> p h d", h=nh), ALU.add, ["SQ"], ["hs"])
                tt(mt, mt, sm[:, 16:16 + nh], ALU.max, ["mt", "hs"], ["mt"])

        def out_proj_T(qb, nchunks):
            b = mbank()
            for c in range(nchunks):
                tr(bankb(b)[:, c * 128:(c + 1) * 128], MIXT[:, c * 128:(c + 1) * 128], identb[:],
                   ["MIXT", "identb"], [("ps", b)], inc=(c == nchunks - 1))
            cp("act", MIXTT[:, 0:nchunks * 128], bankb(b)[:, 0:nchunks * 128], [("ps", b)], ["MIXTT"])

        def out_proj_M(qb, nchunks, first):
            wv = WOUT.rearrange("p (c n) -> p c n", n=1024)
            for half in range(2):
                yb = 6 + half
                for c in range(nchunks):
                    mm(bank(yb), MIXTT[:, c * 128:(c + 1) * 128], wv[:, c, half * 512:(half + 1) * 512],
                       c == 0, c == nchunks - 1, ["MIXTT", "WOUT"], [("ps", yb)], inc=(c == nchunks - 1))
                xs = X[:, qb, half * 512:(half + 1) * 512]
                if first:
                    stt(xs, xs, ALPHA, bank(yb), ALU.mult, ALU.add, [("X", qb), ("ps", yb)], [("X", qb)])
                else:
                    tt(xs, xs, bank(yb), ALU.add, [("X", qb), ("ps", yb)], [("X", qb)])

        def out_proj_partial(qb, nchunks, first, after=None):
            bg.append(lambda: None)
            bg.append(lambda: out_proj_T(qb, nchunks))

            def part2():
                out_proj_M(qb, nchunks, first)
                if after is not None:
                    after(qb)
            bg.append(part2)

        def dbg_store(qb, c0, ncols):
            if dbg_d is None:
                return
            cp("dve", ROPET[:, 0:ncols], MIXT[:, 0:ncols], ["MIXT"], ["ropetmp"])
            P.dma("sp", tm(dbg_d)[:, qb, c0:c0 + ncols], ROPET[:, 0:ncols], reads=["ropetmp"], final=True)

        def ln_stats(t, k):
            o = 32 + 16 * k
            ks = "ln%d" % k
            P.op("dve", lambda e, o_=sm[:, o:o + 6], i=X[:, t, 0:512]: e.bn_stats(out=o_, in_=i), reads=[("X", t)], writes=[ks + "a"])
            P.op("dve", lambda e, o_=sm[:, o + 6:o + 12], i=X[:, t, 512:1024]: e.bn_stats(out=o_, in_=i), reads=[("X", t)], writes=[ks + "b"])
            P.op("dve", lambda e, o_=sm[:, o + 12:o + 14], i=sm[:, o:o + 12]: e.bn_aggr(out=o_, in_=i), reads=[ks + "a", ks + "b"], writes=[ks + "mv"])
            act(sm[:, o + 14:o + 15], sm[:, o + 13:o + 14], AF.Ln, [ks + "mv"], [ks + "lv"], bias=1e-5)
            act(sm[:, o + 15:o + 16], sm[:, o + 14:o + 15], AF.Exp, [ks + "lv"], [ks + "rs"], scale=-0.5)

        def ln_apply(t, k):
            o = 32 + 16 * k
            ks = "ln%d" % k
            xs = X[:, t, :]
            ts(xs, xs, sm[:, o + 12:o + 13], sm[:, o + 15:o + 16], ALU.subtract, ALU.mult, [("X", t), ks + "mv", ks + "rs"], [("X", t)])
            tt(xs, xs, lnp[:, 0, :], ALU.mult, [("X", t), "lnp"], [("X", t)], eng=LNP_ENG)
            tt(xs, xs, lnp[:, 1, :], ALU.add, [("X", t), "lnp"], [("X", t)], eng=LNP_ENG)

        def layer_norm(t, k=0):
            ln_stats(t, k)
            ln_apply(t, k)

        def load_ln(g_d, b_d, l):
            P.dma("sp", lnp[:, 0, :], g_d[l].partition_broadcast(128), writes=["lnp"])
            P.dma("sp", lnp[:, 1, :], b_d[l].partition_broadcast(128), writes=["lnp"])

        def phase_A(l):
            A.off = phase_base
            FM = A.bf16(6 * S).rearrange("p (c n) -> p c n", c=6)
            VA = A.bf16(NT * 2 * 65).rearrange("p (t g d) -> p t g d", t=NT, g=2)
            mt = A.f32(16)[:, 0:10]
            negc = A.f32(2)[:, 0:1]
            esink = A.f32(8)
            den = A.f32(8)
            wv = WIN[:, 0:8 * 768].rearrange("p (c n) -> p c n", c=8)
            P.dma("pool", wv, w_in_d[l].rearrange("(c p) n -> p c n", p=128)[:, :, 0:768], writes=["WIN"])
            P.dma("pool", WOUT[:, 0:4096].rearrange("p (c n) -> p c n", c=4),
                  w_out_d[l, 0:512, :].rearrange("(c p) n -> p c n", p=128), writes=["WOUT"])
            P.dma("sp", sinks[:], sinks_d[l].partition_broadcast(128), writes=["sinks"])
            memset(VA[:, :, :, 64:65], 1.0, ["VAones"])
            def Pf(t):
                in_proj(t, 768, wv, HSB[t % 2])

            def Rf(t):
                hs = HSB[t % 2]
                qk = QKR[t % 2]
                hk = "HSB%d" % (t % 2)
                qkk = "QKR%d" % (t % 2)
                head_sumsq(hs[:, 0:640], 640, 10, mt, t == 0, [hk])
                rope(hs[:, 0:512].rearrange("p (h t d) -> p h t d", h=8, t=2),
                     qk[:, 0:512].rearrange("p (h t d) -> p h t d", h=8, t=2),
                     8, 64, t, cos64, sin64, nsin64, [hk], [qkk])
                kdst = qk[:, 512:768].rearrange("p (g r d) -> p g r d", g=2, r=2)
                rope(hs[:, 512:640].rearrange("p (h t d) -> p h t d", h=2, t=2),
                     kdst[:, :, 0, :].rearrange("p g (t d) -> p g t d", t=2),
                     2, 64, t, cos64, sin64, nsin64, [hk], [qkk])
                cp("dve", kdst[:, :, 1, :], kdst[:, :, 0, :], [qkk], [qkk])
                cp("act", VA[:, t, :, 0:64], hs[:, 640:768].rearrange("p (g d) -> p g d", g=2), [hk], [("VA", t)])

            def Rb(t):
                qk = QKR[t % 2]
                qkk = "QKR%d" % (t % 2)
                for part, (c0, nchk) in enumerate(((0, 4), (4, 2))):
                    b = mbank()
                    for j in range(nchk):
                        c = c0 + j
                        tr(bank(b)[:, j * 128:(j + 1) * 128], qk[:, c * 128:(c + 1) * 128], identf[:],
                           [qkk, "identf"], [("ps", b)], inc=(j == nchk - 1))
                    cp("act", FM[:, c0:c0 + nchk, tcols(t)],
                       bank(b)[:, 0:nchk * 128].rearrange("p (c n) -> p c n", c=nchk),
                       [("ps", b)], [("FM", t)])

            run_stages(Pf, [Rf, Rb])
            if stop_after == "A1":
                return
            global_bound(mt, 10, slice(0, 8), slice(8, 10), SC64, negc)
            act(esink, sinks[:], AF.Exp, ["sinks", "negc"], ["esink"], bias=negc)
            if stop_after == "A2":
                return

            def finA(qb, g):
                def fn():
                    ob = 4 + g
                    o3 = bank(ob)[:, 0:260].rearrange("p (h d) -> p h d", h=4)
                    tt(den[:, 4 * g:4 * g + 4], o3[:, :, 64], esink[:, 4 * g:4 * g + 4], ALU.add,
                       [("ps", ob), "esink"], ["den"])
                    recip(den[:, 4 * g:4 * g + 4], den[:, 4 * g:4 * g + 4], ["den"], ["den"])
                    tt(MIXT[:, g * 256:(g + 1) * 256].rearrange("p (h d) -> p h d", h=4), o3[:, :, 0:64],
                       den[:, 4 * g:4 * g + 4].unsqueeze(2).to_broadcast([128, 4, 64]), ALU.mult,
                       [("ps", ob), "den"], ["MIXT"])
                    if g == 1:
                        dbg_store(qb, 0, 512)
                        out_proj_partial(qb, 4, True)
                return fn

            seq = []
            for qb in range(NT):
                kts = [kt for kt in (qb - 1, qb) if kt >= 0]
                nk_ = len(kts)
                for g in range(2):
                    for par in range(2):
                        po = 64 * par
                        slots = []
                        for h in (4 * g + par, 4 * g + par + 2):
                            for kt in kts:
                                slots.append({"K": (FM[po:po + 64, 4 + g, tcols(kt)], [("FM", kt)]),
                                              "Q": (FM[po:po + 64, h // 2, tcols(qb)], [("FM", qb)]),
                                              "V": (VA[:, kt, g, :], [("VA", kt), "VAones"]),
                                              "ob": 4 + g, "oreg": (h % 4) * 65,
                                              "start": kt == kts[0], "stop": kt == kts[-1]})
                        if nk_ == 2:
                            masks = [(0, 512, ("p (h n) -> p h n", {"h": 2}),
                                      maskPC[:].unsqueeze(1).to_broadcast([128, 2, 256]), ["maskPC"])]
                        else:
                            masks = [(0, 256, ("p (h n) -> p h n", {"h": 2}),
                                      maskPC[:, 128:256].unsqueeze(1).to_broadcast([128, 2, 128]), ["maskPC"])]
                        seq.append(({"kind": "att", "slots": slots, "scale": SC64, "negc": negc, "masks": masks,
                                     "fin": finA(qb, g) if par == 1 else None}, []))
            run_seq(seq, bg_per_iter=1)

        def phase_B(l):
            A.off = phase_base
            FM = A.bf16(6 * S).rearrange("p (c n) -> p c n", c=6)
            VB = A.bf16(NT * 65 + 1)[:, 0:NT * 65].rearrange("p (t d) -> p t d", t=NT)
            SCO = A.f32(S)
            MSK = A.bf16(S)
            RB = [HSB[0][:, 0:512], HSB[1][:, 0:512]]
            mt = A.f32(16)[:, 0:5]
            negc = A.f32(2)[:, 0:1]
            bis = A.f32(8)
            btab = A.f32(32)
            wv = WIN[:, 0:8 * 708].rearrange("p (c n) -> p c n", c=8)
            P.dma("pool", wv, w_in_d[l].rearrange("(c p) n -> p c n", p=128)[:, :, 768:1476], writes=["WIN"])
            P.dma("pool", WOUT[:, 0:2048].rearrange("p (c n) -> p c n", c=2),
                  w_out_d[l, 512:768, :].rearrange("(c p) n -> p c n", p=128), writes=["WOUT"])
            memset(VB[:, :, 64:65], 1.0, ["VBones"])
            def Pf(t):
                in_proj(t, 708, wv, HSB[t % 2])

            def Rf(t):
                hs = HSB[t % 2]
                qk = QKR[t % 2]
                hk = "HSB%d" % (t % 2)
                qkk = "QKR%d" % (t % 2)
                head_sumsq(hs[:, 0:320], 320, 5, mt, t == 0, [hk])
                for (s0, d0) in ((0, 0), (384, 384)):
                    rope(hs[:, s0:s0 + 320].rearrange("p (h t d) -> p h t d", h=5, t=2),
                         qk[:, d0:d0 + 320].rearrange("p (h t d) -> p h t d", h=5, t=2),
                         5, 64, t, cos64, sin64, nsin64, [hk], [qkk])
                    cp("dve", qk[:, d0 + 320:d0 + 384], qk[:, d0 + 256:d0 + 320], [qkk], [qkk])
                cp("act", VB[:, t, 0:64], hs[:, 320:384], [hk], [("VB", t)])
                ts(widx[:, t, :], hs[:, 704:708], IDXW, None, ALU.mult, None, [hk], [("widx", t)])

            def Rb(t):
                qk = QKR[t % 2]
                qkk = "QKR%d" % (t % 2)
                for part, (c0, nchk) in enumerate(((0, 4), (4, 2))):
                    b = mbank()
                    for j in range(nchk):
                        c = c0 + j
                        tr(bank(b)[:, j * 128:(j + 1) * 128], qk[:, c * 128:(c + 1) * 128], identf[:],
                           [qkk, "identf"], [("ps", b)], inc=(j == nchk - 1))
                    cp("act", FM[:, c0:c0 + nchk, tcols(t)],
                       bank(b)[:, 0:nchk * 128].rearrange("p (c n) -> p c n", c=nchk),
                       [("ps", b)], [("FM", t)])

            run_stages(Pf, [Rf, Rb])
            global_bound(mt, 5, slice(0, 4), slice(4, 5), SC64, negc)

            P.barrier()
            SCOs = [SCO, arena[:, 0:S]]
            a_qkr = phase_qkr_off
            MSKT = [arena[:, a_qkr + 1024 * i:a_qkr + 1024 * (i + 1)].bitcast(BF16).rearrange("p (t n) -> p t n", t=NT)
                    for i in range(2)]

            def idx_items(qb):
                nk = (qb + 1) * 128
                par = qb % 2
                out = []
                for k0 in range(0, nk, 512):
                    n = min(512, nk - k0)
                    for h in range(4):
                        po = 64 * (h % 2)
                        ri = (len(out)) % 2
                        out.append({"kind": "idx", "n": n, "h": h, "qb": qb, "k0": k0, "ri": ri, "rb": RB[ri],
                                    "lhsT": FM[po:po + 64, 3 + h // 2, tcols(qb)], "rhs": FM[po:po + 64, 5, k0:k0 + n],
                                    "keys": [("FM", t) for t in range(qb + 1)],
                                    "sco": SCOs[par], "scokey": "SCO%d" % par})
                return out

            def bis_closures(qb):
                nk = (qb + 1) * 128
                par = qb % 2
                sco = SCOs[par]
                sk = "SCO%d" % par
                cl = []

                def init():
                    if qb >= 2:
                        red(bis[:, 0:1], sco[:, 0:nk], ALU.max, [sk], ["bnd"], absv=True)
                    tt(sco[:, qb * 128:nk], sco[:, qb * 128:nk], negmask[:], ALU.add, [sk, "negmask"], [sk])
                    if qb >= 2:
                        ts(bis[:, 2:3], bis[:, 0:1], 2.0, 2.0, ALU.mult, ALU.add, ["bnd"], ["w0"])
                        ts(btab[:], fvec[:], bis[:, 2:3], None, ALU.mult, None, ["w0", "fvec"], ["btab"])
                        memset(bis[:, 3:4], 0.0, ["mid"])
                    else:
                        ts(MSK[:, 0:nk], sco[:, 0:nk], -1.0e29, None, ALU.is_ge, None, [sk], ["MSK"])
                cl.append(init)
                if qb >= 2:
                    def it_fn(k):
                        def fn():
                            ts(MSK[:, 0:nk], sco[:, 0:nk], bis[:, 3:4], None, ALU.is_ge, ALU.add, [sk, "mid"],
                               ["MSK", "cnt"], accum=bis[:, 4:5])
                            if k < NBIS - 1:
                                ts(bis[:, 5:6], bis[:, 4:5], float(TOPK), btab[:, 16 + k + 1:16 + k + 2], ALU.is_ge, ALU.mult,
                                   ["cnt", "btab"], ["stp"])
                                stt(bis[:, 3:4], bis[:, 5:6], btab[:, k + 1:k + 2], bis[:, 3:4], ALU.subtract, ALU.add,
                                    ["stp", "btab", "mid"], ["mid"])
                            else:
                                ts(bis[:, 5:6], bis[:, 4:5], float(TOPK), btab[:, k:k + 1], ALU.is_ge, ALU.mult,
                                   ["cnt", "btab"], ["stp"])
                                stt(bis[:, 1:2], bis[:, 5:6], btab[:, k:k + 1], bis[:, 3:4], ALU.subtract, ALU.add,
                                    ["stp", "btab", "mid"], ["lo"])
                        return fn
                    for k in range(NBIS):
                        cl.append(it_fn(k))
                    cl.append(lambda: ts(MSK[:, 0:nk], sco[:, 0:nk], bis[:, 1:2], None, ALU.is_ge, None, [sk, "lo"], ["MSK"]))
                return cl

            def mskT_items(qb):
                par = qb % 2
                out = []
                for kt0 in range(0, qb + 1, 4):
                    kts = list(range(kt0, min(kt0 + 4, qb + 1)))
                    out.append({"kind": "mskT", "kts": kts, "src": MSK, "srckey": "MSK",
                                "dst": MSKT[par], "dstkey": ("MSKT", par)})
                return out

            def att_items(qb):
                par = qb % 2
                out = []
                for h in range(4):
                    po = 64 * (h % 2)
                    for kt0 in range(0, qb + 1, 4):
                        kts = list(range(kt0, min(kt0 + 4, qb + 1)))
                        slots = [{"K": (FM[po:po + 64, 2, tcols(kt)], [("FM", kt)]),
                                  "Q": (FM[po:po + 64, h // 2, tcols(qb)], [("FM", qb)]),
                                  "V": (VB[:, kt, :], [("VB", kt), "VBones"]),
                                  "ob": 4 + par, "oreg": h * 65, "start": kt == 0, "stop": kt == qb} for kt in kts]
                        ns = len(kts)
                        masks = [(0, 128 * ns, ("p (c n) -> p c n", {"c": ns}), MSKT[par][:, kt0:kt0 + ns, :], [("MSKT", par)])]
                        last = (h == 3 and kts[-1] == qb)
                        out.append({"kind": "att", "slots": slots, "scale": SC64, "negc": negc, "masks": masks,
                                    "fin": fin_generic(qb, 4 + par, 4, 2, 512, None) if last else None})
                return out

            def merge(a, b):
                out = []
                na, nb = len(a), len(b)
                ia = ib = 0
                while ia < na or ib < nb:
                    if ib >= nb or (ia < na and ia * nb <= ib * na):
                        out.append(a[ia])
                        ia += 1
                    else:
                        out.append(b[ib])
                        ib += 1
                return out

            seq = []
            for r in range(-2, NT):
                ia = att_items(r) if r >= 0 else []
                ii = idx_items(r + 2) if r + 2 < NT else []
                cl = bis_closures(r + 1) if 0 <= r + 1 < NT else []
                im = mskT_items(r + 1) if 0 <= r + 1 < NT else []
                ents = [[it, []] for it in merge(ia, ii)]
                if not ents:
                    ents = [[None, []]]
                ne = len(ents)
                for ci, c in enumerate(cl):
                    ents[min(ne - 1, (ci * ne) // max(1, len(cl)))][1].append(c)
                seq.extend((e[0], e[1]) for e in ents)
                seq.extend((it, []) for it in im)
            run_seq(seq, bg_per_iter=1)

        def phase_C(l):
            A.off = phase_base
            FQ = A.bf16(4 * S).rearrange("p (c n) -> p c n", c=4)
            FK = A.bf16(4 * S).rearrange("p (c n) -> p c n", c=4)
            VC = A.bf16(NT * 4 * 65).rearrange("p (t h d) -> p t h d", t=NT, h=4)
            QCF = [QKR[0][:, 0:384], QKR[0][:, 384:768]]
            KCF = [QKR[1][:, 0:384], QKR[1][:, 384:768]]
            CQN = [A.bf16(256), A.bf16(256)]
            CKN = [A.bf16(128), A.bf16(128)]
            CQT = [A.bf16(256), A.bf16(256)]
            CKT = [A.bf16(128), A.bf16(128)]
            mt = A.f32(16)[:, 0:8]
            negc = A.f32(2)[:, 0:1]
            rt = A.f32(128)
            rtk = [A.f32(32), A.f32(32)]
            SQJ = MIXT_f32
            wv = WIN[:, 0:8 * 416].rearrange("p (c n) -> p c n", c=8)
            WUQ = WIN[:, 8 * 416:8 * 416 + 768].rearrange("p (c n) -> p c n", c=2)
            WUKV = WIN[:, 8 * 416 + 768:8 * 416 + 768 + 512]
            P.dma("pool", wv, w_in_d[l].rearrange("(c p) n -> p c n", p=128)[:, :, 1476:1892], writes=["WIN"])
            P.dma("pool", WUQ, w_uq_d[l].rearrange("(c p) n -> p c n", p=128), writes=["WIN"])
            P.dma("pool", WUKV, w_ukv_d[l], writes=["WIN"])
            P.dma("pool", WOUT[:, 0:2048].rearrange("p (c n) -> p c n", c=2),
                  w_out_d[l, 768:1024, :].rearrange("(c p) n -> p c n", p=128), writes=["WOUT"])
            P.dma("sp", gq[:], gq_d[l].partition_broadcast(128), writes=["gq"])
            P.dma("sp", gkv[:], gkv_d[l].partition_broadcast(128), writes=["gkv"])
            load_ln(ln1g_d, ln1b_d, l)
            memset(VC[:, :, :, 64:65], 1.0, ["VCones"])
            def Pf(t):
                in_proj(t, 416, wv, HSB[t % 2])

            def c1(t):
                p = t % 2
                hs = HSB[p]
                hk = "HSB%d" % p
                o = 80 + 8 * p
                ck = "c1s%d" % p
                act(SQJ[:, 0:256], hs[:, 0:256], AF.Square, [hk], ["SQJ"], accum=sm[:, o:o + 1])
                act(SQJ[:, 256:384], hs[:, 256:384], AF.Square, [hk], ["SQJ2"], accum=sm[:, o + 1:o + 2])
                act(sm[:, o + 2:o + 3], sm[:, o:o + 1], AF.Ln, ["SQJ"], [ck + "l1"], scale=1.0 / 256, bias=1e-6)
                act(sm[:, o + 3:o + 4], sm[:, o + 1:o + 2], AF.Ln, ["SQJ2"], [ck + "l2"], scale=1.0 / 128, bias=1e-6)
                act(sm[:, o + 4:o + 5], sm[:, o + 2:o + 3], AF.Exp, [ck + "l1"], [ck + "r1"], scale=-0.5)
                act(sm[:, o + 5:o + 6], sm[:, o + 3:o + 4], AF.Exp, [ck + "l2"], [ck + "r2"], scale=-0.5)
                stt(CQN[p][:, 0:256], hs[:, 0:256], sm[:, o + 4:o + 5], gq[:], ALU.mult, ALU.mult, [hk, ck + "r1", "gq"], ["CQN%d" % p])
                stt(CKN[p][:, 0:128], hs[:, 256:384], sm[:, o + 5:o + 6], gkv[:], ALU.mult, ALU.mult, [hk, ck + "r2", "gkv"], ["CKN%d" % p])
                rope(hs[:, 384:416].rearrange("p (h t d) -> p h t d", h=1, t=2),
                     rtk[p][:, 0:32].rearrange("p (h t d) -> p h t d", h=1, t=2), 1, 32, t,
                     cos32, sin32, nsin32, [hk], ["rtk%d" % p])

            def c23(t):
                p = t % 2
                qck, kck = "QCF%d" % p, "KCF%d" % p
                b = mbank()
                tr(bankb(b)[:, 0:128], CQN[p][:, 0:128], identb[:], ["CQN%d" % p, "identb"], [("ps", b)], inc=False)
                tr(bankb(b)[:, 128:256], CQN[p][:, 128:256], identb[:], ["CQN%d" % p, "identb"], [("ps", b)], inc=False)
                tr(bankb(b)[:, 256:384], CKN[p][:, 0:128], identb[:], ["CKN%d" % p, "identb"], [("ps", b)], inc=True)
                cp("act", CQT[p][:, 0:256], bankb(b)[:, 0:256], [("ps", b)], ["CQT%d" % p])
                cp("dve", CKT[p][:, 0:128], bankb(b)[:, 256:384], [("ps", b)], ["CKT%d" % p])
                bq = mbank()
                mm(bank(bq)[:, 0:384], CQT[p][:, 0:128], WUQ[:, 0, :], True, False, ["CQT%d" % p, "WIN"], [("ps", bq)], False)
                mm(bank(bq)[:, 0:384], CQT[p][:, 128:256], WUQ[:, 1, :], False, True, ["CQT%d" % p, "WIN"], [("ps", bq)], True)
                bk = mbank()
                mm(bank(bk)[:, 0:512], CKT[p][:, 0:128], WUKV, True, True, ["CKT%d" % p, "WIN"], [("ps", bk)], True)
                cp("act", QCF[p], bank(bq)[:, 0:384], [("ps", bq)], [qck])
                q4 = QCF[p].rearrange("p (h d) -> p h d", h=4)
                qr = q4[:, :, 64:96].rearrange("p h (t d) -> p h t d", t=2)
                cp("dve", rt[:, 0:128].rearrange("p (h d) -> p h d", h=4), q4[:, :, 64:96], [qck], ["rt"])
                rope(rt[:, 0:128].rearrange("p (h t d) -> p h t d", h=4, t=2), qr, 4, 32, t,
                     cos32, sin32, nsin32, ["rt"], [qck])
                kv4 = bank(bk)[:, 0:512].rearrange("p (h d) -> p h d", h=4)
                k4 = KCF[p].rearrange("p (h d) -> p h d", h=4)
                cp("act", k4[:, :, 0:64], kv4[:, :, 0:64], [("ps", bk)], [kck])
                cp("dve", VC[:, t, :, 0:64], kv4[:, :, 64:128], [("ps", bk)], [("VC", t)])
                cp("dve", k4[:, :, 64:96], rtk[p][:, 0:32].unsqueeze(1).to_broadcast([128, 4, 32]), ["rtk%d" % p], [kck])
                act(SQ[:, 0:384], QCF[p], AF.Square, [qck], ["SQ"])
                act(SQ[:, 384:768], KCF[p], AF.Square, [kck], ["SQ"])
                if t == 0:
                    red(mt, SQ[:, 0:768].rearrange("p (h d) -> p h d", h=8), ALU.add, ["SQ"], ["mt"])
                else:
                    red(sm[:, 16:24], SQ[:, 0:768].rearrange("p (h d) -> p h d", h=8), ALU.add, ["SQ"], ["hs"])
                    tt(mt, mt, sm[:, 16:24], ALU.max, ["mt", "hs"], ["mt"])

            def c4(t):
                p = t % 2
                for (src_, dstF, key, fkey, eng) in ((QCF[p], FQ, "QCF%d" % p, "FQKR0", "act"), (KCF[p], FK, "KCF%d" % p, "FQKR1", "dve")):
                    b = mbank()
                    for h in range(4):
                        tr(bank(b)[0:96, h * 128:(h + 1) * 128], src_[:, h * 96:(h + 1) * 96], identf[:],
                           [key, "identf"], [("ps", b)], inc=(h == 3))
                    cp(eng, dstF[0:96, :, tcols(t)], bank(b)[0:96, :].rearrange("p (c n) -> p c n", c=4),
                       [("ps", b)], [(fkey, t)])

            run_stages(Pf, [c1, c23, c4])
            global_bound(mt, 8, slice(0, 4), slice(4, 8), SC96, negc)

            P.barrier()
            RT = arena[:, phase_qkr_off:phase_qkr_off + 2304]

            def xpose(qb, half):
                b = mbank()
                for j in range(4):
                    c = half * 4 + j
                    tr(bank(b)[:, j * 128:(j + 1) * 128], X[:, qb, c * 128:(c + 1) * 128], identf[:],
                       [("X", qb), "identf"], [("ps", b)], inc=(j == 3))
                cp("act", xT[:, half * 4:half * 4 + 4, tcols(qb)], bank(b).rearrange("p (c n) -> p c n", c=4),
                   [("ps", b)], [("xT", qb)])
                cp("dve", HSB[half][:, 0:512], bank(b), [("ps", b)], ["HSB%d" % half])

            def router_a(qb):
                b = mbank()
                for c in range(8):
                    mm(bank(b)[:, 0:16], HSB[c // 4][:, (c % 4) * 128:(c % 4 + 1) * 128], wr[:, c, :], c == 0, c == 7,
                       ["HSB0", "HSB1", "wr"], [("ps", b)], inc=(c == 7))
                act(RT[:, qb * 16:(qb + 1) * 16], bank(b)[:, 0:16], AF.Exp, [("ps", b)], [("r_sc", qb)], scale=-1.0)

            def finC(qb, ob):
                def fn():
                    o3 = bank(ob)[:, 0:260].rearrange("p (h d) -> p h d", h=4)
                    rc = sm[:, 72:76]
                    recip(rc, o3[:, :, 64], [("ps", ob)], ["rc"])
                    tt(MIXT[:, 0:256].rearrange("p (h d) -> p h d", h=4), o3[:, :, 0:64],
                       rc.unsqueeze(2).to_broadcast([128, 4, 64]), ALU.mult, [("ps", ob), "rc"], ["MIXT"])
                    dbg_store(qb, 768, 256)
                    pv = qb - 1
                    if pv >= 0:
                        bg.append(lambda: xpose(pv, 0))
                        bg.append(lambda: None)
                    bg.append(lambda: out_proj_T(qb, 2))
                    if pv >= 0:
                        bg.append(lambda: xpose(pv, 1))
                    bg.append(lambda: out_proj_M(qb, 2, False))
                    if pv >= 0:
                        bg.append(lambda: router_a(pv))
                    bg.append(lambda: ln_stats(qb, qb % 2))
                    bg.append(lambda: ln_apply(qb, qb % 2))
                return fn

            seq = []
            for qb in range(NT):
                par = qb % 2
                for h in range(4):
                    for kt0 in range(0, qb + 1, 4):
                        kts = list(range(kt0, min(kt0 + 4, qb + 1)))
                        slots = [{"K": (FK[0:96, h, tcols(kt)], [("FQKR1", kt)]),
                                  "Q": (FQ[0:96, h, tcols(qb)], [("FQKR0", qb)]),
                                  "V": (VC[:, kt, h, :], [("VC", kt), "VCones"]),
                                  "ob": 4 + par, "oreg": h * 65, "start": kt == 0, "stop": kt == qb} for kt in kts]
                        ns = len(kts)
                        masks = []
                        if kts[-1] == qb:
                            masks = [(128 * (ns - 1), 128 * ns, None, causalT[:], ["causalT"])]
                        last = (h == 3 and kts[-1] == qb)
                        seq.append(({"kind": "att", "slots": slots, "scale": SC96, "negc": negc, "masks": masks,
                                     "fin": finC(qb, 4 + par) if last else None}, []))
            run_seq(seq, bg_per_iter=2)
            xpose(NT - 1, 0)
            xpose(NT - 1, 1)
            router_a(NT - 1)

            sc = RT[:, 0:256]
            bi = RT[:, 256:512]
            m1 = RT[:, 512:576]
            eq = RT[:, 576:832]
            msk = RT[:, 832:1088]
            m2 = RT[:, 1088:1152]
            gs = RT[:, 1152:1216]
            gm = RT[:, 1216:1232]
            gsel = RT[:, 1232:1296]
            t2 = RT[:, 1296:1552]
            wgt = RT[:, 1552:1808]
            ws = RT[:, 1808:1824]
            rsk = [("r_sc", t) for t in range(NT)]

            def v3(ap, a):
                return ap.rearrange("p (a b) -> p a b", a=a)

            ts(sc, sc, 1.0, None, ALU.add, None, rsk, ["r_s"])
            recip(sc, sc, ["r_s"], ["r_s"])
            tt(v3(bi, 16), v3(sc, 16), rb[:].unsqueeze(1).to_broadcast([128, 16, 16]), ALU.add, ["r_s", "rb"], ["r_bi"])
            red(m1, v3(bi, 64), ALU.max, ["r_bi"], ["r_m1"])
            tt(v3(eq, 64), v3(bi, 64), m1.unsqueeze(2).to_broadcast([128, 64, 4]), ALU.is_equal, ["r_bi", "r_m1"], ["r_eq"])
            stt(msk, eq, NEG, bi, ALU.mult, ALU.add, ["r_eq", "r_bi"], ["r_msk"])
            red(m2, v3(msk, 64), ALU.max, ["r_msk"], ["r_m2"])
            tt(gs, m1, m2, ALU.add, ["r_m1", "r_m2"], ["r_gs"])
            red(gm, v3(gs, 16), ALU.max, ["r_gs"], ["r_gm"])
            tt(v3(gsel, 16), v3(gs, 16), gm.unsqueeze(2).to_broadcast([128, 16, 4]), ALU.is_equal, ["r_gs", "r_gm"], ["r_gsel"])
            tt(v3(t2, 64), v3(bi, 64), m2.unsqueeze(2).to_broadcast([128, 64, 4]), ALU.is_ge, ["r_bi", "r_m2"], ["r_t2"])
            tt(v3(t2, 64), v3(t2, 64), gsel.unsqueeze(2).to_broadcast([128, 64, 4]), ALU.mult, ["r_t2", "r_gsel"], ["r_t2"])
            tt(wgt, sc, t2, ALU.mult, ["r_s", "r_t2"], ["r_w"])
            red(ws, v3(wgt, 16), ALU.add, ["r_w"], ["r_ws"])
            recip(ws, ws, ["r_ws"], ["r_ws"])
            tt(gates[:], v3(wgt, 16), ws.unsqueeze(2).to_broadcast([128, 16, 16]), ALU.mult, ["r_w", "r_ws"],
               [("gates", t) for t in range(NT)])

        def phase_moe(l, last_layer):
            A.off = 0
            NSLOT = 7
            WGU = [A.bf16(8 * 512).rearrange("p (c n) -> p c n", c=8) for _ in range(NSLOT)]
            WD = [A.bf16(2 * 1024).rearrange("p (c n) -> p c n", c=2) for _ in range(NSLOT)]
            SB = [A.bf16(256) for _ in range(2)]
            TB = [A.bf16(256) for _ in range(2)]
            TTB = [A.bf16(256) for _ in range(2)]
            load_ln(ln2g_d, ln2b_d, l)
            def load_expert(e):
                s = e % NSLOT
                P.dma("pool", WGU[s][:, :, 0:256], w_gate_d[l, e].rearrange("(c p) f -> p c f", p=128), writes=[("WGU", s)])
                P.dma("pool", WGU[s][:, :, 256:512], w_up_d[l, e].rearrange("(c p) f -> p c f", p=128), writes=[("WGU", s)])
                P.dma("pool", WD[s], w_down_d[l, e].rearrange("(c p) d -> p c d", p=128), writes=[("WD", s)])

            for e in range(NSLOT):
                load_expert(e)
            items = [(0, t, e) for e in range(4) for t in range(NT)]
            items += [(G, t, e) for G in range(1, 4) for t in range(NT) for e in range(4 * G, 4 * G + 4)]
            sd = {}
            cnt = [0]

            def s1(it):
                G, t, e = it
                s = e % NSLOT
                b = rbank()
                sd[it] = {"b": b, "i": cnt[0] % 2}
                cnt[0] += 1
                for c in range(8):
                    mm(bank(b), xT[:, c, tcols(t)], WGU[s][:, c, :], c == 0, c == 7,
                       [("xT", t), ("WGU", s)], [("ps", b)], inc=(c == 7))

            def s2(it):
                G, t, e = it
                d = sd[it]
                b, i = d["b"], d["i"]
                act(SB[i][:, 0:256], bank(b)[:, 0:256], AF.Silu, [("ps", b)], [("SB", i)])
                stt(TB[i][:, 0:256], bank(b)[:, 256:512], gates[:, t, e:e + 1], SB[i][:, 0:256], ALU.mult, ALU.mult,
                    [("ps", b), ("gates", t), ("SB", i)], [("TB", i)])
                b2 = rbank()
                d["b2"] = b2
                for fc in range(2):
                    tr(bankb(b2)[:, fc * 128:(fc + 1) * 128], TB[i][:, fc * 128:(fc + 1) * 128], identb[:],
                       [("TB", i), "identb"], [("ps", b2)], inc=(fc == 1))

            def s3(it):
                G, t, e = it
                d = sd[it]
                b2, i = d["b2"], d["i"]
                s = e % NSLOT
                cp("act", TTB[i][:, 0:256], bankb(b2)[:, 0:256], [("ps", b2)], [("TTB", i)])
                first = (e % 4 == 0) or G == 0
                lastx = (e % 4 == 3) or G == 0
                for half in range(2):
                    yb = 4 + 2 * (t % 2) + half
                    for fc in range(2):
                        mm(bank(yb), TTB[i][:, fc * 128:(fc + 1) * 128], WD[s][:, fc, half * 512:(half + 1) * 512],
                           first and fc == 0, lastx and fc == 1, [("TTB", i), ("WD", s)], [("ps", yb)],
                           inc=(fc == 1))
                if t == NT - 1 and e + NSLOT < 16:
                    load_expert(e + NSLOT)
                if lastx:
                    for half in range(2):
                        yb = 4 + 2 * (t % 2) + half
                        xs = X[:, t, half * 512:(half + 1) * 512]
                        if e == 0:
                            stt(xs, xs, ALPHA, bank(yb), ALU.mult, ALU.add, [("X", t), ("ps", yb)], [("X", t)])
                        else:
                            tt(xs, xs, bank(yb), ALU.add, [("X", t), ("ps", yb)], [("X", t)])
                    if G == 3:
                        k = t % 2
                        o = 32 + 16 * k
                        mvo = 96 + 2 * t

                        def st_a(t=t, o=o, k=k):
                            P.op("dve", lambda e, o_=sm[:, o:o + 6], i=X[:, t, 0:512]: e.bn_stats(out=o_, in_=i),
                                 reads=[("X", t)], writes=["bs%da" % k])

                        def st_b(t=t, o=o, k=k, mvo=mvo):
                            P.op("dve", lambda e, o_=sm[:, o + 6:o + 12], i=X[:, t, 512:1024]: e.bn_stats(out=o_, in_=i),
                                 reads=[("X", t)], writes=["bs%db" % k])
                            P.op("dve", lambda e, o_=sm[:, mvo:mvo + 2], i=sm[:, o:o + 12]: e.bn_aggr(out=o_, in_=i),
                                 reads=["bs%da" % k, "bs%db" % k], writes=[("mv2", t)])

                        bg.append(st_a)
                        bg.append(st_b)
                        if t % 8 == 7:
                            t0 = t - 7

                            def rstd8(t0=t0):
                                var8 = sm[:, 96 + 2 * t0:96 + 2 * t0 + 16].rearrange("p (t two) -> p t two", two=2)[:, :, 1]
                                rs8 = sm[:, 128 + t0:128 + t0 + 8]
                                act(rs8, var8, AF.Ln, [("mv2", u) for u in range(t0, t0 + 8)], [("rs2", t0)], bias=1e-5)
                                act(rs8, rs8, AF.Exp, [("rs2", t0)], [("rs2", t0)], scale=-0.5)
                            bg.append(rstd8)
                            for u in range(t0, t0 + 8):
                                for half in range(2):
                                    hsl = slice(half * 512, (half + 1) * 512)

                                    def ap1(u=u, t0=t0, hsl=hsl):
                                        xs = X[:, u, hsl]
                                        ts(xs, xs, sm[:, 96 + 2 * u:96 + 2 * u + 1], sm[:, 128 + u:128 + u + 1], ALU.subtract, ALU.mult,
                                           [("X", u), ("mv2", u), ("rs2", t0)], [("X", u)])

                                    def ap2(u=u, hsl=hsl):
                                        xs = X[:, u, hsl]
                                        tt(xs, xs, lnp[:, 0, hsl], ALU.mult, [("X", u), "lnp"], [("X", u)])

                                    def ap3(u=u, hsl=hsl, half=half):
                                        xs = X[:, u, hsl]
                                        tt(xs, xs, lnp[:, 1, hsl], ALU.add, [("X", u), "lnp"], [("X", u)])
                                        if last_layer and half == 1:
                                            P.dma("sp", tm(out_d)[:, u, :], X[:, u, :], reads=[("X", u)], final=True)
                                    bg.append(ap1)
                                    bg.append(ap2)
                                    bg.append(ap3)

            pipeline(items, s1, s2, s3, bg_per_iter=2)

        def run_layers():
            for l in range(nlayers):
                for t in range(NT):
                    build_xT(t)
                if stop_after == "xT":
                    return False
                phase_A(l)
                P.barrier()
                if stop_after in ("A", "A1", "A2"):
                    return False
                phase_B(l)
                P.barrier()
                if stop_after == "B":
                    return False
                phase_C(l)
                P.barrier()
                if stop_after == "C":
                    return False
                phase_moe(l, l == nlayers - 1)
                P.barrier()
            return True

        if not run_layers():
            for t in range(NT):
                P.dma("sp", tm(out_d)[:, t, :], X[:, t, :], reads=[("X", t)], final=True)

        P.emit(nc, sems)
    return nc


_CACHE = {}


def kernel(**inputs):
    consts = _consts()
    if "nc" not in _CACHE:
        _CACHE["nc"] = build_program()
    nc = _CACHE["nc"]
    x = np.ascontiguousarray(np.asarray(inputs["x"], dtype=np.float32))
    shared = {}
    for k, v in inputs.items():
        if k == "x":
            continue
        shared[k] = np.ascontiguousarray(np.asarray(v, dtype=np.float32))
    shared.update(consts)
    in_maps = []
    for c in range(NCORES):
        m = dict(shared)
        m["x"] = x[c]
        in_maps.append(m)
    res = run_bass_kernel_spmd(nc, in_maps, core_ids=list(range(NCORES)))
    out = np.stack([np.asarray(r["out"], dtype=np.float32) for r in res.results], axis=0)
    return out
```

```python
import contextlib
import numpy as np
import concourse.bass as bass
import concourse.mybir as mybir
from concourse.bass_utils import run_bass_kernel_spmd

F32 = mybir.dt.float32
BF16 = mybir.dt.bfloat16
AF = mybir.ActivationFunctionType
ALU = mybir.AluOpType
AX = mybir.AxisListType

S = 2048
D = 1024
NT = 16
NCORES = 8
DEPTH = 2
ALPHA = float((2 * DEPTH) ** 0.25)
IDXW = float((4 * 64) ** -0.5)
SC64 = float(64 ** -0.5)
SC96 = float(96 ** -0.5)
TOPK = 256
NBIS = 14
NEG = -1.0e30
NDMA_SEMS = 8
ACC_ENG = "dve"
LNP_ENG = "dve"


class Prog:
    ENGS = ("pe", "act", "dve", "pool", "sp")

    def __init__(self):
        self.ops = {e: [] for e in self.ENGS}
        self.cnt = {e: 0 for e in self.ENGS}
        self.last_w = {}
        self.readers = {}
        self.waited = {e: {} for e in self.ENGS}
        self.dma_val = {}
        self.dma_rr = {"sp": 0, "pool": 0}
        self.final_tokens = []

    def _deps(self, eng, reads, writes):
        deps = {}

        def add(tok, raw):
            src, val = tok
            if src == eng and eng == "pe":
                return
            if deps.get(src, 0) < val:
                deps[src] = val

        for k in reads:
            if k in self.last_w:
                add(self.last_w[k], True)
            if isinstance(k, tuple) and k[0] == "ps":
                for r in self.readers.get(k, ()):
                    if r[0] != eng:
                        add(r, False)
        for k in writes:
            if k in self.last_w:
                add(self.last_w[k], False)
            for r in self.readers.get(k, ()):
                add(r, False)
        waits = []
        for src, val in deps.items():
            if self.waited[eng].get(src, 0) >= val:
                continue
            self.waited[eng][src] = val
            waits.append((src, val))
        return waits

    def _record(self, tok, reads, writes):
        for k in writes:
            self.last_w[k] = tok
            self.readers[k] = []
        for k in reads:
            self.readers.setdefault(k, []).append(tok)

    def op(self, eng, fn, reads=(), writes=(), inc=True):
        waits = self._deps(eng, reads, writes)
        if inc:
            self.cnt[eng] += 1
            idx = self.cnt[eng]
        else:
            idx = self.cnt[eng] + 1
        tok = (eng, idx)
        self._record(tok, reads, writes)
        self.ops[eng].append((waits, fn, ("eng", eng) if inc else None))
        return tok

    def dma(self, q, out_ap, in_ap, reads=(), writes=(), final=False):
        i = self.dma_rr[q]
        self.dma_rr[q] = (i + 1) % NDMA_SEMS
        src = ("dma", q, i)
        prev = self.dma_val.get(src, 0)
        waits = self._deps(q, reads, writes)
        if prev and self.waited[q].get(src, 0) < prev:
            self.waited[q][src] = prev
            waits.append((src, prev))
        val = prev + 16
        self.dma_val[src] = val
        tok = (src, val)
        self._record(tok, reads, writes)

        def fn(e, out_ap=out_ap, in_ap=in_ap):
            return e.dma_start(out=out_ap, in_=in_ap)

        self.ops[q].append((waits, fn, ("dma", src)))
        if final:
            self.final_tokens.append(tok)
        return tok

    def barrier(self):
        snap = [(e, self.cnt[e]) for e in self.ENGS if self.cnt[e] > 0]
        snap += [(src, v) for src, v in self.dma_val.items()]
        for e in self.ENGS:
            waits = []
            for src, val in snap:
                if src == e:
                    continue
                if self.waited[e].get(src, 0) >= val:
                    continue
                self.waited[e][src] = val
                waits.append((src, val))
            if waits:
                self.ops[e].append((waits, None, None))

    def emit(self, nc, sems):
        fin = list(self.final_tokens)

        def replay(eng, e):
            for waits, fn, inc in self.ops[eng]:
                for src, val in waits:
                    e.wait_ge(sems[src], val)
                if fn is None:
                    continue
                ins = fn(e)
                if inc is not None:
                    if inc[0] == "eng":
                        ins.then_inc(sems[inc[1]], 1)
                    else:
                        ins.then_inc(sems[inc[1]], 16)
            if eng == "sp":
                for src, val in fin:
                    e.wait_ge(sems[src], val)

        with nc.Block() as block:
            @block.tensor
            def _(e):
                replay("pe", e)

            @block.scalar
            def _(e):
                replay("act", e)

            @block.vector
            def _(e):
                replay("dve", e)

            @block.gpsimd
            def _(e):
                replay("pool", e)

            @block.sync
            def _(e):
                replay("sp", e)


def _consts():
    pos = np.arange(S, dtype=np.float64)
    c = {}
    for dim, nm in ((64, "64"), (32, "32")):
        inv = 1.0 / (10000.0 ** (np.arange(0, dim, 2, dtype=np.float64) / dim))
        inv = inv.astype(np.float32).astype(np.float64)
        ang = (pos.astype(np.float32)[:, None] * inv.astype(np.float32)[None, :]).astype(np.float32)
        c["cos" + nm] = np.cos(ang.astype(np.float64)).astype(np.float32)
        c["sin" + nm] = np.sin(ang.astype(np.float64)).astype(np.float32)
    c["ident"] = np.eye(128, dtype=np.float32)
    fv = np.concatenate([2.0 ** -(np.arange(16) + 1.0), 2.0 ** -np.arange(16).astype(np.float64)])
    c["fvec"] = np.tile(fv.astype(np.float32)[None, :], (128, 1))
    qi = np.arange(128)[:, None]
    kj = np.arange(256)[None, :]
    diff = qi + 128 - kj
    kk = np.arange(128)[None, :]
    c["negmask"] = np.where(kk <= qi, 0.0, NEG).astype(np.float32)
    c["causalT"] = (qi <= kk).astype(np.float32)
    c["maskPC"] = np.concatenate([(qi > kk).astype(np.float32), c["causalT"]], axis=1)
    return c


def build_program(nlayers=DEPTH, debug_mix=False, stop_after=None):
    nc = bass.Bass("TRN2", target_bir_lowering=False)

    def din(name, shape):
        return nc.dram_tensor(name, list(shape), F32, kind="ExternalInput").ap()

    x_d = din("x", [S, D])
    w_in_d = din("w_in", [DEPTH, D, 1892])
    sinks_d = din("attn_sinks", [DEPTH, 8])
    gq_d = din("c_q_norm_g", [DEPTH, 256])
    gkv_d = din("c_kv_norm_g", [DEPTH, 128])
    w_uq_d = din("w_uq", [DEPTH, 256, 384])
    w_ukv_d = din("w_ukv", [DEPTH, 128, 512])
    w_out_d = din("w_out", [DEPTH, D, D])
    ln1g_d = din("ln1_g", [DEPTH, D])
    ln1b_d = din("ln1_b", [DEPTH, D])
    w_router_d = din("w_router", [D, 16])
    rbias_d = din("router_bias", [16])
    w_gate_d = din("w_gate", [DEPTH, 16, D, 256])
    w_up_d = din("w_up", [DEPTH, 16, D, 256])
    w_down_d = din("w_down", [DEPTH, 16, 256, D])
    ln2g_d = din("ln2_g", [DEPTH, D])
    ln2b_d = din("ln2_b", [DEPTH, D])
    cos64_d = din("cos64", [S, 32])
    sin64_d = din("sin64", [S, 32])
    cos32_d = din("cos32", [S, 16])
    sin32_d = din("sin32", [S, 16])
    ident_d = din("ident", [128, 128])
    fvec_d = din("fvec", [128, 32])
    negmask_d = din("negmask", [128, 128])
    causalT_d = din("causalT", [128, 128])
    maskPC_d = din("maskPC", [128, 256])
    out_d = nc.dram_tensor("out", [S, D], F32, kind="ExternalOutput").ap()
    dbg_d = None
    if debug_mix:
        dbg_d = nc.dram_tensor("dbg", [S, D], F32, kind="ExternalOutput").ap()

    P = Prog()
    st = contextlib.ExitStack()
    with st:
        sems = {}
        for e in Prog.ENGS:
            sems[e] = st.enter_context(nc.semaphore("s_" + e))
        for q in ("sp", "pool"):
            for i in range(NDMA_SEMS):
                sems[("dma", q, i)] = st.enter_context(nc.semaphore(f"d_{q}{i}"))

        def T(name, shape, dt):
            return st.enter_context(nc.sbuf_tensor("sb_" + name, list(shape), dt))

        X = T("X", [128, NT, D], F32)
        xT = T("xT", [128, 8, S], BF16)
        cos64 = T("cos64", [128, NT, 32], F32)
        sin64 = T("sin64", [128, NT, 32], F32)
        nsin64 = T("nsin64", [128, NT, 32], F32)
        cos32 = T("cos32", [128, NT, 16], F32)
        sin32 = T("sin32", [128, NT, 16], F32)
        nsin32 = T("nsin32", [128, NT, 16], F32)
        identf = T("identf", [128, 128], F32)
        fvec = T("fvec", [128, 32], F32)
        identb = T("identb", [128, 128], BF16)
        negmask = T("negmask", [128, 128], F32)
        causalT = T("causalT", [128, 128], BF16)
        maskPC = T("maskPC", [128, 256], BF16)
        ones1 = T("ones1", [1, 128], F32)
        lnp = T("lnp", [128, 2, D], F32)
        gates = T("gates", [128, NT, 16], F32)
        widx = T("widx", [128, NT, 4], F32)
        wr = T("wr", [128, 8, 16], F32)
        rb = T("rb", [128, 16], F32)
        sinks = T("sinks", [128, 8], F32)
        gq = T("gq", [128, 256], F32)
        gkv = T("gkv", [128, 128], F32)
        sm = T("sm", [128, 256], F32)
        ARW = 22400
        arena = T("arena", [128, ARW], F32)
        psum = st.enter_context(nc.psum_tensor("psum", [128, 4096], F32))

        def bank(i):
            return psum[:, 512 * i:512 * (i + 1)]

        def bankb(i):
            return psum[:, 512 * i:512 * (i + 1)].bitcast(BF16)

        class Arena:
            def __init__(self):
                self.off = 0

            def f32(self, n):
                o = self.off
                self.off += n
                assert self.off <= ARW, self.off
                return arena[:, o:o + n]

            def bf16(self, n):
                w = (n + 1) // 2
                o = self.off
                self.off += w
                assert self.off <= ARW, self.off
                return arena[:, o:o + w].bitcast(BF16)

        A = Arena()
        WIN = A.bf16(8 * 768)
        WOUT = A.bf16(4 * 1024)
        HSB = [A.f32(768), A.f32(768)]
        phase_qkr_off = A.off
        QKR = [A.f32(768), A.f32(768)]
        SQ = A.f32(768)
        PB = [A.bf16(512), A.bf16(512)]
        PTB = [A.bf16(512), A.bf16(512)]
        ROPET = A.f32(640)
        mixt_off = A.off
        MIXT = A.bf16(512)
        MIXTT = A.bf16(512)
        MIXT_f32 = arena[:, mixt_off:mixt_off + 512]
        phase_base = A.off

        rot = [0]
        rot4 = [0]
        inpipe = [False]

        def rbank():
            i = rot[0]
            rot[0] = (i + 1) % 3
            return i

        def mbank():
            if inpipe[0]:
                return 3
            i = rot4[0]
            rot4[0] = (i + 1) % 4
            return i

        def mm(out, lhsT, rhs, start, stop, reads, writes, inc):
            P.op("pe", lambda e, o=out, l=lhsT, r=rhs, s0=start, s1=stop: e.matmul(o, lhsT=l, rhs=r, start=s0, stop=s1),
                 reads=reads, writes=writes, inc=inc)

        def tr(out, in_, ident, reads, writes, inc=True):
            P.op("pe", lambda e, o=out, i=in_, d=ident: e.transpose(out=o, in_=i, identity=d),
                 reads=reads, writes=writes, inc=inc)

        def act(out, in_, func, reads, writes, bias=None, scale=None, accum=None):
            kw = {}
            if bias is not None:
                kw["bias"] = bias
            if scale is not None:
                kw["scale"] = scale
            if accum is not None:
                kw["accum_out"] = accum
            P.op("act", lambda e, o=out, i=in_, f=func, kw=kw: e.activation(out=o, in_=i, func=f, **kw),
                 reads=reads, writes=writes)

        def tt(out, in0, in1, op, reads, writes, eng="dve"):
            P.op(eng, lambda e, o=out, a=in0, b=in1, p=op: e.tensor_tensor(out=o, in0=a, in1=b, op=p),
                 reads=reads, writes=writes)

        def ts(out, in0, s1, s2, op0, op1, reads, writes, accum=None, eng="dve"):
            def fn(e, o=out, a=in0, s1=s1, s2=s2, op0=op0, op1=op1, accum=accum):
                kw = {}
                if op1 is not None:
                    kw["op1"] = op1
                if accum is not None:
                    kw["accum_out"] = accum
                return e.tensor_scalar(out=o, in0=a, scalar1=s1, scalar2=s2, op0=op0, **kw)
            P.op(eng, fn, reads=reads, writes=writes)

        def stt(out, in0, scalar, in1, op0, op1, reads, writes, eng="dve"):
            P.op(eng, lambda e, o=out, a=in0, s=scalar, b=in1, p0=op0, p1=op1:
                 e.scalar_tensor_tensor(out=o, in0=a, scalar=s, in1=b, op0=p0, op1=p1),
                 reads=reads, writes=writes)

        def cp(eng, out, in_, reads, writes):
            if eng == "act":
                act(out, in_, AF.Copy, reads, writes)
            else:
                P.op(eng, lambda e, o=out, i=in_: e.tensor_copy(out=o, in_=i), reads=reads, writes=writes)

        def red(out, in_, op, reads, writes, absv=False):
            def fn(e, o=out, i=in_, p=op, a=absv):
                if a:
                    return e.tensor_reduce(out=o, in_=i, axis=AX.X, op=p, apply_absolute_value=True)
                return e.tensor_reduce(out=o, in_=i, axis=AX.X, op=p)
            P.op("dve", fn, reads=reads, writes=writes)

        def memset(ap, val, writes, eng="dve"):
            P.op(eng, lambda e, a=ap, v=val: e.memset(a, v), writes=writes)

        def recip(out, in_, reads, writes):
            P.op("dve", lambda e, o=out, i=in_: e.reciprocal(out=o, in_=i), reads=reads, writes=writes)

        bg = []

        def bg_run(k):
            for _ in range(k):
                if not bg:
                    return
                bg.pop(0)()

        def pipeline(items, s1, s2, s3, bg_per_iter=0):
            n = len(items)
            inpipe[0] = True
            for i in range(n + 2):
                if i < n:
                    s1(items[i])
                if 0 <= i - 1 < n:
                    s2(items[i - 1])
                if 0 <= i - 2 < n:
                    s3(items[i - 2])
                if bg_per_iter:
                    bg_run(bg_per_iter)
            bg_run(len(bg))
            inpipe[0] = False

        def run_tiles(Pf, Rf):
            Pf(0)
            for t in range(NT):
                if t + 1 < NT:
                    Pf(t + 1)
                Rf(t)

        def run_stages(Pf, stages):
            ns = len(stages)
            Pf(0)
            for i in range(NT + ns - 1):
                if i < NT:
                    stages[0](i)
                if i + 1 < NT:
                    Pf(i + 1)
                for k, st_ in enumerate(stages):
                    if k >= 1 and 0 <= i - k < NT:
                        st_(i - k)

        def tcols(t):
            return slice(t * 128, (t + 1) * 128)

        PBs = [PB[0], PB[1], PTB[0]]
        pbrot = [0]

        def st1(it):
            b = rbank()
            it["b"] = b
            k = it["kind"]
            if k == "att":
                it["pi"] = pbrot[0]
                pbrot[0] = (pbrot[0] + 1) % 3
                sl = it["slots"]
                for j, s in enumerate(sl):
                    ka, kk = s["K"]
                    qa, qk = s["Q"]
                    mm(bank(b)[:, j * 128:(j + 1) * 128], ka, qa, True, True, kk + qk, [("ps", b)], inc=(j == len(sl) - 1))
            elif k == "idx":
                mm(bank(b)[:, 0:it["n"]], it["lhsT"], it["rhs"], True, True, it["keys"], [("ps", b)], True)
            elif k == "mskT":
                kts = it["kts"]
                for j, kt in enumerate(kts):
                    tr(bankb(b)[:, j * 128:(j + 1) * 128], it["src"][:, kt * 128:(kt + 1) * 128], identb[:],
                       [it["srckey"], "identb"], [("ps", b)], inc=(j == len(kts) - 1))

        def st2(it):
            b = it["b"]
            k = it["kind"]
            if k == "att":
                pi = it["pi"]
                n = 128 * len(it["slots"])
                act(PBs[pi][:, 0:n], bank(b)[:, 0:n], AF.Exp, [("ps", b), "negc"], [("PB", pi)], bias=it["negc"], scale=it["scale"])
                for (c0, c1, view, m_ap, mk) in it["masks"]:
                    pv = PBs[pi][:, c0:c1]
                    if view is not None:
                        pv = pv.rearrange(view[0], **view[1])
                    tt(pv, pv, m_ap, ALU.mult, [("PB", pi)] + mk, [("PB", pi)])
            elif k == "idx":
                ri, n, h, qb, k0 = it["ri"], it["n"], it["h"], it["qb"], it["k0"]
                sco, sk = it["sco"], it["scokey"]
                act(it["rb"][:, 0:n], bank(b)[:, 0:n], AF.Relu, [("ps", b)], ["RB%d" % ri])
                if h == 0:
                    ts(sco[:, k0:k0 + n], it["rb"][:, 0:n], widx[:, qb, 0:1], None, ALU.mult, None,
                       ["RB%d" % ri, ("widx", qb)], [sk], eng=ACC_ENG)
                elif ACC_ENG == "dve":
                    stt(sco[:, k0:k0 + n], it["rb"][:, 0:n], widx[:, qb, h:h + 1], sco[:, k0:k0 + n],
                        ALU.mult, ALU.add, ["RB%d" % ri, ("widx", qb), sk], [sk])
                else:
                    ts(it["rb"][:, 0:n], it["rb"][:, 0:n], widx[:, qb, h:h + 1], None, ALU.mult, None,
                       ["RB%d" % ri, ("widx", qb)], ["RB%d" % ri], eng=ACC_ENG)
                    tt(sco[:, k0:k0 + n], sco[:, k0:k0 + n], it["rb"][:, 0:n], ALU.add, ["RB%d" % ri, sk], [sk], eng=ACC_ENG)
            elif k == "mskT":
                kts = it["kts"]
                nj = len(kts)
                cp("act", it["dst"][:, kts[0]:kts[0] + nj, :],
                   bankb(b)[:, 0:nj * 128].rearrange("p (c n) -> p c n", c=nj), [("ps", b)], [it["dstkey"]])

        def st3(it):
            if it["kind"] != "att":
                return
            pi = it["pi"]
            sl = it["slots"]
            for j, s in enumerate(sl):
                va, vk = s["V"]
                ob = s["ob"]
                mm(bank(ob)[:, s["oreg"]:s["oreg"] + 65], PBs[pi][:, j * 128:(j + 1) * 128], va, s["start"], s["stop"],
                   [("PB", pi)] + vk, [("ps", ob)], inc=(j == len(sl) - 1 or sl[j + 1]["ob"] != ob))
            if it.get("fin") is not None:
                it["fin"]()

        def run_seq(seq, bg_per_iter=0):
            n = len(seq)
            inpipe[0] = True
            for i in range(n + 2):
                if i < n and seq[i][0] is not None:
                    st1(seq[i][0])
                if 0 <= i - 1 < n and seq[i - 1][0] is not None:
                    st2(seq[i - 1][0])
                if 0 <= i - 2 < n and seq[i - 2][0] is not None:
                    st3(seq[i - 2][0])
                if i < n:
                    for c in seq[i][1]:
                        c()
                if bg_per_iter:
                    bg_run(bg_per_iter)
            bg_run(len(bg))
            inpipe[0] = False

        def fin_generic(qb, ob, nheads, nchunks_w, mix_c0, epilogue):
            def fn():
                o3 = bank(ob)[:, 0:nheads * 65].rearrange("p (h d) -> p h d", h=nheads)
                rc = sm[:, 72:72 + nheads]
                recip(rc, o3[:, :, 64], [("ps", ob)], ["rc"])
                tt(MIXT[:, 0:nheads * 64].rearrange("p (h d) -> p h d", h=nheads), o3[:, :, 0:64],
                   rc.unsqueeze(2).to_broadcast([128, nheads, 64]), ALU.mult, [("ps", ob), "rc"], ["MIXT"])
                dbg_store(qb, mix_c0, nheads * 64)
                out_proj_partial(qb, nchunks_w, False, after=epilogue)
            return fn

        def tm(ap_d):
            return ap_d.rearrange("(t p) d -> p t d", p=128)

        P.dma("sp", cos64[:], tm(cos64_d), writes=["cos64"])
        P.dma("sp", sin64[:], tm(sin64_d), writes=["sin64"])
        P.dma("sp", cos32[:], tm(cos32_d), writes=["cos32"])
        P.dma("sp", sin32[:], tm(sin32_d), writes=["sin32"])
        P.dma("sp", identf[:], ident_d, writes=["identf"])
        P.dma("sp", fvec[:], fvec_d, writes=["fvec"])
        P.dma("pool", identb[:], ident_d, writes=["identb"])
        P.dma("pool", causalT[:], causalT_d, writes=["causalT"])
        P.dma("pool", maskPC[:], maskPC_d, writes=["maskPC"])
        P.dma("sp", negmask[:], negmask_d, writes=["negmask"])
        P.dma("sp", wr[:], w_router_d.rearrange("(c p) n -> p c n", p=128), writes=["wr"])
        P.dma("sp", rb[:], rbias_d.partition_broadcast(128), writes=["rb"])
        xv = tm(x_d)
        for t4 in range(4):
            P.dma("sp", X[:, 4 * t4:4 * t4 + 4, :], xv[:, 4 * t4:4 * t4 + 4, :],
                  writes=[("X", t) for t in range(4 * t4, 4 * t4 + 4)])
        ts(nsin64[:], sin64[:], -1.0, None, ALU.mult, None, ["sin64"], ["nsin64"])
        ts(nsin32[:], sin32[:], -1.0, None, ALU.mult, None, ["sin32"], ["nsin32"])
        memset(ones1[:], 1.0, ["ones1"])

        def build_xT(t):
            for half in range(2):
                b = mbank()
                for j in range(4):
                    c = half * 4 + j
                    tr(bank(b)[:, j * 128:(j + 1) * 128], X[:, t, c * 128:(c + 1) * 128], identf[:],
                       [("X", t), "identf"], [("ps", b)], inc=(j == 3))
                cp("act" if half == 0 else "dve",
                   xT[:, half * 4:half * 4 + 4, tcols(t)],
                   bank(b).rearrange("p (c n) -> p c n", c=4),
                   [("ps", b)], [("xT", t)])

        def in_proj(t, ncols, wview, hs):
            n0 = 0
            while n0 < ncols:
                n1 = min(ncols, n0 + 512)
                b = mbank()
                for c in range(8):
                    mm(bank(b)[:, 0:n1 - n0], xT[:, c, tcols(t)], wview[:, c, n0:n1], c == 0, c == 7,
                       [("xT", t), "WIN"], [("ps", b)], inc=(c == 7))
                cp("act", hs[:, n0:n1], bank(b)[:, 0:n1 - n0], [("ps", b)], ["HSB%d" % (t % 2)])
                n0 = n1

        def rope(src, dst, nh, hd, t, cosT, sinT, nsinT, rk, wk, tmp=None):
            h2 = hd // 2
            cb = cosT[:, t, :].unsqueeze(1).unsqueeze(1).to_broadcast([128, nh, 2, h2])
            sb = sinT[:, t, :].unsqueeze(1).to_broadcast([128, nh, h2])
            nb = nsinT[:, t, :].unsqueeze(1).to_broadcast([128, nh, h2])
            tv = ROPET[:, 0:nh * hd].rearrange("p (h t d) -> p h t d", h=nh, t=2)
            rk = rk + ["cos64", "sin64", "nsin64", "cos32", "sin32", "nsin32"]
            tt(dst, src, cb, ALU.mult, rk, wk)
            tt(tv[:, :, 0, :], src[:, :, 1, :], nb, ALU.mult, rk, ["ropetmp"])
            tt(tv[:, :, 1, :], src[:, :, 0, :], sb, ALU.mult, rk, ["ropetmp"])
            tt(dst, dst, tv, ALU.add, wk + ["ropetmp"], wk)

        def global_bound(mt, nh, qsl, ksl, scale, negc):
            b = mbank()
            tr(bank(b)[0:nh, 0:128], mt, identf[:], ["mt", "identf"], [("ps", b)])
            red(sm[0:nh, 0:1], bank(b)[0:nh, 0:128], ALU.max, [("ps", b)], ["gb1"])
            b2 = mbank()
            tr(bank(b2)[0:1, 0:nh], sm[0:nh, 0:1], identf[0:nh, 0:nh], ["gb1", "identf"], [("ps", b2)])
            red(sm[0:1, 1:2], bank(b2)[0:1, qsl], ALU.max, [("ps", b2)], ["gb2"])
            red(sm[0:1, 2:3], bank(b2)[0:1, ksl], ALU.max, [("ps", b2)], ["gb3"])
            tt(sm[0:1, 3:4], sm[0:1, 1:2], sm[0:1, 2:3], ALU.mult, ["gb2", "gb3"], ["gb4"])
            act(sm[0:1, 4:5], sm[0:1, 3:4], AF.Ln, ["gb4"], ["gb5"])
            act(sm[0:1, 5:6], sm[0:1, 4:5], AF.Exp, ["gb5"], ["gb6"], scale=0.5)
            ts(sm[0:1, 6:7], sm[0:1, 5:6], -scale, None, ALU.mult, None, ["gb6"], ["gb7"])
            b3 = mbank()
            mm(bank(b3)[:, 0:1], ones1[0:1, :], sm[0:1, 6:7], True, True, ["gb7", "ones1"], [("ps", b3)], True)
            cp("dve", negc, bank(b3)[:, 0:1], [("ps", b3)], ["negc"])

        def head_sumsq(src, ncols, nh, mt, first, rk):
            act(SQ[:, 0:ncols], src, AF.Square, rk, ["SQ"])
            hd = ncols // nh
            if first:
                red(mt, SQ[:, 0:ncols].rearrange("p (h d) -> p h d", h=nh), ALU.add, ["SQ"], ["mt"])
            else:
                red(sm[:, 16:16 + nh], SQ[:, 0:ncols].rearrange("p (h d) -> p h d", h=nh), ALU.add, ["SQ"], ["hs"])
                tt(mt, mt, sm[:, 16:16 + nh], ALU.max, ["mt", "hs"], ["mt"])

        def out_proj_T(qb, nchunks):
            b = mbank()
            for c in range(nchunks):
                tr(bankb(b)[:, c * 128:(c + 1) * 128], MIXT[:, c * 128:(c + 1) * 128], identb[:],
                   ["MIXT", "identb"], [("ps", b)], inc=(c == nchunks - 1))
            cp("act", MIXTT[:, 0:nchunks * 128], bankb(b)[:, 0:nchunks * 128], [("ps", b)], ["MIXTT"])

        def out_proj_M(qb, nchunks, first):
            wv = WOUT.rearrange("p (c n) -> p c n", n=1024)
            for half in range(2):
                yb = 6 + half
                for c in range(nchunks):
                    mm(bank(yb), MIXTT[:, c * 128:(c + 1) * 128], wv[:, c, half * 512:(half + 1) * 512],
                       c == 0, c == nchunks - 1, ["MIXTT", "WOUT"], [("ps", yb)], inc=(c == nchunks - 1))
                xs = X[:, qb, half * 512:(half + 1) * 512]
                if first:
                    stt(xs, xs, ALPHA, bank(yb), ALU.mult, ALU.add, [("X", qb), ("ps", yb)], [("X", qb)])
                else:
                    tt(xs, xs, bank(yb), ALU.add, [("X", qb), ("ps", yb)], [("X", qb)])

        def out_proj_partial(qb, nchunks, first, after=None):
            bg.append(lambda: None)
            bg.append(lambda: out_proj_T(qb, nchunks))

            def part2():
                out_proj_M(qb, nchunks, first)
                if after is not None:
                    after(qb)
            bg.append(part2)

        def dbg_store(qb, c0, ncols):
            if dbg_d is None:
                return
            cp("dve", ROPET[:, 0:ncols], MIXT[:, 0:ncols], ["MIXT"], ["ropetmp"])
            P.dma("sp", tm(dbg_d)[:, qb, c0:c0 + ncols], ROPET[:, 0:ncols], reads=["ropetmp"], final=True)

        def ln_stats(t, k):
            o = 32 + 16 * k
            ks = "ln%d" % k
            P.op("dve", lambda e, o_=sm[:, o:o + 6], i=X[:, t, 0:512]: e.bn_stats(out=o_, in_=i), reads=[("X", t)], writes=[ks + "a"])
            P.op("dve", lambda e, o_=sm[:, o + 6:o + 12], i=X[:, t, 512:1024]: e.bn_stats(out=o_, in_=i), reads=[("X", t)], writes=[ks + "b"])
            P.op("dve", lambda e, o_=sm[:, o + 12:o + 14], i=sm[:, o:o + 12]: e.bn_aggr(out=o_, in_=i), reads=[ks + "a", ks + "b"], writes=[ks + "mv"])
            act(sm[:, o + 14:o + 15], sm[:, o + 13:o + 14], AF.Ln, [ks + "mv"], [ks + "lv"], bias=1e-5)
            act(sm[:, o + 15:o + 16], sm[:, o + 14:o + 15], AF.Exp, [ks + "lv"], [ks + "rs"], scale=-0.5)

        def ln_apply(t, k):
            o = 32 + 16 * k
            ks = "ln%d" % k
            xs = X[:, t, :]
            ts(xs, xs, sm[:, o + 12:o + 13], sm[:, o + 15:o + 16], ALU.subtract, ALU.mult, [("X", t), ks + "mv", ks + "rs"], [("X", t)])
            tt(xs, xs, lnp[:, 0, :], ALU.mult, [("X", t), "lnp"], [("X", t)], eng=LNP_ENG)
            tt(xs, xs, lnp[:, 1, :], ALU.add, [("X", t), "lnp"], [("X", t)], eng=LNP_ENG)

        def layer_norm(t, k=0):
            ln_stats(t, k)
            ln_apply(t, k)

        def load_ln(g_d, b_d, l):
            P.dma("sp", lnp[:, 0, :], g_d[l].partition_broadcast(128), writes=["lnp"])
            P.dma("sp", lnp[:, 1, :], b_d[l].partition_broadcast(128), writes=["lnp"])

        def phase_A(l):
            A.off = phase_base
            FM = A.bf16(6 * S).rearrange("p (c n) -> p c n", c=6)
            VA = A.bf16(NT * 2 * 65).rearrange("p (t g d) -> p t g d", t=NT, g=2)
            mt = A.f32(16)[:, 0:10]
            negc = A.f32(2)[:, 0:1]
            esink = A.f32(8)
            den = A.f32(8)
            wv = WIN[:, 0:8 * 768].rearrange("p (c n) -> p c n", c=8)
            P.dma("pool", wv, w_in_d[l].rearrange("(c p) n -> p c n", p=128)[:, :, 0:768], writes=["WIN"])
            P.dma("pool", WOUT[:, 0:4096].rearrange("p (c n) -> p c n", c=4),
                  w_out_d[l, 0:512, :].rearrange("(c p) n -> p c n", p=128), writes=["WOUT"])
            P.dma("sp", sinks[:], sinks_d[l].partition_broadcast(128), writes=["sinks"])
            memset(VA[:, :, :, 64:65], 1.0, ["VAones"])
            def Pf(t):
                in_proj(t, 768, wv, HSB[t % 2])

            def Rf(t):
                hs = HSB[t % 2]
                qk = QKR[t % 2]
                hk = "HSB%d" % (t % 2)
                qkk = "QKR%d" % (t % 2)
                head_sumsq(hs[:, 0:640], 640, 10, mt, t == 0, [hk])
                rope(hs[:, 0:512].rearrange("p (h t d) -> p h t d", h=8, t=2),
                     qk[:, 0:512].rearrange("p (h t d) -> p h t d", h=8, t=2),
                     8, 64, t, cos64, sin64, nsin64, [hk], [qkk])
                kdst = qk[:, 512:768].rearrange("p (g r d) -> p g r d", g=2, r=2)
                rope(hs[:, 512:640].rearrange("p (h t d) -> p h t d", h=2, t=2),
                     kdst[:, :, 0, :].rearrange("p g (t d) -> p g t d", t=2),
                     2, 64, t, cos64, sin64, nsin64, [hk], [qkk])
                cp("dve", kdst[:, :, 1, :], kdst[:, :, 0, :], [qkk], [qkk])
                cp("act", VA[:, t, :, 0:64], hs[:, 640:768].rearrange("p (g d) -> p g d", g=2), [hk], [("VA", t)])

            def Rb(t):
                qk = QKR[t % 2]
                qkk = "QKR%d" % (t % 2)
                for part, (c0, nchk) in enumerate(((0, 4), (4, 2))):
                    b = mbank()
                    for j in range(nchk):
                        c = c0 + j
                        tr(bank(b)[:, j * 128:(j + 1) * 128], qk[:, c * 128:(c + 1) * 128], identf[:],
                           [qkk, "identf"], [("ps", b)], inc=(j == nchk - 1))
                    cp("act", FM[:, c0:c0 + nchk, tcols(t)],
                       bank(b)[:, 0:nchk * 128].rearrange("p (c n) -> p c n", c=nchk),
                       [("ps", b)], [("FM", t)])

            run_stages(Pf, [Rf, Rb])
            if stop_after == "A1":
                return
            global_bound(mt, 10, slice(0, 8), slice(8, 10), SC64, negc)
            act(esink, sinks[:], AF.Exp, ["sinks", "negc"], ["esink"], bias=negc)
            if stop_after == "A2":
                return

            def finA(qb, g):
                def fn():
                    ob = 4 + g
                    o3 = bank(ob)[:, 0:260].rearrange("p (h d) -> p h d", h=4)
                    tt(den[:, 4 * g:4 * g + 4], o3[:, :, 64], esink[:, 4 * g:4 * g + 4], ALU.add,
                       [("ps", ob), "esink"], ["den"])
                    recip(den[:, 4 * g:4 * g + 4], den[:, 4 * g:4 * g + 4], ["den"], ["den"])
                    tt(MIXT[:, g * 256:(g + 1) * 256].rearrange("p (h d) -> p h d", h=4), o3[:, :, 0:64],
                       den[:, 4 * g:4 * g + 4].unsqueeze(2).to_broadcast([128, 4, 64]), ALU.mult,
                       [("ps", ob), "den"], ["MIXT"])
                    if g == 1:
                        dbg_store(qb, 0, 512)
                        out_proj_partial(qb, 4, True)
                return fn

            seq = []
            for qb in range(NT):
                kts = [kt for kt in (qb - 1, qb) if kt >= 0]
                nk_ = len(kts)
                for g in range(2):
                    for par in range(2):
                        po = 64 * par
                        slots = []
                        for h in (4 * g + par, 4 * g + par + 2):
                            for kt in kts:
                                slots.append({"K": (FM[po:po + 64, 4 + g, tcols(kt)], [("FM", kt)]),
                                              "Q": (FM[po:po + 64, h // 2, tcols(qb)], [("FM", qb)]),
                                              "V": (VA[:, kt, g, :], [("VA", kt), "VAones"]),
                                              "ob": 4 + g, "oreg": (h % 4) * 65,
                                              "start": kt == kts[0], "stop": kt == kts[-1]})
                        if nk_ == 2:
                            masks = [(0, 512, ("p (h n) -> p h n", {"h": 2}),
                                      maskPC[:].unsqueeze(1).to_broadcast([128, 2, 256]), ["maskPC"])]
                        else:
                            masks = [(0, 256, ("p (h n) -> p h n", {"h": 2}),
                                      maskPC[:, 128:256].unsqueeze(1).to_broadcast([128, 2, 128]), ["maskPC"])]
                        seq.append(({"kind": "att", "slots": slots, "scale": SC64, "negc": negc, "masks": masks,
                                     "fin": finA(qb, g) if par == 1 else None}, []))
            run_seq(seq, bg_per_iter=1)

        def phase_B(l):
            A.off = phase_base
            FM = A.bf16(6 * S).rearrange("p (c n) -> p c n", c=6)
            VB = A.bf16(NT * 65 + 1)[:, 0:NT * 65].rearrange("p (t d) -> p t d", t=NT)
            SCO = A.f32(S)
            MSK = A.bf16(S)
            RB = [HSB[0][:, 0:512], HSB[1][:, 0:512]]
            mt = A.f32(16)[:, 0:5]
            negc = A.f32(2)[:, 0:1]
            bis = A.f32(8)
            btab = A.f32(32)
            wv = WIN[:, 0:8 * 708].rearrange("p (c n) -> p c n", c=8)
            P.dma("pool", wv, w_in_d[l].rearrange("(c p) n -> p c n", p=128)[:, :, 768:1476], writes=["WIN"])
            P.dma("pool", WOUT[:, 0:2048].rearrange("p (c n) -> p c n", c=2),
                  w_out_d[l, 512:768, :].rearrange("(c p) n -> p c n", p=128), writes=["WOUT"])
            memset(VB[:, :, 64:65], 1.0, ["VBones"])
            def Pf(t):
                in_proj(t, 708, wv, HSB[t % 2])

            def Rf(t):
                hs = HSB[t % 2]
                qk = QKR[t % 2]
                hk = "HSB%d" % (t % 2)
                qkk = "QKR%d" % (t % 2)
                head_sumsq(hs[:, 0:320], 320, 5, mt, t == 0, [hk])
                for (s0, d0) in ((0, 0), (384, 384)):
                    rope(hs[:, s0:s0 + 320].rearrange("p (h t d) -> p h t d", h=5, t=2),
                         qk[:, d0:d0 + 320].rearrange("p (h t d) -> p h t d", h=5, t=2),
                         5, 64, t, cos64, sin64, nsin64, [hk], [qkk])
                    cp("dve", qk[:, d0 + 320:d0 + 384], qk[:, d0 + 256:d0 + 320], [qkk], [qkk])
                cp("act", VB[:, t, 0:64], hs[:, 320:384], [hk], [("VB", t)])
                ts(widx[:, t, :], hs[:, 704:708], IDXW, None, ALU.mult, None, [hk], [("widx", t)])

            def Rb(t):
                qk = QKR[t % 2]
                qkk = "QKR%d" % (t % 2)
                for part, (c0, nchk) in enumerate(((0, 4), (4, 2))):
                    b = mbank()
                    for j in range(nchk):
                        c = c0 + j
                        tr(bank(b)[:, j * 128:(j + 1) * 128], qk[:, c * 128:(c + 1) * 128], identf[:],
                           [qkk, "identf"], [("ps", b)], inc=(j == nchk - 1))
                    cp("act", FM[:, c0:c0 + nchk, tcols(t)],
                       bank(b)[:, 0:nchk * 128].rearrange("p (c n) -> p c n", c=nchk),
                       [("ps", b)], [("FM", t)])

            run_stages(Pf, [Rf, Rb])
            global_bound(mt, 5, slice(0, 4), slice(4, 5), SC64, negc)

            P.barrier()
            SCOs = [SCO, arena[:, 0:S]]
            a_qkr = phase_qkr_off
            MSKT = [arena[:, a_qkr + 1024 * i:a_qkr + 1024 * (i + 1)].bitcast(BF16).rearrange("p (t n) -> p t n", t=NT)
                    for i in range(2)]

            def idx_items(qb):
                nk = (qb + 1) * 128
                par = qb % 2
                out = []
                for k0 in range(0, nk, 512):
                    n = min(512, nk - k0)
                    for h in range(4):
                        po = 64 * (h % 2)
                        ri = (len(out)) % 2
                        out.append({"kind": "idx", "n": n, "h": h, "qb": qb, "k0": k0, "ri": ri, "rb": RB[ri],
                                    "lhsT": FM[po:po + 64, 3 + h // 2, tcols(qb)], "rhs": FM[po:po + 64, 5, k0:k0 + n],
                                    "keys": [("FM", t) for t in range(qb + 1)],
                                    "sco": SCOs[par], "scokey": "SCO%d" % par})
                return out

            def bis_closures(qb):
                nk = (qb + 1) * 128
                par = qb % 2
                sco = SCOs[par]
                sk = "SCO%d" % par
                cl = []

                def init():
                    if qb >= 2:
                        red(bis[:, 0:1], sco[:, 0:nk], ALU.max, [sk], ["bnd"], absv=True)
                    tt(sco[:, qb * 128:nk], sco[:, qb * 128:nk], negmask[:], ALU.add, [sk, "negmask"], [sk])
                    if qb >= 2:
                        ts(bis[:, 2:3], bis[:, 0:1], 2.0, 2.0, ALU.mult, ALU.add, ["bnd"], ["w0"])
                        ts(btab[:], fvec[:], bis[:, 2:3], None, ALU.mult, None, ["w0", "fvec"], ["btab"])
                        memset(bis[:, 3:4], 0.0, ["mid"])
                    else:
                        ts(MSK[:, 0:nk], sco[:, 0:nk], -1.0e29, None, ALU.is_ge, None, [sk], ["MSK"])
                cl.append(init)
                if qb >= 2:
                    def it_fn(k):
                        def fn():
                            ts(MSK[:, 0:nk], sco[:, 0:nk], bis[:, 3:4], None, ALU.is_ge, ALU.add, [sk, "mid"],
                               ["MSK", "cnt"], accum=bis[:, 4:5])
                            if k < NBIS - 1:
                                ts(bis[:, 5:6], bis[:, 4:5], float(TOPK), btab[:, 16 + k + 1:16 + k + 2], ALU.is_ge, ALU.mult,
                                   ["cnt", "btab"], ["stp"])
                                stt(bis[:, 3:4], bis[:, 5:6], btab[:, k + 1:k + 2], bis[:, 3:4], ALU.subtract, ALU.add,
                                    ["stp", "btab", "mid"], ["mid"])
                            else:
                                ts(bis[:, 5:6], bis[:, 4:5], float(TOPK), btab[:, k:k + 1], ALU.is_ge, ALU.mult,
                                   ["cnt", "btab"], ["stp"])
                                stt(bis[:, 1:2], bis[:, 5:6], btab[:, k:k + 1], bis[:, 3:4], ALU.subtract, ALU.add,
                                    ["stp", "btab", "mid"], ["lo"])
                        return fn
                    for k in range(NBIS):
                        cl.append(it_fn(k))
                    cl.append(lambda: ts(MSK[:, 0:nk], sco[:, 0:nk], bis[:, 1:2], None, ALU.is_ge, None, [sk, "lo"], ["MSK"]))
                return cl

            def mskT_items(qb):
                par = qb % 2
                out = []
                for kt0 in range(0, qb + 1, 4):
                    kts = list(range(kt0, min(kt0 + 4, qb + 1)))
                    out.append({"kind": "mskT", "kts": kts, "src": MSK, "srckey": "MSK",
                                "dst": MSKT[par], "dstkey": ("MSKT", par)})
                return out

            def att_items(qb):
                par = qb % 2
                out = []
                for h in range(4):
                    po = 64 * (h % 2)
                    for kt0 in range(0, qb + 1, 4):
                        kts = list(range(kt0, min(kt0 + 4, qb + 1)))
                        slots = [{"K": (FM[po:po + 64, 2, tcols(kt)], [("FM", kt)]),
                                  "Q": (FM[po:po + 64, h // 2, tcols(qb)], [("FM", qb)]),
                                  "V": (VB[:, kt, :], [("VB", kt), "VBones"]),
                                  "ob": 4 + par, "oreg": h * 65, "start": kt == 0, "stop": kt == qb} for kt in kts]
                        ns = len(kts)
                        masks = [(0, 128 * ns, ("p (c n) -> p c n", {"c": ns}), MSKT[par][:, kt0:kt0 + ns, :], [("MSKT", par)])]
                        last = (h == 3 and kts[-1] == qb)
                        out.append({"kind": "att", "slots": slots, "scale": SC64, "negc": negc, "masks": masks,
                                    "fin": fin_generic(qb, 4 + par, 4, 2, 512, None) if last else None})
                return out

            def merge(a, b):
                out = []
                na, nb = len(a), len(b)
                ia = ib = 0
                while ia < na or ib < nb:
                    if ib >= nb or (ia < na and ia * nb <= ib * na):
                        out.append(a[ia])
                        ia += 1
                    else:
                        out.append(b[ib])
                        ib += 1
                return out

            seq = []
            for r in range(-2, NT):
                ia = att_items(r) if r >= 0 else []
                ii = idx_items(r + 2) if r + 2 < NT else []
                cl = bis_closures(r + 1) if 0 <= r + 1 < NT else []
                im = mskT_items(r + 1) if 0 <= r + 1 < NT else []
                ents = [[it, []] for it in merge(ia, ii)]
                if not ents:
                    ents = [[None, []]]
                ne = len(ents)
                for ci, c in enumerate(cl):
                    ents[min(ne - 1, (ci * ne) // max(1, len(cl)))][1].append(c)
                seq.extend((e[0], e[1]) for e in ents)
                seq.extend((it, []) for it in im)
            run_seq(seq, bg_per_iter=1)

        def phase_C(l):
            A.off = phase_base
            FQ = A.bf16(4 * S).rearrange("p (c n) -> p c n", c=4)
            FK = A.bf16(4 * S).rearrange("p (c n) -> p c n", c=4)
            VC = A.bf16(NT * 4 * 65).rearrange("p (t h d) -> p t h d", t=NT, h=4)
            QCF = [QKR[0][:, 0:384], QKR[0][:, 384:768]]
            KCF = [QKR[1][:, 0:384], QKR[1][:, 384:768]]
            CQN = [A.bf16(256), A.bf16(256)]
            CKN = [A.bf16(128), A.bf16(128)]
            CQT = [A.bf16(256), A.bf16(256)]
            CKT = [A.bf16(128), A.bf16(128)]
            mt = A.f32(16)[:, 0:8]
            negc = A.f32(2)[:, 0:1]
            rt = A.f32(128)
            rtk = [A.f32(32), A.f32(32)]
            SQJ = MIXT_f32
            wv = WIN[:, 0:8 * 416].rearrange("p (c n) -> p c n", c=8)
            WUQ = WIN[:, 8 * 416:8 * 416 + 768].rearrange("p (c n) -> p c n", c=2)
            WUKV = WIN[:, 8 * 416 + 768:8 * 416 + 768 + 512]
            P.dma("pool", wv, w_in_d[l].rearrange("(c p) n -> p c n", p=128)[:, :, 1476:1892], writes=["WIN"])
            P.dma("pool", WUQ, w_uq_d[l].rearrange("(c p) n -> p c n", p=128), writes=["WIN"])
            P.dma("pool", WUKV, w_ukv_d[l], writes=["WIN"])
            P.dma("pool", WOUT[:, 0:2048].rearrange("p (c n) -> p c n", c=2),
                  w_out_d[l, 768:1024, :].rearrange("(c p) n -> p c n", p=128), writes=["WOUT"])
            P.dma("sp", gq[:], gq_d[l].partition_broadcast(128), writes=["gq"])
            P.dma("sp", gkv[:], gkv_d[l].partition_broadcast(128), writes=["gkv"])
            load_ln(ln1g_d, ln1b_d, l)
            memset(VC[:, :, :, 64:65], 1.0, ["VCones"])
            def Pf(t):
                in_proj(t, 416, wv, HSB[t % 2])

            def c1(t):
                p = t % 2
                hs = HSB[p]
                hk = "HSB%d" % p
                o = 80 + 8 * p
                ck = "c1s%d" % p
                act(SQJ[:, 0:256], hs[:, 0:256], AF.Square, [hk], ["SQJ"], accum=sm[:, o:o + 1])
                act(SQJ[:, 256:384], hs[:, 256:384], AF.Square, [hk], ["SQJ2"], accum=sm[:, o + 1:o + 2])
                act(sm[:, o + 2:o + 3], sm[:, o:o + 1], AF.Ln, ["SQJ"], [ck + "l1"], scale=1.0 / 256, bias=1e-6)
                act(sm[:, o + 3:o + 4], sm[:, o + 1:o + 2], AF.Ln, ["SQJ2"], [ck + "l2"], scale=1.0 / 128, bias=1e-6)
                act(sm[:, o + 4:o + 5], sm[:, o + 2:o + 3], AF.Exp, [ck + "l1"], [ck + "r1"], scale=-0.5)
                act(sm[:, o + 5:o + 6], sm[:, o + 3:o + 4], AF.Exp, [ck + "l2"], [ck + "r2"], scale=-0.5)
                stt(CQN[p][:, 0:256], hs[:, 0:256], sm[:, o + 4:o + 5], gq[:], ALU.mult, ALU.mult, [hk, ck + "r1", "gq"], ["CQN%d" % p])
                stt(CKN[p][:, 0:128], hs[:, 256:384], sm[:, o + 5:o + 6], gkv[:], ALU.mult, ALU.mult, [hk, ck + "r2", "gkv"], ["CKN%d" % p])
                rope(hs[:, 384:416].rearrange("p (h t d) -> p h t d", h=1, t=2),
                     rtk[p][:, 0:32].rearrange("p (h t d) -> p h t d", h=1, t=2), 1, 32, t,
                     cos32, sin32, nsin32, [hk], ["rtk%d" % p])

            def c23(t):
                p = t % 2
                qck, kck = "QCF%d" % p, "KCF%d" % p
                b = mbank()
                tr(bankb(b)[:, 0:128], CQN[p][:, 0:128], identb[:], ["CQN%d" % p, "identb"], [("ps", b)], inc=False)
                tr(bankb(b)[:, 128:256], CQN[p][:, 128:256], identb[:], ["CQN%d" % p, "identb"], [("ps", b)], inc=False)
                tr(bankb(b)[:, 256:384], CKN[p][:, 0:128], identb[:], ["CKN%d" % p, "identb"], [("ps", b)], inc=True)
                cp("act", CQT[p][:, 0:256], bankb(b)[:, 0:256], [("ps", b)], ["CQT%d" % p])
                cp("dve", CKT[p][:, 0:128], bankb(b)[:, 256:384], [("ps", b)], ["CKT%d" % p])
                bq = mbank()
                mm(bank(bq)[:, 0:384], CQT[p][:, 0:128], WUQ[:, 0, :], True, False, ["CQT%d" % p, "WIN"], [("ps", bq)], False)
                mm(bank(bq)[:, 0:384], CQT[p][:, 128:256], WUQ[:, 1, :], False, True, ["CQT%d" % p, "WIN"], [("ps", bq)], True)
                bk = mbank()
                mm(bank(bk)[:, 0:512], CKT[p][:, 0:128], WUKV, True, True, ["CKT%d" % p, "WIN"], [("ps", bk)], True)
                cp("act", QCF[p], bank(bq)[:, 0:384], [("ps", bq)], [qck])
                q4 = QCF[p].rearrange("p (h d) -> p h d", h=4)
                qr = q4[:, :, 64:96].rearrange("p h (t d) -> p h t d", t=2)
                cp("dve", rt[:, 0:128].rearrange("p (h d) -> p h d", h=4), q4[:, :, 64:96], [qck], ["rt"])
                rope(rt[:, 0:128].rearrange("p (h t d) -> p h t d", h=4, t=2), qr, 4, 32, t,
                     cos32, sin32, nsin32, ["rt"], [qck])
                kv4 = bank(bk)[:, 0:512].rearrange("p (h d) -> p h d", h=4)
                k4 = KCF[p].rearrange("p (h d) -> p h d", h=4)
                cp("act", k4[:, :, 0:64], kv4[:, :, 0:64], [("ps", bk)], [kck])
                cp("dve", VC[:, t, :, 0:64], kv4[:, :, 64:128], [("ps", bk)], [("VC", t)])
                cp("dve", k4[:, :, 64:96], rtk[p][:, 0:32].unsqueeze(1).to_broadcast([128, 4, 32]), ["rtk%d" % p], [kck])
                act(SQ[:, 0:384], QCF[p], AF.Square, [qck], ["SQ"])
                act(SQ[:, 384:768], KCF[p], AF.Square, [kck], ["SQ"])
                if t == 0:
                    red(mt, SQ[:, 0:768].rearrange("p (h d) -> p h d", h=8), ALU.add, ["SQ"], ["mt"])
                else:
                    red(sm[:, 16:24], SQ[:, 0:768].rearrange("p (h d) -> p h d", h=8), ALU.add, ["SQ"], ["hs"])
                    tt(mt, mt, sm[:, 16:24], ALU.max, ["mt", "hs"], ["mt"])

            def c4(t):
                p = t % 2
                for (src_, dstF, key, fkey, eng) in ((QCF[p], FQ, "QCF%d" % p, "FQKR0", "act"), (KCF[p], FK, "KCF%d" % p, "FQKR1", "dve")):
                    b = mbank()
                    for h in range(4):
                        tr(bank(b)[0:96, h * 128:(h + 1) * 128], src_[:, h * 96:(h + 1) * 96], identf[:],
                           [key, "identf"], [("ps", b)], inc=(h == 3))
                    cp(eng, dstF[0:96, :, tcols(t)], bank(b)[0:96, :].rearrange("p (c n) -> p c n", c=4),
                       [("ps", b)], [(fkey, t)])

            run_stages(Pf, [c1, c23, c4])
            global_bound(mt, 8, slice(0, 4), slice(4, 8), SC96, negc)

            P.barrier()
            RT = arena[:, phase_qkr_off:phase_qkr_off + 2304]

            def xpose(qb, half):
                b = mbank()
                for j in range(4):
                    c = half * 4 + j
                    tr(bank(b)[:, j * 128:(j + 1) * 128], X[:, qb, c * 128:(c + 1) * 128], identf[:],
                       [("X", qb), "identf"], [("ps", b)], inc=(j == 3))
                cp("act", xT[:, half * 4:half * 4 + 4, tcols(qb)], bank(b).rearrange("p (c n) -> p c n", c=4),
                   [("ps", b)], [("xT", qb)])
                cp("dve", HSB[half][:, 0:512], bank(b), [("ps", b)], ["HSB%d" % half])

            def router_a(qb):
                b = mbank()
                for c in range(8):
                    mm(bank(b)[:, 0:16], HSB[c // 4][:, (c % 4) * 128:(c % 4 + 1) * 128], wr[:, c, :], c == 0, c == 7,
                       ["HSB0", "HSB1", "wr"], [("ps", b)], inc=(c == 7))
                act(RT[:, qb * 16:(qb + 1) * 16], bank(b)[:, 0:16], AF.Exp, [("ps", b)], [("r_sc", qb)], scale=-1.0)

            def finC(qb, ob):
                def fn():
                    o3 = bank(ob)[:, 0:260].rearrange("p (h d) -> p h d", h=4)
                    rc = sm[:, 72:76]
                    recip(rc, o3[:, :, 64], [("ps", ob)], ["rc"])
                    tt(MIXT[:, 0:256].rearrange("p (h d) -> p h d", h=4), o3[:, :, 0:64],
                       rc.unsqueeze(2).to_broadcast([128, 4, 64]), ALU.mult, [("ps", ob), "rc"], ["MIXT"])
                    dbg_store(qb, 768, 256)
                    pv = qb - 1
                    if pv >= 0:
                        bg.append(lambda: xpose(pv, 0))
                        bg.append(lambda: None)
                    bg.append(lambda: out_proj_T(qb, 2))
                    if pv >= 0:
                        bg.append(lambda: xpose(pv, 1))
                    bg.append(lambda: out_proj_M(qb, 2, False))
                    if pv >= 0:
                        bg.append(lambda: router_a(pv))
                    bg.append(lambda: ln_stats(qb, qb % 2))
                    bg.append(lambda: ln_apply(qb, qb % 2))
                return fn

            seq = []
            for qb in range(NT):
                par = qb % 2
                for h in range(4):
                    for kt0 in range(0, qb + 1, 4):
                        kts = list(range(kt0, min(kt0 + 4, qb + 1)))
                        slots = [{"K": (FK[0:96, h, tcols(kt)], [("FQKR1", kt)]),
                                  "Q": (FQ[0:96, h, tcols(qb)], [("FQKR0", qb)]),
                                  "V": (VC[:, kt, h, :], [("VC", kt), "VCones"]),
                                  "ob": 4 + par, "oreg": h * 65, "start": kt == 0, "stop": kt == qb} for kt in kts]
                        ns = len(kts)
                        masks = []
                        if kts[-1] == qb:
                            masks = [(128 * (ns - 1), 128 * ns, None, causalT[:], ["causalT"])]
                        last = (h == 3 and kts[-1] == qb)
                        seq.append(({"kind": "att", "slots": slots, "scale": SC96, "negc": negc, "masks": masks,
                                     "fin": finC(qb, 4 + par) if last else None}, []))
            run_seq(seq, bg_per_iter=2)
            xpose(NT - 1, 0)
            xpose(NT - 1, 1)
            router_a(NT - 1)

            sc = RT[:, 0:256]
            bi = RT[:, 256:512]
            m1 = RT[:, 512:576]
            eq = RT[:, 576:832]
            msk = RT[:, 832:1088]
            m2 = RT[:, 1088:1152]
            gs = RT[:, 1152:1216]
            gm = RT[:, 1216:1232]
            gsel = RT[:, 1232:1296]
            t2 = RT[:, 1296:1552]
            wgt = RT[:, 1552:1808]
            ws = RT[:, 1808:1824]
            rsk = [("r_sc", t) for t in range(NT)]

            def v3(ap, a):
                return ap.rearrange("p (a b) -> p a b", a=a)

            ts(sc, sc, 1.0, None, ALU.add, None, rsk, ["r_s"])
            recip(sc, sc, ["r_s"], ["r_s"])
            tt(v3(bi, 16), v3(sc, 16), rb[:].unsqueeze(1).to_broadcast([128, 16, 16]), ALU.add, ["r_s", "rb"], ["r_bi"])
            red(m1, v3(bi, 64), ALU.max, ["r_bi"], ["r_m1"])
            tt(v3(eq, 64), v3(bi, 64), m1.unsqueeze(2).to_broadcast([128, 64, 4]), ALU.is_equal, ["r_bi", "r_m1"], ["r_eq"])
            stt(msk, eq, NEG, bi, ALU.mult, ALU.add, ["r_eq", "r_bi"], ["r_msk"])
            red(m2, v3(msk, 64), ALU.max, ["r_msk"], ["r_m2"])
            tt(gs, m1, m2, ALU.add, ["r_m1", "r_m2"], ["r_gs"])
            red(gm, v3(gs, 16), ALU.max, ["r_gs"], ["r_gm"])
            tt(v3(gsel, 16), v3(gs, 16), gm.unsqueeze(2).to_broadcast([128, 16, 4]), ALU.is_equal, ["r_gs", "r_gm"], ["r_gsel"])
            tt(v3(t2, 64), v3(bi, 64), m2.unsqueeze(2).to_broadcast([128, 64, 4]), ALU.is_ge, ["r_bi", "r_m2"], ["r_t2"])
            tt(v3(t2, 64), v3(t2, 64), gsel.unsqueeze(2).to_broadcast([128, 64, 4]), ALU.mult, ["r_t2", "r_gsel"], ["r_t2"])
            tt(wgt, sc, t2, ALU.mult, ["r_s", "r_t2"], ["r_w"])
            red(ws, v3(wgt, 16), ALU.add, ["r_w"], ["r_ws"])
            recip(ws, ws, ["r_ws"], ["r_ws"])
            tt(gates[:], v3(wgt, 16), ws.unsqueeze(2).to_broadcast([128, 16, 16]), ALU.mult, ["r_w", "r_ws"],
               [("gates", t) for t in range(NT)])

        def phase_moe(l, last_layer):
            A.off = 0
            NSLOT = 7
            WGU = [A.bf16(8 * 512).rearrange("p (c n) -> p c n", c=8) for _ in range(NSLOT)]
            WD = [A.bf16(2 * 1024).rearrange("p (c n) -> p c n", c=2) for _ in range(NSLOT)]
            SB = [A.bf16(256) for _ in range(2)]
            TB = [A.bf16(256) for _ in range(2)]
            TTB = [A.bf16(256) for _ in range(2)]
            load_ln(ln2g_d, ln2b_d, l)
            def load_expert(e):
                s = e % NSLOT
                P.dma("pool", WGU[s][:, :, 0:256], w_gate_d[l, e].rearrange("(c p) f -> p c f", p=128), writes=[("WGU", s)])
                P.dma("pool", WGU[s][:, :, 256:512], w_up_d[l, e].rearrange("(c p) f -> p c f", p=128), writes=[("WGU", s)])
                P.dma("pool", WD[s], w_down_d[l, e].rearrange("(c p) d -> p c d", p=128), writes=[("WD", s)])

            for e in range(NSLOT):
                load_expert(e)
            items = [(0, t, e) for e in range(4) for t in range(NT)]
            items += [(G, t, e) for G in range(1, 4) for t in range(NT) for e in range(4 * G, 4 * G + 4)]
            sd = {}
            cnt = [0]

            def s1(it):
                G, t, e = it
                s = e % NSLOT
                b = rbank()
                sd[it] = {"b": b, "i": cnt[0] % 2}
                cnt[0] += 1
                for c in range(8):
                    mm(bank(b), xT[:, c, tcols(t)], WGU[s][:, c, :], c == 0, c == 7,
                       [("xT", t), ("WGU", s)], [("ps", b)], inc=(c == 7))

            def s2(it):
                G, t, e = it
                d = sd[it]
                b, i = d["b"], d["i"]
                act(SB[i][:, 0:256], bank(b)[:, 0:256], AF.Silu, [("ps", b)], [("SB", i)])
                stt(TB[i][:, 0:256], bank(b)[:, 256:512], gates[:, t, e:e + 1], SB[i][:, 0:256], ALU.mult, ALU.mult,
                    [("ps", b), ("gates", t), ("SB", i)], [("TB", i)])
                b2 = rbank()
                d["b2"] = b2
                for fc in range(2):
                    tr(bankb(b2)[:, fc * 128:(fc + 1) * 128], TB[i][:, fc * 128:(fc + 1) * 128], identb[:],
                       [("TB", i), "identb"], [("ps", b2)], inc=(fc == 1))

            def s3(it):
                G, t, e = it
                d = sd[it]
                b2, i = d["b2"], d["i"]
                s = e % NSLOT
                cp("act", TTB[i][:, 0:256], bankb(b2)[:, 0:256], [("ps", b2)], [("TTB", i)])
                first = (e % 4 == 0) or G == 0
                lastx = (e % 4 == 3) or G == 0
                for half in range(2):
                    yb = 4 + 2 * (t % 2) + half
                    for fc in range(2):
                        mm(bank(yb), TTB[i][:, fc * 128:(fc + 1) * 128], WD[s][:, fc, half * 512:(half + 1) * 512],
                           first and fc == 0, lastx and fc == 1, [("TTB", i), ("WD", s)], [("ps", yb)],
                           inc=(fc == 1))
                if t == NT - 1 and e + NSLOT < 16:
                    load_expert(e + NSLOT)
                if lastx:
                    for half in range(2):
                        yb = 4 + 2 * (t % 2) + half
                        xs = X[:, t, half * 512:(half + 1) * 512]
                        if e == 0:
                            stt(xs, xs, ALPHA, bank(yb), ALU.mult, ALU.add, [("X", t), ("ps", yb)], [("X", t)])
                        else:
                            tt(xs, xs, bank(yb), ALU.add, [("X", t), ("ps", yb)], [("X", t)])
                    if G == 3:
                        k = t % 2
                        o = 32 + 16 * k
                        mvo = 96 + 2 * t

                        def st_a(t=t, o=o, k=k):
                            P.op("dve", lambda e, o_=sm[:, o:o + 6], i=X[:, t, 0:512]: e.bn_stats(out=o_, in_=i),
                                 reads=[("X", t)], writes=["bs%da" % k])

                        def st_b(t=t, o=o, k=k, mvo=mvo):
                            P.op("dve", lambda e, o_=sm[:, o + 6:o + 12], i=X[:, t, 512:1024]: e.bn_stats(out=o_, in_=i),
                                 reads=[("X", t)], writes=["bs%db" % k])
                            P.op("dve", lambda e, o_=sm[:, mvo:mvo + 2], i=sm[:, o:o + 12]: e.bn_aggr(out=o_, in_=i),
                                 reads=["bs%da" % k, "bs%db" % k], writes=[("mv2", t)])

                        bg.append(st_a)
                        bg.append(st_b)
                        if t % 8 == 7:
                            t0 = t - 7

                            def rstd8(t0=t0):
                                var8 = sm[:, 96 + 2 * t0:96 + 2 * t0 + 16].rearrange("p (t two) -> p t two", two=2)[:, :, 1]
                                rs8 = sm[:, 128 + t0:128 + t0 + 8]
                                act(rs8, var8, AF.Ln, [("mv2", u) for u in range(t0, t0 + 8)], [("rs2", t0)], bias=1e-5)
                                act(rs8, rs8, AF.Exp, [("rs2", t0)], [("rs2", t0)], scale=-0.5)
                            bg.append(rstd8)
                            for u in range(t0, t0 + 8):
                                for half in range(2):
                                    hsl = slice(half * 512, (half + 1) * 512)

                                    def ap1(u=u, t0=t0, hsl=hsl):
                                        xs = X[:, u, hsl]
                                        ts(xs, xs, sm[:, 96 + 2 * u:96 + 2 * u + 1], sm[:, 128 + u:128 + u + 1], ALU.subtract, ALU.mult,
                                           [("X", u), ("mv2", u), ("rs2", t0)], [("X", u)])

                                    def ap2(u=u, hsl=hsl):
                                        xs = X[:, u, hsl]
                                        tt(xs, xs, lnp[:, 0, hsl], ALU.mult, [("X", u), "lnp"], [("X", u)])

                                    def ap3(u=u, hsl=hsl, half=half):
                                        xs = X[:, u, hsl]
                                        tt(xs, xs, lnp[:, 1, hsl], ALU.add, [("X", u), "lnp"], [("X", u)])
                                        if last_layer and half == 1:
                                            P.dma("sp", tm(out_d)[:, u, :], X[:, u, :], reads=[("X", u)], final=True)
                                    bg.append(ap1)
                                    bg.append(ap2)
                                    bg.append(ap3)

            pipeline(items, s1, s2, s3, bg_per_iter=2)

        def run_layers():
            for l in range(nlayers):
                for t in range(NT):
                    build_xT(t)
                if stop_after == "xT":
                    return False
                phase_A(l)
                P.barrier()
                if stop_after in ("A", "A1", "A2"):
                    return False
                phase_B(l)
                P.barrier()
                if stop_after == "B":
                    return False
                phase_C(l)
                P.barrier()
                if stop_after == "C":
                    return False
                phase_moe(l, l == nlayers - 1)
                P.barrier()
            return True

        if not run_layers():
            for t in range(NT):
                P.dma("sp", tm(out_d)[:, t, :], X[:, t, :], reads=[("X", t)], final=True)

        P.emit(nc, sems)
    return nc


_CACHE = {}


def kernel(**inputs):
    consts = _consts()
    if "nc" not in _CACHE:
        _CACHE["nc"] = build_program()
    nc = _CACHE["nc"]
    x = np.ascontiguousarray(np.asarray(inputs["x"], dtype=np.float32))
    shared = {}
    for k, v in inputs.items():
        if k == "x":
            continue
        shared[k] = np.ascontiguousarray(np.asarray(v, dtype=np.float32))
    shared.update(consts)
    in_maps = []
    for c in range(NCORES):
        m = dict(shared)
        m["x"] = x[c]
        in_maps.append(m)
    res = run_bass_kernel_spmd(nc, in_maps, core_ids=list(range(NCORES)))
    out = np.stack([np.asarray(r["out"], dtype=np.float32) for r in res.results], axis=0)
    return out
```

```python
import contextlib
import numpy as np
import concourse.bass as bass
import concourse.mybir as mybir
from concourse.bass_utils import run_bass_kernel_spmd

F32 = mybir.dt.float32
BF16 = mybir.dt.bfloat16
AF = mybir.ActivationFunctionType
ALU = mybir.AluOpType
AX = mybir.AxisListType

S = 2048
D = 1024
NT = 16
NCORES = 8
DEPTH = 2
ALPHA = float((2 * DEPTH) ** 0.25)
IDXW = float((4 * 64) ** -0.5)
SC64 = float(64 ** -0.5)
SC96 = float(96 ** -0.5)
TOPK = 256
NBIS = 14
NEG = -1.0e30
NDMA_SEMS = 8
ACC_ENG = "dve"
LNP_ENG = "dve"


class Prog:
    ENGS = ("pe", "act", "dve", "pool", "sp")

    def __init__(self):
        self.ops = {e: [] for e in self.ENGS}
        self.cnt = {e: 0 for e in self.ENGS}
        self.last_w = {}
        self.readers = {}
        self.waited = {e: {} for e in self.ENGS}
        self.dma_val = {}
        self.dma_rr = {"sp": 0, "pool": 0}
        self.final_tokens = []

    def _deps(self, eng, reads, writes):
        deps = {}

        def add(tok, raw):
            src, val = tok
            if src == eng and eng == "pe":
                return
            if deps.get(src, 0) < val:
                deps[src] = val

        for k in reads:
            if k in self.last_w:
                add(self.last_w[k], True)
            if isinstance(k, tuple) and k[0] == "ps":
                for r in self.readers.get(k, ()):
                    if r[0] != eng:
                        add(r, False)
        for k in writes:
            if k in self.last_w:
                add(self.last_w[k], False)
            for r in self.readers.get(k, ()):
                add(r, False)
        waits = []
        for src, val in deps.items():
            if self.waited[eng].get(src, 0) >= val:
                continue
            self.waited[eng][src] = val
            waits.append((src, val))
        return waits

    def _record(self, tok, reads, writes):
        for k in writes:
            self.last_w[k] = tok
            self.readers[k] = []
        for k in reads:
            self.readers.setdefault(k, []).append(tok)

    def op(self, eng, fn, reads=(), writes=(), inc=True):
        waits = self._deps(eng, reads, writes)
        if inc:
            self.cnt[eng] += 1
            idx = self.cnt[eng]
        else:
            idx = self.cnt[eng] + 1
        tok = (eng, idx)
        self._record(tok, reads, writes)
        self.ops[eng].append((waits, fn, ("eng", eng) if inc else None))
        return tok

    def dma(self, q, out_ap, in_ap, reads=(), writes=(), final=False):
        i = self.dma_rr[q]
        self.dma_rr[q] = (i + 1) % NDMA_SEMS
        src = ("dma", q, i)
        prev = self.dma_val.get(src, 0)
        waits = self._deps(q, reads, writes)
        if prev and self.waited[q].get(src, 0) < prev:
            self.waited[q][src] = prev
            waits.append((src, prev))
        val = prev + 16
        self.dma_val[src] = val
        tok = (src, val)
        self._record(tok, reads, writes)

        def fn(e, out_ap=out_ap, in_ap=in_ap):
            return e.dma_start(out=out_ap, in_=in_ap)

        self.ops[q].append((waits, fn, ("dma", src)))
        if final:
            self.final_tokens.append(tok)
        return tok

    def barrier(self):
        snap = [(e, self.cnt[e]) for e in self.ENGS if self.cnt[e] > 0]
        snap += [(src, v) for src, v in self.dma_val.items()]
        for e in self.ENGS:
            waits = []
            for src, val in snap:
                if src == e:
                    continue
                if self.waited[e].get(src, 0) >= val:
                    continue
                self.waited[e][src] = val
                waits.append((src, val))
            if waits:
                self.ops[e].append((waits, None, None))

    def emit(self, nc, sems):
        fin = list(self.final_tokens)

        def replay(eng, e):
            for waits, fn, inc in self.ops[eng]:
                for src, val in waits:
                    e.wait_ge(sems[src], val)
                if fn is None:
                    continue
                ins = fn(e)
                if inc is not None:
                    if inc[0] == "eng":
                        ins.then_inc(sems[inc[1]], 1)
                    else:
                        ins.then_inc(sems[inc[1]], 16)
            if eng == "sp":
                for src, val in fin:
                    e.wait_ge(sems[src], val)

        with nc.Block() as block:
            @block.tensor
            def _(e):
                replay("pe", e)

            @block.scalar
            def _(e):
                replay("act", e)

            @block.vector
            def _(e):
                replay("dve", e)

            @block.gpsimd
            def _(e):
                replay("pool", e)

            @block.sync
            def _(e):
                replay("sp", e)


def _consts():
    pos = np.arange(S, dtype=np.float64)
    c = {}
    for dim, nm in ((64, "64"), (32, "32")):
        inv = 1.0 / (10000.0 ** (np.arange(0, dim, 2, dtype=np.float64) / dim))
        inv = inv.astype(np.float32).astype(np.float64)
        ang = (pos.astype(np.float32)[:, None] * inv.astype(np.float32)[None, :]).astype(np.float32)
        c["cos" + nm] = np.cos(ang.astype(np.float64)).astype(np.float32)
        c["sin" + nm] = np.sin(ang.astype(np.float64)).astype(np.float32)
    c["ident"] = np.eye(128, dtype=np.float32)
    fv = np.concatenate([2.0 ** -(np.arange(16) + 1.0), 2.0 ** -np.arange(16).astype(np.float64)])
    c["fvec"] = np.tile(fv.astype(np.float32)[None, :], (128, 1))
    qi = np.arange(128)[:, None]
    kj = np.arange(256)[None, :]
    diff = qi + 128 - kj
    kk = np.arange(128)[None, :]
    c["negmask"] = np.where(kk <= qi, 0.0, NEG).astype(np.float32)
    c["causalT"] = (qi <= kk).astype(np.float32)
    c["maskPC"] = np.concatenate([(qi > kk).astype(np.float32), c["causalT"]], axis=1)
    return c


def build_program(nlayers=DEPTH, debug_mix=False, stop_after=None):
    nc = bass.Bass("TRN2", target_bir_lowering=False)

    def din(name, shape):
        return nc.dram_tensor(name, list(shape), F32, kind="ExternalInput").ap()

    x_d = din("x", [S, D])
    w_in_d = din("w_in", [DEPTH, D, 1892])
    sinks_d = din("attn_sinks", [DEPTH, 8])
    gq_d = din("c_q_norm_g", [DEPTH, 256])
    gkv_d = din("c_kv_norm_g", [DEPTH, 128])
    w_uq_d = din("w_uq", [DEPTH, 256, 384])
    w_ukv_d = din("w_ukv", [DEPTH, 128, 512])
    w_out_d = din("w_out", [DEPTH, D, D])
    ln1g_d = din("ln1_g", [DEPTH, D])
    ln1b_d = din("ln1_b", [DEPTH, D])
    w_router_d = din("w_router", [D, 16])
    rbias_d = din("router_bias", [16])
    w_gate_d = din("w_gate", [DEPTH, 16, D, 256])
    w_up_d = din("w_up", [DEPTH, 16, D, 256])
    w_down_d = din("w_down", [DEPTH, 16, 256, D])
    ln2g_d = din("ln2_g", [DEPTH, D])
    ln2b_d = din("ln2_b", [DEPTH, D])
    cos64_d = din("cos64", [S, 32])
    sin64_d = din("sin64", [S, 32])
    cos32_d = din("cos32", [S, 16])
    sin32_d = din("sin32", [S, 16])
    ident_d = din("ident", [128, 128])
    fvec_d = din("fvec", [128, 32])
    negmask_d = din("negmask", [128, 128])
    causalT_d = din("causalT", [128, 128])
    maskPC_d = din("maskPC", [128, 256])
    out_d = nc.dram_tensor("out", [S, D], F32, kind="ExternalOutput").ap()
    dbg_d = None
    if debug_mix:
        dbg_d = nc.dram_tensor("dbg", [S, D], F32, kind="ExternalOutput").ap()

    P = Prog()
    st = contextlib.ExitStack()
    with st:
        sems = {}
        for e in Prog.ENGS:
            sems[e] = st.enter_context(nc.semaphore("s_" + e))
        for q in ("sp", "pool"):
            for i in range(NDMA_SEMS):
                sems[("dma", q, i)] = st.enter_context(nc.semaphore(f"d_{q}{i}"))

        def T(name, shape, dt):
            return st.enter_context(nc.sbuf_tensor("sb_" + name, list(shape), dt))

        X = T("X", [128, NT, D], F32)
        xT = T("xT", [128, 8, S], BF16)
        cos64 = T("cos64", [128, NT, 32], F32)
        sin64 = T("sin64", [128, NT, 32], F32)
        nsin64 = T("nsin64", [128, NT, 32], F32)
        cos32 = T("cos32", [128, NT, 16], F32)
        sin32 = T("sin32", [128, NT, 16], F32)
        nsin32 = T("nsin32", [128, NT, 16], F32)
        identf = T("identf", [128, 128], F32)
        fvec = T("fvec", [128, 32], F32)
        identb = T("identb", [128, 128], BF16)
        negmask = T("negmask", [128, 128], F32)
        causalT = T("causalT", [128, 128], BF16)
        maskPC = T("maskPC", [128, 256], BF16)
        ones1 = T("ones1", [1, 128], F32)
        lnp = T("lnp", [128, 2, D], F32)
        gates = T("gates", [128, NT, 16], F32)
        widx = T("widx", [128, NT, 4], F32)
        wr = T("wr", [128, 8, 16], F32)
        rb = T("rb", [128, 16], F32)
        sinks = T("sinks", [128, 8], F32)
        gq = T("gq", [128, 256], F32)
        gkv = T("gkv", [128, 128], F32)
        sm = T("sm", [128, 256], F32)
        ARW = 22400
        arena = T("arena", [128, ARW], F32)
        psum = st.enter_context(nc.psum_tensor("psum", [128, 4096], F32))

        def bank(i):
            return psum[:, 512 * i:512 * (i + 1)]

        def bankb(i):
            return psum[:, 512 * i:512 * (i + 1)].bitcast(BF16)

        class Arena:
            def __init__(self):
                self.off = 0

            def f32(self, n):
                o = self.off
                self.off += n
                assert self.off <= ARW, self.off
                return arena[:, o:o + n]

            def bf16(self, n):
                w = (n + 1) // 2
                o = self.off
                self.off += w
                assert self.off <= ARW, self.off
                return arena[:, o:o + w].bitcast(BF16)

        A = Arena()
        WIN = A.bf16(8 * 768)
        WOUT = A.bf16(4 * 1024)
        HSB = [A.f32(768), A.f32(768)]
        phase_qkr_off = A.off
        QKR = [A.f32(768), A.f32(768)]
        SQ = A.f32(768)
        PB = [A.bf16(512), A.bf16(512)]
        PTB = [A.bf16(512), A.bf16(512)]
        ROPET = A.f32(640)
        mixt_off = A.off
        MIXT = A.bf16(512)
        MIXTT = A.bf16(512)
        MIXT_f32 = arena[:, mixt_off:mixt_off + 512]
        phase_base = A.off

        rot = [0]
        rot4 = [0]
        inpipe = [False]

        def rbank():
            i = rot[0]
            rot[0] = (i + 1) % 3
            return i

        def mbank():
            if inpipe[0]:
                return 3
            i = rot4[0]
            rot4[0] = (i + 1) % 4
            return i

        def mm(out, lhsT, rhs, start, stop, reads, writes, inc):
            P.op("pe", lambda e, o=out, l=lhsT, r=rhs, s0=start, s1=stop: e.matmul(o, lhsT=l, rhs=r, start=s0, stop=s1),
                 reads=reads, writes=writes, inc=inc)

        def tr(out, in_, ident, reads, writes, inc=True):
            P.op("pe", lambda e, o=out, i=in_, d=ident: e.transpose(out=o, in_=i, identity=d),
                 reads=reads, writes=writes, inc=inc)

        def act(out, in_, func, reads, writes, bias=None, scale=None, accum=None):
            kw = {}
            if bias is not None:
                kw["bias"] = bias
            if scale is not None:
                kw["scale"] = scale
            if accum is not None:
                kw["accum_out"] = accum
            P.op("act", lambda e, o=out, i=in_, f=func, kw=kw: e.activation(out=o, in_=i, func=f, **kw),
                 reads=reads, writes=writes)

        def tt(out, in0, in1, op, reads, writes, eng="dve"):
            P.op(eng, lambda e, o=out, a=in0, b=in1, p=op: e.tensor_tensor(out=o, in0=a, in1=b, op=p),
                 reads=reads, writes=writes)

        def ts(out, in0, s1, s2, op0, op1, reads, writes, accum=None, eng="dve"):
            def fn(e, o=out, a=in0, s1=s1, s2=s2, op0=op0, op1=op1, accum=accum):
                kw = {}
                if op1 is not None:
                    kw["op1"] = op1
                if accum is not None:
                    kw["accum_out"] = accum
                return e.tensor_scalar(out=o, in0=a, scalar1=s1, scalar2=s2, op0=op0, **kw)
            P.op(eng, fn, reads=reads, writes=writes)

        def stt(out, in0, scalar, in1, op0, op1, reads, writes, eng="dve"):
            P.op(eng, lambda e, o=out, a=in0, s=scalar, b=in1, p0=op0, p1=op1:
                 e.scalar_tensor_tensor(out=o, in0=a, scalar=s, in1=b, op0=p0, op1=p1),
                 reads=reads, writes=writes)

        def cp(eng, out, in_, reads, writes):
            if eng == "act":
                act(out, in_, AF.Copy, reads, writes)
            else:
                P.op(eng, lambda e, o=out, i=in_: e.tensor_copy(out=o, in_=i), reads=reads, writes=writes)

        def red(out, in_, op, reads, writes, absv=False):
            def fn(e, o=out, i=in_, p=op, a=absv):
                if a:
                    return e.tensor_reduce(out=o, in_=i, axis=AX.X, op=p, apply_absolute_value=True)
                return e.tensor_reduce(out=o, in_=i, axis=AX.X, op=p)
            P.op("dve", fn, reads=reads, writes=writes)

        def memset(ap, val, writes, eng="dve"):
            P.op(eng, lambda e, a=ap, v=val: e.memset(a, v), writes=writes)

        def recip(out, in_, reads, writes):
            P.op("dve", lambda e, o=out, i=in_: e.reciprocal(out=o, in_=i), reads=reads, writes=writes)

        bg = []

        def bg_run(k):
            for _ in range(k):
                if not bg:
                    return
                bg.pop(0)()

        def pipeline(items, s1, s2, s3, bg_per_iter=0):
            n = len(items)
            inpipe[0] = True
            for i in range(n + 2):
                if i < n:
                    s1(items[i])
                if 0 <= i - 1 < n:
                    s2(items[i - 1])
                if 0 <= i - 2 < n:
                    s3(items[i - 2])
                if bg_per_iter:
                    bg_run(bg_per_iter)
            bg_run(len(bg))
            inpipe[0] = False

        def run_tiles(Pf, Rf):
            Pf(0)
            for t in range(NT):
                if t + 1 < NT:
                    Pf(t + 1)
                Rf(t)

        def run_stages(Pf, stages):
            ns = len(stages)
            Pf(0)
            for i in range(NT + ns - 1):
                if i < NT:
                    stages[0](i)
                if i + 1 < NT:
                    Pf(i + 1)
                for k, st_ in enumerate(stages):
                    if k >= 1 and 0 <= i - k < NT:
                        st_(i - k)

        def tcols(t):
            return slice(t * 128, (t + 1) * 128)

        PBs = [PB[0], PB[1], PTB[0]]
        pbrot = [0]

        def st1(it):
            b = rbank()
            it["b"] = b
            k = it["kind"]
            if k == "att":
                it["pi"] = pbrot[0]
                pbrot[0] = (pbrot[0] + 1) % 3
                sl = it["slots"]
                for j, s in enumerate(sl):
                    ka, kk = s["K"]
                    qa, qk = s["Q"]
                    mm(bank(b)[:, j * 128:(j + 1) * 128], ka, qa, True, True, kk + qk, [("ps", b)], inc=(j == len(sl) - 1))
            elif k == "idx":
                mm(bank(b)[:, 0:it["n"]], it["lhsT"], it["rhs"], True, True, it["keys"], [("ps", b)], True)
            elif k == "mskT":
                kts = it["kts"]
                for j, kt in enumerate(kts):
                    tr(bankb(b)[:, j * 128:(j + 1) * 128], it["src"][:, kt * 128:(kt + 1) * 128], identb[:],
                       [it["srckey"], "identb"], [("ps", b)], inc=(j == len(kts) - 1))

        def st2(it):
            b = it["b"]
            k = it["kind"]
            if k == "att":
                pi = it["pi"]
                n = 128 * len(it["slots"])
                act(PBs[pi][:, 0:n], bank(b)[:, 0:n], AF.Exp, [("ps", b), "negc"], [("PB", pi)], bias=it["negc"], scale=it["scale"])
                for (c0, c1, view, m_ap, mk) in it["masks"]:
                    pv = PBs[pi][:, c0:c1]
                    if view is not None:
                        pv = pv.rearrange(view[0], **view[1])
                    tt(pv, pv, m_ap, ALU.mult, [("PB", pi)] + mk, [("PB", pi)])
            elif k == "idx":
                ri, n, h, qb, k0 = it["ri"], it["n"], it["h"], it["qb"], it["k0"]
                sco, sk = it["sco"], it["scokey"]
                act(it["rb"][:, 0:n], bank(b)[:, 0:n], AF.Relu, [("ps", b)], ["RB%d" % ri])
                if h == 0:
                    ts(sco[:, k0:k0 + n], it["rb"][:, 0:n], widx[:, qb, 0:1], None, ALU.mult, None,
                       ["RB%d" % ri, ("widx", qb)], [sk], eng=ACC_ENG)
                elif ACC_ENG == "dve":
                    stt(sco[:, k0:k0 + n], it["rb"][:, 0:n], widx[:, qb, h:h + 1], sco[:, k0:k0 + n],
                        ALU.mult, ALU.add, ["RB%d" % ri, ("widx", qb), sk], [sk])
                else:
                    ts(it["rb"][:, 0:n], it["rb"][:, 0:n], widx[:, qb, h:h + 1], None, ALU.mult, None,
                       ["RB%d" % ri, ("widx", qb)], ["RB%d" % ri], eng=ACC_ENG)
                    tt(sco[:, k0:k0 + n], sco[:, k0:k0 + n], it["rb"][:, 0:n], ALU.add, ["RB%d" % ri, sk], [sk], eng=ACC_ENG)
            elif k == "mskT":
                kts = it["kts"]
                nj = len(kts)
                cp("act", it["dst"][:, kts[0]:kts[0] + nj, :],
                   bankb(b)[:, 0:nj * 128].rearrange("p (c n) -> p c n", c=nj), [("ps", b)], [it["dstkey"]])

        def st3(it):
            if it["kind"] != "att":
                return
            pi = it["pi"]
            sl = it["slots"]
            for j, s in enumerate(sl):
                va, vk = s["V"]
                ob = s["ob"]
                mm(bank(ob)[:, s["oreg"]:s["oreg"] + 65], PBs[pi][:, j * 128:(j + 1) * 128], va, s["start"], s["stop"],
                   [("PB", pi)] + vk, [("ps", ob)], inc=(j == len(sl) - 1 or sl[j + 1]["ob"] != ob))
            if it.get("fin") is not None:
                it["fin"]()

        def run_seq(seq, bg_per_iter=0):
            n = len(seq)
            inpipe[0] = True
            for i in range(n + 2):
                if i < n and seq[i][0] is not None:
                    st1(seq[i][0])
                if 0 <= i - 1 < n and seq[i - 1][0] is not None:
                    st2(seq[i - 1][0])
                if 0 <= i - 2 < n and seq[i - 2][0] is not None:
                    st3(seq[i - 2][0])
                if i < n:
                    for c in seq[i][1]:
                        c()
                if bg_per_iter:
                    bg_run(bg_per_iter)
            bg_run(len(bg))
            inpipe[0] = False

        def fin_generic(qb, ob, nheads, nchunks_w, mix_c0, epilogue):
            def fn():
                o3 = bank(ob)[:, 0:nheads * 65].rearrange("p (h d) -> p h d", h=nheads)
                rc = sm[:, 72:72 + nheads]
                recip(rc, o3[:, :, 64], [("ps", ob)], ["rc"])
                tt(MIXT[:, 0:nheads * 64].rearrange("p (h d) -> p h d", h=nheads), o3[:, :, 0:64],
                   rc.unsqueeze(2).to_broadcast([128, nheads, 64]), ALU.mult, [("ps", ob), "rc"], ["MIXT"])
                dbg_store(qb, mix_c0, nheads * 64)
                out_proj_partial(qb, nchunks_w, False, after=epilogue)
            return fn

        def tm(ap_d):
            return ap_d.rearrange("(t p) d -> p t d", p=128)

        P.dma("sp", cos64[:], tm(cos64_d), writes=["cos64"])
        P.dma("sp", sin64[:], tm(sin64_d), writes=["sin64"])
        P.dma("sp", cos32[:], tm(cos32_d), writes=["cos32"])
        P.dma("sp", sin32[:], tm(sin32_d), writes=["sin32"])
        P.dma("sp", identf[:], ident_d, writes=["identf"])
        P.dma("sp", fvec[:], fvec_d, writes=["fvec"])
        P.dma("pool", identb[:], ident_d, writes=["identb"])
        P.dma("pool", causalT[:], causalT_d, writes=["causalT"])
        P.dma("pool", maskPC[:], maskPC_d, writes=["maskPC"])
        P.dma("sp", negmask[:], negmask_d, writes=["negmask"])
        P.dma("sp", wr[:], w_router_d.rearrange("(c p) n -> p c n", p=128), writes=["wr"])
        P.dma("sp", rb[:], rbias_d.partition_broadcast(128), writes=["rb"])
        xv = tm(x_d)
        for t4 in range(4):
            P.dma("sp", X[:, 4 * t4:4 * t4 + 4, :], xv[:, 4 * t4:4 * t4 + 4, :],
                  writes=[("X", t) for t in range(4 * t4, 4 * t4 + 4)])
        ts(nsin64[:], sin64[:], -1.0, None, ALU.mult, None, ["sin64"], ["nsin64"])
        ts(nsin32[:], sin32[:], -1.0, None, ALU.mult, None, ["sin32"], ["nsin32"])
        memset(ones1[:], 1.0, ["ones1"])

        def build_xT(t):
            for half in range(2):
                b = mbank()
                for j in range(4):
                    c = half * 4 + j
                    tr(bank(b)[:, j * 128:(j + 1) * 128], X[:, t, c * 128:(c + 1) * 128], identf[:],
                       [("X", t), "identf"], [("ps", b)], inc=(j == 3))
                cp("act" if half == 0 else "dve",
                   xT[:, half * 4:half * 4 + 4, tcols(t)],
                   bank(b).rearrange("p (c n) -> p c n", c=4),
                   [("ps", b)], [("xT", t)])

        def in_proj(t, ncols, wview, hs):
            n0 = 0
            while n0 < ncols:
                n1 = min(ncols, n0 + 512)
                b = mbank()
                for c in range(8):
                    mm(bank(b)[:, 0:n1 - n0], xT[:, c, tcols(t)], wview[:, c, n0:n1], c == 0, c == 7,
                       [("xT", t), "WIN"], [("ps", b)], inc=(c == 7))
                cp("act", hs[:, n0:n1], bank(b)[:, 0:n1 - n0], [("ps", b)], ["HSB%d" % (t % 2)])
                n0 = n1

        def rope(src, dst, nh, hd, t, cosT, sinT, nsinT, rk, wk, tmp=None):
            h2 = hd // 2
            cb = cosT[:, t, :].unsqueeze(1).unsqueeze(1).to_broadcast([128, nh, 2, h2])
            sb = sinT[:, t, :].unsqueeze(1).to_broadcast([128, nh, h2])
            nb = nsinT[:, t, :].unsqueeze(1).to_broadcast([128, nh, h2])
            tv = ROPET[:, 0:nh * hd].rearrange("p (h t d) -> p h t d", h=nh, t=2)
            rk = rk + ["cos64", "sin64", "nsin64", "cos32", "sin32", "nsin32"]
            tt(dst, src, cb, ALU.mult, rk, wk)
            tt(tv[:, :, 0, :], src[:, :, 1, :], nb, ALU.mult, rk, ["ropetmp"])
            tt(tv[:, :, 1, :], src[:, :, 0, :], sb, ALU.mult, rk, ["ropetmp"])
            tt(dst, dst, tv, ALU.add, wk + ["ropetmp"], wk)

        def global_bound(mt, nh, qsl, ksl, scale, negc):
            b = mbank()
            tr(bank(b)[0:nh, 0:128], mt, identf[:], ["mt", "identf"], [("ps", b)])
            red(sm[0:nh, 0:1], bank(b)[0:nh, 0:128], ALU.max, [("ps", b)], ["gb1"])
            b2 = mbank()
            tr(bank(b2)[0:1, 0:nh], sm[0:nh, 0:1], identf[0:nh, 0:nh], ["gb1", "identf"], [("ps", b2)])
            red(sm[0:1, 1:2], bank(b2)[0:1, qsl], ALU.max, [("ps", b2)], ["gb2"])
            red(sm[0:1, 2:3], bank(b2)[0:1, ksl], ALU.max, [("ps", b2)], ["gb3"])
            tt(sm[0:1, 3:4], sm[0:1, 1:2], sm[0:1, 2:3], ALU.mult, ["gb2", "gb3"], ["gb4"])
            act(sm[0:1, 4:5], sm[0:1, 3:4], AF.Ln, ["gb4"], ["gb5"])
            act(sm[0:1, 5:6], sm[0:1, 4:5], AF.Exp, ["gb5"], ["gb6"], scale=0.5)
            ts(sm[0:1, 6:7], sm[0:1, 5:6], -scale, None, ALU.mult, None, ["gb6"], ["gb7"])
            b3 = mbank()
            mm(bank(b3)[:, 0:1], ones1[0:1, :], sm[0:1, 6:7], True, True, ["gb7", "ones1"], [("ps", b3)], True)
            cp("dve", negc, bank(b3)[:, 0:1], [("ps", b3)], ["negc"])

        def head_sumsq(src, ncols, nh, mt, first, rk):
            act(SQ[:, 0:ncols], src, AF.Square, rk, ["SQ"])
            hd = ncols // nh
            if first:
                red(mt, SQ[:, 0:ncols].rearrange("p (h d) -> p h d", h=nh), ALU.add, ["SQ"], ["mt"])
            else:
                red(sm[:, 16:16 + nh], SQ[:, 0:ncols].rearrange("p (h d) -> p h d", h=nh), ALU.add, ["SQ"], ["hs"])
                tt(mt, mt, sm[:, 16:16 + nh], ALU.max, ["mt", "hs"], ["mt"])

        def out_proj_T(qb, nchunks):
            b = mbank()
            for c in range(nchunks):
                tr(bankb(b)[:, c * 128:(c + 1) * 128], MIXT[:, c * 128:(c + 1) * 128], identb[:],
                   ["MIXT", "identb"], [("ps", b)], inc=(c == nchunks - 1))
            cp("act", MIXTT[:, 0:nchunks * 128], bankb(b)[:, 0:nchunks * 128], [("ps", b)], ["MIXTT"])

        def out_proj_M(qb, nchunks, first):
            wv = WOUT.rearrange("p (c n) -> p c n", n=1024)
            for half in range(2):
                yb = 6 + half
                for c in range(nchunks):
                    mm(bank(yb), MIXTT[:, c * 128:(c + 1) * 128], wv[:, c, half * 512:(half + 1) * 512],
                       c == 0, c == nchunks - 1, ["MIXTT", "WOUT"], [("ps", yb)], inc=(c == nchunks - 1))
                xs = X[:, qb, half * 512:(half + 1) * 512]
                if first:
                    stt(xs, xs, ALPHA, bank(yb), ALU.mult, ALU.add, [("X", qb), ("ps", yb)], [("X", qb)])
                else:
                    tt(xs, xs, bank(yb), ALU.add, [("X", qb), ("ps", yb)], [("X", qb)])

        def out_proj_partial(qb, nchunks, first, after=None):
            bg.append(lambda: None)
            bg.append(lambda: out_proj_T(qb, nchunks))

            def part2():
                out_proj_M(qb, nchunks, first)
                if after is not None:
                    after(qb)
            bg.append(part2)

        def dbg_store(qb, c0, ncols):
            if dbg_d is None:
                return
            cp("dve", ROPET[:, 0:ncols], MIXT[:, 0:ncols], ["MIXT"], ["ropetmp"])
            P.dma("sp", tm(dbg_d)[:, qb, c0:c0 + ncols], ROPET[:, 0:ncols], reads=["ropetmp"], final=True)

        def ln_stats(t, k):
            o = 32 + 16 * k
            ks = "ln%d" % k
            P.op("dve", lambda e, o_=sm[:, o:o + 6], i=X[:, t, 0:512]: e.bn_stats(out=o_, in_=i), reads=[("X", t)], writes=[ks + "a"])
            P.op("dve", lambda e, o_=sm[:, o + 6:o + 12], i=X[:, t, 512:1024]: e.bn_stats(out=o_, in_=i), reads=[("X", t)], writes=[ks + "b"])
            P.op("dve", lambda e, o_=sm[:, o + 12:o + 14], i=sm[:, o:o + 12]: e.bn_aggr(out=o_, in_=i), reads=[ks + "a", ks + "b"], writes=[ks + "mv"])
            act(sm[:, o + 14:o + 15], sm[:, o + 13:o + 14], AF.Ln, [ks + "mv"], [ks + "lv"], bias=1e-5)
            act(sm[:, o + 15:o + 16], sm[:, o + 14:o + 15], AF.Exp, [ks + "lv"], [ks + "rs"], scale=-0.5)

        def ln_apply(t, k):
            o = 32 + 16 * k
            ks = "ln%d" % k
            xs = X[:, t, :]
            ts(xs, xs, sm[:, o + 12:o + 13], sm[:, o + 15:o + 16], ALU.subtract, ALU.mult, [("X", t), ks + "mv", ks + "rs"], [("X", t)])
            tt(xs, xs, lnp[:, 0, :], ALU.mult, [("X", t), "lnp"], [("X", t)], eng=LNP_ENG)
            tt(xs, xs, lnp[:, 1, :], ALU.add, [("X", t), "lnp"], [("X", t)], eng=LNP_ENG)

        def layer_norm(t, k=0):
            ln_stats(t, k)
            ln_apply(t, k)

        def load_ln(g_d, b_d, l):
            P.dma("sp", lnp[:, 0, :], g_d[l].partition_broadcast(128), writes=["lnp"])
            P.dma("sp", lnp[:, 1, :], b_d[l].partition_broadcast(128), writes=["lnp"])

        def phase_A(l):
            A.off = phase_base
            FM = A.bf16(6 * S).rearrange("p (c n) -> p c n", c=6)
            VA = A.bf16(NT * 2 * 65).rearrange("p (t g d) -> p t g d", t=NT, g=2)
            mt = A.f32(16)[:, 0:10]
            negc = A.f32(2)[:, 0:1]
            esink = A.f32(8)
            den = A.f32(8)
            wv = WIN[:, 0:8 * 768].rearrange("p (c n) -> p c n", c=8)
            P.dma("pool", wv, w_in_d[l].rearrange("(c p) n -> p c n", p=128)[:, :, 0:768], writes=["WIN"])
            P.dma("pool", WOUT[:, 0:4096].rearrange("p (c n) -> p c n", c=4),
                  w_out_d[l, 0:512, :].rearrange("(c p) n -> p c n", p=128), writes=["WOUT"])
            P.dma("sp", sinks[:], sinks_d[l].partition_broadcast(128), writes=["sinks"])
            memset(VA[:, :, :, 64:65], 1.0, ["VAones"])
            def Pf(t):
                in_proj(t, 768, wv, HSB[t % 2])

            def Rf(t):
                hs = HSB[t % 2]
                qk = QKR[t % 2]
                hk = "HSB%d" % (t % 2)
                qkk = "QKR%d" % (t % 2)
                head_sumsq(hs[:, 0:640], 640, 10, mt, t == 0, [hk])
                rope(hs[:, 0:512].rearrange("p (h t d) -> p h t d", h=8, t=2),
                     qk[:, 0:512].rearrange("p (h t d) -> p h t d", h=8, t=2),
                     8, 64, t, cos64, sin64, nsin64, [hk], [qkk])
                kdst = qk[:, 512:768].rearrange("p (g r d) -> p g r d", g=2, r=2)
                rope(hs[:, 512:640].rearrange("p (h t d) -> p h t d", h=2, t=2),
                     kdst[:, :, 0, :].rearrange("p g (t d) -> p g t d", t=2),
                     2, 64, t, cos64, sin64, nsin64, [hk], [qkk])
                cp("dve", kdst[:, :, 1, :], kdst[:, :, 0, :], [qkk], [qkk])
                cp("act", VA[:, t, :, 0:64], hs[:, 640:768].rearrange("p (g d) -> p g d", g=2), [hk], [("VA", t)])

            def Rb(t):
                qk = QKR[t % 2]
                qkk = "QKR%d" % (t % 2)
                for part, (c0, nchk) in enumerate(((0, 4), (4, 2))):
                    b = mbank()
                    for j in range(nchk):
                        c = c0 + j
                        tr(bank(b)[:, j * 128:(j + 1) * 128], qk[:, c * 128:(c + 1) * 128], identf[:],
                           [qkk, "identf"], [("ps", b)], inc=(j == nchk - 1))
                    cp("act", FM[:, c0:c0 + nchk, tcols(t)],
                       bank(b)[:, 0:nchk * 128].rearrange("p (c n) -> p c n", c=nchk),
                       [("ps", b)], [("FM", t)])

            run_stages(Pf, [Rf, Rb])
            if stop_after == "A1":
                return
            P.dma("pool", WIN[:, 0:8 * 708].rearrange("p (c n) -> p c n", c=8),
                  w_in_d[l].rearrange("(c p) n -> p c n", p=128)[:, :, 768:1476], writes=["WIN"])
            global_bound(mt, 10, slice(0, 8), slice(8, 10), SC64, negc)
            act(esink, sinks[:], AF.Exp, ["sinks", "negc"], ["esink"], bias=negc)
            if stop_after == "A2":
                return

            def finA(qb, g):
                def fn():
                    ob = 4 + g
                    o3 = bank(ob)[:, 0:260].rearrange("p (h d) -> p h d", h=4)
                    tt(den[:, 4 * g:4 * g + 4], o3[:, :, 64], esink[:, 4 * g:4 * g + 4], ALU.add,
                       [("ps", ob), "esink"], ["den"])
                    recip(den[:, 4 * g:4 * g + 4], den[:, 4 * g:4 * g + 4], ["den"], ["den"])
                    tt(MIXT[:, g * 256:(g + 1) * 256].rearrange("p (h d) -> p h d", h=4), o3[:, :, 0:64],
                       den[:, 4 * g:4 * g + 4].unsqueeze(2).to_broadcast([128, 4, 64]), ALU.mult,
                       [("ps", ob), "den"], ["MIXT"])
                    if g == 1:
                        dbg_store(qb, 0, 512)
                        out_proj_partial(qb, 4, True)
                return fn

            seq = []
            for qb in range(NT):
                kts = [kt for kt in (qb - 1, qb) if kt >= 0]
                nk_ = len(kts)
                for g in range(2):
                    for par in range(2):
                        po = 64 * par
                        slots = []
                        for h in (4 * g + par, 4 * g + par + 2):
                            for kt in kts:
                                slots.append({"K": (FM[po:po + 64, 4 + g, tcols(kt)], [("FM", kt)]),
                                              "Q": (FM[po:po + 64, h // 2, tcols(qb)], [("FM", qb)]),
                                              "V": (VA[:, kt, g, :], [("VA", kt), "VAones"]),
                                              "ob": 4 + g, "oreg": (h % 4) * 65,
                                              "start": kt == kts[0], "stop": kt == kts[-1]})
                        if nk_ == 2:
                            masks = [(0, 512, ("p (h n) -> p h n", {"h": 2}),
                                      maskPC[:].unsqueeze(1).to_broadcast([128, 2, 256]), ["maskPC"])]
                        else:
                            masks = [(0, 256, ("p (h n) -> p h n", {"h": 2}),
                                      maskPC[:, 128:256].unsqueeze(1).to_broadcast([128, 2, 128]), ["maskPC"])]
                        seq.append(({"kind": "att", "slots": slots, "scale": SC64, "negc": negc, "masks": masks,
                                     "fin": finA(qb, g) if par == 1 else None}, []))
            run_seq(seq, bg_per_iter=1)

        def phase_B(l):
            A.off = phase_base
            FM = A.bf16(6 * S).rearrange("p (c n) -> p c n", c=6)
            VB = A.bf16(NT * 65 + 1)[:, 0:NT * 65].rearrange("p (t d) -> p t d", t=NT)
            SCO = A.f32(S)
            MSK = A.bf16(S)
            RB = [HSB[0][:, 0:512], HSB[1][:, 0:512]]
            mt = A.f32(16)[:, 0:5]
            negc = A.f32(2)[:, 0:1]
            bis = A.f32(8)
            btab = A.f32(32)
            wv = WIN[:, 0:8 * 708].rearrange("p (c n) -> p c n", c=8)
            P.dma("pool", WOUT[:, 0:2048].rearrange("p (c n) -> p c n", c=2),
                  w_out_d[l, 512:768, :].rearrange("(c p) n -> p c n", p=128), writes=["WOUT"])
            memset(VB[:, :, 64:65], 1.0, ["VBones"])
            def Pf(t):
                in_proj(t, 708, wv, HSB[t % 2])

            def Rf(t):
                hs = HSB[t % 2]
                qk = QKR[t % 2]
                hk = "HSB%d" % (t % 2)
                qkk = "QKR%d" % (t % 2)
                head_sumsq(hs[:, 0:320], 320, 5, mt, t == 0, [hk])
                for (s0, d0) in ((0, 0), (384, 384)):
                    rope(hs[:, s0:s0 + 320].rearrange("p (h t d) -> p h t d", h=5, t=2),
                         qk[:, d0:d0 + 320].rearrange("p (h t d) -> p h t d", h=5, t=2),
                         5, 64, t, cos64, sin64, nsin64, [hk], [qkk])
                    cp("dve", qk[:, d0 + 320:d0 + 384], qk[:, d0 + 256:d0 + 320], [qkk], [qkk])
                cp("act", VB[:, t, 0:64], hs[:, 320:384], [hk], [("VB", t)])
                ts(widx[:, t, :], hs[:, 704:708], IDXW, None, ALU.mult, None, [hk], [("widx", t)])

            def Rb(t):
                qk = QKR[t % 2]
                qkk = "QKR%d" % (t % 2)
                for part, (c0, nchk) in enumerate(((0, 4), (4, 2))):
                    b = mbank()
                    for j in range(nchk):
                        c = c0 + j
                        tr(bank(b)[:, j * 128:(j + 1) * 128], qk[:, c * 128:(c + 1) * 128], identf[:],
                           [qkk, "identf"], [("ps", b)], inc=(j == nchk - 1))
                    cp("act", FM[:, c0:c0 + nchk, tcols(t)],
                       bank(b)[:, 0:nchk * 128].rearrange("p (c n) -> p c n", c=nchk),
                       [("ps", b)], [("FM", t)])

            run_stages(Pf, [Rf, Rb])
            global_bound(mt, 5, slice(0, 4), slice(4, 5), SC64, negc)

            P.barrier()
            SCOs = [SCO, arena[:, 0:S]]
            a_qkr = phase_qkr_off
            MSKT = [arena[:, a_qkr + 1024 * i:a_qkr + 1024 * (i + 1)].bitcast(BF16).rearrange("p (t n) -> p t n", t=NT)
                    for i in range(2)]

            def idx_items(qb):
                nk = (qb + 1) * 128
                par = qb % 2
                out = []
                for k0 in range(0, nk, 512):
                    n = min(512, nk - k0)
                    for h in range(4):
                        po = 64 * (h % 2)
                        ri = (len(out)) % 2
                        out.append({"kind": "idx", "n": n, "h": h, "qb": qb, "k0": k0, "ri": ri, "rb": RB[ri],
                                    "lhsT": FM[po:po + 64, 3 + h // 2, tcols(qb)], "rhs": FM[po:po + 64, 5, k0:k0 + n],
                                    "keys": [("FM", t) for t in range(qb + 1)],
                                    "sco": SCOs[par], "scokey": "SCO%d" % par})
                return out

            def bis_closures(qb):
                nk = (qb + 1) * 128
                par = qb % 2
                sco = SCOs[par]
                sk = "SCO%d" % par
                cl = []

                def init():
                    if qb >= 2:
                        red(bis[:, 0:1], sco[:, 0:nk], ALU.max, [sk], ["bnd"], absv=True)
                    tt(sco[:, qb * 128:nk], sco[:, qb * 128:nk], negmask[:], ALU.add, [sk, "negmask"], [sk])
                    if qb >= 2:
                        ts(bis[:, 2:3], bis[:, 0:1], 2.0, 2.0, ALU.mult, ALU.add, ["bnd"], ["w0"])
                        ts(btab[:], fvec[:], bis[:, 2:3], None, ALU.mult, None, ["w0", "fvec"], ["btab"])
                        memset(bis[:, 3:4], 0.0, ["mid"])
                    else:
                        ts(MSK[:, 0:nk], sco[:, 0:nk], -1.0e29, None, ALU.is_ge, None, [sk], ["MSK"])
                cl.append(init)
                if qb >= 2:
                    def it_fn(k):
                        def fn():
                            ts(MSK[:, 0:nk], sco[:, 0:nk], bis[:, 3:4], None, ALU.is_ge, ALU.add, [sk, "mid"],
                               ["MSK", "cnt"], accum=bis[:, 4:5])
                            if k < NBIS - 1:
                                ts(bis[:, 5:6], bis[:, 4:5], float(TOPK), btab[:, 16 + k + 1:16 + k + 2], ALU.is_ge, ALU.mult,
                                   ["cnt", "btab"], ["stp"])
                                stt(bis[:, 3:4], bis[:, 5:6], btab[:, k + 1:k + 2], bis[:, 3:4], ALU.subtract, ALU.add,
                                    ["stp", "btab", "mid"], ["mid"])
                            else:
                                ts(bis[:, 5:6], bis[:, 4:5], float(TOPK), btab[:, k:k + 1], ALU.is_ge, ALU.mult,
                                   ["cnt", "btab"], ["stp"])
                                stt(bis[:, 1:2], bis[:, 5:6], btab[:, k:k + 1], bis[:, 3:4], ALU.subtract, ALU.add,
                                    ["stp", "btab", "mid"], ["lo"])
                        return fn
                    for k in range(NBIS):
                        cl.append(it_fn(k))
                    cl.append(lambda: ts(MSK[:, 0:nk], sco[:, 0:nk], bis[:, 1:2], None, ALU.is_ge, None, [sk, "lo"], ["MSK"]))
                return cl

            def mskT_items(qb):
                par = qb % 2
                out = []
                for kt0 in range(0, qb + 1, 4):
                    kts = list(range(kt0, min(kt0 + 4, qb + 1)))
                    out.append({"kind": "mskT", "kts": kts, "src": MSK, "srckey": "MSK",
                                "dst": MSKT[par], "dstkey": ("MSKT", par)})
                return out

            def att_items(qb):
                par = qb % 2
                out = []
                for h in range(4):
                    po = 64 * (h % 2)
                    for kt0 in range(0, qb + 1, 4):
                        kts = list(range(kt0, min(kt0 + 4, qb + 1)))
                        slots = [{"K": (FM[po:po + 64, 2, tcols(kt)], [("FM", kt)]),
                                  "Q": (FM[po:po + 64, h // 2, tcols(qb)], [("FM", qb)]),
                                  "V": (VB[:, kt, :], [("VB", kt), "VBones"]),
                                  "ob": 4 + par, "oreg": h * 65, "start": kt == 0, "stop": kt == qb} for kt in kts]
                        ns = len(kts)
                        masks = [(0, 128 * ns, ("p (c n) -> p c n", {"c": ns}), MSKT[par][:, kt0:kt0 + ns, :], [("MSKT", par)])]
                        last = (h == 3 and kts[-1] == qb)
                        out.append({"kind": "att", "slots": slots, "scale": SC64, "negc": negc, "masks": masks,
                                    "fin": fin_generic(qb, 4 + par, 4, 2, 512, None) if last else None})
                return out

            def merge(a, b):
                out = []
                na, nb = len(a), len(b)
                ia = ib = 0
                while ia < na or ib < nb:
                    if ib >= nb or (ia < na and ia * nb <= ib * na):
                        out.append(a[ia])
                        ia += 1
                    else:
                        out.append(b[ib])
                        ib += 1
                return out

            seq = []
            for r in range(-2, NT):
                ia = att_items(r) if r >= 0 else []
                ii = idx_items(r + 2) if r + 2 < NT else []
                cl = bis_closures(r + 1) if 0 <= r + 1 < NT else []
                im = mskT_items(r + 1) if 0 <= r + 1 < NT else []
                ents = [[it, []] for it in merge(ia, ii)]
                if not ents:
                    ents = [[None, []]]
                ne = len(ents)
                for ci, c in enumerate(cl):
                    ents[min(ne - 1, (ci * ne) // max(1, len(cl)))][1].append(c)
                seq.extend((e[0], e[1]) for e in ents)
                seq.extend((it, []) for it in im)
            run_seq(seq, bg_per_iter=1)

        def phase_C(l):
            A.off = phase_base
            FQ = A.bf16(4 * S).rearrange("p (c n) -> p c n", c=4)
            FK = A.bf16(4 * S).rearrange("p (c n) -> p c n", c=4)
            VC = A.bf16(NT * 4 * 65).rearrange("p (t h d) -> p t h d", t=NT, h=4)
            QCF = [QKR[0][:, 0:384], QKR[0][:, 384:768]]
            KCF = [QKR[1][:, 0:384], QKR[1][:, 384:768]]
            CQN = [A.bf16(256), A.bf16(256)]
            CKN = [A.bf16(128), A.bf16(128)]
            CQT = [A.bf16(256), A.bf16(256)]
            CKT = [A.bf16(128), A.bf16(128)]
            mt = A.f32(16)[:, 0:8]
            negc = A.f32(2)[:, 0:1]
            rt = A.f32(128)
            rtk = [A.f32(32), A.f32(32)]
            SQJ = MIXT_f32
            wv = WIN[:, 0:8 * 416].rearrange("p (c n) -> p c n", c=8)
            WUQ = WIN[:, 8 * 416:8 * 416 + 768].rearrange("p (c n) -> p c n", c=2)
            WUKV = WIN[:, 8 * 416 + 768:8 * 416 + 768 + 512]
            P.dma("pool", wv, w_in_d[l].rearrange("(c p) n -> p c n", p=128)[:, :, 1476:1892], writes=["WIN"])
            P.dma("pool", WUQ, w_uq_d[l].rearrange("(c p) n -> p c n", p=128), writes=["WIN"])
            P.dma("pool", WUKV, w_ukv_d[l], writes=["WIN"])
            P.dma("pool", WOUT[:, 0:2048].rearrange("p (c n) -> p c n", c=2),
                  w_out_d[l, 768:1024, :].rearrange("(c p) n -> p c n", p=128), writes=["WOUT"])
            P.dma("sp", gq[:], gq_d[l].partition_broadcast(128), writes=["gq"])
            P.dma("sp", gkv[:], gkv_d[l].partition_broadcast(128), writes=["gkv"])
            load_ln(ln1g_d, ln1b_d, l)
            memset(VC[:, :, :, 64:65], 1.0, ["VCones"])
            def Pf(t):
                in_proj(t, 416, wv, HSB[t % 2])

            def c1(t):
                p = t % 2
                hs = HSB[p]
                hk = "HSB%d" % p
                o = 80 + 8 * p
                ck = "c1s%d" % p
                act(SQJ[:, 0:256], hs[:, 0:256], AF.Square, [hk], ["SQJ"], accum=sm[:, o:o + 1])
                act(SQJ[:, 256:384], hs[:, 256:384], AF.Square, [hk], ["SQJ2"], accum=sm[:, o + 1:o + 2])
                act(sm[:, o + 2:o + 3], sm[:, o:o + 1], AF.Ln, ["SQJ"], [ck + "l1"], scale=1.0 / 256, bias=1e-6)
                act(sm[:, o + 3:o + 4], sm[:, o + 1:o + 2], AF.Ln, ["SQJ2"], [ck + "l2"], scale=1.0 / 128, bias=1e-6)
                act(sm[:, o + 4:o + 5], sm[:, o + 2:o + 3], AF.Exp, [ck + "l1"], [ck + "r1"], scale=-0.5)
                act(sm[:, o + 5:o + 6], sm[:, o + 3:o + 4], AF.Exp, [ck + "l2"], [ck + "r2"], scale=-0.5)
                stt(CQN[p][:, 0:256], hs[:, 0:256], sm[:, o + 4:o + 5], gq[:], ALU.mult, ALU.mult, [hk, ck + "r1", "gq"], ["CQN%d" % p])
                stt(CKN[p][:, 0:128], hs[:, 256:384], sm[:, o + 5:o + 6], gkv[:], ALU.mult, ALU.mult, [hk, ck + "r2", "gkv"], ["CKN%d" % p])
                rope(hs[:, 384:416].rearrange("p (h t d) -> p h t d", h=1, t=2),
                     rtk[p][:, 0:32].rearrange("p (h t d) -> p h t d", h=1, t=2), 1, 32, t,
                     cos32, sin32, nsin32, [hk], ["rtk%d" % p])

            def c23(t):
                p = t % 2
                qck, kck = "QCF%d" % p, "KCF%d" % p
                b = mbank()
                tr(bankb(b)[:, 0:128], CQN[p][:, 0:128], identb[:], ["CQN%d" % p, "identb"], [("ps", b)], inc=False)
                tr(bankb(b)[:, 128:256], CQN[p][:, 128:256], identb[:], ["CQN%d" % p, "identb"], [("ps", b)], inc=False)
                tr(bankb(b)[:, 256:384], CKN[p][:, 0:128], identb[:], ["CKN%d" % p, "identb"], [("ps", b)], inc=True)
                cp("act", CQT[p][:, 0:256], bankb(b)[:, 0:256], [("ps", b)], ["CQT%d" % p])
                cp("dve", CKT[p][:, 0:128], bankb(b)[:, 256:384], [("ps", b)], ["CKT%d" % p])
                bq = mbank()
                mm(bank(bq)[:, 0:384], CQT[p][:, 0:128], WUQ[:, 0, :], True, False, ["CQT%d" % p, "WIN"], [("ps", bq)], False)
                mm(bank(bq)[:, 0:384], CQT[p][:, 128:256], WUQ[:, 1, :], False, True, ["CQT%d" % p, "WIN"], [("ps", bq)], True)
                bk = mbank()
                mm(bank(bk)[:, 0:512], CKT[p][:, 0:128], WUKV, True, True, ["CKT%d" % p, "WIN"], [("ps", bk)], True)
                cp("act", QCF[p], bank(bq)[:, 0:384], [("ps", bq)], [qck])
                q4 = QCF[p].rearrange("p (h d) -> p h d", h=4)
                qr = q4[:, :, 64:96].rearrange("p h (t d) -> p h t d", t=2)
                cp("dve", rt[:, 0:128].rearrange("p (h d) -> p h d", h=4), q4[:, :, 64:96], [qck], ["rt"])
                rope(rt[:, 0:128].rearrange("p (h t d) -> p h t d", h=4, t=2), qr, 4, 32, t,
                     cos32, sin32, nsin32, ["rt"], [qck])
                kv4 = bank(bk)[:, 0:512].rearrange("p (h d) -> p h d", h=4)
                k4 = KCF[p].rearrange("p (h d) -> p h d", h=4)
                cp("act", k4[:, :, 0:64], kv4[:, :, 0:64], [("ps", bk)], [kck])
                cp("dve", VC[:, t, :, 0:64], kv4[:, :, 64:128], [("ps", bk)], [("VC", t)])
                cp("dve", k4[:, :, 64:96], rtk[p][:, 0:32].unsqueeze(1).to_broadcast([128, 4, 32]), ["rtk%d" % p], [kck])
                act(SQ[:, 0:384], QCF[p], AF.Square, [qck], ["SQ"])
                act(SQ[:, 384:768], KCF[p], AF.Square, [kck], ["SQ"])
                if t == 0:
                    red(mt, SQ[:, 0:768].rearrange("p (h d) -> p h d", h=8), ALU.add, ["SQ"], ["mt"])
                else:
                    red(sm[:, 16:24], SQ[:, 0:768].rearrange("p (h d) -> p h d", h=8), ALU.add, ["SQ"], ["hs"])
                    tt(mt, mt, sm[:, 16:24], ALU.max, ["mt", "hs"], ["mt"])

            def c4(t):
                p = t % 2
                for (src_, dstF, key, fkey, eng) in ((QCF[p], FQ, "QCF%d" % p, "FQKR0", "act"), (KCF[p], FK, "KCF%d" % p, "FQKR1", "dve")):
                    b = mbank()
                    for h in range(4):
                        tr(bank(b)[0:96, h * 128:(h + 1) * 128], src_[:, h * 96:(h + 1) * 96], identf[:],
                           [key, "identf"], [("ps", b)], inc=(h == 3))
                    cp(eng, dstF[0:96, :, tcols(t)], bank(b)[0:96, :].rearrange("p (c n) -> p c n", c=4),
                       [("ps", b)], [(fkey, t)])

            run_stages(Pf, [c1, c23, c4])
            global_bound(mt, 8, slice(0, 4), slice(4, 8), SC96, negc)

            P.barrier()
            RT = arena[:, phase_qkr_off:phase_qkr_off + 2304]

            def xpose(qb, half):
                b = mbank()
                for j in range(4):
                    c = half * 4 + j
                    tr(bank(b)[:, j * 128:(j + 1) * 128], X[:, qb, c * 128:(c + 1) * 128], identf[:],
                       [("X", qb), "identf"], [("ps", b)], inc=(j == 3))
                cp("act", xT[:, half * 4:half * 4 + 4, tcols(qb)], bank(b).rearrange("p (c n) -> p c n", c=4),
                   [("ps", b)], [("xT", qb)])
                cp("dve", HSB[half][:, 0:512], bank(b), [("ps", b)], ["HSB%d" % half])

            def router_a(qb):
                b = mbank()
                for c in range(8):
                    mm(bank(b)[:, 0:16], HSB[c // 4][:, (c % 4) * 128:(c % 4 + 1) * 128], wr[:, c, :], c == 0, c == 7,
                       ["HSB0", "HSB1", "wr"], [("ps", b)], inc=(c == 7))
                act(RT[:, qb * 16:(qb + 1) * 16], bank(b)[:, 0:16], AF.Exp, [("ps", b)], [("r_sc", qb)], scale=-1.0)

            def finC(qb, ob):
                def fn():
                    o3 = bank(ob)[:, 0:260].rearrange("p (h d) -> p h d", h=4)
                    rc = sm[:, 72:76]
                    recip(rc, o3[:, :, 64], [("ps", ob)], ["rc"])
                    tt(MIXT[:, 0:256].rearrange("p (h d) -> p h d", h=4), o3[:, :, 0:64],
                       rc.unsqueeze(2).to_broadcast([128, 4, 64]), ALU.mult, [("ps", ob), "rc"], ["MIXT"])
                    dbg_store(qb, 768, 256)
                    pv = qb - 1
                    if pv >= 0:
                        bg.append(lambda: xpose(pv, 0))
                        bg.append(lambda: None)
                    bg.append(lambda: out_proj_T(qb, 2))
                    if pv >= 0:
                        bg.append(lambda: xpose(pv, 1))
                    bg.append(lambda: out_proj_M(qb, 2, False))
                    if pv >= 0:
                        bg.append(lambda: router_a(pv))
                    bg.append(lambda: ln_stats(qb, qb % 2))
                    bg.append(lambda: ln_apply(qb, qb % 2))
                return fn

            seq = []
            for qb in range(NT):
                par = qb % 2
                for h in range(4):
                    for kt0 in range(0, qb + 1, 4):
                        kts = list(range(kt0, min(kt0 + 4, qb + 1)))
                        slots = [{"K": (FK[0:96, h, tcols(kt)], [("FQKR1", kt)]),
                                  "Q": (FQ[0:96, h, tcols(qb)], [("FQKR0", qb)]),
                                  "V": (VC[:, kt, h, :], [("VC", kt), "VCones"]),
                                  "ob": 4 + par, "oreg": h * 65, "start": kt == 0, "stop": kt == qb} for kt in kts]
                        ns = len(kts)
                        masks = []
                        if kts[-1] == qb:
                            masks = [(128 * (ns - 1), 128 * ns, None, causalT[:], ["causalT"])]
                        last = (h == 3 and kts[-1] == qb)
                        seq.append(({"kind": "att", "slots": slots, "scale": SC96, "negc": negc, "masks": masks,
                                     "fin": finC(qb, 4 + par) if last else None}, []))
            run_seq(seq, bg_per_iter=2)
            xpose(NT - 1, 0)
            xpose(NT - 1, 1)
            router_a(NT - 1)

            sc = RT[:, 0:256]
            bi = RT[:, 256:512]
            m1 = RT[:, 512:576]
            eq = RT[:, 576:832]
            msk = RT[:, 832:1088]
            m2 = RT[:, 1088:1152]
            gs = RT[:, 1152:1216]
            gm = RT[:, 1216:1232]
            gsel = RT[:, 1232:1296]
            t2 = RT[:, 1296:1552]
            wgt = RT[:, 1552:1808]
            ws = RT[:, 1808:1824]
            rsk = [("r_sc", t) for t in range(NT)]

            def v3(ap, a):
                return ap.rearrange("p (a b) -> p a b", a=a)

            ts(sc, sc, 1.0, None, ALU.add, None, rsk, ["r_s"])
            recip(sc, sc, ["r_s"], ["r_s"])
            tt(v3(bi, 16), v3(sc, 16), rb[:].unsqueeze(1).to_broadcast([128, 16, 16]), ALU.add, ["r_s", "rb"], ["r_bi"])
            red(m1, v3(bi, 64), ALU.max, ["r_bi"], ["r_m1"])
            tt(v3(eq, 64), v3(bi, 64), m1.unsqueeze(2).to_broadcast([128, 64, 4]), ALU.is_equal, ["r_bi", "r_m1"], ["r_eq"])
            stt(msk, eq, NEG, bi, ALU.mult, ALU.add, ["r_eq", "r_bi"], ["r_msk"])
            red(m2, v3(msk, 64), ALU.max, ["r_msk"], ["r_m2"])
            tt(gs, m1, m2, ALU.add, ["r_m1", "r_m2"], ["r_gs"])
            red(gm, v3(gs, 16), ALU.max, ["r_gs"], ["r_gm"])
            tt(v3(gsel, 16), v3(gs, 16), gm.unsqueeze(2).to_broadcast([128, 16, 4]), ALU.is_equal, ["r_gs", "r_gm"], ["r_gsel"])
            tt(v3(t2, 64), v3(bi, 64), m2.unsqueeze(2).to_broadcast([128, 64, 4]), ALU.is_ge, ["r_bi", "r_m2"], ["r_t2"])
            tt(v3(t2, 64), v3(t2, 64), gsel.unsqueeze(2).to_broadcast([128, 64, 4]), ALU.mult, ["r_t2", "r_gsel"], ["r_t2"])
            tt(wgt, sc, t2, ALU.mult, ["r_s", "r_t2"], ["r_w"])
            red(ws, v3(wgt, 16), ALU.add, ["r_w"], ["r_ws"])
            recip(ws, ws, ["r_ws"], ["r_ws"])
            tt(gates[:], v3(wgt, 16), ws.unsqueeze(2).to_broadcast([128, 16, 16]), ALU.mult, ["r_w", "r_ws"],
               [("gates", t) for t in range(NT)])

        def phase_moe(l, last_layer):
            A.off = 0
            NSLOT = 7
            WGU = [A.bf16(8 * 512).rearrange("p (c n) -> p c n", c=8) for _ in range(NSLOT)]
            WD = [A.bf16(2 * 1024).rearrange("p (c n) -> p c n", c=2) for _ in range(NSLOT)]
            SB = [A.bf16(256) for _ in range(2)]
            TB = [A.bf16(256) for _ in range(2)]
            TTB = [A.bf16(256) for _ in range(2)]
            load_ln(ln2g_d, ln2b_d, l)
            def load_expert(e):
                s = e % NSLOT
                P.dma("pool", WGU[s][:, :, 0:256], w_gate_d[l, e].rearrange("(c p) f -> p c f", p=128), writes=[("WGU", s)])
                P.dma("pool", WGU[s][:, :, 256:512], w_up_d[l, e].rearrange("(c p) f -> p c f", p=128), writes=[("WGU", s)])
                P.dma("pool", WD[s], w_down_d[l, e].rearrange("(c p) d -> p c d", p=128), writes=[("WD", s)])

            for e in range(NSLOT):
                load_expert(e)
            items = [(0, t, e) for e in range(4) for t in range(NT)]
            items += [(G, t, e) for G in range(1, 4) for t in range(NT) for e in range(4 * G, 4 * G + 4)]
            sd = {}
            cnt = [0]

            def s1(it):
                G, t, e = it
                s = e % NSLOT
                b = rbank()
                sd[it] = {"b": b, "i": cnt[0] % 2}
                cnt[0] += 1
                for c in range(8):
                    mm(bank(b), xT[:, c, tcols(t)], WGU[s][:, c, :], c == 0, c == 7,
                       [("xT", t), ("WGU", s)], [("ps", b)], inc=(c == 7))

            def s2(it):
                G, t, e = it
                d = sd[it]
                b, i = d["b"], d["i"]
                act(SB[i][:, 0:256], bank(b)[:, 0:256], AF.Silu, [("ps", b)], [("SB", i)])
                stt(TB[i][:, 0:256], bank(b)[:, 256:512], gates[:, t, e:e + 1], SB[i][:, 0:256], ALU.mult, ALU.mult,
                    [("ps", b), ("gates", t), ("SB", i)], [("TB", i)])
                b2 = rbank()
                d["b2"] = b2
                for fc in range(2):
                    tr(bankb(b2)[:, fc * 128:(fc + 1) * 128], TB[i][:, fc * 128:(fc + 1) * 128], identb[:],
                       [("TB", i), "identb"], [("ps", b2)], inc=(fc == 1))

            def s3(it):
                G, t, e = it
                d = sd[it]
                b2, i = d["b2"], d["i"]
                s = e % NSLOT
                cp("act", TTB[i][:, 0:256], bankb(b2)[:, 0:256], [("ps", b2)], [("TTB", i)])
                first = (e % 4 == 0) or G == 0
                lastx = (e % 4 == 3) or G == 0
                for half in range(2):
                    yb = 4 + 2 * (t % 2) + half
                    for fc in range(2):
                        mm(bank(yb), TTB[i][:, fc * 128:(fc + 1) * 128], WD[s][:, fc, half * 512:(half + 1) * 512],
                           first and fc == 0, lastx and fc == 1, [("TTB", i), ("WD", s)], [("ps", yb)],
                           inc=(fc == 1))
                if t == NT - 1 and e + NSLOT < 16:
                    load_expert(e + NSLOT)
                if lastx:
                    for half in range(2):
                        yb = 4 + 2 * (t % 2) + half
                        xs = X[:, t, half * 512:(half + 1) * 512]
                        if e == 0:
                            stt(xs, xs, ALPHA, bank(yb), ALU.mult, ALU.add, [("X", t), ("ps", yb)], [("X", t)])
                        else:
                            tt(xs, xs, bank(yb), ALU.add, [("X", t), ("ps", yb)], [("X", t)])
                    if G == 3:
                        k = t % 2
                        o = 32 + 16 * k
                        mvo = 96 + 2 * t

                        def st_a(t=t, o=o, k=k):
                            P.op("dve", lambda e, o_=sm[:, o:o + 6], i=X[:, t, 0:512]: e.bn_stats(out=o_, in_=i),
                                 reads=[("X", t)], writes=["bs%da" % k])

                        def st_b(t=t, o=o, k=k, mvo=mvo):
                            P.op("dve", lambda e, o_=sm[:, o + 6:o + 12], i=X[:, t, 512:1024]: e.bn_stats(out=o_, in_=i),
                                 reads=[("X", t)], writes=["bs%db" % k])
                            P.op("dve", lambda e, o_=sm[:, mvo:mvo + 2], i=sm[:, o:o + 12]: e.bn_aggr(out=o_, in_=i),
                                 reads=["bs%da" % k, "bs%db" % k], writes=[("mv2", t)])

                        bg.append(st_a)
                        bg.append(st_b)
                        if t % 8 == 7:
                            t0 = t - 7

                            def rstd8(t0=t0):
                                var8 = sm[:, 96 + 2 * t0:96 + 2 * t0 + 16].rearrange("p (t two) -> p t two", two=2)[:, :, 1]
                                rs8 = sm[:, 128 + t0:128 + t0 + 8]
                                act(rs8, var8, AF.Ln, [("mv2", u) for u in range(t0, t0 + 8)], [("rs2", t0)], bias=1e-5)
                                act(rs8, rs8, AF.Exp, [("rs2", t0)], [("rs2", t0)], scale=-0.5)
                            bg.append(rstd8)
                            for u in range(t0, t0 + 8):
                                for half in range(2):
                                    hsl = slice(half * 512, (half + 1) * 512)

                                    def ap1(u=u, t0=t0, hsl=hsl):
                                        xs = X[:, u, hsl]
                                        ts(xs, xs, sm[:, 96 + 2 * u:96 + 2 * u + 1], sm[:, 128 + u:128 + u + 1], ALU.subtract, ALU.mult,
                                           [("X", u), ("mv2", u), ("rs2", t0)], [("X", u)])

                                    def ap2(u=u, hsl=hsl):
                                        xs = X[:, u, hsl]
                                        tt(xs, xs, lnp[:, 0, hsl], ALU.mult, [("X", u), "lnp"], [("X", u)])

                                    def ap3(u=u, hsl=hsl, half=half):
                                        xs = X[:, u, hsl]
                                        tt(xs, xs, lnp[:, 1, hsl], ALU.add, [("X", u), "lnp"], [("X", u)])
                                        if last_layer and half == 1:
                                            P.dma("sp", tm(out_d)[:, u, :], X[:, u, :], reads=[("X", u)], final=True)
                                    bg.append(ap1)
                                    bg.append(ap2)
                                    bg.append(ap3)

            pipeline(items, s1, s2, s3, bg_per_iter=2)

        def run_layers():
            for l in range(nlayers):
                for t in range(NT):
                    build_xT(t)
                if stop_after == "xT":
                    return False
                phase_A(l)
                P.barrier()
                if stop_after in ("A", "A1", "A2"):
                    return False
                phase_B(l)
                P.barrier()
                if stop_after == "B":
                    return False
                phase_C(l)
                P.barrier()
                if stop_after == "C":
                    return False
                phase_moe(l, l == nlayers - 1)
                P.barrier()
            return True

        if not run_layers():
            for t in range(NT):
                P.dma("sp", tm(out_d)[:, t, :], X[:, t, :], reads=[("X", t)], final=True)

        P.emit(nc, sems)
    return nc


_CACHE = {}


def kernel(**inputs):
    consts = _consts()
    if "nc" not in _CACHE:
        _CACHE["nc"] = build_program()
    nc = _CACHE["nc"]
    x = np.ascontiguousarray(np.asarray(inputs["x"], dtype=np.float32))
    shared = {}
    for k, v in inputs.items():
        if k == "x":
            continue
        shared[k] = np.ascontiguousarray(np.asarray(v, dtype=np.float32))
    shared.update(consts)
    in_maps = []
    for c in range(NCORES):
        m = dict(shared)
        m["x"] = x[c]
        in_maps.append(m)
    res = run_bass_kernel_spmd(nc, in_maps, core_ids=list(range(NCORES)))
    out = np.stack([np.asarray(r["out"], dtype=np.float32) for r in res.results], axis=0)
    return out
```
